# Optimizing a Trainium2 kernel written in Bass

```python
import math
import jax, jax.numpy as jnp
from jax import lax
import numpy as np

D_MODEL = 1024
BATCH = 8
SEQ = 4096
DEPTH = 2

EPS = 1e-6
GLA_HEADS = 4
GLA_DV = D_MODEL // (2 * GLA_HEADS)
GLA_DK = GLA_DV // 2
GLA_RANK = 16
GLA_GATE_NORM = 16.0
GLA_CHUNK = 64
DSA_HEADS = 4
DSA_DH = D_MODEL // (2 * DSA_HEADS)
IDX_HEADS = 8
IDX_DIM = 64
TOPK_MAX = 256
Q_BLOCK = 128
RET_HEADS = 4
RET_DK = D_MODEL // RET_HEADS
RET_DV = 2 * RET_DK
RET_CHUNK = 64
ROPE_BASE = 10000.0
D_FF = ((8 * D_MODEL + 3 * 256 - 1) // (3 * 256)) * 256

EVEN_SPLITS = (GLA_HEADS * GLA_DK,
               GLA_HEADS * GLA_DK,
               GLA_HEADS * GLA_DV,
               GLA_RANK,
               GLA_HEADS * GLA_DV,
               DSA_HEADS * DSA_DH,
               DSA_DH,
               DSA_DH,
               IDX_HEADS * IDX_DIM,
               IDX_DIM,
               IDX_HEADS)
EVEN_IN = sum(EVEN_SPLITS)
EVEN_MIX = GLA_HEADS * GLA_DV + DSA_HEADS * DSA_DH
ODD_SPLITS = (RET_HEADS * RET_DK, RET_HEADS * RET_DK, RET_HEADS * RET_DV, RET_HEADS * RET_DV)
ODD_IN = sum(ODD_SPLITS)
ODD_MIX = RET_HEADS * RET_DV

kernel_name = "hybrid_gla_dsa_retention_trunk"


def _split(z, sizes):
    out, start = [], 0
    for s in sizes:
        out.append(z[..., start:start + s])
        start += s
    return out


def rmsnorm(x, g):
    xf = x.astype(jnp.float32)
    y = xf * lax.rsqrt(jnp.mean(xf * xf, axis=-1, keepdims=True) + EPS)
    return (y * g.astype(jnp.float32)).astype(x.dtype)


def _to_chunks(z, c):
    b, t, h, d = z.shape
    return z.astype(jnp.float32).reshape(b, t // c, c, h, d).transpose(1, 0, 3, 2, 4)


def _from_chunks(z):
    n, b, h, c, d = z.shape
    return z.transpose(1, 0, 3, 2, 4).reshape(b, n * c, h, d)


def gla_chunked(q, k, v, log_a):
    b_, t_, h_, dk = q.shape
    dv = v.shape[-1]
    c = GLA_CHUNK
    qc, kc, vc, gc = (_to_chunks(z, c) for z in (q, k, v, log_a))
    cum = jnp.cumsum(gc, axis=3)
    last = cum[:, :, :, -1:, :]
    q_t = qc * jnp.exp(cum) * (dk ** -0.5)
    k_t = kc * jnp.exp(-cum)
    k_end = kc * jnp.exp(last - cum)
    causal = jnp.tril(jnp.ones((c, c), dtype=bool))
    a = jnp.einsum('nbhid,nbhjd->nbhij', q_t, k_t)
    a = jnp.where(causal, a, 0.0)
    o_intra = jnp.einsum('nbhij,nbhjv->nbhiv', a, vc)

    def step(state, inp):
        qq, ke, vv, ll = inp
        o = jnp.einsum('bhid,bhdv->bhiv', qq, state)
        state = jnp.exp(ll)[:, :, 0, :, None] * state + jnp.einsum('bhjd,bhjv->bhdv', ke, vv)
        return state, o

    s0 = jnp.zeros((b_, h_, dk, dv), jnp.float32)
    _, o_inter = lax.scan(step, s0, (q_t, k_end, vc, last))
    return _from_chunks(o_intra + o_inter).astype(q.dtype)


def dsa_attention(q, k, v, iq, ik, iw, topk):
    b_, t_, h_, dh = q.shape
    nb = t_ // Q_BLOCK
    key_pos = jnp.arange(t_)
    ikf = ik.astype(jnp.float32)

    def blk(z):
        return z.reshape(b_, nb, Q_BLOCK, *z.shape[2:]).swapaxes(0, 1)

    def one_block(args):
        qb, iqb, iwb, bi = args
        q_pos = bi * Q_BLOCK + jnp.arange(Q_BLOCK)
        allowed = key_pos[None, :] <= q_pos[:, None]
        logits = jnp.einsum('bqhd,bsd->bqhs', iqb.astype(jnp.float32), ikf) * (IDX_DIM ** -0.5)
        score = jnp.einsum('bqh,bqhs->bqs', iwb.astype(jnp.float32), jax.nn.relu(logits))
        score = jnp.where(allowed[None], score, -jnp.inf)
        _, idx = lax.top_k(score, topk)
        valid = idx <= q_pos[None, :, None]
        k_sel = jax.vmap(lambda kk, ii: kk[ii])(k, idx).astype(jnp.float32)
        v_sel = jax.vmap(lambda vv, ii: vv[ii])(v, idx).astype(jnp.float32)
        s = jnp.einsum('bqhd,bqkd->bqhk', qb.astype(jnp.float32), k_sel) * (dh ** -0.5)
        s = jnp.where(valid[:, :, None, :], s, -jnp.inf)
        p = jax.nn.softmax(s, axis=-1)
        return jnp.einsum('bqhk,bqkd->bqhd', p, v_sel).astype(q.dtype)

    out = lax.map(one_block, (blk(q), blk(iq), blk(iw), jnp.arange(nb)))
    return out.swapaxes(0, 1).reshape(b_, t_, h_, dh)


def rotary(x, pos):
    half = x.shape[-1] // 2
    inv = ROPE_BASE ** (-jnp.arange(half, dtype=jnp.float32) / half)
    ang = pos.astype(jnp.float32)[:, None] * inv[None, :]
    cos = jnp.cos(ang)[None, :, None, :]
    sin = jnp.sin(ang)[None, :, None, :]
    xf = x.astype(jnp.float32)
    x1, x2 = xf[..., :half], xf[..., half:]
    return jnp.concatenate([x1 * cos - x2 * sin, x2 * cos + x1 * sin], axis=-1)


def retention_chunked(q, k, v):
    b_, t_, h_, dk = q.shape
    dv = v.shape[-1]
    c = RET_CHUNK
    log_g = jnp.log1p(-jnp.exp2(-5.0 - jnp.arange(h_, dtype=jnp.float32)))
    qc = _to_chunks(q, c)
    kc = _to_chunks(k, c) * (dk ** -0.5)
    vc = _to_chunks(v, c)
    idx = jnp.arange(c, dtype=jnp.float32)
    rel = idx[:, None] - idx[None, :]
    dmat = jnp.where(rel >= 0, jnp.exp(log_g[:, None, None] * jnp.maximum(rel, 0.0)), 0.0)
    inner = jnp.einsum('nbhid,nbhjd->nbhij', qc, kc) * dmat[None, None]
    o_intra = jnp.einsum('nbhij,nbhjv->nbhiv', inner, vc)
    xi = jnp.exp(log_g[:, None] * (idx[None, :] + 1.0))
    zeta = jnp.exp(log_g[:, None] * (c - 1.0 - idx[None, :]))
    decay_c = jnp.exp(log_g * c)

    def step(state, inp):
        qq, kk, vv = inp
        o = jnp.einsum('bhid,bhdv->bhiv', qq, state) * xi[None, :, :, None]
        state = decay_c[None, :, None, None] * state + jnp.einsum(
            'bhjd,bhjv->bhdv', kk * zeta[None, :, :, None], vv)
        return state, o

    s0 = jnp.zeros((b_, h_, dk, dv), jnp.float32)
    _, o_inter = lax.scan(step, s0, (qc, kc, vc))
    return _from_chunks(o_intra + o_inter).astype(q.dtype)


def even_mixer(h, w_in, gla_wa2, gla_ba2, gla_norm, w_out, topk):
    b_, t_, _ = h.shape
    proj = h @ w_in
    gq, gk, gv, ga, gr, dq, dk_, dv_, iq, ik, iw = _split(proj, EVEN_SPLITS)
    log_a = jax.nn.log_sigmoid((ga @ gla_wa2 + gla_ba2).astype(jnp.float32)) / GLA_GATE_NORM
    o_gla = gla_chunked(gq.reshape(b_, t_, GLA_HEADS, GLA_DK),
                        gk.reshape(b_, t_, GLA_HEADS, GLA_DK),
                        gv.reshape(b_, t_, GLA_HEADS, GLA_DV),
                        log_a.reshape(b_, t_, GLA_HEADS, GLA_DK))
    o_gla = rmsnorm(o_gla, gla_norm).reshape(b_, t_, GLA_HEADS * GLA_DV) * jax.nn.silu(gr)
    o_dsa = dsa_attention(dq.reshape(b_, t_, DSA_HEADS, DSA_DH), dk_, dv_,
                          iq.reshape(b_, t_, IDX_HEADS, IDX_DIM), ik,
                          iw * (IDX_HEADS ** -0.5), topk)
    o_dsa = o_dsa.reshape(b_, t_, DSA_HEADS * DSA_DH)
    return jnp.concatenate([o_gla.astype(h.dtype), o_dsa.astype(h.dtype)], axis=-1) @ w_out


def odd_mixer(h, w_in, ret_norm, w_out, pos):
    b_, t_, _ = h.shape
    q, k, v, g = _split(h @ w_in, ODD_SPLITS)
    q = rotary(q.reshape(b_, t_, RET_HEADS, RET_DK), pos)
    k = rotary(k.reshape(b_, t_, RET_HEADS, RET_DK), pos)
    o = retention_chunked(q, k, v.reshape(b_, t_, RET_HEADS, RET_DV))
    o = rmsnorm(o, ret_norm).reshape(b_, t_, ODD_MIX) * jax.nn.silu(g)
    return o.astype(h.dtype) @ w_out


def swiglu(h, w_gate, w_up, w_down):
    return (jax.nn.silu(h @ w_gate) * (h @ w_up)) @ w_down


def setup_inputs(seed: int = 0) -> dict:
    key = jax.random.key(seed)
    ks = jax.random.split(key, 20)
    ne = (DEPTH + 1) // 2
    no = DEPTH // 2
    f32 = jnp.float32

    def dense(k, shape, fan_in):
        return jax.random.normal(k, shape, f32) * (fan_in ** -0.5)

    def gain(k, shape):
        return 1.0 + 0.02 * jax.random.normal(k, shape, f32)

    return {
        "x": jax.random.normal(ks[0], (BATCH, SEQ, D_MODEL), f32),
        "even_attn_norm": gain(ks[1], (ne, D_MODEL)),
        "even_w_in": dense(ks[2], (ne, D_MODEL, EVEN_IN), D_MODEL),
        "even_gla_wa2": dense(ks[3], (ne, GLA_RANK, GLA_HEADS * GLA_DK), GLA_RANK),
        "even_gla_ba2": 0.1 * jax.random.normal(ks[4], (ne, GLA_HEADS * GLA_DK), f32),
        "even_gla_norm": gain(ks[5], (ne, GLA_DV)),
        "even_w_out": dense(ks[6], (ne, EVEN_MIX, D_MODEL), EVEN_MIX),
        "odd_attn_norm": gain(ks[7], (no, D_MODEL)),
        "odd_w_in": dense(ks[8], (no, D_MODEL, ODD_IN), D_MODEL),
        "odd_ret_norm": gain(ks[9], (no, RET_DV)),
        "odd_w_out": dense(ks[10], (no, ODD_MIX, D_MODEL), ODD_MIX),
        "ffn_norm": gain(ks[11], (DEPTH, D_MODEL)),
        "ffn_w_gate": dense(ks[12], (DEPTH, D_MODEL, D_FF), D_MODEL),
        "ffn_w_up": dense(ks[13], (DEPTH, D_MODEL, D_FF), D_MODEL),
        "ffn_w_down": dense(ks[14], (DEPTH, D_FF, D_MODEL), D_FF),
        "final_norm": gain(ks[15], (D_MODEL,)),
    }


def reference(x, even_attn_norm, even_w_in, even_gla_wa2, even_gla_ba2, even_gla_norm, even_w_out,
              odd_attn_norm, odd_w_in, odd_ret_norm, odd_w_out,
              ffn_norm, ffn_w_gate, ffn_w_up, ffn_w_down, final_norm):
    t_ = x.shape[1]
    topk = min(TOPK_MAX, t_ // 4)
    pos = jnp.arange(t_, dtype=jnp.int32)
    h = x
    for layer in range(DEPTH):
        i = layer // 2
        if layer % 2 == 0:
            h = h + even_mixer(rmsnorm(h, even_attn_norm[i]), even_w_in[i], even_gla_wa2[i],
                               even_gla_ba2[i], even_gla_norm[i], even_w_out[i], topk)
        else:
            h = h + odd_mixer(rmsnorm(h, odd_attn_norm[i]), odd_w_in[i], odd_ret_norm[i],
                              odd_w_out[i], pos)
        h = h + swiglu(rmsnorm(h, ffn_norm[layer]), ffn_w_gate[layer], ffn_w_up[layer],
                       ffn_w_down[layer])
    return rmsnorm(h, final_norm)
```

```python
import math
from contextlib import ExitStack

import numpy as np
import concourse.bass as bass
import concourse.mybir as mybir
from concourse.bass_utils import run_bass_kernel_spmd

F32 = mybir.dt.float32
BF16 = mybir.dt.bfloat16
AF = mybir.ActivationFunctionType
ALU = mybir.AluOpType
AX = mybir.AxisListType

T = 4096
D = 1024
DFF = 2816
NT = T // 128
EPS = 1e-6
EVEN_IN = 2904
ODD_IN = 6144


class _Op:
    __slots__ = ("eng", "fn", "deps", "dma", "sig", "sigidx", "dsem", "dval", "ndep")


class Prog:
    COMPUTE = ("pe", "act", "dve", "pool")

    def __init__(self, nc, n_dma_sems=16):
        self.nc = nc
        self.ops = []
        self.lastw = {}
        self.readers = {}
        self.n_dma_sems = n_dma_sems
        self.dma_rr = {"sp": 0, "pool": 0, "act": 0}
        self.dma_cnt = {}

    def add(self, eng, fn, r=(), w=(), dma=False):
        i = len(self.ops)
        deps = set()
        pr = [k for k in r if k[0] == "ps" and k not in w]
        if pr:
            w = list(w) + pr
        for k in r:
            a = self.lastw.get(k)
            if a is not None:
                deps.add(a)
        for k in w:
            a = self.lastw.get(k)
            if a is not None:
                deps.add(a)
            rd = self.readers.get(k)
            if rd:
                deps.update(rd.values())
        op = _Op()
        op.eng = eng
        op.fn = fn
        op.dma = dma
        op.sig = False
        op.sigidx = 0
        op.dsem = None
        op.dval = 0
        fdeps = []
        for a in deps:
            A = self.ops[a]
            if (not dma) and (not A.dma) and eng == "pe" and A.eng == "pe":
                continue
            fdeps.append(a)
        op.deps = sorted(fdeps)
        if dma:
            q = self.dma_rr[eng]
            self.dma_rr[eng] = (q + 1) % self.n_dma_sems
            key = (eng, q)
            self.dma_cnt[key] = self.dma_cnt.get(key, 0) + 1
            op.dsem = key
            op.dval = 16 * self.dma_cnt[key]
        self.ops.append(op)
        for k in w:
            self.lastw[k] = i
            self.readers[k] = {}
        for k in r:
            d = self.readers.setdefault(k, {})
            d[("dma", i) if dma else eng] = i
        return i

    def emit(self, es):
        nc = self.nc
        ops = self.ops
        for op in ops:
            for a in op.deps:
                if not ops[a].dma:
                    ops[a].sig = True
        cnt = {e: 0 for e in self.COMPUTE + ("sp",)}
        for op in ops:
            if op.sig and not op.dma:
                cnt[op.eng] += 1
                op.sigidx = cnt[op.eng]
        sems = {e: es.enter_context(nc.semaphore("s_" + e)) for e in cnt}
        dsems = {}
        for key in self.dma_cnt:
            dsems[key] = es.enter_context(nc.semaphore("d_%s%d" % key))
        engines = {"pe": "tensor", "act": "scalar", "dve": "vector", "pool": "gpsimd", "sp": "sync"}
        with nc.Block() as block:
            for e, attr in engines.items():
                mine = [op for op in ops if op.eng == e]

                def body(engine, mine=mine, e=e):
                    known = {}

                    def wait(sem_key, sem, val):
                        if known.get(sem_key, 0) >= val:
                            return
                        engine.wait_ge(sem, val)
                        known[sem_key] = val

                    for op in mine:
                        for a in op.deps:
                            A = ops[a]
                            if A.dma:
                                wait(A.dsem, dsems[A.dsem], A.dval)
                            else:
                                wait(A.eng, sems[A.eng], A.sigidx)
                        if op.dma:
                            if op.dval > 16:
                                wait(op.dsem, dsems[op.dsem], op.dval - 16)
                            inst = op.fn(engine)
                            inst.then_inc(dsems[op.dsem], 16)
                        else:
                            inst = op.fn(engine)
                            if op.sig:
                                inst.then_inc(sems[e], 1)
                    for key, c in self.dma_cnt.items():
                        if key[0] == e:
                            wait(key, dsems[key], 16 * c)

                getattr(block, attr)(body)


def _k(name, *idx):
    return (name,) + idx


class Ctx:
    def __init__(self, nc, P, es):
        self.nc = nc
        self.P = P
        self.es = es
        self.rr = 0

    UID = [0]

    def sb(self, name, shape, dt):
        Ctx.UID[0] += 1
        return self.es.enter_context(self.nc.sbuf_tensor("sb%d_%s" % (Ctx.UID[0], name), list(shape), dt))

    def ps(self, name):
        Ctx.UID[0] += 1
        return self.es.enter_context(self.nc.psum_tensor("ps%d_%s" % (Ctx.UID[0], name), [128, 512], F32))


def rsqrt_small(P, out_ap, outkey, in_ap, inkey, scale, eps=EPS):
    inkeys = inkey if isinstance(inkey, list) else [inkey]
    P.add("dve", lambda e: e.tensor_scalar(out_ap, in_ap, scale, eps, ALU.mult, ALU.add),
          r=inkeys, w=[outkey])
    P.add("act", lambda e: e.sqrt(out_ap, out_ap), r=[outkey], w=[outkey])
    P.add("dve", lambda e: e.reciprocal(out_ap, out_ap), r=[outkey], w=[outkey])


def rmsnorm_tile(c, h_ap, hkey, xn_ap, xnkey, junk_ap, junkkey, ss_ap, sskey, rstd_ap, rstdkey):
    P = c.P
    P.add("act", lambda e: e.activation(junk_ap, h_ap, AF.Square, accum_out=ss_ap),
          r=[hkey], w=[junkkey, sskey])
    rsqrt_small(P, rstd_ap, rstdkey, ss_ap, sskey, 1.0 / D)
    P.add("dve", lambda e: e.tensor_scalar(xn_ap, h_ap, rstd_ap, None, ALU.mult),
          r=[hkey, rstdkey], w=[xnkey])


def load_weight_bf16(c, w_dram, rows, cols, dst, dstname, stage, gcol=None, gkey=None, colchunk=704, gmap=None):
    P = c.P
    nk = rows // 128
    nch = (cols + colchunk - 1) // colchunk
    cnt = 0
    for k in range(nk):
        for j in range(nch):
            c0 = j * colchunk
            c1 = min(cols, c0 + colchunk)
            s = cnt % len(stage)
            st = stage[s]
            skey = _k("stage", s)
            src = w_dram[k * 128:(k + 1) * 128, c0:c1]
            P.add("sp", lambda e, st=st, src=src, n=c1 - c0: e.dma_start(out=st[:, 0:n], in_=src),
                  w=[skey], dma=True)
            dap = dst[:, k, c0:c1]
            sap = st[:, 0:c1 - c0]
            eng = ("dve", "pool", "act")[cnt % 3]
            rk = [skey] + ([gkey] if gcol is not None else [])
            if gcol is None:
                if eng == "act":
                    P.add("act", lambda e, dap=dap, sap=sap: e.copy(dap, sap), r=rk, w=[_k(dstname, k, j)])
                else:
                    P.add(eng, lambda e, dap=dap, sap=sap: e.tensor_copy(dap, sap), r=rk, w=[_k(dstname, k, j)])
            else:
                kk_ = gmap(k) if gmap is not None else k
                g = gcol[:, kk_:kk_ + 1]
                if eng == "act":
                    P.add("act", lambda e, dap=dap, sap=sap, g=g: e.activation(dap, sap, AF.Copy, scale=g),
                          r=rk, w=[_k(dstname, k, j)])
                else:
                    P.add(eng, lambda e, dap=dap, sap=sap, g=g: e.tensor_scalar(dap, sap, g, None, ALU.mult),
                          r=rk, w=[_k(dstname, k, j)])
            cnt += 1
    return [_k(dstname, k, j) for k in range(nk) for j in range(nch)]


def wkeys(dstname, k, c0, c1, colchunk=704):
    return [_k(dstname, k, j) for j in range(c0 // colchunk, (c1 - 1) // colchunk + 1)]


def ffn_phase(nc, h_in, h_out, g_dram, wg_d, wu_d, wd_d, ident_d, final_g=None):
    with ExitStack() as es:
        P = Prog(nc)
        c = Ctx(nc, P, es)
        CC = 704
        wg = c.sb("wg", [128, 8, DFF], BF16)
        wu = c.sb("wu", [128, 8, DFF], BF16)
        wd = c.sb("wd", [128, 22, D], BF16)
        stage = [c.sb("stage%d" % i, [128, CC], F32) for i in range(2)]
        gcol = c.sb("gcol", [128, 8], F32)
        ident = c.sb("ident", [128, 128], BF16)
        hbuf = c.sb("hbuf", [128, 4, D], F32)
        xn = [c.sb("xn%d" % i, [128, D], BF16) for i in range(2)]
        xnT = c.sb("xnT", [128, 8, 512], BF16)
        actT = c.sb("actT", [128, 22, 512], BF16)
        sg = [c.sb("sg%d" % i, [128, 512], F32) for i in range(2)]
        junk = c.sb("junk", [128, D], BF16)
        ss = c.sb("ss", [128, 8], F32)
        rstd = c.sb("rstd", [128, 8], F32)
        if final_g is not None:
            fg = c.sb("fg", [128, D], F32)
        psum = [c.ps("ps%d" % i) for i in range(8)]

        P.add("sp", lambda e: e.dma_start(out=gcol[:], in_=g_dram.rearrange("(k p) -> p k", p=128),
                                          allow_slow_non_contiguous=True), w=[_k("gcol")], dma=True)
        P.add("sp", lambda e: e.dma_start(out=ident[:], in_=ident_d), w=[_k("ident")], dma=True)
        if final_g is not None:
            P.add("sp", lambda e: e.dma_start(out=fg[:], in_=final_g.partition_broadcast(128)),
                  w=[_k("fg")], dma=True)
        load_weight_bf16(c, wg_d, D, DFF, wg, "wg", stage, gcol, _k("gcol"), CC)
        load_weight_bf16(c, wu_d, D, DFF, wu, "wu", stage, gcol, _k("gcol"), CC)
        load_weight_bf16(c, wd_d, DFF, D, wd, "wd", stage, None, None, CC)

        nsup = T // 512
        tcount = 0
        for s in range(nsup):
            for j in range(4):
                t0 = s * 512 + j * 128
                hk = _k("hbuf", j)
                hap = hbuf[:, j, :]
                P.add("sp", lambda e, hap=hap, t0=t0: e.dma_start(out=hap, in_=h_in[t0:t0 + 128, :]),
                      w=[hk], dma=True)
                b = tcount % 2
                rmsnorm_tile(c, hap, hk, xn[b][:], _k("xn", b), junk[:], _k("junk"),
                             ss[:, j:j + 1], _k("ss", j), rstd[:, j:j + 1], _k("rstd", j))
                pb = tcount % 2
                pst = psum[pb].bitcast(BF16) if hasattr(psum[pb], "bitcast") else psum[pb][:].bitcast(BF16)

                def tr(e, b=b, pst=pst):
                    inst = None
                    for cc in range(8):
                        inst = e.transpose(pst[:, cc * 128:(cc + 1) * 128], xn[b][:, cc * 128:(cc + 1) * 128], ident[:])
                    return inst
                P.add("pe", tr, r=[_k("xn", b), _k("ident")], w=[_k("ps", pb)])
                dst = xnT[:, :, j * 128:(j + 1) * 128]
                src = pst[:, 0:1024].rearrange("p (c t) -> p c t", c=8)
                P.add("act", lambda e, dst=dst, src=src: e.copy(dst, src), r=[_k("ps", pb)], w=[_k("xnT", j)])
                tcount += 1
            xk = [_k("xnT", j) for j in range(4)]
            for f in range(22):
                pg = 2 + (f % 2)
                pu = 4 + (f % 2)

                def mm(e, w_, pi, f=f):
                    inst = None
                    for k in range(8):
                        inst = e.matmul(psum[pi][:], w_[:, k, f * 128:(f + 1) * 128], xnT[:, k, :],
                                        start=(k == 0), stop=(k == 7))
                    return inst
                wkg = [kk for k in range(8) for kk in wkeys("wg", k, f * 128, (f + 1) * 128, CC)]
                wku = [kk for k in range(8) for kk in wkeys("wu", k, f * 128, (f + 1) * 128, CC)]
                P.add("pe", lambda e, pg=pg, mm=mm: mm(e, wg, pg), r=xk + wkg, w=[_k("ps", pg)])
                P.add("pe", lambda e, pu=pu, mm=mm: mm(e, wu, pu), r=xk + wku, w=[_k("ps", pu)])
                sb_ = f % 2
                P.add("act", lambda e, sb_=sb_, pg=pg: e.activation(sg[sb_][:], psum[pg][:], AF.Silu),
                      r=[_k("ps", pg)], w=[_k("sg", sb_)])
                P.add("dve", lambda e, sb_=sb_, pu=pu, f=f: e.tensor_tensor(actT[:, f, :], sg[sb_][:], psum[pu][:], ALU.mult),
                      r=[_k("sg", sb_), _k("ps", pu)], w=[_k("actT", f)])
            ak = [_k("actT", f) for f in range(22)]
            wdk = [kk for f in range(22) for kk in wkeys("wd", f, 0, D, CC)]
            for j in range(4):
                t0 = s * 512 + j * 128
                for mh in range(2):
                    py = 6 + mh

                    def mmd(e, j=j, mh=mh, py=py):
                        inst = None
                        for f in range(22):
                            inst = e.matmul(psum[py][:], actT[:, f, j * 128:(j + 1) * 128],
                                            wd[:, f, mh * 512:(mh + 1) * 512], start=(f == 0), stop=(f == 21))
                        return inst
                    P.add("pe", mmd, r=ak + wdk, w=[_k("ps", py)])
                    hs = hbuf[:, j, mh * 512:(mh + 1) * 512]
                    P.add("dve", lambda e, hs=hs, py=py: e.tensor_tensor(hs, hs, psum[py][:], ALU.add),
                          r=[_k("ps", py), _k("hbuf", j)], w=[_k("hbuf", j)])
                hap = hbuf[:, j, :]
                hk = _k("hbuf", j)
                if final_g is not None:
                    sj = 4 + j
                    P.add("act", lambda e, hap=hap, sj=sj: e.activation(junk[:], hap, AF.Square, accum_out=ss[:, sj:sj + 1]),
                          r=[hk], w=[_k("junk"), _k("ss", sj)])
                    rsqrt_small(P, rstd[:, sj:sj + 1], _k("rstd", sj), ss[:, sj:sj + 1], _k("ss", sj), 1.0 / D)
                    P.add("dve", lambda e, hap=hap, sj=sj: e.scalar_tensor_tensor(hap, hap, rstd[:, sj:sj + 1], fg[:], ALU.mult, ALU.mult),
                          r=[hk, _k("rstd", sj), _k("fg")], w=[hk])
                P.add("sp", lambda e, hap=hap, t0=t0: e.dma_start(out=h_out[t0:t0 + 128, :], in_=hap),
                      r=[hk], dma=True)
        P.emit(es)


RET_GAMMA = [1.0 - 2.0 ** (-5.0 - h) for h in range(4)]


DBG = {"ntiles": NT, "stage": 99}


def mix1_phase(nc, h_in, h_out, g_dram, win_d, retnorm_d, wout_d, ident_d, cos_d, sin_d, dt_d, xi8_d, zeta_d):
    with ExitStack() as es:
        P = Prog(nc)
        c = Ctx(nc, P, es)
        CC = 512
        win = c.sb("win", [128, 8, ODD_IN], BF16)
        wout = c.sb("wout", [128, 16, D], BF16)
        stage = [c.sb("stage%d" % i, [128, CC], F32) for i in range(2)]
        gcol = c.sb("gcol", [128, 8], F32)
        rcol = c.sb("rcol", [128, 4], F32)
        ident = c.sb("ident", [128, 128], BF16)
        S = c.sb("S", [128, 2, 4, 512], F32)
        Sb = c.sb("Sb", [128, 2, 4, 512], BF16)
        hbuf = c.sb("hbuf", [128, D], F32)
        xn = c.sb("xn", [128, D], BF16)
        xnT = c.sb("xnT", [128, 8, 128], BF16)
        tmp = [c.sb("tmp%d" % i, [128, 2, 128], F32) for i in range(4)]
        qrot = c.sb("qrot", [128, D], BF16)
        krot = c.sb("krot", [128, D], BF16)
        kz = c.sb("kz", [128, D], BF16)
        qT = c.sb("qT", [128, 8, 128], BF16)
        qxiT = c.sb("qxiT", [128, 8, 128], BF16)
        kT = c.sb("kT", [128, 8, 128], BF16)
        vb = c.sb("vb", [128, 2048], BF16)
        gs = c.sb("gs", [128, 2048], BF16)
        atm = c.sb("atm", [128, 4, 128], BF16)
        og = c.sb("og", [128, 2048], BF16)
        ogT = c.sb("ogT", [128, 16, 128], BF16)
        junk = c.sb("junk", [128, 512], BF16)
        cos_t = c.sb("cos", [128, 128], F32)
        sin_t = c.sb("sin", [128, 128], F32)
        dtm = c.sb("dtm", [128, 4, 128], F32)
        xi8 = c.sb("xi8", [128, 8, 128], F32)
        zeta = c.sb("zeta", [128, 4], F32)
        ss = c.sb("ss", [128, 8], F32)
        rstd = c.sb("rstd", [128, 8], F32)
        psum = [c.ps("ps%d" % i) for i in range(8)]

        def dma(out_ap, in_ap, wk, **kw):
            P.add("sp", lambda e: e.dma_start(out=out_ap, in_=in_ap, **kw), w=[wk], dma=True)

        dma(gcol[:], g_dram.rearrange("(k p) -> p k", p=128), _k("gcol"), allow_slow_non_contiguous=True)
        dma(rcol[:], retnorm_d.rearrange("(k p) -> p k", p=128), _k("rcol"), allow_slow_non_contiguous=True)
        dma(ident[:], ident_d, _k("ident"))
        dma(dtm[:], dt_d, _k("dtm"))
        dma(xi8[:], xi8_d, _k("xi8"))
        dma(zeta[:], zeta_d, _k("zeta"))
        P.add("dve", lambda e: e.memset(S[:], 0.0), w=[_k("S", cc, h) for cc in range(2) for h in range(4)])
        P.add("pool", lambda e: e.memset(Sb[:], 0.0), w=[_k("Sb", cc, h) for cc in range(2) for h in range(4)])
        load_weight_bf16(c, win_d, D, ODD_IN, win, "win", stage, gcol, _k("gcol"), CC)
        load_weight_bf16(c, wout_d, 2048, D, wout, "wout", stage, rcol, _k("rcol"), CC, gmap=lambda k: k % 4)

        def bfv(i):
            return psum[i][:].bitcast(BF16)

        for i in range(DBG["ntiles"]):
            t0 = i * 128
            hk = _k("hbuf")
            dma(hbuf[:], h_in[t0:t0 + 128, :], hk)
            dma(cos_t[:], cos_d[t0:t0 + 128, :], _k("cos"))
            dma(sin_t[:], sin_d[t0:t0 + 128, :], _k("sin"))
            rmsnorm_tile(c, hbuf[:], hk, xn[:], _k("xn"), ogT[:, 0:8, :].rearrange("p c t -> p (c t)"), _k("ogT", 0),
                         ss[:, 4:5], _k("ss", 4), rstd[:, 4:5], _k("rstd", 4))

            def tr8(e, src, pb):
                inst = None
                v = bfv(pb)
                for cc in range(8):
                    inst = e.transpose(v[:, cc * 128:(cc + 1) * 128], src[:, cc * 128:(cc + 1) * 128], ident[:])
                return inst
            P.add("pe", lambda e: tr8(e, xn, 0), r=[_k("xn"), _k("ident")], w=[_k("ps", 0)])
            P.add("act", lambda e: e.copy(xnT[:].rearrange("p c t -> p (c t)"), bfv(0)), r=[_k("ps", 0)], w=[_k("xnT")])
            for g in range(12 if DBG["stage"] >= 2 else 0):
                pb = 1 + (g % 2)

                def mm(e, g=g, pb=pb):
                    inst = None
                    for k in range(8):
                        inst = e.matmul(psum[pb][:], xnT[:, k, :], win[:, k, g * 512:(g + 1) * 512],
                                        start=(k == 0), stop=(k == 7))
                    return inst
                wk_ = [_k("win", k, g) for k in range(8)]
                P.add("pe", mm, r=[_k("xnT")] + wk_, w=[_k("ps", pb)])
                if g < 4:
                    dstt = qrot if g < 2 else krot
                    dname = "qrot" if g < 2 else "krot"
                    hh = (g % 2) * 2
                    X = psum[pb][:].rearrange("p (h x d) -> p h x d", h=2, x=2)
                    X1 = X[:, :, 0, :]
                    X2 = X[:, :, 1, :]
                    Dv = dstt[:, hh * 256:(hh + 2) * 256].rearrange("p (h x d) -> p h x d", h=2, x=2)
                    cb = cos_t[:].unsqueeze(1).to_broadcast([128, 2, 128])
                    sb_ = sin_t[:].unsqueeze(1).to_broadcast([128, 2, 128])
                    pk = _k("ps", pb)

                    def tt(e, o, a, b, op):
                        return e.tensor_tensor(o, a, b, op)
                    P.add("dve", lambda e, X1=X1, cb=cb: tt(e, tmp[0][:], X1, cb, ALU.mult), r=[pk, _k("cos")], w=[_k("tmp", 0)])
                    P.add("dve", lambda e, X2=X2, sb_=sb_: tt(e, tmp[1][:], X2, sb_, ALU.mult), r=[pk, _k("sin")], w=[_k("tmp", 1)])
                    P.add("dve", lambda e, X2=X2, cb=cb: tt(e, tmp[2][:], X2, cb, ALU.mult), r=[pk, _k("cos")], w=[_k("tmp", 2)])
                    P.add("dve", lambda e, X1=X1, sb_=sb_: tt(e, tmp[3][:], X1, sb_, ALU.mult), r=[pk, _k("sin")], w=[_k("tmp", 3)])
                    P.add("dve", lambda e, Dv=Dv: tt(e, Dv[:, :, 0, :], tmp[0][:], tmp[1][:], ALU.subtract),
                          r=[_k("tmp", 0), _k("tmp", 1)], w=[_k(dname, g % 2)])
                    P.add("dve", lambda e, Dv=Dv: tt(e, Dv[:, :, 1, :], tmp[2][:], tmp[3][:], ALU.add),
                          r=[_k("tmp", 2), _k("tmp", 3)], w=[_k(dname, g % 2)])
                elif g < 8:
                    vv = g - 4
                    P.add("act", lambda e, vv=vv, pb=pb: e.copy(vb[:, vv * 512:(vv + 1) * 512], psum[pb][:]),
                          r=[_k("ps", pb)], w=[_k("vb", vv)])
                else:
                    vv = g - 8
                    P.add("act", lambda e, vv=vv, pb=pb: e.activation(gs[:, vv * 512:(vv + 1) * 512], psum[pb][:], AF.Silu),
                          r=[_k("ps", pb)], w=[_k("gs", vv)])
            if DBG["stage"] < 3:
                P.add("sp", lambda e, t0=t0: e.dma_start(out=h_out[t0:t0 + 128, :], in_=hbuf[:]), r=[hk], dma=True)
                continue
            P.add("pool", lambda e: e.tensor_tensor(kz[:].rearrange("p (h d) -> p h d", h=4),
                                                    krot[:].rearrange("p (h d) -> p h d", h=4),
                                                    zeta[:].unsqueeze(2).to_broadcast([128, 4, 256]), ALU.mult),
                  r=[_k("krot", 0), _k("krot", 1), _k("zeta")], w=[_k("kz")])
            P.add("pe", lambda e: tr8(e, qrot, 0), r=[_k("qrot", 0), _k("qrot", 1), _k("ident")], w=[_k("ps", 0)])
            P.add("act", lambda e: e.copy(qT[:].rearrange("p c t -> p (c t)"), bfv(0)), r=[_k("ps", 0)], w=[_k("qT")])
            P.add("dve", lambda e: e.tensor_tensor(qxiT[:].rearrange("p c t -> p (c t)"), bfv(0),
                                                   xi8[:].rearrange("p c t -> p (c t)"), ALU.mult),
                  r=[_k("ps", 0), _k("xi8")], w=[_k("qxiT")])
            P.add("pe", lambda e: tr8(e, krot, 3), r=[_k("krot", 0), _k("krot", 1), _k("ident")], w=[_k("ps", 3)])
            P.add("act", lambda e: e.copy(kT[:].rearrange("p c t -> p (c t)"), bfv(3)), r=[_k("ps", 3)], w=[_k("kT")])

            if DBG["stage"] < 4:
                P.add("sp", lambda e, t0=t0: e.dma_start(out=h_out[t0:t0 + 128, :], in_=hbuf[:]), r=[hk], dma=True)
                continue
            def amm(e):
                inst = None
                for h in range(4):
                    for cc in range(2):
                        inst = e.matmul(psum[0][:, h * 128:(h + 1) * 128], kT[:, 2 * h + cc, :], qT[:, 2 * h + cc, :],
                                        start=(cc == 0), stop=(cc == 1))
                return inst
            P.add("pe", amm, r=[_k("kT"), _k("qT")], w=[_k("ps", 0)])
            P.add("dve", lambda e: e.tensor_tensor(atm[:].rearrange("p h t -> p (h t)"), psum[0][:],
                                                   dtm[:].rearrange("p h t -> p (h t)"), ALU.mult),
                  r=[_k("ps", 0), _k("dtm")], w=[_k("atm")])
            if DBG["stage"] < 5:
                P.add("sp", lambda e, t0=t0: e.dma_start(out=h_out[t0:t0 + 128, :], in_=hbuf[:]), r=[hk], dma=True)
                continue
            for h in range(4):
                def omm(e, h=h):
                    e.matmul(psum[4 + h][:], atm[:, h, :], vb[:, h * 512:(h + 1) * 512], start=True, stop=False)
                    e.matmul(psum[4 + h][:], qxiT[:, 2 * h, :], Sb[:, 0, h, :], start=False, stop=False)
                    return e.matmul(psum[4 + h][:], qxiT[:, 2 * h + 1, :], Sb[:, 1, h, :], start=False, stop=True)
                P.add("pe", omm, r=[_k("atm"), _k("vb", h), _k("qxiT"), _k("Sb", 0, h), _k("Sb", 1, h)], w=[_k("ps", 4 + h)])
                P.add("act", lambda e, h=h: e.activation(junk[:], psum[4 + h][:], AF.Square, accum_out=ss[:, h:h + 1]),
                      r=[_k("ps", 4 + h)], w=[_k("junk"), _k("ss", h)])
            if DBG["stage"] < 6:
                P.add("sp", lambda e, t0=t0: e.dma_start(out=h_out[t0:t0 + 128, :], in_=hbuf[:]), r=[hk], dma=True)
                continue
            for h in range(4):
                for cc in range(2):
                    pb = 1 + ((h * 2 + cc) % 2)
                    P.add("pe", lambda e, h=h, cc=cc, pb=pb: e.matmul(psum[pb][:], kz[:, h * 256 + cc * 128:h * 256 + (cc + 1) * 128],
                                                                        vb[:, h * 512:(h + 1) * 512], start=True, stop=True),
                          r=[_k("kz"), _k("vb", h)], w=[_k("ps", pb)])
                    dec = RET_GAMMA[h] ** 128
                    P.add("dve", lambda e, h=h, cc=cc, pb=pb, dec=dec: e.scalar_tensor_tensor(S[:, cc, h, :], S[:, cc, h, :], dec, psum[pb][:], ALU.mult, ALU.add),
                          r=[_k("S", cc, h), _k("ps", pb)], w=[_k("S", cc, h)])
                    P.add("pool", lambda e, h=h, cc=cc: e.tensor_copy(Sb[:, cc, h, :], S[:, cc, h, :]),
                          r=[_k("S", cc, h)], w=[_k("Sb", cc, h)])
            if DBG["stage"] < 7:
                P.add("sp", lambda e, t0=t0: e.dma_start(out=h_out[t0:t0 + 128, :], in_=hbuf[:]), r=[hk], dma=True)
                continue
            rsqrt_small(P, rstd[:, 0:4], _k("rstd", 0), ss[:, 0:4], [_k("ss", h) for h in range(4)], 1.0 / 512)
            for h in range(4):
                P.add("dve", lambda e, h=h: e.scalar_tensor_tensor(og[:, h * 512:(h + 1) * 512], psum[4 + h][:], rstd[:, h:h + 1],
                                                                    gs[:, h * 512:(h + 1) * 512], ALU.mult, ALU.mult),
                      r=[_k("ps", 4 + h), _k("rstd", 0), _k("gs", h)], w=[_k("og", h)])
            for half in range(2):
                pb = 0 if half == 0 else 3

                def tro(e, half=half, pb=pb):
                    inst = None
                    v = bfv(pb)
                    for cc in range(8):
                        c2 = half * 8 + cc
                        inst = e.transpose(v[:, cc * 128:(cc + 1) * 128], og[:, c2 * 128:(c2 + 1) * 128], ident[:])
                    return inst
                P.add("pe", tro, r=[_k("og", half * 2), _k("og", half * 2 + 1), _k("ident")], w=[_k("ps", pb)])
                P.add("act", lambda e, half=half, pb=pb: e.copy(ogT[:, half * 8:(half + 1) * 8, :].rearrange("p c t -> p (c t)"), bfv(pb)),
                      r=[_k("ps", pb)], w=[_k("ogT", half)])
            for mh in range(2):
                pb = 1 + mh

                def ymm(e, mh=mh, pb=pb):
                    inst = None
                    for cc in range(16):
                        inst = e.matmul(psum[pb][:], ogT[:, cc, :], wout[:, cc, mh * 512:(mh + 1) * 512],
                                        start=(cc == 0), stop=(cc == 15))
                    return inst
                P.add("pe", ymm, r=[_k("ogT", 0), _k("ogT", 1)] + [_k("wout", cc, jj) for cc in range(16) for jj in range(2)],
                      w=[_k("ps", pb)])
                P.add("dve", lambda e, mh=mh, pb=pb: e.tensor_tensor(hbuf[:, mh * 512:(mh + 1) * 512], hbuf[:, mh * 512:(mh + 1) * 512],
                                                                      psum[pb][:], ALU.add),
                      r=[_k("ps", pb), hk], w=[hk])
            P.add("sp", lambda e, t0=t0: e.dma_start(out=h_out[t0:t0 + 128, :], in_=hbuf[:]), r=[hk], dma=True)
        P.emit(es)


N_IT = 22
TOPK = 256
IDX_C0 = (64.0 ** -0.5) * (8.0 ** -0.5)


def mix0_phase(nc, h_in, h_out, I, C):
    with ExitStack() as es:
        P = Prog(nc)
        c = Ctx(nc, P, es)
        CC = 512
        win = c.sb("win", [128, 8, EVEN_IN], BF16)
        wout = c.sb("wout", [128, 8, D], BF16)
        stage = [c.sb("stage%d" % i, [128, CC], F32) for i in range(2)]
        gcol = c.sb("gcol", [128, 8], F32)
        gcol2 = c.sb("gcol2", [128, 8], F32)
        ident = c.sb("ident", [128, 128], BF16)
        ones = c.sb("ones", [128, 128], BF16)
        trim = c.sb("trim", [128, 128], F32)
        trir = c.sb("trir", [128, 128], F32)
        m01 = c.sb("m01", [128, 4, 128], F32)
        negm = c.sb("negm", [128, 128], F32)
        pw = c.sb("pw", [128, N_IT], F32)
        wa2 = c.sb("wa2", [16, 256], F32)
        ba2 = c.sb("ba2", [128, 256], F32)
        hbuf = c.sb("hbuf", [128, D], F32)
        xn = c.sb("xn", [128, D], BF16)
        xnT = c.sb("xnT", [128, 8, 128], BF16)
        junk = c.sb("junk", [128, D], BF16)
        gqk = c.sb("gqk", [128, 512], BF16)
        gv = c.sb("gv", [128, 512], BF16)
        gsl = c.sb("gsl", [128, 512], BF16)
        dq = c.sb("dq", [128, 512], BF16)
        dkb = c.sb("dkb", [128, 128], BF16)
        iq = c.sb("iq", [128, 512], BF16)
        ikb = c.sb("ikb", [128, 64], BF16)
        iw = c.sb("iw", [128, 8], F32)
        wabs = c.sb("wabs", [128, 8], F32)
        sgn = c.sb("sgn", [128, 8], F32)
        gaT = c.sb("gaT", [16, 128], F32)
        zb = c.sb("zb", [128, 256], F32)
        sp_ = c.sb("sp", [128, 256], F32)
        ecum = c.sb("ecum", [64, 4, 128], F32)
        encum = c.sb("encum", [64, 4, 128], F32)
        erev = c.sb("erev", [128, 256], F32)
        qtT = c.sb("qtT", [64, 4, 128], BF16)
        ktT = c.sb("ktT", [64, 4, 128], BF16)
        kend = c.sb("kend", [128, 256], BF16)
        atm = c.sb("atm", [128, 4, 128], BF16)
        Sg = c.sb("Sg", [64, 4, 128], F32)
        Sgb = c.sb("Sgb", [64, 4, 128], BF16)
        og = c.sb("og", [128, 512], BF16)
        mixT = c.sb("mixT", [128, 8, 128], BF16)
        qT = c.sb("qT", [128, 4, 128], BF16)
        kTc = c.sb("kTc", [128, T], BF16)
        vc = c.sb("vc", [128, NT, 128], BF16)
        ikTc = c.sb("ikTc", [64, T], BF16)
        iqT = c.sb("iqT", [64, 8, 128], BF16)
        sc = c.sb("sc", [128, T], F32)
        msk = c.sb("msk", [128, T], BF16)
        mskT = c.sb("mskT", [128, NT, 128], BF16)
        Rb = [c.sb("Rb%d" % i, [128, 512], F32) for i in range(2)]
        Eb = [c.sb("Eb%d" % i, [128, 512], BF16) for i in range(2)]
        PTb = [c.sb("PTb%d" % i, [128, 4, 128], BF16) for i in range(2)]
        rden = c.sb("rden", [128, 512], F32)
        ss = c.sb("ss", [128, 8], F32)
        rstd = c.sb("rstd", [128, 8], F32)
        st = c.sb("st", [128, 8], F32)
        hst = c.sb("hst", [128, N_IT], F32)
        psum = [c.ps("ps%d" % i) for i in range(8)]

        def dma(out_ap, in_ap, wk, **kw):
            P.add("sp", lambda e: e.dma_start(out=out_ap, in_=in_ap, **kw), w=[wk], dma=True)

        def bfv(i):
            return psum[i][:].bitcast(BF16)

        dma(gcol[:], I["even_attn_norm"][0].rearrange("(k p) -> p k", p=128), _k("gcol"), allow_slow_non_contiguous=True)
        P.add("dve", lambda e: e.memset(gcol2[:], 1.0), w=[_k("gcol2")])
        for cc in range(4):
            dma(gcol2[:, cc:cc + 1], I["even_gla_norm"][0].rearrange("(p o) -> p o", o=1), _k("gcol2"))
        dma(ident[:], C["ident"], _k("ident"))
        dma(ones[:], C["ones"], _k("ones"))
        dma(trim[:], C["gla_trim"], _k("trim"))
        dma(trir[:], C["gla_trir"], _k("trir"))
        dma(m01[:], C["gla_m01"], _k("m01"))
        dma(negm[:], C["dsa_negm"], _k("negm"))
        dma(pw[:], C["dsa_pw"], _k("pw"))
        dma(wa2[:], I["even_gla_wa2"][0], _k("wa2"))
        dma(ba2[:], I["even_gla_ba2"][0].partition_broadcast(128), _k("ba2"))
        P.add("dve", lambda e: e.memset(Sg[:], 0.0), w=[_k("Sg")])
        P.add("pool", lambda e: e.memset(Sgb[:], 0.0), w=[_k("Sgb")])
        load_weight_bf16(c, I["even_w_in"][0], D, EVEN_IN, win, "win", stage, gcol, _k("gcol"), CC)
        load_weight_bf16(c, I["even_w_out"][0], D, D, wout, "wout", stage, gcol2, _k("gcol2"), CC)
        WIN_ALL = [_k("win", k, j) for k in range(8) for j in range(6)]
        WOUT_ALL = [_k("wout", k, j) for k in range(8) for j in range(2)]

        for i in range(DBG["ntiles"]):
            t0 = i * 128
            hk = _k("hbuf")
            dma(hbuf[:], h_in[t0:t0 + 128, :], hk)
            rmsnorm_tile(c, hbuf[:], hk, xn[:], _k("xn"), junk[:], _k("junk"),
                         ss[:, 4:5], _k("ss", 4), rstd[:, 4:5], _k("rstd", 4))

            def tr8(e):
                inst = None
                v = bfv(0)
                for cc in range(8):
                    inst = e.transpose(v[:, cc * 128:(cc + 1) * 128], xn[:, cc * 128:(cc + 1) * 128], ident[:])
                return inst
            P.add("pe", tr8, r=[_k("xn"), _k("ident")], w=[_k("ps", 0)])
            P.add("act", lambda e: e.copy(xnT[:].rearrange("p c t -> p (c t)"), bfv(0)), r=[_k("ps", 0)], w=[_k("xnT")])

            def proj(c0, c1, pb, col0=0, m=128):
                def f(e):
                    inst = None
                    for k in range(8):
                        inst = e.matmul(psum[pb][:, col0:col0 + (c1 - c0)], xnT[:, k, :], win[:, k, c0:c1],
                                        start=(k == 0), stop=(k == 7))
                    return inst
                P.add("pe", f, r=[_k("xnT")] + WIN_ALL, w=[_k("ps", pb)])

            proj(0, 512, 2)
            P.add("act", lambda e: e.copy(gqk[:], psum[2][:]), r=[_k("ps", 2)], w=[_k("gqk")])
            proj(512, 1024, 3)
            P.add("dve", lambda e: e.tensor_copy(gv[:], psum[3][:]), r=[_k("ps", 3)], w=[_k("gv")])
            proj(1040, 1552, 2)
            P.add("act", lambda e: e.activation(gsl[:], psum[2][:], AF.Silu), r=[_k("ps", 2)], w=[_k("gsl")])
            proj(1552, 2064, 3)
            P.add("dve", lambda e: e.tensor_copy(dq[:], psum[3][:]), r=[_k("ps", 3)], w=[_k("dq")])
            proj(2064, 2320, 2)
            P.add("act", lambda e: e.copy(dkb[:], psum[2][:, 0:128]), r=[_k("ps", 2)], w=[_k("dkb")])
            P.add("act", lambda e, i=i: e.copy(vc[:, i, :], psum[2][:, 128:256]), r=[_k("ps", 2)], w=[_k("vc", i)])
            proj(2320, 2832, 3)
            P.add("dve", lambda e: e.tensor_copy(iq[:], psum[3][:]), r=[_k("ps", 3)], w=[_k("iq")])
            proj(2832, 2904, 2)
            P.add("act", lambda e: e.copy(ikb[:], psum[2][:, 0:64]), r=[_k("ps", 2)], w=[_k("ikb")])
            P.add("act", lambda e: e.copy(iw[:], psum[2][:, 64:72]), r=[_k("ps", 2)], w=[_k("iw")])

            def gaf(e):
                inst = None
                for k in range(8):
                    inst = e.matmul(psum[3][0:16, 0:128], win[:, k, 1024:1040], xnT[:, k, :], start=(k == 0), stop=(k == 7))
                return inst
            P.add("pe", gaf, r=[_k("xnT")] + WIN_ALL, w=[_k("ps", 3)])
            P.add("act", lambda e: e.copy(gaT[:], psum[3][0:16, 0:128]), r=[_k("ps", 3)], w=[_k("gaT")])

            P.add("pe", lambda e: e.matmul(psum[2][:, 0:256], gaT[:], wa2[:], start=True, stop=True),
                  r=[_k("gaT"), _k("wa2")], w=[_k("ps", 2)])
            P.add("dve", lambda e: e.tensor_tensor(zb[:], psum[2][:, 0:256], ba2[:], ALU.add),
                  r=[_k("ps", 2), _k("ba2")], w=[_k("zb")])
            P.add("act", lambda e: e.activation(zb[:], zb[:], AF.Exp, scale=-1.0), r=[_k("zb")], w=[_k("zb")])
            P.add("act", lambda e: e.activation(sp_[:], zb[:], AF.Ln, bias=1.0), r=[_k("zb")], w=[_k("sp")])

            def cumf(e):
                inst = None
                for h in range(4):
                    inst = e.matmul(psum[3][0:64, h * 128:(h + 1) * 128], sp_[:, h * 64:(h + 1) * 64], trim[:], start=True, stop=True)
                return inst
            P.add("pe", cumf, r=[_k("sp"), _k("trim")], w=[_k("ps", 3)])
            P.add("pe", lambda e: e.matmul(psum[2][:, 0:256], trir[:], sp_[:], start=True, stop=True),
                  r=[_k("sp"), _k("trir")], w=[_k("ps", 2)])
            P.add("act", lambda e: e.activation(ecum[:].rearrange("p h t -> p (h t)"), psum[3][0:64, :], AF.Exp),
                  r=[_k("ps", 3)], w=[_k("ecum")])
            P.add("act", lambda e: e.activation(encum[:].rearrange("p h t -> p (h t)"), psum[3][0:64, :], AF.Exp, scale=-1.0),
                  r=[_k("ps", 3)], w=[_k("encum")])
            P.add("act", lambda e: e.activation(erev[:], psum[2][:, 0:256], AF.Exp), r=[_k("ps", 2)], w=[_k("erev")])

            def trqk(e):
                inst = None
                v = bfv(0)
                for hh in range(8):
                    inst = e.transpose(v[0:64, hh * 128:(hh + 1) * 128], gqk[:, hh * 64:(hh + 1) * 64], ident[:])
                return inst
            P.add("pe", trqk, r=[_k("gqk"), _k("ident")], w=[_k("ps", 0)])
            P.add("dve", lambda e: e.scalar_tensor_tensor(qtT[:].rearrange("p h t -> p (h t)"), bfv(0)[0:64, 0:512], 0.125,
                                                          ecum[:].rearrange("p h t -> p (h t)"), ALU.mult, ALU.mult),
                  r=[_k("ps", 0), _k("ecum")], w=[_k("qtT")])
            P.add("dve", lambda e: e.tensor_tensor(ktT[:].rearrange("p h t -> p (h t)"), bfv(0)[0:64, 512:1024],
                                                   encum[:].rearrange("p h t -> p (h t)"), ALU.mult),
                  r=[_k("ps", 0), _k("encum")], w=[_k("ktT")])
            P.add("dve", lambda e: e.tensor_tensor(kend[:], gqk[:, 256:512], erev[:], ALU.mult),
                  r=[_k("gqk"), _k("erev")], w=[_k("kend")])

            def atf(e):
                inst = None
                for h in range(4):
                    inst = e.matmul(psum[3][:, h * 128:(h + 1) * 128], ktT[:, h, :], qtT[:, h, :], start=True, stop=True)
                return inst
            P.add("pe", atf, r=[_k("ktT"), _k("qtT")], w=[_k("ps", 3)])
            P.add("dve", lambda e: e.tensor_tensor(atm[:].rearrange("p h t -> p (h t)"), psum[3][:],
                                                   m01[:].rearrange("p h t -> p (h t)"), ALU.mult),
                  r=[_k("ps", 3), _k("m01")], w=[_k("atm")])

            def of(e):
                inst = None
                for h in range(4):
                    e.matmul(psum[2][:, h * 128:(h + 1) * 128], atm[:, h, :], gv[:, h * 128:(h + 1) * 128], start=True, stop=False)
                    inst = e.matmul(psum[2][:, h * 128:(h + 1) * 128], qtT[:, h, :], Sgb[:, h, :], start=False, stop=True)
                return inst
            P.add("pe", of, r=[_k("atm"), _k("gv"), _k("qtT"), _k("Sgb")], w=[_k("ps", 2)])

            def dsf(e):
                inst = None
                for h in range(4):
                    inst = e.matmul(psum[3][0:64, h * 128:(h + 1) * 128], kend[:, h * 64:(h + 1) * 64], gv[:, h * 128:(h + 1) * 128],
                                    start=True, stop=True)
                return inst
            P.add("pe", dsf, r=[_k("kend"), _k("gv")], w=[_k("ps", 3)])
            elast = ecum[:, :, 127:128].to_broadcast([64, 4, 128])
            P.add("dve", lambda e, elast=elast: e.tensor_tensor(Sg[:], Sg[:], elast, ALU.mult), r=[_k("Sg"), _k("ecum")], w=[_k("Sg")])
            P.add("dve", lambda e: e.tensor_tensor(Sg[:].rearrange("p h t -> p (h t)"), Sg[:].rearrange("p h t -> p (h t)"),
                                                   psum[3][0:64, :], ALU.add), r=[_k("Sg"), _k("ps", 3)], w=[_k("Sg")])
            P.add("pool", lambda e: e.tensor_copy(Sgb[:], Sg[:]), r=[_k("Sg")], w=[_k("Sgb")])
            for h in range(4):
                P.add("act", lambda e, h=h: e.activation(junk[:, 0:128], psum[2][:, h * 128:(h + 1) * 128], AF.Square, accum_out=ss[:, h:h + 1]),
                      r=[_k("ps", 2)], w=[_k("junk"), _k("ss", h)])
            rsqrt_small(P, rstd[:, 0:4], _k("rstd", 0), ss[:, 0:4], [_k("ss", h) for h in range(4)], 1.0 / 128)
            for h in range(4):
                P.add("dve", lambda e, h=h: e.scalar_tensor_tensor(og[:, h * 128:(h + 1) * 128], psum[2][:, h * 128:(h + 1) * 128], rstd[:, h:h + 1],
                                                                    gsl[:, h * 128:(h + 1) * 128], ALU.mult, ALU.mult),
                      r=[_k("ps", 2), _k("rstd", 0), _k("gsl")], w=[_k("og")])

            def trog(e):
                inst = None
                v = bfv(0)
                for cc in range(4):
                    inst = e.transpose(v[:, cc * 128:(cc + 1) * 128], og[:, cc * 128:(cc + 1) * 128], ident[:])
                return inst
            P.add("pe", trog, r=[_k("og"), _k("ident")], w=[_k("ps", 0)])
            P.add("act", lambda e: e.copy(mixT[:, 0:4, :].rearrange("p c t -> p (c t)"), bfv(0)[:, 0:512]),
                  r=[_k("ps", 0)], w=[_k("mixT", 0)])

            def trdq(e):
                inst = None
                v = bfv(1)
                for cc in range(4):
                    inst = e.transpose(v[:, cc * 128:(cc + 1) * 128], dq[:, cc * 128:(cc + 1) * 128], ident[:])
                inst = e.transpose(v[:, 512:640], dkb[:], ident[:])
                return inst
            P.add("pe", trdq, r=[_k("dq"), _k("dkb"), _k("ident")], w=[_k("ps", 1)])
            P.add("act", lambda e: e.copy(qT[:].rearrange("p c t -> p (c t)"), bfv(1)[:, 0:512]), r=[_k("ps", 1)], w=[_k("qT")])
            P.add("act", lambda e, t0=t0: e.copy(kTc[:, t0:t0 + 128], bfv(1)[:, 512:640]), r=[_k("ps", 1)], w=[_k("kTc", i)])

            def triq(e):
                inst = None
                v = bfv(0)
                for hh in range(8):
                    inst = e.transpose(v[0:64, hh * 128:(hh + 1) * 128], iq[:, hh * 64:(hh + 1) * 64], ident[:])
                return inst
            P.add("pe", triq, r=[_k("iq"), _k("ident")], w=[_k("ps", 0)])
            P.add("act", lambda e: e.copy(iqT[:].rearrange("p c t -> p (c t)"), bfv(0)[0:64, :]), r=[_k("ps", 0)], w=[_k("iqT")])
            P.add("pe", lambda e: e.transpose(bfv(1)[0:64, 0:128], ikb[:], ident[:]), r=[_k("ikb"), _k("ident")], w=[_k("ps", 1)])
            P.add("act", lambda e, t0=t0: e.copy(ikTc[:, t0:t0 + 128], bfv(1)[0:64, 0:128]), r=[_k("ps", 1)], w=[_k("ikTc", i)])
            P.add("act", lambda e: e.activation(wabs[:], iw[:], AF.Abs, scale=IDX_C0), r=[_k("iw")], w=[_k("wabs")])
            P.add("act", lambda e: e.sign(sgn[:], iw[:]), r=[_k("iw")], w=[_k("sgn")])

            nkeys = (i + 1) * 128
            ngrp = (nkeys + 511) // 512
            for gk in range(ngrp):
                k0 = gk * 512
                n = min(512, nkeys - k0)
                kk = [_k("ikTc", b) for b in range(k0 // 128, (k0 + n) // 128)]
                for hI in range(8):
                    pb = 2 + (hI % 2)
                    rb = hI % 2
                    P.add("pe", lambda e, pb=pb, hI=hI, k0=k0, n=n: e.matmul(psum[pb][:, 0:n], iqT[:, hI, :], ikTc[:, k0:k0 + n], start=True, stop=True),
                          r=[_k("iqT")] + kk, w=[_k("ps", pb)])
                    P.add("act", lambda e, pb=pb, rb=rb, hI=hI, n=n: e.activation(Rb[rb][:, 0:n], psum[pb][:, 0:n], AF.Relu, scale=wabs[:, hI:hI + 1]),
                          r=[_k("ps", pb), _k("wabs")], w=[_k("Rb", rb)])
                    if hI == 0:
                        P.add("dve", lambda e, rb=rb, k0=k0, n=n: e.tensor_scalar(sc[:, k0:k0 + n], Rb[rb][:, 0:n], sgn[:, 0:1], None, ALU.mult),
                              r=[_k("Rb", rb), _k("sgn")], w=[_k("sc")])
                    else:
                        P.add("dve", lambda e, rb=rb, k0=k0, n=n, hI=hI: e.scalar_tensor_tensor(sc[:, k0:k0 + n], Rb[rb][:, 0:n], sgn[:, hI:hI + 1],
                                                                                               sc[:, k0:k0 + n], ALU.mult, ALU.add),
                              r=[_k("Rb", rb), _k("sgn"), _k("sc")], w=[_k("sc")])
            scv = sc[:, 0:nkeys]
            P.add("dve", lambda e, scv=scv: e.tensor_reduce(st[:, 0:1], scv, AX.X, ALU.max), r=[_k("sc")], w=[_k("st", 0)])
            P.add("dve", lambda e, scv=scv: e.tensor_reduce(st[:, 1:2], scv, AX.X, ALU.min), r=[_k("sc")], w=[_k("st", 1)])
            P.add("dve", lambda e, t0=t0: e.tensor_tensor(sc[:, t0:t0 + 128], sc[:, t0:t0 + 128], negm[:], ALU.add),
                  r=[_k("sc"), _k("negm")], w=[_k("sc")])
            P.add("dve", lambda e: e.tensor_tensor(st[:, 2:3], st[:, 0:1], st[:, 1:2], ALU.subtract), r=[_k("st", 0), _k("st", 1)], w=[_k("st", 2)])
            P.add("dve", lambda e: e.tensor_scalar(st[:, 2:3], st[:, 2:3], 1.000001, 1e-30, ALU.mult, ALU.add), r=[_k("st", 2)], w=[_k("st", 2)])
            P.add("dve", lambda e: e.tensor_scalar(hst[:], pw[:], st[:, 2:3], None, ALU.mult), r=[_k("st", 2), _k("pw")], w=[_k("hst")])
            for k in range(N_IT):
                P.add("dve", lambda e, k=k: e.tensor_tensor(st[:, 3:4], st[:, 1:2], hst[:, k:k + 1], ALU.add),
                      r=[_k("st", 1), _k("hst")], w=[_k("st", 3)])
                P.add("dve", lambda e, scv=scv, nkeys=nkeys: e.tensor_scalar(msk[:, 0:nkeys], scv, st[:, 3:4], None, ALU.is_ge, ALU.add, accum_out=st[:, 4:5]),
                      r=[_k("sc"), _k("st", 3)], w=[_k("msk"), _k("st", 4)])
                P.add("dve", lambda e, k=k: e.tensor_scalar(st[:, 5:6], st[:, 4:5], TOPK - 0.5, hst[:, k:k + 1], ALU.is_ge, ALU.mult),
                      r=[_k("st", 4), _k("hst")], w=[_k("st", 5)])
                P.add("dve", lambda e: e.tensor_tensor(st[:, 1:2], st[:, 1:2], st[:, 5:6], ALU.add),
                      r=[_k("st", 1), _k("st", 5)], w=[_k("st", 1)])
            P.add("dve", lambda e, scv=scv, nkeys=nkeys: e.tensor_scalar(msk[:, 0:nkeys], scv, st[:, 1:2], None, ALU.is_ge),
                  r=[_k("sc"), _k("st", 1)], w=[_k("msk")])
            for b0 in range(0, i + 1, 8):
                nb = min(8, i + 1 - b0)
                pb = (b0 // 8) % 2

                def trm(e, b0=b0, nb=nb, pb=pb):
                    inst = None
                    v = bfv(pb)
                    for bb in range(nb):
                        inst = e.transpose(v[:, bb * 128:(bb + 1) * 128], msk[:, (b0 + bb) * 128:(b0 + bb + 1) * 128], ident[:])
                    return inst
                P.add("pe", trm, r=[_k("msk"), _k("ident")], w=[_k("ps", pb)])
                P.add("act", lambda e, b0=b0, nb=nb, pb=pb: e.copy(mskT[:, b0:b0 + nb, :].rearrange("p c t -> p (c t)"), bfv(pb)[:, 0:nb * 128]),
                      r=[_k("ps", pb)], w=[_k("mskT", b0 // 8)])
            for j in range(i + 1):
                pb = 4 + (j % 2)
                eb = j % 2
                P.add("pe", lambda e, pb=pb, j=j: e.matmul(psum[pb][:], kTc[:, j * 128:(j + 1) * 128], qT[:].rearrange("p c t -> p (c t)"), start=True, stop=True),
                      r=[_k("kTc", j), _k("qT")], w=[_k("ps", pb)])
                P.add("act", lambda e, pb=pb, eb=eb: e.activation(Eb[eb][:], psum[pb][:], AF.Exp, scale=128.0 ** -0.5),
                      r=[_k("ps", pb)], w=[_k("Eb", eb)])
                mb = mskT[:, j:j + 1, :].to_broadcast([128, 4, 128])
                P.add("dve", lambda e, eb=eb, mb=mb: e.tensor_tensor(PTb[eb][:], Eb[eb][:].rearrange("p (h t) -> p h t", h=4), mb, ALU.mult),
                      r=[_k("Eb", eb), _k("mskT", j // 8)], w=[_k("PTb", eb)])

                def pv(e, eb=eb, j=j, i=i):
                    e.matmul(psum[6][:], vc[:, j, :], PTb[eb][:].rearrange("p h t -> p (h t)"), start=(j == 0), stop=(j == i))
                    return e.matmul(psum[7][:], ones[:], PTb[eb][:].rearrange("p h t -> p (h t)"), start=(j == 0), stop=(j == i))
                P.add("pe", pv, r=[_k("vc", j), _k("PTb", eb), _k("ones")], w=[_k("ps", 6), _k("ps", 7)])
            P.add("dve", lambda e: e.reciprocal(rden[:], psum[7][:]), r=[_k("ps", 7)], w=[_k("rden")])
            P.add("dve", lambda e: e.tensor_tensor(mixT[:, 4:8, :].rearrange("p c t -> p (c t)"), psum[6][:], rden[:], ALU.mult),
                  r=[_k("ps", 6), _k("rden")], w=[_k("mixT", 1)])
            for mh in range(2):
                pb = 2 + mh

                def ymm(e, mh=mh, pb=pb):
                    inst = None
                    for cc in range(8):
                        inst = e.matmul(psum[pb][:], mixT[:, cc, :], wout[:, cc, mh * 512:(mh + 1) * 512], start=(cc == 0), stop=(cc == 7))
                    return inst
                P.add("pe", ymm, r=[_k("mixT", 0), _k("mixT", 1)] + WOUT_ALL, w=[_k("ps", pb)])
                P.add("dve", lambda e, mh=mh, pb=pb: e.tensor_tensor(hbuf[:, mh * 512:(mh + 1) * 512], hbuf[:, mh * 512:(mh + 1) * 512], psum[pb][:], ALU.add),
                      r=[_k("ps", pb), hk], w=[hk])
            P.add("sp", lambda e, t0=t0: e.dma_start(out=h_out[t0:t0 + 128, :], in_=hbuf[:]), r=[hk], dma=True)
        P.emit(es)


def host_consts():
    import ml_dtypes
    cst = {}
    cst["ident"] = np.eye(128, dtype=ml_dtypes.bfloat16)
    half = 128
    inv = 10000.0 ** (-np.arange(half, dtype=np.float32) / half)
    ang = np.arange(T, dtype=np.float32)[:, None] * inv[None, :].astype(np.float32)
    cst["rope_cos"] = np.cos(ang).astype(np.float32)
    cst["rope_sin"] = np.sin(ang).astype(np.float32)
    g = np.array(RET_GAMMA, dtype=np.float64)
    ii = np.arange(128)
    rel = ii[None, :] - ii[:, None]
    dtm = np.zeros((128, 4, 128), np.float64)
    for h in range(4):
        dtm[:, h, :] = np.where(rel >= 0, g[h] ** np.maximum(rel, 0), 0.0) / 16.0
    cst["ret_dt"] = dtm.astype(np.float32)
    xi8 = np.zeros((128, 8, 128), np.float64)
    for cc in range(8):
        xi8[:, cc, :] = (g[cc // 2] ** (ii + 1.0))[None, :]
    cst["ret_xi8"] = xi8.astype(np.float32)
    cst["ones"] = np.ones((128, 128), dtype=ml_dtypes.bfloat16)
    le = (ii[:, None] <= ii[None, :])
    cst["gla_trim"] = np.where(le, -1.0 / 16.0, 0.0).astype(np.float32)
    cst["gla_trir"] = np.where(~le, -1.0 / 16.0, 0.0).astype(np.float32)
    cst["gla_m01"] = np.repeat(le[:, None, :], 4, axis=1).astype(np.float32)
    cst["dsa_negm"] = np.where(ii[None, :] <= ii[:, None], 0.0, -1e30).astype(np.float32)
    cst["dsa_pw"] = np.repeat((0.5 ** (np.arange(N_IT) + 1.0))[None, :], 128, axis=0).astype(np.float32)
    zeta = np.zeros((128, 4), np.float64)
    for h in range(4):
        zeta[:, h] = g[h] ** (127.0 - ii) / 16.0
    cst["ret_zeta"] = zeta.astype(np.float32)
    return cst


INPUT_SHAPES = {
    "even_attn_norm": [1, D], "even_w_in": [1, D, EVEN_IN], "even_gla_wa2": [1, 16, 256],
    "even_gla_ba2": [1, 256], "even_gla_norm": [1, 128], "even_w_out": [1, D, D],
    "odd_attn_norm": [1, D], "odd_w_in": [1, D, ODD_IN], "odd_ret_norm": [1, 512], "odd_w_out": [1, 2048, D],
    "ffn_norm": [2, D], "ffn_w_gate": [2, D, DFF], "ffn_w_up": [2, D, DFF], "ffn_w_down": [2, DFF, D],
    "final_norm": [D],
}


def build(phases=("mix0", "ffn0", "mix1", "ffn1")):
    nc = bass.Bass("TRN2", target_bir_lowering=False)
    x = nc.dram_tensor("x", [T, D], F32, kind="ExternalInput").ap()
    out = nc.dram_tensor("out", [T, D], F32, kind="ExternalOutput").ap()
    I = {k: nc.dram_tensor(k, shp, F32, kind="ExternalInput").ap() for k, shp in INPUT_SHAPES.items()}
    cst = host_consts()
    C = {}
    for k, v in cst.items():
        C[k] = nc.dram_tensor(k, list(v.shape), BF16 if v.dtype != np.float32 else F32, kind="ExternalInput").ap()
    scr = [nc.dram_tensor("scr%d" % i, [T, D], F32, kind="Internal").ap() for i in range(3)]
    bufs = [x] + scr[:len(phases) - 1] + [out]
    for pi, ph in enumerate(phases):
        hin, hout = bufs[pi], bufs[pi + 1]
        if ph == "ffn0":
            ffn_phase(nc, hin, hout, I["ffn_norm"][0], I["ffn_w_gate"][0], I["ffn_w_up"][0], I["ffn_w_down"][0], C["ident"])
        elif ph == "ffn1":
            ffn_phase(nc, hin, hout, I["ffn_norm"][1], I["ffn_w_gate"][1], I["ffn_w_up"][1], I["ffn_w_down"][1], C["ident"],
                      final_g=I["final_norm"])
        elif ph == "mix1":
            mix1_phase(nc, hin, hout, I["odd_attn_norm"][0], I["odd_w_in"][0], I["odd_ret_norm"][0], I["odd_w_out"][0],
                       C["ident"], C["rope_cos"], C["rope_sin"], C["ret_dt"], C["ret_xi8"], C["ret_zeta"])
        elif ph == "mix0":
            mix0_phase(nc, hin, hout, I, C)
    return nc


def make_inputs(inputs, b):
    m = {k: np.ascontiguousarray(np.asarray(inputs[k], dtype=np.float32)) for k in INPUT_SHAPES}
    m["x"] = np.ascontiguousarray(np.asarray(inputs["x"][b], dtype=np.float32))
    m.update(host_consts())
    return m


def kernel(**inputs):
    nc = build()
    in_maps = [make_inputs(inputs, b) for b in range(8)]
    res = run_bass_kernel_spmd(nc, in_maps, core_ids=list(range(8)))
    return np.stack([np.asarray(r["out"], dtype=np.float32) for r in res.results], axis=0)
```

```python
import math
from contextlib import ExitStack

import numpy as np
import concourse.bass as bass
import concourse.mybir as mybir
from concourse.bass_utils import run_bass_kernel_spmd

F32 = mybir.dt.float32
BF16 = mybir.dt.bfloat16
AF = mybir.ActivationFunctionType
ALU = mybir.AluOpType
AX = mybir.AxisListType

T = 4096
D = 1024
DFF = 2816
NT = T // 128
EPS = 1e-6
EVEN_IN = 2904
ODD_IN = 6144


class _Op:
    __slots__ = ("eng", "fn", "deps", "dma", "sig", "sigidx", "dsem", "dval", "ndep")


class Prog:
    COMPUTE = ("pe", "act", "dve", "pool")

    def __init__(self, nc, n_dma_sems=16):
        self.nc = nc
        self.ops = []
        self.lastw = {}
        self.readers = {}
        self.n_dma_sems = n_dma_sems
        self.dma_rr = {"sp": 0, "pool": 0, "act": 0}
        self.dma_cnt = {}

    def add(self, eng, fn, r=(), w=(), dma=False):
        i = len(self.ops)
        deps = set()
        pr = [k for k in r if k[0] == "ps" and k not in w]
        if pr:
            w = list(w) + pr
        for k in r:
            a = self.lastw.get(k)
            if a is not None:
                deps.add(a)
        for k in w:
            a = self.lastw.get(k)
            if a is not None:
                deps.add(a)
            rd = self.readers.get(k)
            if rd:
                deps.update(rd.values())
        op = _Op()
        op.eng = eng
        op.fn = fn
        op.dma = dma
        op.sig = False
        op.sigidx = 0
        op.dsem = None
        op.dval = 0
        fdeps = []
        for a in deps:
            A = self.ops[a]
            if (not dma) and (not A.dma) and eng == "pe" and A.eng == "pe":
                continue
            fdeps.append(a)
        op.deps = sorted(fdeps)
        if dma:
            q = self.dma_rr[eng]
            self.dma_rr[eng] = (q + 1) % self.n_dma_sems
            key = (eng, q)
            self.dma_cnt[key] = self.dma_cnt.get(key, 0) + 1
            op.dsem = key
            op.dval = 16 * self.dma_cnt[key]
        self.ops.append(op)
        for k in w:
            self.lastw[k] = i
            self.readers[k] = {}
        for k in r:
            d = self.readers.setdefault(k, {})
            d[("dma", i) if dma else eng] = i
        return i

    def emit(self, es):
        nc = self.nc
        ops = self.ops
        for op in ops:
            for a in op.deps:
                if not ops[a].dma:
                    ops[a].sig = True
        cnt = {e: 0 for e in self.COMPUTE + ("sp",)}
        for op in ops:
            if op.sig and not op.dma:
                cnt[op.eng] += 1
                op.sigidx = cnt[op.eng]
        sems = {e: es.enter_context(nc.semaphore("s_" + e)) for e in cnt}
        dsems = {}
        for key in self.dma_cnt:
            dsems[key] = es.enter_context(nc.semaphore("d_%s%d" % key))
        engines = {"pe": "tensor", "act": "scalar", "dve": "vector", "pool": "gpsimd", "sp": "sync"}
        with nc.Block() as block:
            for e, attr in engines.items():
                mine = [op for op in ops if op.eng == e]

                def body(engine, mine=mine, e=e):
                    known = {}

                    def wait(sem_key, sem, val):
                        if known.get(sem_key, 0) >= val:
                            return
                        engine.wait_ge(sem, val)
                        known[sem_key] = val

                    for op in mine:
                        for a in op.deps:
                            A = ops[a]
                            if A.dma:
                                wait(A.dsem, dsems[A.dsem], A.dval)
                            else:
                                wait(A.eng, sems[A.eng], A.sigidx)
                        if op.dma:
                            if op.dval > 16:
                                wait(op.dsem, dsems[op.dsem], op.dval - 16)
                            inst = op.fn(engine)
                            inst.then_inc(dsems[op.dsem], 16)
                        else:
                            inst = op.fn(engine)
                            if op.sig:
                                inst.then_inc(sems[e], 1)
                    for key, c in self.dma_cnt.items():
                        if key[0] == e:
                            wait(key, dsems[key], 16 * c)

                getattr(block, attr)(body)


def _k(name, *idx):
    return (name,) + idx


class Ctx:
    def __init__(self, nc, P, es):
        self.nc = nc
        self.P = P
        self.es = es
        self.rr = 0

    UID = [0]

    def sb(self, name, shape, dt):
        Ctx.UID[0] += 1
        return self.es.enter_context(self.nc.sbuf_tensor("sb%d_%s" % (Ctx.UID[0], name), list(shape), dt))

    def ps(self, name):
        Ctx.UID[0] += 1
        return self.es.enter_context(self.nc.psum_tensor("ps%d_%s" % (Ctx.UID[0], name), [128, 512], F32))


def rsqrt_small(P, out_ap, outkey, in_ap, inkey, scale, eps=EPS):
    inkeys = inkey if isinstance(inkey, list) else [inkey]
    P.add("dve", lambda e: e.tensor_scalar(out_ap, in_ap, scale, eps, ALU.mult, ALU.add),
          r=inkeys, w=[outkey])
    P.add("act", lambda e: e.activation(out_ap, out_ap, AF.Ln), r=[outkey], w=[outkey])
    P.add("act", lambda e: e.activation(out_ap, out_ap, AF.Exp, scale=-0.5), r=[outkey], w=[outkey])


def rmsnorm_tile(c, h_ap, hkey, xn_ap, xnkey, junk_ap, junkkey, ss_ap, sskey, rstd_ap, rstdkey):
    P = c.P
    P.add("act", lambda e: e.activation(junk_ap, h_ap, AF.Square, accum_out=ss_ap),
          r=[hkey], w=[junkkey, sskey])
    rsqrt_small(P, rstd_ap, rstdkey, ss_ap, sskey, 1.0 / D)
    P.add("dve", lambda e: e.tensor_scalar(xn_ap, h_ap, rstd_ap, None, ALU.mult),
          r=[hkey, rstdkey], w=[xnkey])


def load_weight_bf16(c, w_dram, rows, cols, dst, dstname, stage, gcol=None, gkey=None, colchunk=704, gmap=None):
    P = c.P
    nk = rows // 128
    nch = (cols + colchunk - 1) // colchunk
    cnt = 0
    for k in range(nk):
        for j in range(nch):
            c0 = j * colchunk
            c1 = min(cols, c0 + colchunk)
            s = cnt % len(stage)
            st = stage[s]
            skey = _k("stage", s)
            src = w_dram[k * 128:(k + 1) * 128, c0:c1]
            P.add("sp", lambda e, st=st, src=src, n=c1 - c0: e.dma_start(out=st[:, 0:n], in_=src),
                  w=[skey], dma=True)
            dap = dst[:, k, c0:c1]
            sap = st[:, 0:c1 - c0]
            eng = ("dve", "pool", "act")[cnt % 3]
            rk = [skey] + ([gkey] if gcol is not None else [])
            if gcol is None:
                if eng == "act":
                    P.add("act", lambda e, dap=dap, sap=sap: e.copy(dap, sap), r=rk, w=[_k(dstname, k, j)])
                else:
                    P.add(eng, lambda e, dap=dap, sap=sap: e.tensor_copy(dap, sap), r=rk, w=[_k(dstname, k, j)])
            else:
                kk_ = gmap(k) if gmap is not None else k
                g = gcol[:, kk_:kk_ + 1]
                if eng == "act":
                    P.add("act", lambda e, dap=dap, sap=sap, g=g: e.activation(dap, sap, AF.Copy, scale=g),
                          r=rk, w=[_k(dstname, k, j)])
                else:
                    P.add(eng, lambda e, dap=dap, sap=sap, g=g: e.tensor_scalar(dap, sap, g, None, ALU.mult),
                          r=rk, w=[_k(dstname, k, j)])
            cnt += 1
    return [_k(dstname, k, j) for k in range(nk) for j in range(nch)]


def wkeys(dstname, k, c0, c1, colchunk=704):
    return [_k(dstname, k, j) for j in range(c0 // colchunk, (c1 - 1) // colchunk + 1)]


def ffn_phase(nc, h_in, h_out, g_dram, wg_d, wu_d, wd_d, ident_d, final_g=None):
    with ExitStack() as es:
        P = Prog(nc)
        c = Ctx(nc, P, es)
        CC = 704
        wg = c.sb("wg", [128, 8, DFF], BF16)
        wu = c.sb("wu", [128, 8, DFF], BF16)
        wd = c.sb("wd", [128, 22, D], BF16)
        stage = [c.sb("stage%d" % i, [128, CC], F32) for i in range(2)]
        gcol = c.sb("gcol", [128, 8], F32)
        ident = c.sb("ident", [128, 128], BF16)
        hbuf = c.sb("hbuf", [128, 4, D], F32)
        xn = [c.sb("xn%d" % i, [128, D], BF16) for i in range(2)]
        xnT = c.sb("xnT", [128, 8, 512], BF16)
        actT = c.sb("actT", [128, 22, 512], BF16)
        sg = [c.sb("sg%d" % i, [128, 512], F32) for i in range(2)]
        junk = c.sb("junk", [128, D], BF16)
        ss = c.sb("ss", [128, 8], F32)
        rstd = c.sb("rstd", [128, 8], F32)
        if final_g is not None:
            fg = c.sb("fg", [128, D], F32)
        psum = [c.ps("ps%d" % i) for i in range(8)]

        P.add("sp", lambda e: e.dma_start(out=gcol[:], in_=g_dram.rearrange("(k p) -> p k", p=128),
                                          allow_slow_non_contiguous=True), w=[_k("gcol")], dma=True)
        P.add("sp", lambda e: e.dma_start(out=ident[:], in_=ident_d), w=[_k("ident")], dma=True)
        if final_g is not None:
            P.add("sp", lambda e: e.dma_start(out=fg[:], in_=final_g.partition_broadcast(128)),
                  w=[_k("fg")], dma=True)
        load_weight_bf16(c, wg_d, D, DFF, wg, "wg", stage, gcol, _k("gcol"), CC)
        load_weight_bf16(c, wu_d, D, DFF, wu, "wu", stage, gcol, _k("gcol"), CC)
        load_weight_bf16(c, wd_d, DFF, D, wd, "wd", stage, None, None, CC)

        nsup = T // 512
        tcount = 0
        for s in range(nsup):
            for j in range(4):
                t0 = s * 512 + j * 128
                hk = _k("hbuf", j)
                hap = hbuf[:, j, :]
                P.add("sp", lambda e, hap=hap, t0=t0: e.dma_start(out=hap, in_=h_in[t0:t0 + 128, :]),
                      w=[hk], dma=True)
                b = tcount % 2
                rmsnorm_tile(c, hap, hk, xn[b][:], _k("xn", b), junk[:], _k("junk"),
                             ss[:, j:j + 1], _k("ss", j), rstd[:, j:j + 1], _k("rstd", j))
                pb = tcount % 2
                pst = psum[pb].bitcast(BF16) if hasattr(psum[pb], "bitcast") else psum[pb][:].bitcast(BF16)

                def tr(e, b=b, pst=pst):
                    inst = None
                    for cc in range(8):
                        inst = e.transpose(pst[:, cc * 128:(cc + 1) * 128], xn[b][:, cc * 128:(cc + 1) * 128], ident[:])
                    return inst
                P.add("pe", tr, r=[_k("xn", b), _k("ident")], w=[_k("ps", pb)])
                dst = xnT[:, :, j * 128:(j + 1) * 128]
                src = pst[:, 0:1024].rearrange("p (c t) -> p c t", c=8)
                P.add("act", lambda e, dst=dst, src=src: e.copy(dst, src), r=[_k("ps", pb)], w=[_k("xnT", j)])
                tcount += 1
            xk = [_k("xnT", j) for j in range(4)]
            for f in range(22):
                pg = 2 + (f % 2)
                pu = 4 + (f % 2)

                def mm(e, w_, pi, f=f):
                    inst = None
                    for k in range(8):
                        inst = e.matmul(psum[pi][:], w_[:, k, f * 128:(f + 1) * 128], xnT[:, k, :],
                                        start=(k == 0), stop=(k == 7))
                    return inst
                wkg = [kk for k in range(8) for kk in wkeys("wg", k, f * 128, (f + 1) * 128, CC)]
                wku = [kk for k in range(8) for kk in wkeys("wu", k, f * 128, (f + 1) * 128, CC)]
                P.add("pe", lambda e, pg=pg, mm=mm: mm(e, wg, pg), r=xk + wkg, w=[_k("ps", pg)])
                P.add("pe", lambda e, pu=pu, mm=mm: mm(e, wu, pu), r=xk + wku, w=[_k("ps", pu)])
                sb_ = f % 2
                P.add("act", lambda e, sb_=sb_, pg=pg: e.activation(sg[sb_][:], psum[pg][:], AF.Silu),
                      r=[_k("ps", pg)], w=[_k("sg", sb_)])
                P.add("dve", lambda e, sb_=sb_, pu=pu, f=f: e.tensor_tensor(actT[:, f, :], sg[sb_][:], psum[pu][:], ALU.mult),
                      r=[_k("sg", sb_), _k("ps", pu)], w=[_k("actT", f)])
            ak = [_k("actT", f) for f in range(22)]
            wdk = [kk for f in range(22) for kk in wkeys("wd", f, 0, D, CC)]
            for j in range(4):
                t0 = s * 512 + j * 128
                for mh in range(2):
                    py = 6 + mh

                    def mmd(e, j=j, mh=mh, py=py):
                        inst = None
                        for f in range(22):
                            inst = e.matmul(psum[py][:], actT[:, f, j * 128:(j + 1) * 128],
                                            wd[:, f, mh * 512:(mh + 1) * 512], start=(f == 0), stop=(f == 21))
                        return inst
                    P.add("pe", mmd, r=ak + wdk, w=[_k("ps", py)])
                    hs = hbuf[:, j, mh * 512:(mh + 1) * 512]
                    P.add("dve", lambda e, hs=hs, py=py: e.tensor_tensor(hs, hs, psum[py][:], ALU.add),
                          r=[_k("ps", py), _k("hbuf", j)], w=[_k("hbuf", j)])
                hap = hbuf[:, j, :]
                hk = _k("hbuf", j)
                if final_g is not None:
                    sj = 4 + j
                    P.add("act", lambda e, hap=hap, sj=sj: e.activation(junk[:], hap, AF.Square, accum_out=ss[:, sj:sj + 1]),
                          r=[hk], w=[_k("junk"), _k("ss", sj)])
                    rsqrt_small(P, rstd[:, sj:sj + 1], _k("rstd", sj), ss[:, sj:sj + 1], _k("ss", sj), 1.0 / D)
                    P.add("dve", lambda e, hap=hap, sj=sj: e.scalar_tensor_tensor(hap, hap, rstd[:, sj:sj + 1], fg[:], ALU.mult, ALU.mult),
                          r=[hk, _k("rstd", sj), _k("fg")], w=[hk])
                P.add("sp", lambda e, hap=hap, t0=t0: e.dma_start(out=h_out[t0:t0 + 128, :], in_=hap),
                      r=[hk], dma=True)
        P.emit(es)


RET_GAMMA = [1.0 - 2.0 ** (-5.0 - h) for h in range(4)]


DBG = {"ntiles": NT, "stage": 99}


def mix1_phase(nc, h_in, h_out, g_dram, win_d, retnorm_d, wout_d, ident_d, cos_d, sin_d, dt_d, xi8_d, zeta_d):
    with ExitStack() as es:
        P = Prog(nc)
        c = Ctx(nc, P, es)
        CC = 512
        win = c.sb("win", [128, 8, ODD_IN], BF16)
        wout = c.sb("wout", [128, 16, D], BF16)
        stage = [c.sb("stage%d" % i, [128, CC], F32) for i in range(2)]
        gcol = c.sb("gcol", [128, 8], F32)
        rcol = c.sb("rcol", [128, 4], F32)
        ident = c.sb("ident", [128, 128], BF16)
        S = c.sb("S", [128, 2, 4, 512], F32)
        Sb = c.sb("Sb", [128, 2, 4, 512], BF16)
        hbuf = c.sb("hbuf", [128, D], F32)
        xn = c.sb("xn", [128, D], BF16)
        xnT = c.sb("xnT", [128, 8, 128], BF16)
        tmp = [c.sb("tmp%d" % i, [128, 2, 128], F32) for i in range(4)]
        qrot = c.sb("qrot", [128, D], BF16)
        krot = c.sb("krot", [128, D], BF16)
        kz = c.sb("kz", [128, D], BF16)
        qT = c.sb("qT", [128, 8, 128], BF16)
        qxiT = c.sb("qxiT", [128, 8, 128], BF16)
        kT = c.sb("kT", [128, 8, 128], BF16)
        vb = c.sb("vb", [128, 2048], BF16)
        gs = c.sb("gs", [128, 2048], BF16)
        atm = c.sb("atm", [128, 4, 128], BF16)
        og = c.sb("og", [128, 2048], BF16)
        ogT = c.sb("ogT", [128, 16, 128], BF16)
        junk = c.sb("junk", [128, 512], BF16)
        cos_t = c.sb("cos", [128, 128], F32)
        sin_t = c.sb("sin", [128, 128], F32)
        dtm = c.sb("dtm", [128, 4, 128], F32)
        xi8 = c.sb("xi8", [128, 8, 128], F32)
        zeta = c.sb("zeta", [128, 4], F32)
        ss = c.sb("ss", [128, 8], F32)
        rstd = c.sb("rstd", [128, 8], F32)
        psum = [c.ps("ps%d" % i) for i in range(8)]

        def dma(out_ap, in_ap, wk, **kw):
            P.add("sp", lambda e: e.dma_start(out=out_ap, in_=in_ap, **kw), w=[wk], dma=True)

        dma(gcol[:], g_dram.rearrange("(k p) -> p k", p=128), _k("gcol"), allow_slow_non_contiguous=True)
        dma(rcol[:], retnorm_d.rearrange("(k p) -> p k", p=128), _k("rcol"), allow_slow_non_contiguous=True)
        dma(ident[:], ident_d, _k("ident"))
        dma(dtm[:], dt_d, _k("dtm"))
        dma(xi8[:], xi8_d, _k("xi8"))
        dma(zeta[:], zeta_d, _k("zeta"))
        P.add("dve", lambda e: e.memset(S[:], 0.0), w=[_k("S", cc, h) for cc in range(2) for h in range(4)])
        P.add("pool", lambda e: e.memset(Sb[:], 0.0), w=[_k("Sb", cc, h) for cc in range(2) for h in range(4)])
        load_weight_bf16(c, win_d, D, ODD_IN, win, "win", stage, gcol, _k("gcol"), CC)
        load_weight_bf16(c, wout_d, 2048, D, wout, "wout", stage, rcol, _k("rcol"), CC, gmap=lambda k: k % 4)

        def bfv(i):
            return psum[i][:].bitcast(BF16)

        for i in range(DBG["ntiles"]):
            t0 = i * 128
            hk = _k("hbuf")
            dma(hbuf[:], h_in[t0:t0 + 128, :], hk)
            dma(cos_t[:], cos_d[t0:t0 + 128, :], _k("cos"))
            dma(sin_t[:], sin_d[t0:t0 + 128, :], _k("sin"))
            rmsnorm_tile(c, hbuf[:], hk, xn[:], _k("xn"), ogT[:, 0:8, :].rearrange("p c t -> p (c t)"), _k("ogT", 0),
                         ss[:, 4:5], _k("ss", 4), rstd[:, 4:5], _k("rstd", 4))

            def tr8(e, src, pb):
                inst = None
                v = bfv(pb)
                for cc in range(8):
                    inst = e.transpose(v[:, cc * 128:(cc + 1) * 128], src[:, cc * 128:(cc + 1) * 128], ident[:])
                return inst
            P.add("pe", lambda e: tr8(e, xn, 0), r=[_k("xn"), _k("ident")], w=[_k("ps", 0)])
            P.add("act", lambda e: e.copy(xnT[:].rearrange("p c t -> p (c t)"), bfv(0)), r=[_k("ps", 0)], w=[_k("xnT")])
            for g in range(12 if DBG["stage"] >= 2 else 0):
                pb = 1 + (g % 2)

                def mm(e, g=g, pb=pb):
                    inst = None
                    for k in range(8):
                        inst = e.matmul(psum[pb][:], xnT[:, k, :], win[:, k, g * 512:(g + 1) * 512],
                                        start=(k == 0), stop=(k == 7))
                    return inst
                wk_ = [_k("win", k, g) for k in range(8)]
                P.add("pe", mm, r=[_k("xnT")] + wk_, w=[_k("ps", pb)])
                if g < 4:
                    dstt = qrot if g < 2 else krot
                    dname = "qrot" if g < 2 else "krot"
                    hh = (g % 2) * 2
                    X = psum[pb][:].rearrange("p (h x d) -> p h x d", h=2, x=2)
                    X1 = X[:, :, 0, :]
                    X2 = X[:, :, 1, :]
                    Dv = dstt[:, hh * 256:(hh + 2) * 256].rearrange("p (h x d) -> p h x d", h=2, x=2)
                    cb = cos_t[:].unsqueeze(1).to_broadcast([128, 2, 128])
                    sb_ = sin_t[:].unsqueeze(1).to_broadcast([128, 2, 128])
                    pk = _k("ps", pb)

                    def tt(e, o, a, b, op):
                        return e.tensor_tensor(o, a, b, op)
                    P.add("dve", lambda e, X1=X1, cb=cb: tt(e, tmp[0][:], X1, cb, ALU.mult), r=[pk, _k("cos")], w=[_k("tmp", 0)])
                    P.add("dve", lambda e, X2=X2, sb_=sb_: tt(e, tmp[1][:], X2, sb_, ALU.mult), r=[pk, _k("sin")], w=[_k("tmp", 1)])
                    P.add("dve", lambda e, X2=X2, cb=cb: tt(e, tmp[2][:], X2, cb, ALU.mult), r=[pk, _k("cos")], w=[_k("tmp", 2)])
                    P.add("dve", lambda e, X1=X1, sb_=sb_: tt(e, tmp[3][:], X1, sb_, ALU.mult), r=[pk, _k("sin")], w=[_k("tmp", 3)])
                    P.add("dve", lambda e, Dv=Dv: tt(e, Dv[:, :, 0, :], tmp[0][:], tmp[1][:], ALU.subtract),
                          r=[_k("tmp", 0), _k("tmp", 1)], w=[_k(dname, g % 2)])
                    P.add("dve", lambda e, Dv=Dv: tt(e, Dv[:, :, 1, :], tmp[2][:], tmp[3][:], ALU.add),
                          r=[_k("tmp", 2), _k("tmp", 3)], w=[_k(dname, g % 2)])
                elif g < 8:
                    vv = g - 4
                    P.add("act", lambda e, vv=vv, pb=pb: e.copy(vb[:, vv * 512:(vv + 1) * 512], psum[pb][:]),
                          r=[_k("ps", pb)], w=[_k("vb", vv)])
                else:
                    vv = g - 8
                    P.add("act", lambda e, vv=vv, pb=pb: e.activation(gs[:, vv * 512:(vv + 1) * 512], psum[pb][:], AF.Silu),
                          r=[_k("ps", pb)], w=[_k("gs", vv)])
            if DBG["stage"] < 3:
                P.add("sp", lambda e, t0=t0: e.dma_start(out=h_out[t0:t0 + 128, :], in_=hbuf[:]), r=[hk], dma=True)
                continue
            P.add("pool", lambda e: e.tensor_tensor(kz[:].rearrange("p (h d) -> p h d", h=4),
                                                    krot[:].rearrange("p (h d) -> p h d", h=4),
                                                    zeta[:].unsqueeze(2).to_broadcast([128, 4, 256]), ALU.mult),
                  r=[_k("krot", 0), _k("krot", 1), _k("zeta")], w=[_k("kz")])
            P.add("pe", lambda e: tr8(e, qrot, 0), r=[_k("qrot", 0), _k("qrot", 1), _k("ident")], w=[_k("ps", 0)])
            P.add("act", lambda e: e.copy(qT[:].rearrange("p c t -> p (c t)"), bfv(0)), r=[_k("ps", 0)], w=[_k("qT")])
            P.add("dve", lambda e: e.tensor_tensor(qxiT[:].rearrange("p c t -> p (c t)"), bfv(0),
                                                   xi8[:].rearrange("p c t -> p (c t)"), ALU.mult),
                  r=[_k("ps", 0), _k("xi8")], w=[_k("qxiT")])
            P.add("pe", lambda e: tr8(e, krot, 3), r=[_k("krot", 0), _k("krot", 1), _k("ident")], w=[_k("ps", 3)])
            P.add("act", lambda e: e.copy(kT[:].rearrange("p c t -> p (c t)"), bfv(3)), r=[_k("ps", 3)], w=[_k("kT")])

            if DBG["stage"] < 4:
                P.add("sp", lambda e, t0=t0: e.dma_start(out=h_out[t0:t0 + 128, :], in_=hbuf[:]), r=[hk], dma=True)
                continue
            def amm(e):
                inst = None
                for h in range(4):
                    for cc in range(2):
                        inst = e.matmul(psum[0][:, h * 128:(h + 1) * 128], kT[:, 2 * h + cc, :], qT[:, 2 * h + cc, :],
                                        start=(cc == 0), stop=(cc == 1))
                return inst
            P.add("pe", amm, r=[_k("kT"), _k("qT")], w=[_k("ps", 0)])
            P.add("dve", lambda e: e.tensor_tensor(atm[:].rearrange("p h t -> p (h t)"), psum[0][:],
                                                   dtm[:].rearrange("p h t -> p (h t)"), ALU.mult),
                  r=[_k("ps", 0), _k("dtm")], w=[_k("atm")])
            if DBG["stage"] < 5:
                P.add("sp", lambda e, t0=t0: e.dma_start(out=h_out[t0:t0 + 128, :], in_=hbuf[:]), r=[hk], dma=True)
                continue
            for h in range(4):
                def omm(e, h=h):
                    e.matmul(psum[4 + h][:], atm[:, h, :], vb[:, h * 512:(h + 1) * 512], start=True, stop=False)
                    e.matmul(psum[4 + h][:], qxiT[:, 2 * h, :], Sb[:, 0, h, :], start=False, stop=False)
                    return e.matmul(psum[4 + h][:], qxiT[:, 2 * h + 1, :], Sb[:, 1, h, :], start=False, stop=True)
                P.add("pe", omm, r=[_k("atm"), _k("vb", h), _k("qxiT"), _k("Sb", 0, h), _k("Sb", 1, h)], w=[_k("ps", 4 + h)])
                P.add("act", lambda e, h=h: e.activation(junk[:], psum[4 + h][:], AF.Square, accum_out=ss[:, h:h + 1]),
                      r=[_k("ps", 4 + h)], w=[_k("junk"), _k("ss", h)])
            if DBG["stage"] < 6:
                P.add("sp", lambda e, t0=t0: e.dma_start(out=h_out[t0:t0 + 128, :], in_=hbuf[:]), r=[hk], dma=True)
                continue
            for h in range(4):
                for cc in range(2):
                    pb = 1 + ((h * 2 + cc) % 2)
                    P.add("pe", lambda e, h=h, cc=cc, pb=pb: e.matmul(psum[pb][:], kz[:, h * 256 + cc * 128:h * 256 + (cc + 1) * 128],
                                                                        vb[:, h * 512:(h + 1) * 512], start=True, stop=True),
                          r=[_k("kz"), _k("vb", h)], w=[_k("ps", pb)])
                    dec = RET_GAMMA[h] ** 128
                    P.add("dve", lambda e, h=h, cc=cc, pb=pb, dec=dec: e.scalar_tensor_tensor(S[:, cc, h, :], S[:, cc, h, :], dec, psum[pb][:], ALU.mult, ALU.add),
                          r=[_k("S", cc, h), _k("ps", pb)], w=[_k("S", cc, h)])
                    P.add("pool", lambda e, h=h, cc=cc: e.tensor_copy(Sb[:, cc, h, :], S[:, cc, h, :]),
                          r=[_k("S", cc, h)], w=[_k("Sb", cc, h)])
            if DBG["stage"] < 7:
                P.add("sp", lambda e, t0=t0: e.dma_start(out=h_out[t0:t0 + 128, :], in_=hbuf[:]), r=[hk], dma=True)
                continue
            rsqrt_small(P, rstd[:, 0:4], _k("rstd", 0), ss[:, 0:4], [_k("ss", h) for h in range(4)], 1.0 / 512)
            for h in range(4):
                P.add("dve", lambda e, h=h: e.scalar_tensor_tensor(og[:, h * 512:(h + 1) * 512], psum[4 + h][:], rstd[:, h:h + 1],
                                                                    gs[:, h * 512:(h + 1) * 512], ALU.mult, ALU.mult),
                      r=[_k("ps", 4 + h), _k("rstd", 0), _k("gs", h)], w=[_k("og", h)])
            for half in range(2):
                pb = 0 if half == 0 else 3

                def tro(e, half=half, pb=pb):
                    inst = None
                    v = bfv(pb)
                    for cc in range(8):
                        c2 = half * 8 + cc
                        inst = e.transpose(v[:, cc * 128:(cc + 1) * 128], og[:, c2 * 128:(c2 + 1) * 128], ident[:])
                    return inst
                P.add("pe", tro, r=[_k("og", half * 2), _k("og", half * 2 + 1), _k("ident")], w=[_k("ps", pb)])
                P.add("act", lambda e, half=half, pb=pb: e.copy(ogT[:, half * 8:(half + 1) * 8, :].rearrange("p c t -> p (c t)"), bfv(pb)),
                      r=[_k("ps", pb)], w=[_k("ogT", half)])
            for mh in range(2):
                pb = 1 + mh

                def ymm(e, mh=mh, pb=pb):
                    inst = None
                    for cc in range(16):
                        inst = e.matmul(psum[pb][:], ogT[:, cc, :], wout[:, cc, mh * 512:(mh + 1) * 512],
                                        start=(cc == 0), stop=(cc == 15))
                    return inst
                P.add("pe", ymm, r=[_k("ogT", 0), _k("ogT", 1)] + [_k("wout", cc, jj) for cc in range(16) for jj in range(2)],
                      w=[_k("ps", pb)])
                P.add("dve", lambda e, mh=mh, pb=pb: e.tensor_tensor(hbuf[:, mh * 512:(mh + 1) * 512], hbuf[:, mh * 512:(mh + 1) * 512],
                                                                      psum[pb][:], ALU.add),
                      r=[_k("ps", pb), hk], w=[hk])
            P.add("sp", lambda e, t0=t0: e.dma_start(out=h_out[t0:t0 + 128, :], in_=hbuf[:]), r=[hk], dma=True)
        P.emit(es)


N_IT = 22
TOPK = 256
IDX_C0 = (64.0 ** -0.5) * (8.0 ** -0.5)


def _interleave(gens):
    st_ = [[g, max(1, n), 0] for g, n in gens]
    while st_:
        st_.sort(key=lambda x: x[2] / x[1])
        g = st_[0]
        try:
            next(g[0])
            g[2] += 1
        except StopIteration:
            st_.pop(0)


def mix0_phase(nc, h_in, h_out, I, C):
    with ExitStack() as es:
        P = Prog(nc)
        c = Ctx(nc, P, es)
        CC = 512
        win = c.sb("win", [128, 8, EVEN_IN], BF16)
        wout = c.sb("wout", [128, 8, D], BF16)
        stage = [c.sb("stage%d" % i, [128, CC], F32) for i in range(2)]
        gcol = c.sb("gcol", [128, 8], F32)
        gcol2 = c.sb("gcol2", [128, 8], F32)
        ident = c.sb("ident", [128, 128], BF16)
        identf = c.sb("identf", [128, 128], F32)
        ones = c.sb("ones", [128, 128], BF16)
        trim = c.sb("trim", [128, 128], F32)
        trir = c.sb("trir", [128, 128], F32)
        m01 = c.sb("m01", [128, 4, 128], F32)
        negm = c.sb("negm", [128, 128], F32)
        pw = c.sb("pw", [128, N_IT], F32)
        wa2 = c.sb("wa2", [16, 256], F32)
        ba2 = c.sb("ba2", [128, 256], F32)
        hbufA = c.sb("hbufA", [128, D], F32)
        hbufC = c.sb("hbufC", [128, D], F32)
        xn = c.sb("xn", [128, D], BF16)
        xnT = c.sb("xnT", [128, 8, 128], BF16)
        junk = c.sb("junk", [128, D], BF16)
        gqk = c.sb("gqk", [128, 512], BF16)
        gv = c.sb("gv", [128, 512], BF16)
        gsl = c.sb("gsl", [128, 512], BF16)
        dq = c.sb("dq", [128, 512], BF16)
        dkb = c.sb("dkb", [128, 128], BF16)
        iq = c.sb("iq", [128, 512], BF16)
        ikb = c.sb("ikb", [128, 64], BF16)
        iw = c.sb("iw", [128, 8], F32)
        wabs = [c.sb("wabs%d" % i, [128, 8], F32) for i in range(2)]
        sgn = c.sb("sgn", [128, 8], F32)
        Dsg = [c.sb("Dsg%d" % i, [128, 8, 128], BF16) for i in range(2)]
        gaT = c.sb("gaT", [16, 128], F32)
        zb = c.sb("zb", [128, 256], F32)
        sp_ = c.sb("sp", [128, 256], F32)
        ecum = c.sb("ecum", [64, 4, 128], F32)
        encum = c.sb("encum", [64, 4, 128], F32)
        erev = c.sb("erev", [128, 256], F32)
        qtT = c.sb("qtT", [64, 4, 128], BF16)
        ktT = c.sb("ktT", [64, 4, 128], BF16)
        kend = c.sb("kend", [128, 256], BF16)
        atm = c.sb("atm", [128, 4, 128], BF16)
        Sg = c.sb("Sg", [64, 4, 128], F32)
        Sgb = c.sb("Sgb", [64, 4, 128], BF16)
        og = c.sb("og", [128, 512], BF16)
        ogT = [c.sb("ogT%d" % i, [128, 4, 128], BF16) for i in range(4)]
        dsT = c.sb("dsT", [128, 4, 128], BF16)
        qT = [c.sb("qT%d" % i, [128, 4, 128], BF16) for i in range(4)]
        kTc = c.sb("kTc", [128, T], BF16)
        vc = c.sb("vc", [128, NT, 128], BF16)
        ikTc = c.sb("ikTc", [64, T], BF16)
        iqT = [c.sb("iqT%d" % i, [64, 8, 128], BF16) for i in range(2)]
        scb = [c.sb("sc%d" % i, [128, T], F32) for i in range(2)]
        msk = c.sb("msk", [128, T], BF16)
        mskT = c.sb("mskT", [128, NT, 128], BF16)
        Rb = [c.sb("Rb%d" % i, [128, 512], BF16) for i in range(2)]
        Eb = [c.sb("Eb%d" % i, [128, 512], BF16) for i in range(2)]
        PTb = [c.sb("PTb%d" % i, [128, 4, 128], BF16) for i in range(2)]
        rden = c.sb("rden", [128, 512], F32)
        ss = c.sb("ss", [128, 8], F32)
        rstd = c.sb("rstd", [128, 8], F32)
        st = c.sb("st", [128, 8], F32)
        hst = c.sb("hst", [128, N_IT], F32)
        psum = [c.ps("ps%d" % i) for i in range(8)]

        def dma(out_ap, in_ap, wk, **kw):
            P.add("sp", lambda e: e.dma_start(out=out_ap, in_=in_ap, **kw), w=[wk], dma=True)

        def bfv(i):
            return psum[i][:].bitcast(BF16)

        dma(gcol[:], I["even_attn_norm"][0].rearrange("(k p) -> p k", p=128), _k("gcol"), allow_slow_non_contiguous=True)
        P.add("dve", lambda e: e.memset(gcol2[:], 1.0), w=[_k("gcol2")])
        for cc in range(4):
            dma(gcol2[:, cc:cc + 1], I["even_gla_norm"][0].rearrange("(p o) -> p o", o=1), _k("gcol2"))
        dma(ident[:], C["ident"], _k("ident"))
        dma(identf[:], C["identf"], _k("identf"))
        dma(ones[:], C["ones"], _k("ones"))
        dma(trim[:], C["gla_trim"], _k("trim"))
        dma(trir[:], C["gla_trir"], _k("trir"))
        dma(m01[:], C["gla_m01"], _k("m01"))
        dma(negm[:], C["dsa_negm"], _k("negm"))
        dma(pw[:], C["dsa_pw"], _k("pw"))
        dma(wa2[:], I["even_gla_wa2"][0], _k("wa2"))
        dma(ba2[:], I["even_gla_ba2"][0].partition_broadcast(128), _k("ba2"))
        P.add("dve", lambda e: e.memset(Sg[:], 0.0), w=[_k("Sg")])
        P.add("pool", lambda e: e.memset(Sgb[:], 0.0), w=[_k("Sgb")])
        load_weight_bf16(c, I["even_w_in"][0], D, EVEN_IN, win, "win", stage, gcol, _k("gcol"), CC)
        load_weight_bf16(c, I["even_w_out"][0], D, D, wout, "wout", stage, gcol2, _k("gcol2"), CC)
        WIN_ALL = [_k("win", k, j) for k in range(8) for j in range(6)]
        WOUT_ALL = [_k("wout", k, j) for k in range(8) for j in range(2)]

        def stageA(i):
            t0 = i * 128
            p2 = i % 2
            p3 = i % 4
            hb = hbufA
            hk = _k("hbufA")
            dma(hb[:], h_in[t0:t0 + 128, :], hk)
            rmsnorm_tile(c, hb[:], hk, xn[:], _k("xn"), junk[:], _k("junk"),
                         ss[:, 4:5], _k("ss", 4), rstd[:, 4:5], _k("rstd", 4))

            def tr8(e):
                inst = None
                v = bfv(0)
                for cc in range(8):
                    inst = e.transpose(v[:, cc * 128:(cc + 1) * 128], xn[:, cc * 128:(cc + 1) * 128], ident[:])
                return inst
            P.add("pe", tr8, r=[_k("xn"), _k("ident")], w=[_k("ps", 0)])
            P.add("act", lambda e: e.copy(xnT[:].rearrange("p c t -> p (c t)"), bfv(0)), r=[_k("ps", 0)], w=[_k("xnT")])
            yield

            def proj(c0, c1, pb):
                def f(e):
                    inst = None
                    for k in range(8):
                        inst = e.matmul(psum[pb][:, 0:(c1 - c0)], xnT[:, k, :], win[:, k, c0:c1], start=(k == 0), stop=(k == 7))
                    return inst
                P.add("pe", f, r=[_k("xnT")] + WIN_ALL, w=[_k("ps", pb)])

            proj(2320, 2832, 1)
            P.add("dve", lambda e: e.tensor_copy(iq[:], psum[1][:]), r=[_k("ps", 1)], w=[_k("iq")])
            yield
            proj(2832, 2904, 2)
            P.add("act", lambda e: e.copy(ikb[:], psum[2][:, 0:64]), r=[_k("ps", 2)], w=[_k("ikb")])
            P.add("act", lambda e: e.copy(iw[:], psum[2][:, 64:72]), r=[_k("ps", 2)], w=[_k("iw")])
            yield
            P.add("act", lambda e: e.activation(wabs[p2][:], iw[:], AF.Abs, scale=IDX_C0), r=[_k("iw")], w=[_k("wabs", p2)])
            P.add("act", lambda e: e.sign(sgn[:], iw[:]), r=[_k("iw")], w=[_k("sgn")])
            P.add("dve", lambda e: e.tensor_tensor(Dsg[p2][:], identf[:].unsqueeze(1).to_broadcast([128, 8, 128]),
                                                   sgn[:].unsqueeze(2).to_broadcast([128, 8, 128]), ALU.mult),
                  r=[_k("identf"), _k("sgn")], w=[_k("Dsg", p2)])

            def triq(e):
                inst = None
                v = bfv(0)
                for hh in range(8):
                    inst = e.transpose(v[0:64, hh * 128:(hh + 1) * 128], iq[:, hh * 64:(hh + 1) * 64], ident[:])
                return inst
            P.add("pe", triq, r=[_k("iq"), _k("ident")], w=[_k("ps", 0)])
            P.add("act", lambda e: e.copy(iqT[p2][:].rearrange("p c t -> p (c t)"), bfv(0)[0:64, :]), r=[_k("ps", 0)], w=[_k("iqT", p2)])
            yield
            P.add("pe", lambda e: e.transpose(bfv(0)[0:64, 0:128], ikb[:], ident[:]), r=[_k("ikb"), _k("ident")], w=[_k("ps", 0)])
            P.add("act", lambda e: e.copy(ikTc[:, t0:t0 + 128], bfv(0)[0:64, 0:128]), r=[_k("ps", 0)], w=[_k("ikTc", i)])
            yield
            proj(1552, 2064, 1)
            P.add("dve", lambda e: e.tensor_copy(dq[:], psum[1][:]), r=[_k("ps", 1)], w=[_k("dq")])
            yield
            proj(2064, 2320, 2)
            P.add("act", lambda e: e.copy(dkb[:], psum[2][:, 0:128]), r=[_k("ps", 2)], w=[_k("dkb")])
            P.add("act", lambda e: e.copy(vc[:, i, :], psum[2][:, 128:256]), r=[_k("ps", 2)], w=[_k("vc", i)])
            yield

            def trdq(e):
                inst = None
                v = bfv(0)
                for cc in range(4):
                    inst = e.transpose(v[:, cc * 128:(cc + 1) * 128], dq[:, cc * 128:(cc + 1) * 128], ident[:])
                inst = e.transpose(v[:, 512:640], dkb[:], ident[:])
                return inst
            P.add("pe", trdq, r=[_k("dq"), _k("dkb"), _k("ident")], w=[_k("ps", 0)])
            P.add("act", lambda e: e.copy(qT[p3][:].rearrange("p c t -> p (c t)"), bfv(0)[:, 0:512]), r=[_k("ps", 0)], w=[_k("qT", p3)])
            P.add("act", lambda e: e.copy(kTc[:, t0:t0 + 128], bfv(0)[:, 512:640]), r=[_k("ps", 0)], w=[_k("kTc", i)])
            yield
            proj(0, 512, 1)
            P.add("act", lambda e: e.copy(gqk[:], psum[1][:]), r=[_k("ps", 1)], w=[_k("gqk")])
            yield
            proj(512, 1024, 2)
            P.add("dve", lambda e: e.tensor_copy(gv[:], psum[2][:]), r=[_k("ps", 2)], w=[_k("gv")])
            yield
            proj(1040, 1552, 1)
            P.add("act", lambda e: e.activation(gsl[:], psum[1][:], AF.Silu), r=[_k("ps", 1)], w=[_k("gsl")])
            yield

            def gaf(e):
                inst = None
                for k in range(8):
                    inst = e.matmul(psum[2][0:16, 0:128], win[:, k, 1024:1040], xnT[:, k, :], start=(k == 0), stop=(k == 7))
                return inst
            P.add("pe", gaf, r=[_k("xnT")] + WIN_ALL, w=[_k("ps", 2)])
            P.add("act", lambda e: e.copy(gaT[:], psum[2][0:16, 0:128]), r=[_k("ps", 2)], w=[_k("gaT")])
            yield
            P.add("pe", lambda e: e.matmul(psum[1][:, 0:256], gaT[:], wa2[:], start=True, stop=True),
                  r=[_k("gaT"), _k("wa2")], w=[_k("ps", 1)])
            P.add("dve", lambda e: e.tensor_tensor(zb[:], psum[1][:, 0:256], ba2[:], ALU.add),
                  r=[_k("ps", 1), _k("ba2")], w=[_k("zb")])
            P.add("act", lambda e: e.activation(zb[:], zb[:], AF.Exp, scale=-1.0), r=[_k("zb")], w=[_k("zb")])
            P.add("act", lambda e: e.activation(sp_[:], zb[:], AF.Ln, bias=1.0), r=[_k("zb")], w=[_k("sp")])
            yield

            def cumf(e):
                inst = None
                for h in range(4):
                    inst = e.matmul(psum[2][0:64, h * 128:(h + 1) * 128], sp_[:, h * 64:(h + 1) * 64], trim[:], start=True, stop=True)
                return inst
            P.add("pe", cumf, r=[_k("sp"), _k("trim")], w=[_k("ps", 2)])
            P.add("pe", lambda e: e.matmul(psum[1][:, 0:256], trir[:], sp_[:], start=True, stop=True),
                  r=[_k("sp"), _k("trir")], w=[_k("ps", 1)])
            P.add("act", lambda e: e.activation(ecum[:].rearrange("p h t -> p (h t)"), psum[2][0:64, :], AF.Exp),
                  r=[_k("ps", 2)], w=[_k("ecum")])
            P.add("act", lambda e: e.activation(encum[:].rearrange("p h t -> p (h t)"), psum[2][0:64, :], AF.Exp, scale=-1.0),
                  r=[_k("ps", 2)], w=[_k("encum")])
            P.add("act", lambda e: e.activation(erev[:], psum[1][:, 0:256], AF.Exp), r=[_k("ps", 1)], w=[_k("erev")])
            yield

            def trqk(e):
                inst = None
                v = bfv(0)
                for hh in range(8):
                    inst = e.transpose(v[0:64, hh * 128:(hh + 1) * 128], gqk[:, hh * 64:(hh + 1) * 64], ident[:])
                return inst
            P.add("pe", trqk, r=[_k("gqk"), _k("ident")], w=[_k("ps", 0)])
            P.add("dve", lambda e: e.scalar_tensor_tensor(qtT[:].rearrange("p h t -> p (h t)"), bfv(0)[0:64, 0:512], 0.125,
                                                          ecum[:].rearrange("p h t -> p (h t)"), ALU.mult, ALU.mult),
                  r=[_k("ps", 0), _k("ecum")], w=[_k("qtT")])
            P.add("dve", lambda e: e.tensor_tensor(ktT[:].rearrange("p h t -> p (h t)"), bfv(0)[0:64, 512:1024],
                                                   encum[:].rearrange("p h t -> p (h t)"), ALU.mult),
                  r=[_k("ps", 0), _k("encum")], w=[_k("ktT")])
            P.add("dve", lambda e: e.tensor_tensor(kend[:], gqk[:, 256:512], erev[:], ALU.mult),
                  r=[_k("gqk"), _k("erev")], w=[_k("kend")])
            yield

            def atf(e):
                inst = None
                for h in range(4):
                    inst = e.matmul(psum[2][:, h * 128:(h + 1) * 128], ktT[:, h, :], qtT[:, h, :], start=True, stop=True)
                return inst
            P.add("pe", atf, r=[_k("ktT"), _k("qtT")], w=[_k("ps", 2)])
            P.add("dve", lambda e: e.tensor_tensor(atm[:].rearrange("p h t -> p (h t)"), psum[2][:],
                                                   m01[:].rearrange("p h t -> p (h t)"), ALU.mult),
                  r=[_k("ps", 2), _k("m01")], w=[_k("atm")])
            yield

            def of(e):
                inst = None
                for h in range(4):
                    e.matmul(psum[1][:, h * 128:(h + 1) * 128], atm[:, h, :], gv[:, h * 128:(h + 1) * 128], start=True, stop=False)
                    inst = e.matmul(psum[1][:, h * 128:(h + 1) * 128], qtT[:, h, :], Sgb[:, h, :], start=False, stop=True)
                return inst
            P.add("pe", of, r=[_k("atm"), _k("gv"), _k("qtT"), _k("Sgb")], w=[_k("ps", 1)])

            def dsf(e):
                inst = None
                for h in range(4):
                    inst = e.matmul(psum[2][0:64, h * 128:(h + 1) * 128], kend[:, h * 64:(h + 1) * 64], gv[:, h * 128:(h + 1) * 128],
                                    start=True, stop=True)
                return inst
            P.add("pe", dsf, r=[_k("kend"), _k("gv")], w=[_k("ps", 2)])
            elast = ecum[:, :, 127:128].to_broadcast([64, 4, 128])
            P.add("dve", lambda e: e.tensor_tensor(Sg[:], Sg[:], elast, ALU.mult), r=[_k("Sg"), _k("ecum")], w=[_k("Sg")])
            P.add("dve", lambda e: e.tensor_tensor(Sg[:].rearrange("p h t -> p (h t)"), Sg[:].rearrange("p h t -> p (h t)"),
                                                   psum[2][0:64, :], ALU.add), r=[_k("Sg"), _k("ps", 2)], w=[_k("Sg")])
            P.add("pool", lambda e: e.tensor_copy(Sgb[:], Sg[:]), r=[_k("Sg")], w=[_k("Sgb")])
            yield
            for h in range(4):
                P.add("act", lambda e, h=h: e.activation(junk[:, 0:128], psum[1][:, h * 128:(h + 1) * 128], AF.Square, accum_out=ss[:, h:h + 1]),
                      r=[_k("ps", 1)], w=[_k("junk"), _k("ss", h)])
            rsqrt_small(P, rstd[:, 0:4], _k("rstd", 0), ss[:, 0:4], [_k("ss", h) for h in range(4)], 1.0 / 128)
            yield
            for h in range(4):
                P.add("dve", lambda e, h=h: e.scalar_tensor_tensor(og[:, h * 128:(h + 1) * 128], psum[1][:, h * 128:(h + 1) * 128], rstd[:, h:h + 1],
                                                                    gsl[:, h * 128:(h + 1) * 128], ALU.mult, ALU.mult),
                      r=[_k("ps", 1), _k("rstd", 0), _k("gsl")], w=[_k("og")])
            yield

            def trog(e):
                inst = None
                v = bfv(0)
                for cc in range(4):
                    inst = e.transpose(v[:, cc * 128:(cc + 1) * 128], og[:, cc * 128:(cc + 1) * 128], ident[:])
                return inst
            P.add("pe", trog, r=[_k("og"), _k("ident")], w=[_k("ps", 0)])
            P.add("act", lambda e: e.copy(ogT[p3][:].rearrange("p c t -> p (c t)"), bfv(0)[:, 0:512]),
                  r=[_k("ps", 0)], w=[_k("ogT", p3)])
            yield

        def stageBs(i):
            p2 = i % 2
            sc = scb[p2]
            nkeys = (i + 1) * 128
            ngrp = (nkeys + 511) // 512
            pend = None
            for gk in range(ngrp):
                k0 = gk * 512
                n = min(512, nkeys - k0)
                kk = [_k("ikTc", b) for b in range(k0 // 128, (k0 + n) // 128)]
                for hI in range(8):
                    rb = hI % 2
                    P.add("pe", lambda e, hI=hI, k0=k0, n=n: e.matmul(psum[3][:, 0:n], iqT[p2][:, hI, :], ikTc[:, k0:k0 + n], start=True, stop=True),
                          r=[_k("iqT", p2)] + kk, w=[_k("ps", 3)])
                    if pend is not None:
                        pend()
                        pend = None
                    P.add("act", lambda e, rb=rb, hI=hI, n=n: e.activation(Rb[rb][:, 0:n], psum[3][:, 0:n], AF.Relu, scale=wabs[p2][:, hI:hI + 1]),
                          r=[_k("ps", 3), _k("wabs", p2)], w=[_k("Rb", rb)])

                    def acc(rb=rb, hI=hI, n=n, k0=k0):
                        P.add("pe", lambda e: e.matmul(psum[4][:, 0:n], Dsg[p2][:, hI, :], Rb[rb][:, 0:n], start=(hI == 0), stop=(hI == 7)),
                              r=[_k("Dsg", p2), _k("Rb", rb)], w=[_k("ps", 4)])
                        if hI == 7:
                            P.add("act", lambda e: e.copy(sc[:, k0:k0 + n], psum[4][:, 0:n]), r=[_k("ps", 4)], w=[_k("sc", p2)])
                    pend = acc
                    yield
            if pend is not None:
                pend()
            yield

        def stageBb(i):
            t0 = i * 128
            p2 = i % 2
            sc = scb[p2]
            nkeys = (i + 1) * 128
            scv = sc[:, 0:nkeys]
            P.add("dve", lambda e: e.tensor_reduce(st[:, 0:1], scv, AX.X, ALU.max), r=[_k("sc", p2)], w=[_k("st", 0)])
            yield
            P.add("dve", lambda e: e.tensor_reduce(st[:, 1:2], scv, AX.X, ALU.min), r=[_k("sc", p2)], w=[_k("st", 1)])
            P.add("dve", lambda e: e.tensor_tensor(sc[:, t0:t0 + 128], sc[:, t0:t0 + 128], negm[:], ALU.add),
                  r=[_k("sc", p2), _k("negm")], w=[_k("sc", p2)])
            P.add("dve", lambda e: e.tensor_tensor(st[:, 2:3], st[:, 0:1], st[:, 1:2], ALU.subtract), r=[_k("st", 0), _k("st", 1)], w=[_k("st", 2)])
            P.add("dve", lambda e: e.tensor_scalar(st[:, 2:3], st[:, 2:3], 1.000001, 1e-30, ALU.mult, ALU.add), r=[_k("st", 2)], w=[_k("st", 2)])
            P.add("dve", lambda e: e.tensor_scalar(hst[:], pw[:], st[:, 2:3], None, ALU.mult), r=[_k("st", 2), _k("pw")], w=[_k("hst")])
            yield
            for k in range(N_IT):
                P.add("dve", lambda e, k=k: e.tensor_tensor(st[:, 3:4], st[:, 1:2], hst[:, k:k + 1], ALU.add),
                      r=[_k("st", 1), _k("hst")], w=[_k("st", 3)])
                P.add("dve", lambda e: e.tensor_scalar(msk[:, 0:nkeys], scv, st[:, 3:4], None, ALU.is_ge, ALU.add, accum_out=st[:, 4:5]),
                      r=[_k("sc", p2), _k("st", 3)], w=[_k("msk"), _k("st", 4)])
                P.add("dve", lambda e, k=k: e.tensor_scalar(st[:, 5:6], st[:, 4:5], TOPK - 0.5, hst[:, k:k + 1], ALU.is_ge, ALU.mult),
                      r=[_k("st", 4), _k("hst")], w=[_k("st", 5)])
                P.add("dve", lambda e: e.tensor_tensor(st[:, 1:2], st[:, 1:2], st[:, 5:6], ALU.add),
                      r=[_k("st", 1), _k("st", 5)], w=[_k("st", 1)])
                yield
            P.add("dve", lambda e: e.tensor_scalar(msk[:, 0:nkeys], scv, st[:, 1:2], None, ALU.is_ge),
                  r=[_k("sc", p2), _k("st", 1)], w=[_k("msk")])
            yield
            for b0 in range(0, i + 1, 8):
                nb = min(8, i + 1 - b0)

                def trm(e, b0=b0, nb=nb):
                    inst = None
                    v = bfv(3)
                    for bb in range(nb):
                        inst = e.transpose(v[:, bb * 128:(bb + 1) * 128], msk[:, (b0 + bb) * 128:(b0 + bb + 1) * 128], ident[:])
                    return inst
                P.add("pe", trm, r=[_k("msk"), _k("ident")], w=[_k("ps", 3)])
                P.add("act", lambda e, b0=b0, nb=nb: e.copy(mskT[:, b0:b0 + nb, :].rearrange("p c t -> p (c t)"), bfv(3)[:, 0:nb * 128]),
                      r=[_k("ps", 3)], w=[_k("mskT", b0 // 8)])
                yield

        def stageC(i):
            t0 = i * 128
            p3 = i % 4
            hb = hbufC
            hk = _k("hbufC")
            dma(hb[:], h_in[t0:t0 + 128, :], hk)
            pendc = []
            for j in range(i + 1):
                eb = j % 2
                P.add("pe", lambda e, j=j: e.matmul(psum[5][:], kTc[:, j * 128:(j + 1) * 128], qT[p3][:].rearrange("p c t -> p (c t)"), start=True, stop=True),
                      r=[_k("kTc", j), _k("qT", p3)], w=[_k("ps", 5)])
                while pendc:
                    pendc.pop(0)()
                P.add("act", lambda e, eb=eb: e.activation(Eb[eb][:], psum[5][:], AF.Exp, scale=128.0 ** -0.5),
                      r=[_k("ps", 5)], w=[_k("Eb", eb)])
                mb = mskT[:, j:j + 1, :].to_broadcast([128, 4, 128])
                P.add("dve", lambda e, eb=eb, mb=mb: e.tensor_tensor(PTb[eb][:], Eb[eb][:].rearrange("p (h t) -> p h t", h=4), mb, ALU.mult),
                      r=[_k("Eb", eb), _k("mskT", j // 8)], w=[_k("PTb", eb)])

                def pv(e, eb=eb, j=j):
                    e.matmul(psum[6][:], vc[:, j, :], PTb[eb][:].rearrange("p h t -> p (h t)"), start=(j == 0), stop=(j == i))
                    return e.matmul(psum[7][:], ones[:], PTb[eb][:].rearrange("p h t -> p (h t)"), start=(j == 0), stop=(j == i))

                def pvadd(pv=pv, j=j, eb=eb):
                    P.add("pe", pv, r=[_k("vc", j), _k("PTb", eb), _k("ones")], w=[_k("ps", 6), _k("ps", 7)])
                pendc.append(pvadd)
                yield
            while pendc:
                pendc.pop(0)()
            P.add("dve", lambda e: e.reciprocal(rden[:], psum[7][:]), r=[_k("ps", 7)], w=[_k("rden")])
            P.add("dve", lambda e: e.tensor_tensor(dsT[:].rearrange("p c t -> p (c t)"), psum[6][:], rden[:], ALU.mult),
                  r=[_k("ps", 6), _k("rden")], w=[_k("dsT")])
            yield
            for mh in range(2):
                def ymm(e, mh=mh):
                    inst = None
                    for cc in range(8):
                        src = ogT[p3][:, cc, :] if cc < 4 else dsT[:, cc - 4, :]
                        inst = e.matmul(psum[5][:], src, wout[:, cc, mh * 512:(mh + 1) * 512], start=(cc == 0), stop=(cc == 7))
                    return inst
                P.add("pe", ymm, r=[_k("ogT", p3), _k("dsT")] + WOUT_ALL, w=[_k("ps", 5)])
                P.add("dve", lambda e, mh=mh: e.tensor_tensor(hb[:, mh * 512:(mh + 1) * 512], hb[:, mh * 512:(mh + 1) * 512], psum[5][:], ALU.add),
                      r=[_k("ps", 5), hk], w=[hk])
                yield
            P.add("sp", lambda e: e.dma_start(out=h_out[t0:t0 + 128, :], in_=hb[:]), r=[hk], dma=True)
            yield

        ntl = DBG["ntiles"]
        for s_ in range(ntl + 3):
            gens = []
            if 0 <= s_ - 3 < ntl:
                gens.append((stageC(s_ - 3), (s_ - 3) + 1 + 4))
            if 0 <= s_ - 2 < ntl:
                gens.append((stageBb(s_ - 2), 4 + N_IT + (s_ - 2) // 8 + 1))
            if 0 <= s_ - 1 < ntl:
                gens.append((stageBs(s_ - 1), 9 * ((s_ - 1) // 4 + 1)))
            if s_ < ntl:
                gens.append((stageA(s_), 24))
            _interleave(gens)
        P.emit(es)


def host_consts():
    import ml_dtypes
    cst = {}
    cst["ident"] = np.eye(128, dtype=ml_dtypes.bfloat16)
    half = 128
    inv = 10000.0 ** (-np.arange(half, dtype=np.float32) / half)
    ang = np.arange(T, dtype=np.float32)[:, None] * inv[None, :].astype(np.float32)
    cst["rope_cos"] = np.cos(ang).astype(np.float32)
    cst["rope_sin"] = np.sin(ang).astype(np.float32)
    g = np.array(RET_GAMMA, dtype=np.float64)
    ii = np.arange(128)
    rel = ii[None, :] - ii[:, None]
    dtm = np.zeros((128, 4, 128), np.float64)
    for h in range(4):
        dtm[:, h, :] = np.where(rel >= 0, g[h] ** np.maximum(rel, 0), 0.0) / 16.0
    cst["ret_dt"] = dtm.astype(np.float32)
    xi8 = np.zeros((128, 8, 128), np.float64)
    for cc in range(8):
        xi8[:, cc, :] = (g[cc // 2] ** (ii + 1.0))[None, :]
    cst["ret_xi8"] = xi8.astype(np.float32)
    cst["ones"] = np.ones((128, 128), dtype=ml_dtypes.bfloat16)
    cst["identf"] = np.eye(128, dtype=np.float32)
    le = (ii[:, None] <= ii[None, :])
    cst["gla_trim"] = np.where(le, -1.0 / 16.0, 0.0).astype(np.float32)
    cst["gla_trir"] = np.where(~le, -1.0 / 16.0, 0.0).astype(np.float32)
    cst["gla_m01"] = np.repeat(le[:, None, :], 4, axis=1).astype(np.float32)
    cst["dsa_negm"] = np.where(ii[None, :] <= ii[:, None], 0.0, -1e30).astype(np.float32)
    cst["dsa_pw"] = np.repeat((0.5 ** (np.arange(N_IT) + 1.0))[None, :], 128, axis=0).astype(np.float32)
    zeta = np.zeros((128, 4), np.float64)
    for h in range(4):
        zeta[:, h] = g[h] ** (127.0 - ii) / 16.0
    cst["ret_zeta"] = zeta.astype(np.float32)
    return cst


INPUT_SHAPES = {
    "even_attn_norm": [1, D], "even_w_in": [1, D, EVEN_IN], "even_gla_wa2": [1, 16, 256],
    "even_gla_ba2": [1, 256], "even_gla_norm": [1, 128], "even_w_out": [1, D, D],
    "odd_attn_norm": [1, D], "odd_w_in": [1, D, ODD_IN], "odd_ret_norm": [1, 512], "odd_w_out": [1, 2048, D],
    "ffn_norm": [2, D], "ffn_w_gate": [2, D, DFF], "ffn_w_up": [2, D, DFF], "ffn_w_down": [2, DFF, D],
    "final_norm": [D],
}


def build(phases=("mix0", "ffn0", "mix1", "ffn1")):
    nc = bass.Bass("TRN2", target_bir_lowering=False)
    x = nc.dram_tensor("x", [T, D], F32, kind="ExternalInput").ap()
    out = nc.dram_tensor("out", [T, D], F32, kind="ExternalOutput").ap()
    I = {k: nc.dram_tensor(k, shp, F32, kind="ExternalInput").ap() for k, shp in INPUT_SHAPES.items()}
    cst = host_consts()
    C = {}
    for k, v in cst.items():
        C[k] = nc.dram_tensor(k, list(v.shape), BF16 if v.dtype != np.float32 else F32, kind="ExternalInput").ap()
    scr = [nc.dram_tensor("scr%d" % i, [T, D], F32, kind="Internal").ap() for i in range(3)]
    bufs = [x] + scr[:len(phases) - 1] + [out]
    for pi, ph in enumerate(phases):
        hin, hout = bufs[pi], bufs[pi + 1]
        if ph == "ffn0":
            ffn_phase(nc, hin, hout, I["ffn_norm"][0], I["ffn_w_gate"][0], I["ffn_w_up"][0], I["ffn_w_down"][0], C["ident"])
        elif ph == "ffn1":
            ffn_phase(nc, hin, hout, I["ffn_norm"][1], I["ffn_w_gate"][1], I["ffn_w_up"][1], I["ffn_w_down"][1], C["ident"],
                      final_g=I["final_norm"])
        elif ph == "mix1":
            mix1_phase(nc, hin, hout, I["odd_attn_norm"][0], I["odd_w_in"][0], I["odd_ret_norm"][0], I["odd_w_out"][0],
                       C["ident"], C["rope_cos"], C["rope_sin"], C["ret_dt"], C["ret_xi8"], C["ret_zeta"])
        elif ph == "mix0":
            mix0_phase(nc, hin, hout, I, C)
    return nc


def make_inputs(inputs, b):
    m = {k: np.ascontiguousarray(np.asarray(inputs[k], dtype=np.float32)) for k in INPUT_SHAPES}
    m["x"] = np.ascontiguousarray(np.asarray(inputs["x"][b], dtype=np.float32))
    m.update(host_consts())
    return m


def kernel(**inputs):
    nc = build()
    in_maps = [make_inputs(inputs, b) for b in range(8)]
    res = run_bass_kernel_spmd(nc, in_maps, core_ids=list(range(8)))
    return np.stack([np.asarray(r["out"], dtype=np.float32) for r in res.results], axis=0)
```

```python
import math
from contextlib import ExitStack

import numpy as np
import concourse.bass as bass
import concourse.mybir as mybir
from concourse.bass_utils import run_bass_kernel_spmd

F32 = mybir.dt.float32
BF16 = mybir.dt.bfloat16
AF = mybir.ActivationFunctionType
ALU = mybir.AluOpType
AX = mybir.AxisListType

T = 4096
D = 1024
DFF = 2816
NT = T // 128
EPS = 1e-6
EVEN_IN = 2904
ODD_IN = 6144


class _Op:
    __slots__ = ("eng", "fn", "deps", "dma", "sig", "sigidx", "dsem", "dval", "ndep")


class Prog:
    COMPUTE = ("pe", "act", "dve", "pool")

    def __init__(self, nc, n_dma_sems=16):
        self.nc = nc
        self.ops = []
        self.lastw = {}
        self.readers = {}
        self.n_dma_sems = n_dma_sems
        self.dma_rr = {"sp": 0, "pool": 0, "act": 0}
        self.dma_cnt = {}

    def add(self, eng, fn, r=(), w=(), dma=False):
        i = len(self.ops)
        deps = set()
        pr = [k for k in r if k[0] == "ps" and k not in w]
        if pr:
            w = list(w) + pr
        for k in r:
            a = self.lastw.get(k)
            if a is not None:
                deps.add(a)
        for k in w:
            a = self.lastw.get(k)
            if a is not None:
                deps.add(a)
            rd = self.readers.get(k)
            if rd:
                deps.update(rd.values())
        op = _Op()
        op.eng = eng
        op.fn = fn
        op.dma = dma
        op.sig = False
        op.sigidx = 0
        op.dsem = None
        op.dval = 0
        fdeps = []
        for a in deps:
            A = self.ops[a]
            if (not dma) and (not A.dma) and eng == "pe" and A.eng == "pe":
                continue
            fdeps.append(a)
        op.deps = sorted(fdeps)
        if dma:
            q = self.dma_rr[eng]
            self.dma_rr[eng] = (q + 1) % self.n_dma_sems
            key = (eng, q)
            self.dma_cnt[key] = self.dma_cnt.get(key, 0) + 1
            op.dsem = key
            op.dval = 16 * self.dma_cnt[key]
        self.ops.append(op)
        for k in w:
            self.lastw[k] = i
            self.readers[k] = {}
        for k in r:
            d = self.readers.setdefault(k, {})
            d[("dma", i) if dma else eng] = i
        return i

    def emit(self, es):
        nc = self.nc
        ops = self.ops
        for op in ops:
            for a in op.deps:
                if not ops[a].dma:
                    ops[a].sig = True
        cnt = {e: 0 for e in self.COMPUTE + ("sp",)}
        for op in ops:
            if op.sig and not op.dma:
                cnt[op.eng] += 1
                op.sigidx = cnt[op.eng]
        sems = {e: es.enter_context(nc.semaphore("s_" + e)) for e in cnt}
        dsems = {}
        for key in self.dma_cnt:
            dsems[key] = es.enter_context(nc.semaphore("d_%s%d" % key))
        engines = {"pe": "tensor", "act": "scalar", "dve": "vector", "pool": "gpsimd", "sp": "sync"}
        with nc.Block() as block:
            for e, attr in engines.items():
                mine = [op for op in ops if op.eng == e]

                def body(engine, mine=mine, e=e):
                    known = {}

                    def wait(sem_key, sem, val):
                        if known.get(sem_key, 0) >= val:
                            return
                        engine.wait_ge(sem, val)
                        known[sem_key] = val

                    for op in mine:
                        for a in op.deps:
                            A = ops[a]
                            if A.dma:
                                wait(A.dsem, dsems[A.dsem], A.dval)
                            else:
                                wait(A.eng, sems[A.eng], A.sigidx)
                        if op.dma:
                            if op.dval > 16:
                                wait(op.dsem, dsems[op.dsem], op.dval - 16)
                            inst = op.fn(engine)
                            inst.then_inc(dsems[op.dsem], 16)
                        else:
                            inst = op.fn(engine)
                            if op.sig:
                                inst.then_inc(sems[e], 1)
                    for key, c in self.dma_cnt.items():
                        if key[0] == e:
                            wait(key, dsems[key], 16 * c)

                getattr(block, attr)(body)


def _k(name, *idx):
    return (name,) + idx


class Ctx:
    def __init__(self, nc, P, es):
        self.nc = nc
        self.P = P
        self.es = es
        self.rr = 0

    UID = [0]

    def sb(self, name, shape, dt):
        Ctx.UID[0] += 1
        return self.es.enter_context(self.nc.sbuf_tensor("sb%d_%s" % (Ctx.UID[0], name), list(shape), dt))

    def ps(self, name):
        Ctx.UID[0] += 1
        return self.es.enter_context(self.nc.psum_tensor("ps%d_%s" % (Ctx.UID[0], name), [128, 512], F32))


def rsqrt_small(P, out_ap, outkey, in_ap, inkey, scale, eps=EPS):
    inkeys = inkey if isinstance(inkey, list) else [inkey]
    P.add("dve", lambda e: e.tensor_scalar(out_ap, in_ap, scale, eps, ALU.mult, ALU.add),
          r=inkeys, w=[outkey])
    P.add("act", lambda e: e.activation(out_ap, out_ap, AF.Ln), r=[outkey], w=[outkey])
    P.add("act", lambda e: e.activation(out_ap, out_ap, AF.Exp, scale=-0.5), r=[outkey], w=[outkey])


def rmsnorm_tile(c, h_ap, hkey, xn_ap, xnkey, junk_ap, junkkey, ss_ap, sskey, rstd_ap, rstdkey):
    P = c.P
    P.add("act", lambda e: e.activation(junk_ap, h_ap, AF.Square, accum_out=ss_ap),
          r=[hkey], w=[junkkey, sskey])
    rsqrt_small(P, rstd_ap, rstdkey, ss_ap, sskey, 1.0 / D)
    P.add("dve", lambda e: e.tensor_scalar(xn_ap, h_ap, rstd_ap, None, ALU.mult),
          r=[hkey, rstdkey], w=[xnkey])


_LW = [0]


def load_weight_bf16(c, w_dram, rows, cols, dst, dstname, stage, gcol=None, gkey=None, colchunk=704, gmap=None):
    P = c.P
    nk = rows // 128
    nch = (cols + colchunk - 1) // colchunk
    if gcol is None:
        for k in range(nk):
            P.add("pool", lambda e, k=k: e.dma_start(out=dst[:, k, :], in_=w_dram[k * 128:(k + 1) * 128, :]),
                  w=[_k(dstname, k, j) for j in range(nch)], dma=True)
        return [_k(dstname, k, j) for k in range(nk) for j in range(nch)]
    for k in range(nk):
        for j in range(nch):
            c0 = j * colchunk
            c1 = min(cols, c0 + colchunk)
            cnt = _LW[0]
            _LW[0] += 1
            st, skeys = stage[cnt % len(stage)]
            src = w_dram[k * 128:(k + 1) * 128, c0:c1]
            P.add("sp", lambda e, st=st, src=src, n=c1 - c0: e.dma_start(out=st[:, 0:n], in_=src),
                  w=skeys, dma=True)
            dap = dst[:, k, c0:c1]
            sap = st[:, 0:c1 - c0]
            eng = ("dve", "act")[cnt % 2]
            rk = list(skeys) + ([gkey] if gcol is not None else [])
            if gcol is None:
                if eng == "act":
                    P.add("act", lambda e, dap=dap, sap=sap: e.copy(dap, sap), r=rk, w=[_k(dstname, k, j)])
                else:
                    P.add(eng, lambda e, dap=dap, sap=sap: e.tensor_copy(dap, sap), r=rk, w=[_k(dstname, k, j)])
            else:
                kk_ = gmap(k) if gmap is not None else k
                g = gcol[:, kk_:kk_ + 1]
                if eng == "act":
                    P.add("act", lambda e, dap=dap, sap=sap, g=g: e.activation(dap, sap, AF.Copy, scale=g),
                          r=rk, w=[_k(dstname, k, j)])
                else:
                    P.add(eng, lambda e, dap=dap, sap=sap, g=g: e.tensor_scalar(dap, sap, g, None, ALU.mult),
                          r=rk, w=[_k(dstname, k, j)])
    return [_k(dstname, k, j) for k in range(nk) for j in range(nch)]


def wkeys(dstname, k, c0, c1, colchunk=704):
    return [_k(dstname, k, j) for j in range(c0 // colchunk, (c1 - 1) // colchunk + 1)]


def ffn_phase(nc, h_in, h_out, g_dram, wg_d, wu_d, wd_d, ident_d, final_g=None):
    with ExitStack() as es:
        P = Prog(nc)
        c = Ctx(nc, P, es)
        CC = 704
        wg = c.sb("wg", [128, 8, DFF], BF16)
        wu = c.sb("wu", [128, 8, DFF], BF16)
        wd = c.sb("wd", [128, 22, D], BF16)
        gcol = c.sb("gcol", [128, 8], F32)
        ident = c.sb("ident", [128, 128], BF16)
        hbuf = c.sb("hbuf", [128, 4, D], F32)
        xn = [c.sb("xn%d" % i, [128, D], BF16) for i in range(2)]
        xnT = c.sb("xnT", [128, 8, 512], BF16)
        actT = c.sb("actT", [128, 22, 512], BF16)
        actf = actT[:].rearrange("p f t -> p (f t)").bitcast(F32)
        stage = []
        for si in range(7):
            stage.append((actf[:, si * 768:si * 768 + CC], [_k("actT", f) for f in range(3 * si, 3 * si + 3)]))
        sg = [c.sb("sg%d" % i, [128, 512], F32) for i in range(2)]
        junk = c.sb("junk", [128, D], BF16)
        ss = c.sb("ss", [128, 8], F32)
        rstd = c.sb("rstd", [128, 8], F32)
        if final_g is not None:
            fg = c.sb("fg", [128, D], F32)
        psum = [c.ps("ps%d" % i) for i in range(8)]

        P.add("sp", lambda e: e.dma_start(out=gcol[:], in_=g_dram.rearrange("(k p) -> p k", p=128),
                                          allow_slow_non_contiguous=True), w=[_k("gcol")], dma=True)
        P.add("sp", lambda e: e.dma_start(out=ident[:], in_=ident_d), w=[_k("ident")], dma=True)
        if final_g is not None:
            P.add("sp", lambda e: e.dma_start(out=fg[:], in_=final_g.partition_broadcast(128)),
                  w=[_k("fg")], dma=True)
        load_weight_bf16(c, wg_d, D, DFF, wg, "wg", stage, gcol, _k("gcol"), CC)
        load_weight_bf16(c, wu_d, D, DFF, wu, "wu", stage, gcol, _k("gcol"), CC)
        load_weight_bf16(c, wd_d, DFF, D, wd, "wd", stage, None, None, CC)

        nsup = T // 512
        tcount = 0
        for s in range(nsup):
            for j in range(4):
                t0 = s * 512 + j * 128
                hk = _k("hbuf", j)
                hap = hbuf[:, j, :]
                P.add("sp", lambda e, hap=hap, t0=t0: e.dma_start(out=hap, in_=h_in[t0:t0 + 128, :]),
                      w=[hk], dma=True)
                b = tcount % 2
                rmsnorm_tile(c, hap, hk, xn[b][:], _k("xn", b), junk[:], _k("junk"),
                             ss[:, j:j + 1], _k("ss", j), rstd[:, j:j + 1], _k("rstd", j))
                pb = tcount % 2
                pst = psum[pb].bitcast(BF16) if hasattr(psum[pb], "bitcast") else psum[pb][:].bitcast(BF16)

                def tr(e, b=b, pst=pst):
                    inst = None
                    for cc in range(8):
                        inst = e.transpose(pst[:, cc * 128:(cc + 1) * 128], xn[b][:, cc * 128:(cc + 1) * 128], ident[:])
                    return inst
                P.add("pe", tr, r=[_k("xn", b), _k("ident")], w=[_k("ps", pb)])
                dst = xnT[:, :, j * 128:(j + 1) * 128]
                src = pst[:, 0:1024].rearrange("p (c t) -> p c t", c=8)
                P.add("act", lambda e, dst=dst, src=src: e.copy(dst, src), r=[_k("ps", pb)], w=[_k("xnT", j)])
                tcount += 1
            xk = [_k("xnT", j) for j in range(4)]
            for f in range(22):
                pg = 2 + (f % 2)
                pu = 4 + (f % 2)

                def mm(e, w_, pi, f=f):
                    inst = None
                    for k in range(8):
                        inst = e.matmul(psum[pi][:], w_[:, k, f * 128:(f + 1) * 128], xnT[:, k, :],
                                        start=(k == 0), stop=(k == 7))
                    return inst
                wkg = [kk for k in range(8) for kk in wkeys("wg", k, f * 128, (f + 1) * 128, CC)]
                wku = [kk for k in range(8) for kk in wkeys("wu", k, f * 128, (f + 1) * 128, CC)]
                P.add("pe", lambda e, pg=pg, mm=mm: mm(e, wg, pg), r=xk + wkg, w=[_k("ps", pg)])
                P.add("pe", lambda e, pu=pu, mm=mm: mm(e, wu, pu), r=xk + wku, w=[_k("ps", pu)])
                sb_ = f % 2
                P.add("act", lambda e, sb_=sb_, pg=pg: e.activation(sg[sb_][:], psum[pg][:], AF.Silu),
                      r=[_k("ps", pg)], w=[_k("sg", sb_)])
                P.add("dve", lambda e, sb_=sb_, pu=pu, f=f: e.tensor_tensor(actT[:, f, :], sg[sb_][:], psum[pu][:], ALU.mult),
                      r=[_k("sg", sb_), _k("ps", pu)], w=[_k("actT", f)])
            ak = [_k("actT", f) for f in range(22)]
            wdk = [kk for f in range(22) for kk in wkeys("wd", f, 0, D, CC)]
            for j in range(4):
                t0 = s * 512 + j * 128
                for mh in range(2):
                    py = 6 + mh

                    def mmd(e, j=j, mh=mh, py=py):
                        inst = None
                        for f in range(22):
                            inst = e.matmul(psum[py][:], actT[:, f, j * 128:(j + 1) * 128],
                                            wd[:, f, mh * 512:(mh + 1) * 512], start=(f == 0), stop=(f == 21))
                        return inst
                    P.add("pe", mmd, r=ak + wdk, w=[_k("ps", py)])
                    hs = hbuf[:, j, mh * 512:(mh + 1) * 512]
                    P.add("dve", lambda e, hs=hs, py=py: e.tensor_tensor(hs, hs, psum[py][:], ALU.add),
                          r=[_k("ps", py), _k("hbuf", j)], w=[_k("hbuf", j)])
                hap = hbuf[:, j, :]
                hk = _k("hbuf", j)
                if final_g is not None:
                    sj = 4 + j
                    P.add("act", lambda e, hap=hap, sj=sj: e.activation(junk[:], hap, AF.Square, accum_out=ss[:, sj:sj + 1]),
                          r=[hk], w=[_k("junk"), _k("ss", sj)])
                    rsqrt_small(P, rstd[:, sj:sj + 1], _k("rstd", sj), ss[:, sj:sj + 1], _k("ss", sj), 1.0 / D)
                    P.add("dve", lambda e, hap=hap, sj=sj: e.scalar_tensor_tensor(hap, hap, rstd[:, sj:sj + 1], fg[:], ALU.mult, ALU.mult),
                          r=[hk, _k("rstd", sj), _k("fg")], w=[hk])
                P.add("sp", lambda e, hap=hap, t0=t0: e.dma_start(out=h_out[t0:t0 + 128, :], in_=hap),
                      r=[hk], dma=True)
        P.emit(es)


RET_GAMMA = [1.0 - 2.0 ** (-5.0 - h) for h in range(4)]


DBG = {"ntiles": NT, "stage": 99}


def mix1_phase(nc, h_in, h_out, g_dram, win_d, retnorm_d, wout_d, ident_d, cos_d, sin_d, dt_d, xi8_d, zeta_d):
    with ExitStack() as es:
        P = Prog(nc)
        c = Ctx(nc, P, es)
        CC = 512
        win = c.sb("win", [128, 8, ODD_IN], BF16)
        wout = c.sb("wout", [128, 16, D], BF16)
        stage = [c.sb("stage%d" % i, [128, CC], F32) for i in range(2)]
        gcol = c.sb("gcol", [128, 8], F32)
        rcol = c.sb("rcol", [128, 4], F32)
        ident = c.sb("ident", [128, 128], BF16)
        S = c.sb("S", [128, 2, 4, 512], F32)
        Sb = c.sb("Sb", [128, 2, 4, 512], BF16)
        hbuf = c.sb("hbuf", [128, D], F32)
        xn = c.sb("xn", [128, D], BF16)
        xnT = c.sb("xnT", [128, 8, 128], BF16)
        tmp = [c.sb("tmp%d" % i, [128, 2, 128], F32) for i in range(4)]
        qrot = c.sb("qrot", [128, D], BF16)
        krot = c.sb("krot", [128, D], BF16)
        kz = c.sb("kz", [128, D], BF16)
        qT = c.sb("qT", [128, 8, 128], BF16)
        qxiT = c.sb("qxiT", [128, 8, 128], BF16)
        kT = c.sb("kT", [128, 8, 128], BF16)
        vb = c.sb("vb", [128, 2048], BF16)
        gs = c.sb("gs", [128, 2048], BF16)
        atm = c.sb("atm", [128, 4, 128], BF16)
        og = c.sb("og", [128, 2048], BF16)
        ogT = c.sb("ogT", [128, 16, 128], BF16)
        junk = c.sb("junk", [128, 512], BF16)
        cos_t = c.sb("cos", [128, 128], F32)
        sin_t = c.sb("sin", [128, 128], F32)
        dtm = c.sb("dtm", [128, 4, 128], F32)
        xi8 = c.sb("xi8", [128, 8, 128], F32)
        zeta = c.sb("zeta", [128, 4], F32)
        ss = c.sb("ss", [128, 8], F32)
        rstd = c.sb("rstd", [128, 8], F32)
        psum = [c.ps("ps%d" % i) for i in range(8)]

        def dma(out_ap, in_ap, wk, **kw):
            P.add("sp", lambda e: e.dma_start(out=out_ap, in_=in_ap, **kw), w=[wk], dma=True)

        dma(gcol[:], g_dram.rearrange("(k p) -> p k", p=128), _k("gcol"), allow_slow_non_contiguous=True)
        dma(rcol[:], retnorm_d.rearrange("(k p) -> p k", p=128), _k("rcol"), allow_slow_non_contiguous=True)
        dma(ident[:], ident_d, _k("ident"))
        dma(dtm[:], dt_d, _k("dtm"))
        dma(xi8[:], xi8_d, _k("xi8"))
        dma(zeta[:], zeta_d, _k("zeta"))
        P.add("dve", lambda e: e.memset(S[:], 0.0), w=[_k("S", cc, h) for cc in range(2) for h in range(4)])
        P.add("pool", lambda e: e.memset(Sb[:], 0.0), w=[_k("Sb", cc, h) for cc in range(2) for h in range(4)])
        ogf = og[:].bitcast(F32)
        ogTf = ogT[:].rearrange("p c t -> p (c t)").bitcast(F32)
        stage = [(stage[0][:], [_k("stage", 0)]), (stage[1][:], [_k("stage", 1)]),
                 (ogf[:, 0:512], [_k("og", 0), _k("og", 1)]), (ogf[:, 512:1024], [_k("og", 2), _k("og", 3)]),
                 (ogTf[:, 0:512], [_k("ogT", 0)]), (ogTf[:, 512:1024], [_k("ogT", 1)])]
        load_weight_bf16(c, win_d, D, ODD_IN, win, "win", stage, gcol, _k("gcol"), CC)
        load_weight_bf16(c, wout_d, 2048, D, wout, "wout", stage, rcol, _k("rcol"), CC, gmap=lambda k: k % 4)

        def bfv(i):
            return psum[i][:].bitcast(BF16)

        for i in range(DBG["ntiles"]):
            t0 = i * 128
            hk = _k("hbuf")
            dma(hbuf[:], h_in[t0:t0 + 128, :], hk)
            dma(cos_t[:], cos_d[t0:t0 + 128, :], _k("cos"))
            dma(sin_t[:], sin_d[t0:t0 + 128, :], _k("sin"))
            rmsnorm_tile(c, hbuf[:], hk, xn[:], _k("xn"), ogT[:, 0:8, :].rearrange("p c t -> p (c t)"), _k("ogT", 0),
                         ss[:, 4:5], _k("ss", 4), rstd[:, 4:5], _k("rstd", 4))

            def tr8(e, src, pb):
                inst = None
                v = bfv(pb)
                for cc in range(8):
                    inst = e.transpose(v[:, cc * 128:(cc + 1) * 128], src[:, cc * 128:(cc + 1) * 128], ident[:])
                return inst
            P.add("pe", lambda e: tr8(e, xn, 0), r=[_k("xn"), _k("ident")], w=[_k("ps", 0)])
            P.add("act", lambda e: e.copy(xnT[:].rearrange("p c t -> p (c t)"), bfv(0)), r=[_k("ps", 0)], w=[_k("xnT")])
            for g in range(12 if DBG["stage"] >= 2 else 0):
                pb = 1 + (g % 2)

                def mm(e, g=g, pb=pb):
                    inst = None
                    for k in range(8):
                        inst = e.matmul(psum[pb][:], xnT[:, k, :], win[:, k, g * 512:(g + 1) * 512],
                                        start=(k == 0), stop=(k == 7))
                    return inst
                wk_ = [_k("win", k, g) for k in range(8)]
                P.add("pe", mm, r=[_k("xnT")] + wk_, w=[_k("ps", pb)])
                if g < 4:
                    dstt = qrot if g < 2 else krot
                    dname = "qrot" if g < 2 else "krot"
                    hh = (g % 2) * 2
                    X = psum[pb][:].rearrange("p (h x d) -> p h x d", h=2, x=2)
                    X1 = X[:, :, 0, :]
                    X2 = X[:, :, 1, :]
                    Dv = dstt[:, hh * 256:(hh + 2) * 256].rearrange("p (h x d) -> p h x d", h=2, x=2)
                    cb = cos_t[:].unsqueeze(1).to_broadcast([128, 2, 128])
                    sb_ = sin_t[:].unsqueeze(1).to_broadcast([128, 2, 128])
                    pk = _k("ps", pb)

                    def tt(e, o, a, b, op):
                        return e.tensor_tensor(o, a, b, op)
                    P.add("dve", lambda e, X1=X1, cb=cb: tt(e, tmp[0][:], X1, cb, ALU.mult), r=[pk, _k("cos")], w=[_k("tmp", 0)])
                    P.add("dve", lambda e, X2=X2, sb_=sb_: tt(e, tmp[1][:], X2, sb_, ALU.mult), r=[pk, _k("sin")], w=[_k("tmp", 1)])
                    P.add("dve", lambda e, X2=X2, cb=cb: tt(e, tmp[2][:], X2, cb, ALU.mult), r=[pk, _k("cos")], w=[_k("tmp", 2)])
                    P.add("dve", lambda e, X1=X1, sb_=sb_: tt(e, tmp[3][:], X1, sb_, ALU.mult), r=[pk, _k("sin")], w=[_k("tmp", 3)])
                    P.add("dve", lambda e, Dv=Dv: tt(e, Dv[:, :, 0, :], tmp[0][:], tmp[1][:], ALU.subtract),
                          r=[_k("tmp", 0), _k("tmp", 1)], w=[_k(dname, g % 2)])
                    P.add("dve", lambda e, Dv=Dv: tt(e, Dv[:, :, 1, :], tmp[2][:], tmp[3][:], ALU.add),
                          r=[_k("tmp", 2), _k("tmp", 3)], w=[_k(dname, g % 2)])
                elif g < 8:
                    vv = g - 4
                    P.add("act", lambda e, vv=vv, pb=pb: e.copy(vb[:, vv * 512:(vv + 1) * 512], psum[pb][:]),
                          r=[_k("ps", pb)], w=[_k("vb", vv)])
                else:
                    vv = g - 8
                    P.add("act", lambda e, vv=vv, pb=pb: e.activation(gs[:, vv * 512:(vv + 1) * 512], psum[pb][:], AF.Silu),
                          r=[_k("ps", pb)], w=[_k("gs", vv)])
            if DBG["stage"] < 3:
                P.add("sp", lambda e, t0=t0: e.dma_start(out=h_out[t0:t0 + 128, :], in_=hbuf[:]), r=[hk], dma=True)
                continue
            P.add("pool", lambda e: e.tensor_tensor(kz[:].rearrange("p (h d) -> p h d", h=4),
                                                    krot[:].rearrange("p (h d) -> p h d", h=4),
                                                    zeta[:].unsqueeze(2).to_broadcast([128, 4, 256]), ALU.mult),
                  r=[_k("krot", 0), _k("krot", 1), _k("zeta")], w=[_k("kz")])
            P.add("pe", lambda e: tr8(e, qrot, 0), r=[_k("qrot", 0), _k("qrot", 1), _k("ident")], w=[_k("ps", 0)])
            P.add("act", lambda e: e.copy(qT[:].rearrange("p c t -> p (c t)"), bfv(0)), r=[_k("ps", 0)], w=[_k("qT")])
            P.add("dve", lambda e: e.tensor_tensor(qxiT[:].rearrange("p c t -> p (c t)"), bfv(0),
                                                   xi8[:].rearrange("p c t -> p (c t)"), ALU.mult),
                  r=[_k("ps", 0), _k("xi8")], w=[_k("qxiT")])
            P.add("pe", lambda e: tr8(e, krot, 3), r=[_k("krot", 0), _k("krot", 1), _k("ident")], w=[_k("ps", 3)])
            P.add("act", lambda e: e.copy(kT[:].rearrange("p c t -> p (c t)"), bfv(3)), r=[_k("ps", 3)], w=[_k("kT")])

            if DBG["stage"] < 4:
                P.add("sp", lambda e, t0=t0: e.dma_start(out=h_out[t0:t0 + 128, :], in_=hbuf[:]), r=[hk], dma=True)
                continue
            def amm(e):
                inst = None
                for h in range(4):
                    for cc in range(2):
                        inst = e.matmul(psum[0][:, h * 128:(h + 1) * 128], kT[:, 2 * h + cc, :], qT[:, 2 * h + cc, :],
                                        start=(cc == 0), stop=(cc == 1))
                return inst
            P.add("pe", amm, r=[_k("kT"), _k("qT")], w=[_k("ps", 0)])
            P.add("dve", lambda e: e.tensor_tensor(atm[:].rearrange("p h t -> p (h t)"), psum[0][:],
                                                   dtm[:].rearrange("p h t -> p (h t)"), ALU.mult),
                  r=[_k("ps", 0), _k("dtm")], w=[_k("atm")])
            if DBG["stage"] < 5:
                P.add("sp", lambda e, t0=t0: e.dma_start(out=h_out[t0:t0 + 128, :], in_=hbuf[:]), r=[hk], dma=True)
                continue
            for h in range(4):
                def omm(e, h=h):
                    e.matmul(psum[4 + h][:], atm[:, h, :], vb[:, h * 512:(h + 1) * 512], start=True, stop=False)
                    e.matmul(psum[4 + h][:], qxiT[:, 2 * h, :], Sb[:, 0, h, :], start=False, stop=False)
                    return e.matmul(psum[4 + h][:], qxiT[:, 2 * h + 1, :], Sb[:, 1, h, :], start=False, stop=True)
                P.add("pe", omm, r=[_k("atm"), _k("vb", h), _k("qxiT"), _k("Sb", 0, h), _k("Sb", 1, h)], w=[_k("ps", 4 + h)])
                P.add("act", lambda e, h=h: e.activation(junk[:], psum[4 + h][:], AF.Square, accum_out=ss[:, h:h + 1]),
                      r=[_k("ps", 4 + h)], w=[_k("junk"), _k("ss", h)])
            if DBG["stage"] < 6:
                P.add("sp", lambda e, t0=t0: e.dma_start(out=h_out[t0:t0 + 128, :], in_=hbuf[:]), r=[hk], dma=True)
                continue
            for h in range(4):
                for cc in range(2):
                    pb = 1 + ((h * 2 + cc) % 2)
                    P.add("pe", lambda e, h=h, cc=cc, pb=pb: e.matmul(psum[pb][:], kz[:, h * 256 + cc * 128:h * 256 + (cc + 1) * 128],
                                                                        vb[:, h * 512:(h + 1) * 512], start=True, stop=True),
                          r=[_k("kz"), _k("vb", h)], w=[_k("ps", pb)])
                    dec = RET_GAMMA[h] ** 128
                    P.add("dve", lambda e, h=h, cc=cc, pb=pb, dec=dec: e.scalar_tensor_tensor(S[:, cc, h, :], S[:, cc, h, :], dec, psum[pb][:], ALU.mult, ALU.add),
                          r=[_k("S", cc, h), _k("ps", pb)], w=[_k("S", cc, h)])
                    P.add("pool", lambda e, h=h, cc=cc: e.tensor_copy(Sb[:, cc, h, :], S[:, cc, h, :]),
                          r=[_k("S", cc, h)], w=[_k("Sb", cc, h)])
            if DBG["stage"] < 7:
                P.add("sp", lambda e, t0=t0: e.dma_start(out=h_out[t0:t0 + 128, :], in_=hbuf[:]), r=[hk], dma=True)
                continue
            rsqrt_small(P, rstd[:, 0:4], _k("rstd", 0), ss[:, 0:4], [_k("ss", h) for h in range(4)], 1.0 / 512)
            for h in range(4):
                P.add("dve", lambda e, h=h: e.scalar_tensor_tensor(og[:, h * 512:(h + 1) * 512], psum[4 + h][:], rstd[:, h:h + 1],
                                                                    gs[:, h * 512:(h + 1) * 512], ALU.mult, ALU.mult),
                      r=[_k("ps", 4 + h), _k("rstd", 0), _k("gs", h)], w=[_k("og", h)])
            for half in range(2):
                pb = 0 if half == 0 else 3

                def tro(e, half=half, pb=pb):
                    inst = None
                    v = bfv(pb)
                    for cc in range(8):
                        c2 = half * 8 + cc
                        inst = e.transpose(v[:, cc * 128:(cc + 1) * 128], og[:, c2 * 128:(c2 + 1) * 128], ident[:])
                    return inst
                P.add("pe", tro, r=[_k("og", half * 2), _k("og", half * 2 + 1), _k("ident")], w=[_k("ps", pb)])
                P.add("act", lambda e, half=half, pb=pb: e.copy(ogT[:, half * 8:(half + 1) * 8, :].rearrange("p c t -> p (c t)"), bfv(pb)),
                      r=[_k("ps", pb)], w=[_k("ogT", half)])
            for mh in range(2):
                pb = 1 + mh

                def ymm(e, mh=mh, pb=pb):
                    inst = None
                    for cc in range(16):
                        inst = e.matmul(psum[pb][:], ogT[:, cc, :], wout[:, cc, mh * 512:(mh + 1) * 512],
                                        start=(cc == 0), stop=(cc == 15))
                    return inst
                P.add("pe", ymm, r=[_k("ogT", 0), _k("ogT", 1)] + [_k("wout", cc, jj) for cc in range(16) for jj in range(2)],
                      w=[_k("ps", pb)])
                P.add("dve", lambda e, mh=mh, pb=pb: e.tensor_tensor(hbuf[:, mh * 512:(mh + 1) * 512], hbuf[:, mh * 512:(mh + 1) * 512],
                                                                      psum[pb][:], ALU.add),
                      r=[_k("ps", pb), hk], w=[hk])
            P.add("sp", lambda e, t0=t0: e.dma_start(out=h_out[t0:t0 + 128, :], in_=hbuf[:]), r=[hk], dma=True)
        P.emit(es)


N_IT = 22
TOPK = 256
IDX_C0 = (64.0 ** -0.5) * (8.0 ** -0.5)


def _interleave(gens):
    st_ = [[g, max(1, n), 0] for g, n in gens]
    while st_:
        st_.sort(key=lambda x: x[2] / x[1])
        g = st_[0]
        try:
            next(g[0])
            g[2] += 1
        except StopIteration:
            st_.pop(0)


def mix0_phase(nc, h_in, h_out, I, C):
    with ExitStack() as es:
        P = Prog(nc)
        c = Ctx(nc, P, es)
        CC = 512
        win = c.sb("win", [128, 8, EVEN_IN], BF16)
        wout = c.sb("wout", [128, 8, D], BF16)
        stage = [c.sb("stage%d" % i, [128, CC], F32) for i in range(2)]
        gcol = c.sb("gcol", [128, 8], F32)
        gcol2 = c.sb("gcol2", [128, 8], F32)
        ident = c.sb("ident", [128, 128], BF16)
        identf = c.sb("identf", [128, 128], F32)
        ones = c.sb("ones", [128, 128], BF16)
        trim = c.sb("trim", [128, 128], F32)
        trir = c.sb("trir", [128, 128], F32)
        m01 = c.sb("m01", [128, 4, 128], F32)
        negm = c.sb("negm", [128, 128], F32)
        pw = c.sb("pw", [128, N_IT], F32)
        wa2 = c.sb("wa2", [16, 256], F32)
        ba2 = c.sb("ba2", [128, 256], F32)
        hbufA = c.sb("hbufA", [128, D], F32)
        hbufC = c.sb("hbufC", [128, D], F32)
        xn = c.sb("xn", [128, D], BF16)
        xnT = c.sb("xnT", [128, 8, 128], BF16)
        junk = c.sb("junk", [128, D], BF16)
        gqk = c.sb("gqk", [128, 512], BF16)
        gv = c.sb("gv", [128, 512], BF16)
        gsl = c.sb("gsl", [128, 512], BF16)
        dq = c.sb("dq", [128, 512], BF16)
        dkb = c.sb("dkb", [128, 128], BF16)
        iq = c.sb("iq", [128, 512], BF16)
        ikb = c.sb("ikb", [128, 64], BF16)
        iw = c.sb("iw", [128, 8], F32)
        wabs = [c.sb("wabs%d" % i, [128, 8], F32) for i in range(2)]
        sgn = c.sb("sgn", [128, 8], F32)
        Dsg = [c.sb("Dsg%d" % i, [128, 8, 128], BF16) for i in range(2)]
        gaT = c.sb("gaT", [16, 128], F32)
        zb = c.sb("zb", [128, 256], F32)
        sp_ = c.sb("sp", [128, 256], F32)
        ecum = c.sb("ecum", [64, 4, 128], F32)
        encum = c.sb("encum", [64, 4, 128], F32)
        erev = c.sb("erev", [128, 256], F32)
        qtT = c.sb("qtT", [64, 4, 128], BF16)
        ktT = c.sb("ktT", [64, 4, 128], BF16)
        kend = c.sb("kend", [128, 256], BF16)
        atm = c.sb("atm", [128, 4, 128], BF16)
        Sg = c.sb("Sg", [64, 4, 128], F32)
        Sgb = c.sb("Sgb", [64, 4, 128], BF16)
        og = c.sb("og", [128, 512], BF16)
        ogT = [c.sb("ogT%d" % i, [128, 4, 128], BF16) for i in range(4)]
        dsT = c.sb("dsT", [128, 4, 128], BF16)
        qT = [c.sb("qT%d" % i, [128, 4, 128], BF16) for i in range(4)]
        kTc = c.sb("kTc", [128, T], BF16)
        vc = c.sb("vc", [128, NT, 128], BF16)
        ikTc = c.sb("ikTc", [64, T], BF16)
        iqT = [c.sb("iqT%d" % i, [64, 8, 128], BF16) for i in range(2)]
        scb = [c.sb("sc%d" % i, [128, T], F32) for i in range(2)]
        msk = c.sb("msk", [128, T], BF16)
        mskT = c.sb("mskT", [128, NT, 128], BF16)
        Rb = [c.sb("Rb%d" % i, [128, 512], BF16) for i in range(2)]
        Eb = [c.sb("Eb%d" % i, [128, 512], BF16) for i in range(2)]
        PTb = [c.sb("PTb%d" % i, [128, 4, 128], BF16) for i in range(2)]
        rden = c.sb("rden", [128, 512], F32)
        ss = c.sb("ss", [128, 8], F32)
        rstd = c.sb("rstd", [128, 8], F32)
        st = c.sb("st", [128, 8], F32)
        hst = c.sb("hst", [128, N_IT], F32)
        psum = [c.ps("ps%d" % i) for i in range(8)]

        def dma(out_ap, in_ap, wk, **kw):
            P.add("sp", lambda e: e.dma_start(out=out_ap, in_=in_ap, **kw), w=[wk], dma=True)

        def bfv(i):
            return psum[i][:].bitcast(BF16)

        dma(gcol[:], I["even_attn_norm"][0].rearrange("(k p) -> p k", p=128), _k("gcol"), allow_slow_non_contiguous=True)
        P.add("dve", lambda e: e.memset(gcol2[:], 1.0), w=[_k("gcol2")])
        for cc in range(4):
            dma(gcol2[:, cc:cc + 1], I["even_gla_norm"][0].rearrange("(p o) -> p o", o=1), _k("gcol2"))
        dma(ident[:], C["ident"], _k("ident"))
        dma(identf[:], C["identf"], _k("identf"))
        dma(ones[:], C["ones"], _k("ones"))
        dma(trim[:], C["gla_trim"], _k("trim"))
        dma(trir[:], C["gla_trir"], _k("trir"))
        dma(m01[:], C["gla_m01"], _k("m01"))
        dma(negm[:], C["dsa_negm"], _k("negm"))
        dma(pw[:], C["dsa_pw"], _k("pw"))
        dma(wa2[:], I["even_gla_wa2"][0], _k("wa2"))
        dma(ba2[:], I["even_gla_ba2"][0].partition_broadcast(128), _k("ba2"))
        P.add("dve", lambda e: e.memset(Sg[:], 0.0), w=[_k("Sg")])
        P.add("pool", lambda e: e.memset(Sgb[:], 0.0), w=[_k("Sgb")])
        stage = [(stage[0][:], [_k("stage", 0)]), (stage[1][:], [_k("stage", 1)])] + \
                [(scb[1][:, si * 512:(si + 1) * 512], [_k("stg", si)]) for si in range(8)]
        load_weight_bf16(c, I["even_w_in"][0], D, EVEN_IN, win, "win", stage, gcol, _k("gcol"), CC)
        load_weight_bf16(c, I["even_w_out"][0], D, D, wout, "wout", stage, gcol2, _k("gcol2"), CC)
        P.add("dve", lambda e: e.memset(st[:, 7:8], 0.0), w=[_k("stg", si) for si in range(8)] + [_k("sc", 1), _k("st", 7)])
        WIN_ALL = [_k("win", k, j) for k in range(8) for j in range(6)]
        WOUT_ALL = [_k("wout", k, j) for k in range(8) for j in range(2)]

        def stageA(i):
            t0 = i * 128
            p2 = i % 2
            p3 = i % 4
            hb = hbufA
            hk = _k("hbufA")
            dma(hb[:], h_in[t0:t0 + 128, :], hk)
            rmsnorm_tile(c, hb[:], hk, xn[:], _k("xn"), junk[:], _k("junk"),
                         ss[:, 4:5], _k("ss", 4), rstd[:, 4:5], _k("rstd", 4))

            def tr8(e):
                inst = None
                v = bfv(0)
                for cc in range(8):
                    inst = e.transpose(v[:, cc * 128:(cc + 1) * 128], xn[:, cc * 128:(cc + 1) * 128], ident[:])
                return inst
            P.add("pe", tr8, r=[_k("xn"), _k("ident")], w=[_k("ps", 0)])
            P.add("act", lambda e: e.copy(xnT[:].rearrange("p c t -> p (c t)"), bfv(0)), r=[_k("ps", 0)], w=[_k("xnT")])
            yield

            def proj(c0, c1, pb):
                def f(e):
                    inst = None
                    for k in range(8):
                        inst = e.matmul(psum[pb][:, 0:(c1 - c0)], xnT[:, k, :], win[:, k, c0:c1], start=(k == 0), stop=(k == 7))
                    return inst
                P.add("pe", f, r=[_k("xnT")] + WIN_ALL, w=[_k("ps", pb)])

            proj(2320, 2832, 1)
            P.add("dve", lambda e: e.tensor_copy(iq[:], psum[1][:]), r=[_k("ps", 1)], w=[_k("iq")])
            yield
            proj(2832, 2904, 2)
            P.add("act", lambda e: e.copy(ikb[:], psum[2][:, 0:64]), r=[_k("ps", 2)], w=[_k("ikb")])
            P.add("act", lambda e: e.copy(iw[:], psum[2][:, 64:72]), r=[_k("ps", 2)], w=[_k("iw")])
            yield
            P.add("act", lambda e: e.activation(wabs[p2][:], iw[:], AF.Abs, scale=IDX_C0), r=[_k("iw")], w=[_k("wabs", p2)])
            P.add("act", lambda e: e.sign(sgn[:], iw[:]), r=[_k("iw")], w=[_k("sgn")])
            P.add("dve", lambda e: e.tensor_tensor(Dsg[p2][:], identf[:].unsqueeze(1).to_broadcast([128, 8, 128]),
                                                   sgn[:].unsqueeze(2).to_broadcast([128, 8, 128]), ALU.mult),
                  r=[_k("identf"), _k("sgn")], w=[_k("Dsg", p2)])

            def triq(e):
                inst = None
                v = bfv(0)
                for hh in range(8):
                    inst = e.transpose(v[0:64, hh * 128:(hh + 1) * 128], iq[:, hh * 64:(hh + 1) * 64], ident[:])
                return inst
            P.add("pe", triq, r=[_k("iq"), _k("ident")], w=[_k("ps", 0)])
            P.add("act", lambda e: e.copy(iqT[p2][:].rearrange("p c t -> p (c t)"), bfv(0)[0:64, :]), r=[_k("ps", 0)], w=[_k("iqT", p2)])
            yield
            P.add("pe", lambda e: e.transpose(bfv(0)[0:64, 0:128], ikb[:], ident[:]), r=[_k("ikb"), _k("ident")], w=[_k("ps", 0)])
            P.add("act", lambda e: e.copy(ikTc[:, t0:t0 + 128], bfv(0)[0:64, 0:128]), r=[_k("ps", 0)], w=[_k("ikTc", i)])
            yield
            proj(1552, 2064, 1)
            P.add("dve", lambda e: e.tensor_copy(dq[:], psum[1][:]), r=[_k("ps", 1)], w=[_k("dq")])
            yield
            proj(2064, 2320, 2)
            P.add("act", lambda e: e.copy(dkb[:], psum[2][:, 0:128]), r=[_k("ps", 2)], w=[_k("dkb")])
            P.add("act", lambda e: e.copy(vc[:, i, :], psum[2][:, 128:256]), r=[_k("ps", 2)], w=[_k("vc", i)])
            yield

            def trdq(e):
                inst = None
                v = bfv(0)
                for cc in range(4):
                    inst = e.transpose(v[:, cc * 128:(cc + 1) * 128], dq[:, cc * 128:(cc + 1) * 128], ident[:])
                inst = e.transpose(v[:, 512:640], dkb[:], ident[:])
                return inst
            P.add("pe", trdq, r=[_k("dq"), _k("dkb"), _k("ident")], w=[_k("ps", 0)])
            P.add("act", lambda e: e.copy(qT[p3][:].rearrange("p c t -> p (c t)"), bfv(0)[:, 0:512]), r=[_k("ps", 0)], w=[_k("qT", p3)])
            P.add("act", lambda e: e.copy(kTc[:, t0:t0 + 128], bfv(0)[:, 512:640]), r=[_k("ps", 0)], w=[_k("kTc", i)])
            yield
            proj(0, 512, 1)
            P.add("act", lambda e: e.copy(gqk[:], psum[1][:]), r=[_k("ps", 1)], w=[_k("gqk")])
            yield
            proj(512, 1024, 2)
            P.add("dve", lambda e: e.tensor_copy(gv[:], psum[2][:]), r=[_k("ps", 2)], w=[_k("gv")])
            yield
            proj(1040, 1552, 1)
            P.add("act", lambda e: e.activation(gsl[:], psum[1][:], AF.Silu), r=[_k("ps", 1)], w=[_k("gsl")])
            yield

            def gaf(e):
                inst = None
                for k in range(8):
                    inst = e.matmul(psum[2][0:16, 0:128], win[:, k, 1024:1040], xnT[:, k, :], start=(k == 0), stop=(k == 7))
                return inst
            P.add("pe", gaf, r=[_k("xnT")] + WIN_ALL, w=[_k("ps", 2)])
            P.add("act", lambda e: e.copy(gaT[:], psum[2][0:16, 0:128]), r=[_k("ps", 2)], w=[_k("gaT")])
            yield
            P.add("pe", lambda e: e.matmul(psum[1][:, 0:256], gaT[:], wa2[:], start=True, stop=True),
                  r=[_k("gaT"), _k("wa2")], w=[_k("ps", 1)])
            P.add("dve", lambda e: e.tensor_tensor(zb[:], psum[1][:, 0:256], ba2[:], ALU.add),
                  r=[_k("ps", 1), _k("ba2")], w=[_k("zb")])
            P.add("act", lambda e: e.activation(zb[:], zb[:], AF.Exp, scale=-1.0), r=[_k("zb")], w=[_k("zb")])
            P.add("act", lambda e: e.activation(sp_[:], zb[:], AF.Ln, bias=1.0), r=[_k("zb")], w=[_k("sp")])
            yield

            def cumf(e):
                inst = None
                for h in range(4):
                    inst = e.matmul(psum[2][0:64, h * 128:(h + 1) * 128], sp_[:, h * 64:(h + 1) * 64], trim[:], start=True, stop=True)
                return inst
            P.add("pe", cumf, r=[_k("sp"), _k("trim")], w=[_k("ps", 2)])
            P.add("pe", lambda e: e.matmul(psum[1][:, 0:256], trir[:], sp_[:], start=True, stop=True),
                  r=[_k("sp"), _k("trir")], w=[_k("ps", 1)])
            P.add("act", lambda e: e.activation(ecum[:].rearrange("p h t -> p (h t)"), psum[2][0:64, :], AF.Exp),
                  r=[_k("ps", 2)], w=[_k("ecum")])
            P.add("act", lambda e: e.activation(encum[:].rearrange("p h t -> p (h t)"), psum[2][0:64, :], AF.Exp, scale=-1.0),
                  r=[_k("ps", 2)], w=[_k("encum")])
            P.add("act", lambda e: e.activation(erev[:], psum[1][:, 0:256], AF.Exp), r=[_k("ps", 1)], w=[_k("erev")])
            yield

            def trqk(e):
                inst = None
                v = bfv(0)
                for hh in range(8):
                    inst = e.transpose(v[0:64, hh * 128:(hh + 1) * 128], gqk[:, hh * 64:(hh + 1) * 64], ident[:])
                return inst
            P.add("pe", trqk, r=[_k("gqk"), _k("ident")], w=[_k("ps", 0)])
            P.add("dve", lambda e: e.scalar_tensor_tensor(qtT[:].rearrange("p h t -> p (h t)"), bfv(0)[0:64, 0:512], 0.125,
                                                          ecum[:].rearrange("p h t -> p (h t)"), ALU.mult, ALU.mult),
                  r=[_k("ps", 0), _k("ecum")], w=[_k("qtT")])
            P.add("dve", lambda e: e.tensor_tensor(ktT[:].rearrange("p h t -> p (h t)"), bfv(0)[0:64, 512:1024],
                                                   encum[:].rearrange("p h t -> p (h t)"), ALU.mult),
                  r=[_k("ps", 0), _k("encum")], w=[_k("ktT")])
            P.add("dve", lambda e: e.tensor_tensor(kend[:], gqk[:, 256:512], erev[:], ALU.mult),
                  r=[_k("gqk"), _k("erev")], w=[_k("kend")])
            yield

            def atf(e):
                inst = None
                for h in range(4):
                    inst = e.matmul(psum[2][:, h * 128:(h + 1) * 128], ktT[:, h, :], qtT[:, h, :], start=True, stop=True)
                return inst
            P.add("pe", atf, r=[_k("ktT"), _k("qtT")], w=[_k("ps", 2)])
            P.add("dve", lambda e: e.tensor_tensor(atm[:].rearrange("p h t -> p (h t)"), psum[2][:],
                                                   m01[:].rearrange("p h t -> p (h t)"), ALU.mult),
                  r=[_k("ps", 2), _k("m01")], w=[_k("atm")])
            yield

            def of(e):
                inst = None
                for h in range(4):
                    e.matmul(psum[1][:, h * 128:(h + 1) * 128], atm[:, h, :], gv[:, h * 128:(h + 1) * 128], start=True, stop=False)
                    inst = e.matmul(psum[1][:, h * 128:(h + 1) * 128], qtT[:, h, :], Sgb[:, h, :], start=False, stop=True)
                return inst
            P.add("pe", of, r=[_k("atm"), _k("gv"), _k("qtT"), _k("Sgb")], w=[_k("ps", 1)])

            def dsf(e):
                inst = None
                for h in range(4):
                    inst = e.matmul(psum[2][0:64, h * 128:(h + 1) * 128], kend[:, h * 64:(h + 1) * 64], gv[:, h * 128:(h + 1) * 128],
                                    start=True, stop=True)
                return inst
            P.add("pe", dsf, r=[_k("kend"), _k("gv")], w=[_k("ps", 2)])
            elast = ecum[:, :, 127:128].to_broadcast([64, 4, 128])
            P.add("dve", lambda e: e.tensor_tensor(Sg[:], Sg[:], elast, ALU.mult), r=[_k("Sg"), _k("ecum")], w=[_k("Sg")])
            P.add("dve", lambda e: e.tensor_tensor(Sg[:].rearrange("p h t -> p (h t)"), Sg[:].rearrange("p h t -> p (h t)"),
                                                   psum[2][0:64, :], ALU.add), r=[_k("Sg"), _k("ps", 2)], w=[_k("Sg")])
            P.add("pool", lambda e: e.tensor_copy(Sgb[:], Sg[:]), r=[_k("Sg")], w=[_k("Sgb")])
            yield
            for h in range(4):
                P.add("act", lambda e, h=h: e.activation(junk[:, 0:128], psum[1][:, h * 128:(h + 1) * 128], AF.Square, accum_out=ss[:, h:h + 1]),
                      r=[_k("ps", 1)], w=[_k("junk"), _k("ss", h)])
            rsqrt_small(P, rstd[:, 0:4], _k("rstd", 0), ss[:, 0:4], [_k("ss", h) for h in range(4)], 1.0 / 128)
            yield
            for h in range(4):
                P.add("dve", lambda e, h=h: e.scalar_tensor_tensor(og[:, h * 128:(h + 1) * 128], psum[1][:, h * 128:(h + 1) * 128], rstd[:, h:h + 1],
                                                                    gsl[:, h * 128:(h + 1) * 128], ALU.mult, ALU.mult),
                      r=[_k("ps", 1), _k("rstd", 0), _k("gsl")], w=[_k("og")])
            yield

            def trog(e):
                inst = None
                v = bfv(0)
                for cc in range(4):
                    inst = e.transpose(v[:, cc * 128:(cc + 1) * 128], og[:, cc * 128:(cc + 1) * 128], ident[:])
                return inst
            P.add("pe", trog, r=[_k("og"), _k("ident")], w=[_k("ps", 0)])
            P.add("act", lambda e: e.copy(ogT[p3][:].rearrange("p c t -> p (c t)"), bfv(0)[:, 0:512]),
                  r=[_k("ps", 0)], w=[_k("ogT", p3)])
            yield

        def stageBs(i):
            p2 = i % 2
            sc = scb[p2]
            nkeys = (i + 1) * 128
            ngrp = (nkeys + 511) // 512
            pend = None
            for gk in range(ngrp):
                k0 = gk * 512
                n = min(512, nkeys - k0)
                kk = [_k("ikTc", b) for b in range(k0 // 128, (k0 + n) // 128)]
                for hI in range(8):
                    rb = hI % 2
                    P.add("pe", lambda e, hI=hI, k0=k0, n=n: e.matmul(psum[3][:, 0:n], iqT[p2][:, hI, :], ikTc[:, k0:k0 + n], start=True, stop=True),
                          r=[_k("iqT", p2)] + kk, w=[_k("ps", 3)])
                    if pend is not None:
                        pend()
                        pend = None
                    P.add("act", lambda e, rb=rb, hI=hI, n=n: e.activation(Rb[rb][:, 0:n], psum[3][:, 0:n], AF.Relu, scale=wabs[p2][:, hI:hI + 1]),
                          r=[_k("ps", 3), _k("wabs", p2)], w=[_k("Rb", rb)])

                    def acc(rb=rb, hI=hI, n=n, k0=k0):
                        P.add("pe", lambda e: e.matmul(psum[4][:, 0:n], Dsg[p2][:, hI, :], Rb[rb][:, 0:n], start=(hI == 0), stop=(hI == 7)),
                              r=[_k("Dsg", p2), _k("Rb", rb)], w=[_k("ps", 4)])
                        if hI == 7:
                            P.add("act", lambda e: e.copy(sc[:, k0:k0 + n], psum[4][:, 0:n]), r=[_k("ps", 4)], w=[_k("sc", p2)])
                    pend = acc
                    yield
            if pend is not None:
                pend()
            yield

        def stageBb(i):
            t0 = i * 128
            p2 = i % 2
            sc = scb[p2]
            nkeys = (i + 1) * 128
            scv = sc[:, 0:nkeys]
            P.add("dve", lambda e: e.tensor_reduce(st[:, 0:1], scv, AX.X, ALU.max), r=[_k("sc", p2)], w=[_k("st", 0)])
            yield
            P.add("dve", lambda e: e.tensor_reduce(st[:, 1:2], scv, AX.X, ALU.min), r=[_k("sc", p2)], w=[_k("st", 1)])
            P.add("dve", lambda e: e.tensor_tensor(sc[:, t0:t0 + 128], sc[:, t0:t0 + 128], negm[:], ALU.add),
                  r=[_k("sc", p2), _k("negm")], w=[_k("sc", p2)])
            P.add("dve", lambda e: e.tensor_tensor(st[:, 2:3], st[:, 0:1], st[:, 1:2], ALU.subtract), r=[_k("st", 0), _k("st", 1)], w=[_k("st", 2)])
            P.add("dve", lambda e: e.tensor_scalar(st[:, 2:3], st[:, 2:3], 1.000001, 1e-30, ALU.mult, ALU.add), r=[_k("st", 2)], w=[_k("st", 2)])
            P.add("dve", lambda e: e.tensor_scalar(hst[:], pw[:], st[:, 2:3], None, ALU.mult), r=[_k("st", 2), _k("pw")], w=[_k("hst")])
            yield
            for k in range(N_IT):
                P.add("dve", lambda e, k=k: e.tensor_tensor(st[:, 3:4], st[:, 1:2], hst[:, k:k + 1], ALU.add),
                      r=[_k("st", 1), _k("hst")], w=[_k("st", 3)])
                P.add("dve", lambda e: e.tensor_scalar(msk[:, 0:nkeys], scv, st[:, 3:4], None, ALU.is_ge, ALU.add, accum_out=st[:, 4:5]),
                      r=[_k("sc", p2), _k("st", 3)], w=[_k("msk"), _k("st", 4)])
                P.add("dve", lambda e, k=k: e.tensor_scalar(st[:, 5:6], st[:, 4:5], TOPK - 0.5, hst[:, k:k + 1], ALU.is_ge, ALU.mult),
                      r=[_k("st", 4), _k("hst")], w=[_k("st", 5)])
                P.add("dve", lambda e: e.tensor_tensor(st[:, 1:2], st[:, 1:2], st[:, 5:6], ALU.add),
                      r=[_k("st", 1), _k("st", 5)], w=[_k("st", 1)])
                yield
            P.add("dve", lambda e: e.tensor_scalar(msk[:, 0:nkeys], scv, st[:, 1:2], None, ALU.is_ge),
                  r=[_k("sc", p2), _k("st", 1)], w=[_k("msk")])
            yield
            for b0 in range(0, i + 1, 8):
                nb = min(8, i + 1 - b0)

                def trm(e, b0=b0, nb=nb):
                    inst = None
                    v = bfv(3)
                    for bb in range(nb):
                        inst = e.transpose(v[:, bb * 128:(bb + 1) * 128], msk[:, (b0 + bb) * 128:(b0 + bb + 1) * 128], ident[:])
                    return inst
                P.add("pe", trm, r=[_k("msk"), _k("ident")], w=[_k("ps", 3)])
                P.add("act", lambda e, b0=b0, nb=nb: e.copy(mskT[:, b0:b0 + nb, :].rearrange("p c t -> p (c t)"), bfv(3)[:, 0:nb * 128]),
                      r=[_k("ps", 3)], w=[_k("mskT", b0 // 8)])
                yield

        def stageC(i):
            t0 = i * 128
            p3 = i % 4
            hb = hbufC
            hk = _k("hbufC")
            dma(hb[:], h_in[t0:t0 + 128, :], hk)
            pendc = []
            for j in range(i + 1):
                eb = j % 2
                P.add("pe", lambda e, j=j: e.matmul(psum[5][:], kTc[:, j * 128:(j + 1) * 128], qT[p3][:].rearrange("p c t -> p (c t)"), start=True, stop=True),
                      r=[_k("kTc", j), _k("qT", p3)], w=[_k("ps", 5)])
                while pendc:
                    pendc.pop(0)()
                P.add("act", lambda e, eb=eb: e.activation(Eb[eb][:], psum[5][:], AF.Exp, scale=128.0 ** -0.5),
                      r=[_k("ps", 5)], w=[_k("Eb", eb)])
                mb = mskT[:, j:j + 1, :].to_broadcast([128, 4, 128])
                P.add("dve", lambda e, eb=eb, mb=mb: e.tensor_tensor(PTb[eb][:], Eb[eb][:].rearrange("p (h t) -> p h t", h=4), mb, ALU.mult),
                      r=[_k("Eb", eb), _k("mskT", j // 8)], w=[_k("PTb", eb)])

                def pv(e, eb=eb, j=j):
                    e.matmul(psum[6][:], vc[:, j, :], PTb[eb][:].rearrange("p h t -> p (h t)"), start=(j == 0), stop=(j == i))
                    return e.matmul(psum[7][:], ones[:], PTb[eb][:].rearrange("p h t -> p (h t)"), start=(j == 0), stop=(j == i))

                def pvadd(pv=pv, j=j, eb=eb):
                    P.add("pe", pv, r=[_k("vc", j), _k("PTb", eb), _k("ones")], w=[_k("ps", 6), _k("ps", 7)])
                pendc.append(pvadd)
                yield
            while pendc:
                pendc.pop(0)()
            P.add("dve", lambda e: e.reciprocal(rden[:], psum[7][:]), r=[_k("ps", 7)], w=[_k("rden")])
            P.add("dve", lambda e: e.tensor_tensor(dsT[:].rearrange("p c t -> p (c t)"), psum[6][:], rden[:], ALU.mult),
                  r=[_k("ps", 6), _k("rden")], w=[_k("dsT")])
            yield
            for mh in range(2):
                def ymm(e, mh=mh):
                    inst = None
                    for cc in range(8):
                        src = ogT[p3][:, cc, :] if cc < 4 else dsT[:, cc - 4, :]
                        inst = e.matmul(psum[5][:], src, wout[:, cc, mh * 512:(mh + 1) * 512], start=(cc == 0), stop=(cc == 7))
                    return inst
                P.add("pe", ymm, r=[_k("ogT", p3), _k("dsT")] + WOUT_ALL, w=[_k("ps", 5)])
                P.add("dve", lambda e, mh=mh: e.tensor_tensor(hb[:, mh * 512:(mh + 1) * 512], hb[:, mh * 512:(mh + 1) * 512], psum[5][:], ALU.add),
                      r=[_k("ps", 5), hk], w=[hk])
                yield
            P.add("sp", lambda e: e.dma_start(out=h_out[t0:t0 + 128, :], in_=hb[:]), r=[hk], dma=True)
            yield

        ntl = DBG["ntiles"]
        for s_ in range(ntl + 3):
            gens = []
            if 0 <= s_ - 3 < ntl:
                gens.append((stageC(s_ - 3), (s_ - 3) + 1 + 4))
            if 0 <= s_ - 2 < ntl:
                gens.append((stageBb(s_ - 2), 4 + N_IT + (s_ - 2) // 8 + 1))
            if 0 <= s_ - 1 < ntl:
                gens.append((stageBs(s_ - 1), 9 * ((s_ - 1) // 4 + 1)))
            if s_ < ntl:
                gens.append((stageA(s_), 24))
            _interleave(gens)
        P.emit(es)


def host_consts():
    import ml_dtypes
    cst = {}
    cst["ident"] = np.eye(128, dtype=ml_dtypes.bfloat16)
    half = 128
    inv = 10000.0 ** (-np.arange(half, dtype=np.float32) / half)
    ang = np.arange(T, dtype=np.float32)[:, None] * inv[None, :].astype(np.float32)
    cst["rope_cos"] = np.cos(ang).astype(np.float32)
    cst["rope_sin"] = np.sin(ang).astype(np.float32)
    g = np.array(RET_GAMMA, dtype=np.float64)
    ii = np.arange(128)
    rel = ii[None, :] - ii[:, None]
    dtm = np.zeros((128, 4, 128), np.float64)
    for h in range(4):
        dtm[:, h, :] = np.where(rel >= 0, g[h] ** np.maximum(rel, 0), 0.0) / 16.0
    cst["ret_dt"] = dtm.astype(np.float32)
    xi8 = np.zeros((128, 8, 128), np.float64)
    for cc in range(8):
        xi8[:, cc, :] = (g[cc // 2] ** (ii + 1.0))[None, :]
    cst["ret_xi8"] = xi8.astype(np.float32)
    cst["ones"] = np.ones((128, 128), dtype=ml_dtypes.bfloat16)
    cst["identf"] = np.eye(128, dtype=np.float32)
    le = (ii[:, None] <= ii[None, :])
    cst["gla_trim"] = np.where(le, -1.0 / 16.0, 0.0).astype(np.float32)
    cst["gla_trir"] = np.where(~le, -1.0 / 16.0, 0.0).astype(np.float32)
    cst["gla_m01"] = np.repeat(le[:, None, :], 4, axis=1).astype(np.float32)
    cst["dsa_negm"] = np.where(ii[None, :] <= ii[:, None], 0.0, -1e30).astype(np.float32)
    cst["dsa_pw"] = np.repeat((0.5 ** (np.arange(N_IT) + 1.0))[None, :], 128, axis=0).astype(np.float32)
    zeta = np.zeros((128, 4), np.float64)
    for h in range(4):
        zeta[:, h] = g[h] ** (127.0 - ii) / 16.0
    cst["ret_zeta"] = zeta.astype(np.float32)
    return cst


INPUT_SHAPES = {
    "even_attn_norm": [1, D], "even_w_in": [1, D, EVEN_IN], "even_gla_wa2": [1, 16, 256],
    "even_gla_ba2": [1, 256], "even_gla_norm": [1, 128], "even_w_out": [1, D, D],
    "odd_attn_norm": [1, D], "odd_w_in": [1, D, ODD_IN], "odd_ret_norm": [1, 512], "odd_w_out": [1, 2048, D],
    "ffn_norm": [2, D], "ffn_w_gate": [2, D, DFF], "ffn_w_up": [2, D, DFF], "ffn_w_down": [2, DFF, D],
    "final_norm": [D],
}


def build(phases=("mix0", "ffn0", "mix1", "ffn1")):
    nc = bass.Bass("TRN2", target_bir_lowering=False)
    x = nc.dram_tensor("x", [T, D], F32, kind="ExternalInput").ap()
    out = nc.dram_tensor("out", [T, D], F32, kind="ExternalOutput").ap()
    I = {k: nc.dram_tensor(k, shp, F32, kind="ExternalInput").ap() for k, shp in INPUT_SHAPES.items()}
    cst = host_consts()
    C = {}
    for k, v in cst.items():
        C[k] = nc.dram_tensor(k, list(v.shape), BF16 if v.dtype != np.float32 else F32, kind="ExternalInput").ap()
    scr = [nc.dram_tensor("scr%d" % i, [T, D], F32, kind="Internal").ap() for i in range(3)]
    bufs = [x] + scr[:len(phases) - 1] + [out]
    for pi, ph in enumerate(phases):
        hin, hout = bufs[pi], bufs[pi + 1]
        if ph == "ffn0":
            ffn_phase(nc, hin, hout, I["ffn_norm"][0], I["ffn_w_gate"][0], I["ffn_w_up"][0], I["ffn_w_down"][0], C["ident"])
        elif ph == "ffn1":
            ffn_phase(nc, hin, hout, I["ffn_norm"][1], I["ffn_w_gate"][1], I["ffn_w_up"][1], I["ffn_w_down"][1], C["ident"],
                      final_g=I["final_norm"])
        elif ph == "mix1":
            mix1_phase(nc, hin, hout, I["odd_attn_norm"][0], I["odd_w_in"][0], I["odd_ret_norm"][0], I["odd_w_out"][0],
                       C["ident"], C["rope_cos"], C["rope_sin"], C["ret_dt"], C["ret_xi8"], C["ret_zeta"])
        elif ph == "mix0":
            mix0_phase(nc, hin, hout, I, C)
    return nc


def make_inputs(inputs, b):
    m = {k: np.ascontiguousarray(np.asarray(inputs[k], dtype=np.float32)) for k in INPUT_SHAPES}
    m["x"] = np.ascontiguousarray(np.asarray(inputs["x"][b], dtype=np.float32))
    m.update(host_consts())
    return m


def kernel(**inputs):
    nc = build()
    in_maps = [make_inputs(inputs, b) for b in range(8)]
    res = run_bass_kernel_spmd(nc, in_maps, core_ids=list(range(8)))
    return np.stack([np.asarray(r["out"], dtype=np.float32) for r in res.results], axis=0)
```

```python
import math
from contextlib import ExitStack

import numpy as np
import concourse.bass as bass
import concourse.mybir as mybir
from concourse.bass_utils import run_bass_kernel_spmd

F32 = mybir.dt.float32
BF16 = mybir.dt.bfloat16
AF = mybir.ActivationFunctionType
ALU = mybir.AluOpType
AX = mybir.AxisListType

T = 4096
D = 1024
DFF = 2816
NT = T // 128
EPS = 1e-6
EVEN_IN = 2904
ODD_IN = 6144


class _Op:
    __slots__ = ("eng", "fn", "deps", "dma", "sig", "sigidx", "dsem", "dval", "ndep")


class Prog:
    COMPUTE = ("pe", "act", "dve", "pool")

    def __init__(self, nc, n_dma_sems=16):
        self.nc = nc
        self.ops = []
        self.lastw = {}
        self.readers = {}
        self.n_dma_sems = n_dma_sems
        self.dma_rr = {"sp": 0, "pool": 0, "act": 0}
        self.dma_cnt = {}

    def add(self, eng, fn, r=(), w=(), dma=False):
        i = len(self.ops)
        deps = set()
        pr = [k for k in r if k[0] == "ps" and k not in w]
        if pr:
            w = list(w) + pr
        for k in r:
            a = self.lastw.get(k)
            if a is not None:
                deps.add(a)
        for k in w:
            a = self.lastw.get(k)
            if a is not None:
                deps.add(a)
            rd = self.readers.get(k)
            if rd:
                deps.update(rd.values())
        op = _Op()
        op.eng = eng
        op.fn = fn
        op.dma = dma
        op.sig = False
        op.sigidx = 0
        op.dsem = None
        op.dval = 0
        fdeps = []
        for a in deps:
            A = self.ops[a]
            if (not dma) and (not A.dma) and eng == "pe" and A.eng == "pe":
                continue
            fdeps.append(a)
        op.deps = sorted(fdeps)
        if dma:
            q = self.dma_rr[eng]
            self.dma_rr[eng] = (q + 1) % self.n_dma_sems
            key = (eng, q)
            self.dma_cnt[key] = self.dma_cnt.get(key, 0) + 1
            op.dsem = key
            op.dval = 16 * self.dma_cnt[key]
        self.ops.append(op)
        for k in w:
            self.lastw[k] = i
            self.readers[k] = {}
        for k in r:
            d = self.readers.setdefault(k, {})
            d[("dma", i) if dma else eng] = i
        return i

    def emit(self, es):
        nc = self.nc
        ops = self.ops
        for op in ops:
            for a in op.deps:
                if not ops[a].dma:
                    ops[a].sig = True
        cnt = {e: 0 for e in self.COMPUTE + ("sp",)}
        for op in ops:
            if op.sig and not op.dma:
                cnt[op.eng] += 1
                op.sigidx = cnt[op.eng]
        sems = {e: es.enter_context(nc.semaphore("s_" + e)) for e in cnt}
        dsems = {}
        for key in self.dma_cnt:
            dsems[key] = es.enter_context(nc.semaphore("d_%s%d" % key))
        engines = {"pe": "tensor", "act": "scalar", "dve": "vector", "pool": "gpsimd", "sp": "sync"}
        with nc.Block() as block:
            for e, attr in engines.items():
                mine = [op for op in ops if op.eng == e]

                def body(engine, mine=mine, e=e):
                    known = {}

                    def wait(sem_key, sem, val):
                        if known.get(sem_key, 0) >= val:
                            return
                        engine.wait_ge(sem, val)
                        known[sem_key] = val

                    for op in mine:
                        for a in op.deps:
                            A = ops[a]
                            if A.dma:
                                wait(A.dsem, dsems[A.dsem], A.dval)
                            else:
                                wait(A.eng, sems[A.eng], A.sigidx)
                        if op.dma:
                            if op.dval > 16:
                                wait(op.dsem, dsems[op.dsem], op.dval - 16)
                            inst = op.fn(engine)
                            inst.then_inc(dsems[op.dsem], 16)
                        else:
                            inst = op.fn(engine)
                            if op.sig:
                                inst.then_inc(sems[e], 1)
                    for key, c in self.dma_cnt.items():
                        if key[0] == e:
                            wait(key, dsems[key], 16 * c)

                getattr(block, attr)(body)


def _k(name, *idx):
    return (name,) + idx


class Ctx:
    def __init__(self, nc, P, es):
        self.nc = nc
        self.P = P
        self.es = es
        self.rr = 0

    UID = [0]

    def sb(self, name, shape, dt):
        Ctx.UID[0] += 1
        return self.es.enter_context(self.nc.sbuf_tensor("sb%d_%s" % (Ctx.UID[0], name), list(shape), dt))

    def ps(self, name):
        Ctx.UID[0] += 1
        return self.es.enter_context(self.nc.psum_tensor("ps%d_%s" % (Ctx.UID[0], name), [128, 512], F32))


def rsqrt_small(P, out_ap, outkey, in_ap, inkey, scale, eps=EPS):
    inkeys = inkey if isinstance(inkey, list) else [inkey]
    P.add("dve", lambda e: e.tensor_scalar(out_ap, in_ap, scale, eps, ALU.mult, ALU.add),
          r=inkeys, w=[outkey])
    P.add("act", lambda e: e.activation(out_ap, out_ap, AF.Ln), r=[outkey], w=[outkey])
    P.add("act", lambda e: e.activation(out_ap, out_ap, AF.Exp, scale=-0.5), r=[outkey], w=[outkey])


def rmsnorm_tile(c, h_ap, hkey, xn_ap, xnkey, junk_ap, junkkey, ss_ap, sskey, rstd_ap, rstdkey):
    P = c.P
    P.add("act", lambda e: e.activation(junk_ap, h_ap, AF.Square, accum_out=ss_ap),
          r=[hkey], w=[junkkey, sskey])
    rsqrt_small(P, rstd_ap, rstdkey, ss_ap, sskey, 1.0 / D)
    P.add("dve", lambda e: e.tensor_scalar(xn_ap, h_ap, rstd_ap, None, ALU.mult),
          r=[hkey, rstdkey], w=[xnkey])


_LW = [0]


def load_weight_bf16(c, w_dram, rows, cols, dst, dstname, stage, gcol=None, gkey=None, colchunk=704, gmap=None):
    P = c.P
    nk = rows // 128
    nch = (cols + colchunk - 1) // colchunk
    if gcol is None:
        for k in range(nk):
            P.add("pool", lambda e, k=k: e.dma_start(out=dst[:, k, :], in_=w_dram[k * 128:(k + 1) * 128, :]),
                  w=[_k(dstname, k, j) for j in range(nch)], dma=True)
        return [_k(dstname, k, j) for k in range(nk) for j in range(nch)]
    for k in range(nk):
        for j in range(nch):
            c0 = j * colchunk
            c1 = min(cols, c0 + colchunk)
            cnt = _LW[0]
            _LW[0] += 1
            st, skeys = stage[cnt % len(stage)]
            src = w_dram[k * 128:(k + 1) * 128, c0:c1]
            P.add("sp", lambda e, st=st, src=src, n=c1 - c0: e.dma_start(out=st[:, 0:n], in_=src),
                  w=skeys, dma=True)
            dap = dst[:, k, c0:c1]
            sap = st[:, 0:c1 - c0]
            eng = ("dve", "act")[cnt % 2]
            rk = list(skeys) + ([gkey] if gcol is not None else [])
            if gcol is None:
                if eng == "act":
                    P.add("act", lambda e, dap=dap, sap=sap: e.copy(dap, sap), r=rk, w=[_k(dstname, k, j)])
                else:
                    P.add(eng, lambda e, dap=dap, sap=sap: e.tensor_copy(dap, sap), r=rk, w=[_k(dstname, k, j)])
            else:
                kk_ = gmap(k) if gmap is not None else k
                g = gcol[:, kk_:kk_ + 1]
                if eng == "act":
                    P.add("act", lambda e, dap=dap, sap=sap, g=g: e.activation(dap, sap, AF.Copy, scale=g),
                          r=rk, w=[_k(dstname, k, j)])
                else:
                    P.add(eng, lambda e, dap=dap, sap=sap, g=g: e.tensor_scalar(dap, sap, g, None, ALU.mult),
                          r=rk, w=[_k(dstname, k, j)])
    return [_k(dstname, k, j) for k in range(nk) for j in range(nch)]


def wkeys(dstname, k, c0, c1, colchunk=704):
    return [_k(dstname, k, j) for j in range(c0 // colchunk, (c1 - 1) // colchunk + 1)]


def ffn_phase(nc, h_in, h_out, g_dram, wg_d, wu_d, wd_d, ident_d, final_g=None):
    with ExitStack() as es:
        P = Prog(nc)
        c = Ctx(nc, P, es)
        CC = 704
        wg = c.sb("wg", [128, 8, DFF], BF16)
        wu = c.sb("wu", [128, 8, DFF], BF16)
        wd = c.sb("wd", [128, 22, D], BF16)
        gcol = c.sb("gcol", [128, 8], F32)
        ident = c.sb("ident", [128, 128], BF16)
        hbuf = c.sb("hbuf", [128, 4, D], F32)
        xn = [c.sb("xn%d" % i, [128, D], BF16) for i in range(2)]
        xnT = c.sb("xnT", [128, 8, 512], BF16)
        actT = c.sb("actT", [128, 22, 512], BF16)
        actf = actT[:].rearrange("p f t -> p (f t)").bitcast(F32)
        stage = []
        for si in range(7):
            stage.append((actf[:, si * 768:si * 768 + CC], [_k("actT", f) for f in range(3 * si, 3 * si + 3)]))
        sg = [c.sb("sg%d" % i, [128, 512], F32) for i in range(2)]
        junk = c.sb("junk", [128, D], BF16)
        ss = c.sb("ss", [128, 8], F32)
        rstd = c.sb("rstd", [128, 8], F32)
        if final_g is not None:
            fg = c.sb("fg", [128, D], F32)
        psum = [c.ps("ps%d" % i) for i in range(8)]

        P.add("sp", lambda e: e.dma_start(out=gcol[:], in_=g_dram.rearrange("(k p) -> p k", p=128),
                                          allow_slow_non_contiguous=True), w=[_k("gcol")], dma=True)
        P.add("sp", lambda e: e.dma_start(out=ident[:], in_=ident_d), w=[_k("ident")], dma=True)
        if final_g is not None:
            P.add("sp", lambda e: e.dma_start(out=fg[:], in_=final_g.partition_broadcast(128)),
                  w=[_k("fg")], dma=True)
        load_weight_bf16(c, wg_d, D, DFF, wg, "wg", stage, gcol, _k("gcol"), CC)
        load_weight_bf16(c, wu_d, D, DFF, wu, "wu", stage, gcol, _k("gcol"), CC)
        load_weight_bf16(c, wd_d, DFF, D, wd, "wd", stage, None, None, CC)

        nsup = T // 512
        tcount = 0
        for s in range(nsup):
            for j in range(4):
                t0 = s * 512 + j * 128
                hk = _k("hbuf", j)
                hap = hbuf[:, j, :]
                P.add("sp", lambda e, hap=hap, t0=t0: e.dma_start(out=hap, in_=h_in[t0:t0 + 128, :]),
                      w=[hk], dma=True)
                b = tcount % 2
                rmsnorm_tile(c, hap, hk, xn[b][:], _k("xn", b), junk[:], _k("junk"),
                             ss[:, j:j + 1], _k("ss", j), rstd[:, j:j + 1], _k("rstd", j))
                pb = tcount % 2
                pst = psum[pb].bitcast(BF16) if hasattr(psum[pb], "bitcast") else psum[pb][:].bitcast(BF16)

                def tr(e, b=b, pst=pst):
                    inst = None
                    for cc in range(8):
                        inst = e.transpose(pst[:, cc * 128:(cc + 1) * 128], xn[b][:, cc * 128:(cc + 1) * 128], ident[:])
                    return inst
                P.add("pe", tr, r=[_k("xn", b), _k("ident")], w=[_k("ps", pb)])
                dst = xnT[:, :, j * 128:(j + 1) * 128]
                src = pst[:, 0:1024].rearrange("p (c t) -> p c t", c=8)
                P.add("act", lambda e, dst=dst, src=src: e.copy(dst, src), r=[_k("ps", pb)], w=[_k("xnT", j)])
                tcount += 1
            xk = [_k("xnT", j) for j in range(4)]
            for f in range(22):
                pg = 2 + (f % 2)
                pu = 4 + (f % 2)

                def mm(e, w_, pi, f=f):
                    inst = None
                    for k in range(8):
                        inst = e.matmul(psum[pi][:], w_[:, k, f * 128:(f + 1) * 128], xnT[:, k, :],
                                        start=(k == 0), stop=(k == 7))
                    return inst
                wkg = [kk for k in range(8) for kk in wkeys("wg", k, f * 128, (f + 1) * 128, CC)]
                wku = [kk for k in range(8) for kk in wkeys("wu", k, f * 128, (f + 1) * 128, CC)]
                P.add("pe", lambda e, pg=pg, mm=mm: mm(e, wg, pg), r=xk + wkg, w=[_k("ps", pg)])
                P.add("pe", lambda e, pu=pu, mm=mm: mm(e, wu, pu), r=xk + wku, w=[_k("ps", pu)])
                sb_ = f % 2
                P.add("act", lambda e, sb_=sb_, pg=pg: e.activation(sg[sb_][:], psum[pg][:], AF.Silu),
                      r=[_k("ps", pg)], w=[_k("sg", sb_)])
                P.add("dve", lambda e, sb_=sb_, pu=pu, f=f: e.tensor_tensor(actT[:, f, :], sg[sb_][:], psum[pu][:], ALU.mult),
                      r=[_k("sg", sb_), _k("ps", pu)], w=[_k("actT", f)])
            ak = [_k("actT", f) for f in range(22)]
            wdk = [kk for f in range(22) for kk in wkeys("wd", f, 0, D, CC)]
            for j in range(4):
                t0 = s * 512 + j * 128
                for mh in range(2):
                    py = 6 + mh

                    def mmd(e, j=j, mh=mh, py=py):
                        inst = None
                        for f in range(22):
                            inst = e.matmul(psum[py][:], actT[:, f, j * 128:(j + 1) * 128],
                                            wd[:, f, mh * 512:(mh + 1) * 512], start=(f == 0), stop=(f == 21))
                        return inst
                    P.add("pe", mmd, r=ak + wdk, w=[_k("ps", py)])
                    hs = hbuf[:, j, mh * 512:(mh + 1) * 512]
                    P.add("dve", lambda e, hs=hs, py=py: e.tensor_tensor(hs, hs, psum[py][:], ALU.add),
                          r=[_k("ps", py), _k("hbuf", j)], w=[_k("hbuf", j)])
                hap = hbuf[:, j, :]
                hk = _k("hbuf", j)
                if final_g is not None:
                    sj = 4 + j
                    P.add("act", lambda e, hap=hap, sj=sj: e.activation(junk[:], hap, AF.Square, accum_out=ss[:, sj:sj + 1]),
                          r=[hk], w=[_k("junk"), _k("ss", sj)])
                    rsqrt_small(P, rstd[:, sj:sj + 1], _k("rstd", sj), ss[:, sj:sj + 1], _k("ss", sj), 1.0 / D)
                    P.add("dve", lambda e, hap=hap, sj=sj: e.scalar_tensor_tensor(hap, hap, rstd[:, sj:sj + 1], fg[:], ALU.mult, ALU.mult),
                          r=[hk, _k("rstd", sj), _k("fg")], w=[hk])
                P.add("sp", lambda e, hap=hap, t0=t0: e.dma_start(out=h_out[t0:t0 + 128, :], in_=hap),
                      r=[hk], dma=True)
        P.emit(es)


RET_GAMMA = [1.0 - 2.0 ** (-5.0 - h) for h in range(4)]


DBG = {"ntiles": NT, "stage": 99}


def mix1_phase(nc, h_in, h_out, g_dram, win_d, retnorm_d, wout_d, ident_d, cos_d, sin_d, dt_d, xi8_d, zeta_d):
    with ExitStack() as es:
        P = Prog(nc)
        c = Ctx(nc, P, es)
        CC = 512
        win = c.sb("win", [128, 8, ODD_IN], BF16)
        wout = c.sb("wout", [128, 16, D], BF16)
        stage = [c.sb("stage%d" % i, [128, CC], F32) for i in range(2)]
        gcol = c.sb("gcol", [128, 8], F32)
        rcol = c.sb("rcol", [128, 4], F32)
        ident = c.sb("ident", [128, 128], BF16)
        S = c.sb("S", [128, 2, 4, 512], F32)
        Sb = c.sb("Sb", [128, 2, 4, 512], BF16)
        hbuf = c.sb("hbuf", [128, D], F32)
        xn = c.sb("xn", [128, D], BF16)
        xnT = c.sb("xnT", [128, 8, 128], BF16)
        tmp = [c.sb("tmp%d" % i, [128, 2, 128], F32) for i in range(4)]
        qrot = c.sb("qrot", [128, D], BF16)
        krot = c.sb("krot", [128, D], BF16)
        kz = c.sb("kz", [128, D], BF16)
        qT = c.sb("qT", [128, 8, 128], BF16)
        qxiT = c.sb("qxiT", [128, 8, 128], BF16)
        kT = c.sb("kT", [128, 8, 128], BF16)
        vb = c.sb("vb", [128, 2048], BF16)
        gs = c.sb("gs", [128, 2048], BF16)
        atm = c.sb("atm", [128, 4, 128], BF16)
        og = c.sb("og", [128, 2048], BF16)
        ogT = c.sb("ogT", [128, 16, 128], BF16)
        junk = c.sb("junk", [128, 512], BF16)
        cos_t = c.sb("cos", [128, 128], F32)
        sin_t = c.sb("sin", [128, 128], F32)
        dtm = c.sb("dtm", [128, 4, 128], F32)
        xi8 = c.sb("xi8", [128, 8, 128], F32)
        zeta = c.sb("zeta", [128, 4], F32)
        ss = c.sb("ss", [128, 8], F32)
        rstd = c.sb("rstd", [128, 8], F32)
        psum = [c.ps("ps%d" % i) for i in range(8)]

        def dma(out_ap, in_ap, wk, **kw):
            P.add("sp", lambda e: e.dma_start(out=out_ap, in_=in_ap, **kw), w=[wk], dma=True)

        dma(gcol[:], g_dram.rearrange("(k p) -> p k", p=128), _k("gcol"), allow_slow_non_contiguous=True)
        dma(rcol[:], retnorm_d.rearrange("(k p) -> p k", p=128), _k("rcol"), allow_slow_non_contiguous=True)
        dma(ident[:], ident_d, _k("ident"))
        dma(dtm[:], dt_d, _k("dtm"))
        dma(xi8[:], xi8_d, _k("xi8"))
        dma(zeta[:], zeta_d, _k("zeta"))
        P.add("dve", lambda e: e.memset(S[:], 0.0), w=[_k("S", cc, h) for cc in range(2) for h in range(4)])
        P.add("pool", lambda e: e.memset(Sb[:], 0.0), w=[_k("Sb", cc, h) for cc in range(2) for h in range(4)])
        ogf = og[:].bitcast(F32)
        ogTf = ogT[:].rearrange("p c t -> p (c t)").bitcast(F32)
        stage = [(stage[0][:], [_k("stage", 0)]), (stage[1][:], [_k("stage", 1)]),
                 (ogf[:, 0:512], [_k("og", 0), _k("og", 1)]), (ogf[:, 512:1024], [_k("og", 2), _k("og", 3)]),
                 (ogTf[:, 0:512], [_k("ogT", 0)]), (ogTf[:, 512:1024], [_k("ogT", 1)])]
        load_weight_bf16(c, win_d, D, ODD_IN, win, "win", stage, gcol, _k("gcol"), CC)
        load_weight_bf16(c, wout_d, 2048, D, wout, "wout", stage, rcol, _k("rcol"), CC, gmap=lambda k: k % 4)

        def bfv(i):
            return psum[i][:].bitcast(BF16)

        for i in range(DBG["ntiles"]):
            t0 = i * 128
            hk = _k("hbuf")
            dma(hbuf[:], h_in[t0:t0 + 128, :], hk)
            dma(cos_t[:], cos_d[t0:t0 + 128, :], _k("cos"))
            dma(sin_t[:], sin_d[t0:t0 + 128, :], _k("sin"))
            rmsnorm_tile(c, hbuf[:], hk, xn[:], _k("xn"), ogT[:, 0:8, :].rearrange("p c t -> p (c t)"), _k("ogT", 0),
                         ss[:, 4:5], _k("ss", 4), rstd[:, 4:5], _k("rstd", 4))

            def tr8(e, src, pb):
                inst = None
                v = bfv(pb)
                for cc in range(8):
                    inst = e.transpose(v[:, cc * 128:(cc + 1) * 128], src[:, cc * 128:(cc + 1) * 128], ident[:])
                return inst
            P.add("pe", lambda e: tr8(e, xn, 0), r=[_k("xn"), _k("ident")], w=[_k("ps", 0)])
            P.add("act", lambda e: e.copy(xnT[:].rearrange("p c t -> p (c t)"), bfv(0)), r=[_k("ps", 0)], w=[_k("xnT")])
            for g in range(12 if DBG["stage"] >= 2 else 0):
                pb = 1 + (g % 2)

                def mm(e, g=g, pb=pb):
                    inst = None
                    for k in range(8):
                        inst = e.matmul(psum[pb][:], xnT[:, k, :], win[:, k, g * 512:(g + 1) * 512],
                                        start=(k == 0), stop=(k == 7))
                    return inst
                wk_ = [_k("win", k, g) for k in range(8)]
                P.add("pe", mm, r=[_k("xnT")] + wk_, w=[_k("ps", pb)])
                if g < 4:
                    dstt = qrot if g < 2 else krot
                    dname = "qrot" if g < 2 else "krot"
                    hh = (g % 2) * 2
                    X = psum[pb][:].rearrange("p (h x d) -> p h x d", h=2, x=2)
                    X1 = X[:, :, 0, :]
                    X2 = X[:, :, 1, :]
                    Dv = dstt[:, hh * 256:(hh + 2) * 256].rearrange("p (h x d) -> p h x d", h=2, x=2)
                    cb = cos_t[:].unsqueeze(1).to_broadcast([128, 2, 128])
                    sb_ = sin_t[:].unsqueeze(1).to_broadcast([128, 2, 128])
                    pk = _k("ps", pb)

                    def tt(e, o, a, b, op):
                        return e.tensor_tensor(o, a, b, op)
                    P.add("dve", lambda e, X1=X1, cb=cb: tt(e, tmp[0][:], X1, cb, ALU.mult), r=[pk, _k("cos")], w=[_k("tmp", 0)])
                    P.add("dve", lambda e, X2=X2, sb_=sb_: tt(e, tmp[1][:], X2, sb_, ALU.mult), r=[pk, _k("sin")], w=[_k("tmp", 1)])
                    P.add("dve", lambda e, X2=X2, cb=cb: tt(e, tmp[2][:], X2, cb, ALU.mult), r=[pk, _k("cos")], w=[_k("tmp", 2)])
                    P.add("dve", lambda e, X1=X1, sb_=sb_: tt(e, tmp[3][:], X1, sb_, ALU.mult), r=[pk, _k("sin")], w=[_k("tmp", 3)])
                    P.add("dve", lambda e, Dv=Dv: tt(e, Dv[:, :, 0, :], tmp[0][:], tmp[1][:], ALU.subtract),
                          r=[_k("tmp", 0), _k("tmp", 1)], w=[_k(dname, g % 2)])
                    P.add("dve", lambda e, Dv=Dv: tt(e, Dv[:, :, 1, :], tmp[2][:], tmp[3][:], ALU.add),
                          r=[_k("tmp", 2), _k("tmp", 3)], w=[_k(dname, g % 2)])
                elif g < 8:
                    vv = g - 4
                    P.add("act", lambda e, vv=vv, pb=pb: e.copy(vb[:, vv * 512:(vv + 1) * 512], psum[pb][:]),
                          r=[_k("ps", pb)], w=[_k("vb", vv)])
                else:
                    vv = g - 8
                    P.add("act", lambda e, vv=vv, pb=pb: e.activation(gs[:, vv * 512:(vv + 1) * 512], psum[pb][:], AF.Silu),
                          r=[_k("ps", pb)], w=[_k("gs", vv)])
            if DBG["stage"] < 3:
                P.add("sp", lambda e, t0=t0: e.dma_start(out=h_out[t0:t0 + 128, :], in_=hbuf[:]), r=[hk], dma=True)
                continue
            P.add("pool", lambda e: e.tensor_tensor(kz[:].rearrange("p (h d) -> p h d", h=4),
                                                    krot[:].rearrange("p (h d) -> p h d", h=4),
                                                    zeta[:].unsqueeze(2).to_broadcast([128, 4, 256]), ALU.mult),
                  r=[_k("krot", 0), _k("krot", 1), _k("zeta")], w=[_k("kz")])
            P.add("pe", lambda e: tr8(e, qrot, 0), r=[_k("qrot", 0), _k("qrot", 1), _k("ident")], w=[_k("ps", 0)])
            P.add("act", lambda e: e.copy(qT[:].rearrange("p c t -> p (c t)"), bfv(0)), r=[_k("ps", 0)], w=[_k("qT")])
            P.add("dve", lambda e: e.tensor_tensor(qxiT[:].rearrange("p c t -> p (c t)"), bfv(0),
                                                   xi8[:].rearrange("p c t -> p (c t)"), ALU.mult),
                  r=[_k("ps", 0), _k("xi8")], w=[_k("qxiT")])
            P.add("pe", lambda e: tr8(e, krot, 3), r=[_k("krot", 0), _k("krot", 1), _k("ident")], w=[_k("ps", 3)])
            P.add("act", lambda e: e.copy(kT[:].rearrange("p c t -> p (c t)"), bfv(3)), r=[_k("ps", 3)], w=[_k("kT")])

            if DBG["stage"] < 4:
                P.add("sp", lambda e, t0=t0: e.dma_start(out=h_out[t0:t0 + 128, :], in_=hbuf[:]), r=[hk], dma=True)
                continue
            def amm(e):
                inst = None
                for h in range(4):
                    for cc in range(2):
                        inst = e.matmul(psum[0][:, h * 128:(h + 1) * 128], kT[:, 2 * h + cc, :], qT[:, 2 * h + cc, :],
                                        start=(cc == 0), stop=(cc == 1))
                return inst
            P.add("pe", amm, r=[_k("kT"), _k("qT")], w=[_k("ps", 0)])
            P.add("dve", lambda e: e.tensor_tensor(atm[:].rearrange("p h t -> p (h t)"), psum[0][:],
                                                   dtm[:].rearrange("p h t -> p (h t)"), ALU.mult),
                  r=[_k("ps", 0), _k("dtm")], w=[_k("atm")])
            if DBG["stage"] < 5:
                P.add("sp", lambda e, t0=t0: e.dma_start(out=h_out[t0:t0 + 128, :], in_=hbuf[:]), r=[hk], dma=True)
                continue
            for h in range(4):
                def omm(e, h=h):
                    e.matmul(psum[4 + h][:], atm[:, h, :], vb[:, h * 512:(h + 1) * 512], start=True, stop=False)
                    e.matmul(psum[4 + h][:], qxiT[:, 2 * h, :], Sb[:, 0, h, :], start=False, stop=False)
                    return e.matmul(psum[4 + h][:], qxiT[:, 2 * h + 1, :], Sb[:, 1, h, :], start=False, stop=True)
                P.add("pe", omm, r=[_k("atm"), _k("vb", h), _k("qxiT"), _k("Sb", 0, h), _k("Sb", 1, h)], w=[_k("ps", 4 + h)])
                P.add("act", lambda e, h=h: e.activation(junk[:], psum[4 + h][:], AF.Square, accum_out=ss[:, h:h + 1]),
                      r=[_k("ps", 4 + h)], w=[_k("junk"), _k("ss", h)])
            if DBG["stage"] < 6:
                P.add("sp", lambda e, t0=t0: e.dma_start(out=h_out[t0:t0 + 128, :], in_=hbuf[:]), r=[hk], dma=True)
                continue
            for h in range(4):
                for cc in range(2):
                    pb = 1 + ((h * 2 + cc) % 2)
                    P.add("pe", lambda e, h=h, cc=cc, pb=pb: e.matmul(psum[pb][:], kz[:, h * 256 + cc * 128:h * 256 + (cc + 1) * 128],
                                                                        vb[:, h * 512:(h + 1) * 512], start=True, stop=True),
                          r=[_k("kz"), _k("vb", h)], w=[_k("ps", pb)])
                    dec = RET_GAMMA[h] ** 128
                    P.add("dve", lambda e, h=h, cc=cc, pb=pb, dec=dec: e.scalar_tensor_tensor(S[:, cc, h, :], S[:, cc, h, :], dec, psum[pb][:], ALU.mult, ALU.add),
                          r=[_k("S", cc, h), _k("ps", pb)], w=[_k("S", cc, h)])
                    P.add("pool", lambda e, h=h, cc=cc: e.tensor_copy(Sb[:, cc, h, :], S[:, cc, h, :]),
                          r=[_k("S", cc, h)], w=[_k("Sb", cc, h)])
            if DBG["stage"] < 7:
                P.add("sp", lambda e, t0=t0: e.dma_start(out=h_out[t0:t0 + 128, :], in_=hbuf[:]), r=[hk], dma=True)
                continue
            rsqrt_small(P, rstd[:, 0:4], _k("rstd", 0), ss[:, 0:4], [_k("ss", h) for h in range(4)], 1.0 / 512)
            for h in range(4):
                P.add("dve", lambda e, h=h: e.scalar_tensor_tensor(og[:, h * 512:(h + 1) * 512], psum[4 + h][:], rstd[:, h:h + 1],
                                                                    gs[:, h * 512:(h + 1) * 512], ALU.mult, ALU.mult),
                      r=[_k("ps", 4 + h), _k("rstd", 0), _k("gs", h)], w=[_k("og", h)])
            for half in range(2):
                pb = 0 if half == 0 else 3

                def tro(e, half=half, pb=pb):
                    inst = None
                    v = bfv(pb)
                    for cc in range(8):
                        c2 = half * 8 + cc
                        inst = e.transpose(v[:, cc * 128:(cc + 1) * 128], og[:, c2 * 128:(c2 + 1) * 128], ident[:])
                    return inst
                P.add("pe", tro, r=[_k("og", half * 2), _k("og", half * 2 + 1), _k("ident")], w=[_k("ps", pb)])
                P.add("act", lambda e, half=half, pb=pb: e.copy(ogT[:, half * 8:(half + 1) * 8, :].rearrange("p c t -> p (c t)"), bfv(pb)),
                      r=[_k("ps", pb)], w=[_k("ogT", half)])
            for mh in range(2):
                pb = 1 + mh

                def ymm(e, mh=mh, pb=pb):
                    inst = None
                    for cc in range(16):
                        inst = e.matmul(psum[pb][:], ogT[:, cc, :], wout[:, cc, mh * 512:(mh + 1) * 512],
                                        start=(cc == 0), stop=(cc == 15))
                    return inst
                P.add("pe", ymm, r=[_k("ogT", 0), _k("ogT", 1)] + [_k("wout", cc, jj) for cc in range(16) for jj in range(2)],
                      w=[_k("ps", pb)])
                P.add("dve", lambda e, mh=mh, pb=pb: e.tensor_tensor(hbuf[:, mh * 512:(mh + 1) * 512], hbuf[:, mh * 512:(mh + 1) * 512],
                                                                      psum[pb][:], ALU.add),
                      r=[_k("ps", pb), hk], w=[hk])
            P.add("sp", lambda e, t0=t0: e.dma_start(out=h_out[t0:t0 + 128, :], in_=hbuf[:]), r=[hk], dma=True)
        P.emit(es)


MASK_BIG = 30000.0
N_IT = 22
TOPK = 256
IDX_C0 = (64.0 ** -0.5) * (8.0 ** -0.5)
ACT_BISECT_EVERY = 10 ** 9


def _interleave(gens):
    st_ = [[g, max(1, n), 0] for g, n in gens]
    while st_:
        st_.sort(key=lambda x: x[2] / x[1])
        g = st_[0]
        try:
            next(g[0])
            g[2] += 1
        except StopIteration:
            st_.pop(0)


def mix0_phase(nc, h_in, h_out, I, C):
    with ExitStack() as es:
        P = Prog(nc)
        c = Ctx(nc, P, es)
        CC = 512
        win = c.sb("win", [128, 8, EVEN_IN], BF16)
        wout = c.sb("wout", [128, 8, D], BF16)
        stage = [c.sb("stage%d" % i, [128, CC], F32) for i in range(2)]
        gcol = c.sb("gcol", [128, 8], F32)
        gcol2 = c.sb("gcol2", [128, 8], F32)
        ident = c.sb("ident", [128, 128], BF16)
        identf = c.sb("identf", [128, 128], F32)
        ones = c.sb("ones", [128, 128], BF16)
        ident4 = c.sb("ident4", [128, 512], BF16)
        trim = c.sb("trim", [128, 128], F32)
        trir = c.sb("trir", [128, 128], F32)
        m01 = c.sb("m01", [128, 4, 128], F32)
        negm = c.sb("negm", [128, 128], F32)
        pw = c.sb("pw", [128, N_IT + 1], F32)
        wa2 = c.sb("wa2", [16, 256], F32)
        ba2 = c.sb("ba2", [128, 256], F32)
        hbufA = c.sb("hbufA", [128, D], F32)
        hbufC = c.sb("hbufC", [128, D], F32)
        xn = c.sb("xn", [128, D], BF16)
        xnT = c.sb("xnT", [128, 8, 128], BF16)
        junk = c.sb("junk", [128, D], BF16)
        gqk = c.sb("gqk", [128, 512], BF16)
        gv = c.sb("gv", [128, 512], BF16)
        gsl = c.sb("gsl", [128, 512], BF16)
        dq = c.sb("dq", [128, 512], BF16)
        dkb = c.sb("dkb", [128, 128], BF16)
        iq = c.sb("iq", [128, 512], BF16)
        ikb = c.sb("ikb", [128, 64], BF16)
        iw = c.sb("iw", [128, 8], F32)
        wabs = [c.sb("wabs%d" % i, [128, 8], F32) for i in range(2)]
        sgn = c.sb("sgn", [128, 8], F32)
        Dsg = [c.sb("Dsg%d" % i, [128, 8, 128], BF16) for i in range(2)]
        gaT = c.sb("gaT", [16, 128], F32)
        zb = c.sb("zb", [128, 256], F32)
        sp_ = c.sb("sp", [128, 256], F32)
        ecum = c.sb("ecum", [64, 4, 128], F32)
        encum = c.sb("encum", [64, 4, 128], F32)
        erev = c.sb("erev", [128, 256], F32)
        qtT = c.sb("qtT", [64, 4, 128], BF16)
        ktT = c.sb("ktT", [64, 4, 128], BF16)
        kend = c.sb("kend", [128, 256], BF16)
        atm = c.sb("atm", [128, 4, 128], BF16)
        Sg = c.sb("Sg", [64, 4, 128], F32)
        Sgb = c.sb("Sgb", [64, 4, 128], BF16)
        og = c.sb("og", [128, 512], BF16)
        ogT = [c.sb("ogT%d" % i, [128, 4, 128], BF16) for i in range(4)]
        dsT = c.sb("dsT", [128, 4, 128], BF16)
        qT = [c.sb("qT%d" % i, [128, 4, 128], BF16) for i in range(4)]
        kTc = c.sb("kTc", [128, T], BF16)
        vc = c.sb("vc", [128, NT, 128], BF16)
        ikTc = c.sb("ikTc", [64, T], BF16)
        iqT = [c.sb("iqT%d" % i, [64, 8, 128], BF16) for i in range(2)]
        scb = [c.sb("sc%d" % i, [128, T], F32) for i in range(2)]
        mskb = [c.sb("msk%d" % i, [128, T], BF16) for i in range(2)]
        junkb = c.sb("junkb", [128, T], mybir.dt.int8)
        Rb = [c.sb("Rb%d" % i, [128, 512], BF16) for i in range(2)]
        Eb = [c.sb("Eb%d" % i, [128, 512], BF16) for i in range(2)]
        rden = c.sb("rden", [128, 512], F32)
        ss = c.sb("ss", [128, 8], F32)
        rstd = c.sb("rstd", [128, 8], F32)
        st = c.sb("st", [128, 8], F32)
        hst = c.sb("hst", [128, N_IT + 1], F32)
        nhst = c.sb("nhst", [128, N_IT], F32)
        hhst = c.sb("hhst", [128, N_IT], F32)
        psum = [c.ps("ps%d" % i) for i in range(8)]

        def dma(out_ap, in_ap, wk, **kw):
            P.add("sp", lambda e: e.dma_start(out=out_ap, in_=in_ap, **kw), w=[wk], dma=True)

        def bfv(i):
            return psum[i][:].bitcast(BF16)

        dma(gcol[:], I["even_attn_norm"][0].rearrange("(k p) -> p k", p=128), _k("gcol"), allow_slow_non_contiguous=True)
        P.add("dve", lambda e: e.memset(gcol2[:], 1.0), w=[_k("gcol2")])
        for cc in range(4):
            dma(gcol2[:, cc:cc + 1], I["even_gla_norm"][0].rearrange("(p o) -> p o", o=1), _k("gcol2"))
        dma(ident[:], C["ident"], _k("ident"))
        dma(identf[:], C["identf"], _k("identf"))
        dma(ones[:], C["ones"], _k("ones"))
        dma(ident4[:], C["ident4"], _k("ident4"))
        dma(trim[:], C["gla_trim"], _k("trim"))
        dma(trir[:], C["gla_trir"], _k("trir"))
        dma(m01[:], C["gla_m01"], _k("m01"))
        dma(negm[:], C["dsa_negm"], _k("negm"))
        dma(pw[:], C["dsa_pw"], _k("pw"))
        dma(wa2[:], I["even_gla_wa2"][0], _k("wa2"))
        dma(ba2[:], I["even_gla_ba2"][0].partition_broadcast(128), _k("ba2"))
        P.add("dve", lambda e: e.memset(Sg[:], 0.0), w=[_k("Sg")])
        P.add("pool", lambda e: e.memset(Sgb[:], 0.0), w=[_k("Sgb")])
        stage = [(stage[0][:], [_k("stage", 0)]), (stage[1][:], [_k("stage", 1)])] + \
                [(scb[1][:, si * 512:(si + 1) * 512], [_k("stg", si)]) for si in range(8)]
        load_weight_bf16(c, I["even_w_in"][0], D, EVEN_IN, win, "win", stage, gcol, _k("gcol"), CC)
        load_weight_bf16(c, I["even_w_out"][0], D, D, wout, "wout", stage, gcol2, _k("gcol2"), CC)
        P.add("dve", lambda e: e.memset(st[:, 7:8], 0.0), w=[_k("stg", si) for si in range(8)] + [_k("sc", 1), _k("st", 7)])
        WIN_ALL = [_k("win", k, j) for k in range(8) for j in range(6)]
        WOUT_ALL = [_k("wout", k, j) for k in range(8) for j in range(2)]

        def stageA(i):
            t0 = i * 128
            p2 = i % 2
            p3 = i % 4
            hb = hbufA
            hk = _k("hbufA")
            dma(hb[:], h_in[t0:t0 + 128, :], hk)
            rmsnorm_tile(c, hb[:], hk, xn[:], _k("xn"), junk[:], _k("junk"),
                         ss[:, 4:5], _k("ss", 4), rstd[:, 4:5], _k("rstd", 4))

            def tr8(e):
                inst = None
                v = bfv(0)
                for cc in range(8):
                    inst = e.transpose(v[:, cc * 128:(cc + 1) * 128], xn[:, cc * 128:(cc + 1) * 128], ident[:])
                return inst
            P.add("pe", tr8, r=[_k("xn"), _k("ident")], w=[_k("ps", 0)])
            P.add("act", lambda e: e.copy(xnT[:].rearrange("p c t -> p (c t)"), bfv(0)), r=[_k("ps", 0)], w=[_k("xnT")])
            yield

            def proj(c0, c1, pb):
                def f(e):
                    inst = None
                    for k in range(8):
                        inst = e.matmul(psum[pb][:, 0:(c1 - c0)], xnT[:, k, :], win[:, k, c0:c1], start=(k == 0), stop=(k == 7))
                    return inst
                P.add("pe", f, r=[_k("xnT")] + WIN_ALL, w=[_k("ps", pb)])

            proj(2320, 2832, 1)
            P.add("dve", lambda e: e.tensor_copy(iq[:], psum[1][:]), r=[_k("ps", 1)], w=[_k("iq")])
            yield
            proj(2832, 2904, 2)
            P.add("act", lambda e: e.copy(ikb[:], psum[2][:, 0:64]), r=[_k("ps", 2)], w=[_k("ikb")])
            P.add("act", lambda e: e.copy(iw[:], psum[2][:, 64:72]), r=[_k("ps", 2)], w=[_k("iw")])
            yield
            P.add("act", lambda e: e.activation(wabs[p2][:], iw[:], AF.Abs, scale=IDX_C0), r=[_k("iw")], w=[_k("wabs", p2)])
            P.add("act", lambda e: e.sign(sgn[:], iw[:]), r=[_k("iw")], w=[_k("sgn")])
            P.add("dve", lambda e: e.tensor_tensor(Dsg[p2][:], identf[:].unsqueeze(1).to_broadcast([128, 8, 128]),
                                                   sgn[:].unsqueeze(2).to_broadcast([128, 8, 128]), ALU.mult),
                  r=[_k("identf"), _k("sgn")], w=[_k("Dsg", p2)])

            def triq(e):
                inst = None
                v = bfv(0)
                for hh in range(8):
                    inst = e.transpose(v[0:64, hh * 128:(hh + 1) * 128], iq[:, hh * 64:(hh + 1) * 64], ident[:])
                return inst
            P.add("pe", triq, r=[_k("iq"), _k("ident")], w=[_k("ps", 0)])
            P.add("act", lambda e: e.copy(iqT[p2][:].rearrange("p c t -> p (c t)"), bfv(0)[0:64, :]), r=[_k("ps", 0)], w=[_k("iqT", p2)])
            yield
            P.add("pe", lambda e: e.transpose(bfv(0)[0:64, 0:128], ikb[:], ident[:]), r=[_k("ikb"), _k("ident")], w=[_k("ps", 0)])
            P.add("act", lambda e: e.copy(ikTc[:, t0:t0 + 128], bfv(0)[0:64, 0:128]), r=[_k("ps", 0)], w=[_k("ikTc", i)])
            yield
            proj(1552, 2064, 1)
            P.add("dve", lambda e: e.tensor_copy(dq[:], psum[1][:]), r=[_k("ps", 1)], w=[_k("dq")])
            yield
            proj(2064, 2320, 2)
            P.add("act", lambda e: e.copy(dkb[:], psum[2][:, 0:128]), r=[_k("ps", 2)], w=[_k("dkb")])
            P.add("act", lambda e: e.copy(vc[:, i, :], psum[2][:, 128:256]), r=[_k("ps", 2)], w=[_k("vc", i)])
            yield

            def trdq(e):
                inst = None
                v = bfv(0)
                for cc in range(4):
                    inst = e.transpose(v[:, cc * 128:(cc + 1) * 128], dq[:, cc * 128:(cc + 1) * 128], ident[:])
                inst = e.transpose(v[:, 512:640], dkb[:], ident[:])
                return inst
            P.add("pe", trdq, r=[_k("dq"), _k("dkb"), _k("ident")], w=[_k("ps", 0)])
            P.add("act", lambda e: e.copy(qT[p3][:].rearrange("p c t -> p (c t)"), bfv(0)[:, 0:512]), r=[_k("ps", 0)], w=[_k("qT", p3)])
            P.add("act", lambda e: e.copy(kTc[:, t0:t0 + 128], bfv(0)[:, 512:640]), r=[_k("ps", 0)], w=[_k("kTc", i)])
            yield
            proj(0, 512, 1)
            P.add("act", lambda e: e.copy(gqk[:], psum[1][:]), r=[_k("ps", 1)], w=[_k("gqk")])
            yield
            proj(512, 1024, 2)
            P.add("dve", lambda e: e.tensor_copy(gv[:], psum[2][:]), r=[_k("ps", 2)], w=[_k("gv")])
            yield
            proj(1040, 1552, 1)
            P.add("act", lambda e: e.activation(gsl[:], psum[1][:], AF.Silu), r=[_k("ps", 1)], w=[_k("gsl")])
            yield

            def gaf(e):
                inst = None
                for k in range(8):
                    inst = e.matmul(psum[2][0:16, 0:128], win[:, k, 1024:1040], xnT[:, k, :], start=(k == 0), stop=(k == 7))
                return inst
            P.add("pe", gaf, r=[_k("xnT")] + WIN_ALL, w=[_k("ps", 2)])
            P.add("act", lambda e: e.copy(gaT[:], psum[2][0:16, 0:128]), r=[_k("ps", 2)], w=[_k("gaT")])
            yield
            P.add("pe", lambda e: e.matmul(psum[1][:, 0:256], gaT[:], wa2[:], start=True, stop=True),
                  r=[_k("gaT"), _k("wa2")], w=[_k("ps", 1)])
            P.add("dve", lambda e: e.tensor_tensor(zb[:], psum[1][:, 0:256], ba2[:], ALU.add),
                  r=[_k("ps", 1), _k("ba2")], w=[_k("zb")])
            P.add("act", lambda e: e.activation(zb[:], zb[:], AF.Exp, scale=-1.0), r=[_k("zb")], w=[_k("zb")])
            P.add("act", lambda e: e.activation(sp_[:], zb[:], AF.Ln, bias=1.0), r=[_k("zb")], w=[_k("sp")])
            yield

            def cumf(e):
                inst = None
                for h in range(4):
                    inst = e.matmul(psum[2][0:64, h * 128:(h + 1) * 128], sp_[:, h * 64:(h + 1) * 64], trim[:], start=True, stop=True)
                return inst
            P.add("pe", cumf, r=[_k("sp"), _k("trim")], w=[_k("ps", 2)])
            P.add("pe", lambda e: e.matmul(psum[1][:, 0:256], trir[:], sp_[:], start=True, stop=True),
                  r=[_k("sp"), _k("trir")], w=[_k("ps", 1)])
            P.add("act", lambda e: e.activation(ecum[:].rearrange("p h t -> p (h t)"), psum[2][0:64, :], AF.Exp),
                  r=[_k("ps", 2)], w=[_k("ecum")])
            P.add("act", lambda e: e.activation(encum[:].rearrange("p h t -> p (h t)"), psum[2][0:64, :], AF.Exp, scale=-1.0),
                  r=[_k("ps", 2)], w=[_k("encum")])
            P.add("act", lambda e: e.activation(erev[:], psum[1][:, 0:256], AF.Exp), r=[_k("ps", 1)], w=[_k("erev")])
            yield

            def trqk(e):
                inst = None
                v = bfv(0)
                for hh in range(8):
                    inst = e.transpose(v[0:64, hh * 128:(hh + 1) * 128], gqk[:, hh * 64:(hh + 1) * 64], ident[:])
                return inst
            P.add("pe", trqk, r=[_k("gqk"), _k("ident")], w=[_k("ps", 0)])
            P.add("dve", lambda e: e.scalar_tensor_tensor(qtT[:].rearrange("p h t -> p (h t)"), bfv(0)[0:64, 0:512], 0.125,
                                                          ecum[:].rearrange("p h t -> p (h t)"), ALU.mult, ALU.mult),
                  r=[_k("ps", 0), _k("ecum")], w=[_k("qtT")])
            P.add("dve", lambda e: e.tensor_tensor(ktT[:].rearrange("p h t -> p (h t)"), bfv(0)[0:64, 512:1024],
                                                   encum[:].rearrange("p h t -> p (h t)"), ALU.mult),
                  r=[_k("ps", 0), _k("encum")], w=[_k("ktT")])
            P.add("dve", lambda e: e.tensor_tensor(kend[:], gqk[:, 256:512], erev[:], ALU.mult),
                  r=[_k("gqk"), _k("erev")], w=[_k("kend")])
            yield

            def atf(e):
                inst = None
                for h in range(4):
                    inst = e.matmul(psum[2][:, h * 128:(h + 1) * 128], ktT[:, h, :], qtT[:, h, :], start=True, stop=True)
                return inst
            P.add("pe", atf, r=[_k("ktT"), _k("qtT")], w=[_k("ps", 2)])
            P.add("dve", lambda e: e.tensor_tensor(atm[:].rearrange("p h t -> p (h t)"), psum[2][:],
                                                   m01[:].rearrange("p h t -> p (h t)"), ALU.mult),
                  r=[_k("ps", 2), _k("m01")], w=[_k("atm")])
            yield

            def of(e):
                inst = None
                for h in range(4):
                    e.matmul(psum[1][:, h * 128:(h + 1) * 128], atm[:, h, :], gv[:, h * 128:(h + 1) * 128], start=True, stop=False)
                    inst = e.matmul(psum[1][:, h * 128:(h + 1) * 128], qtT[:, h, :], Sgb[:, h, :], start=False, stop=True)
                return inst
            P.add("pe", of, r=[_k("atm"), _k("gv"), _k("qtT"), _k("Sgb")], w=[_k("ps", 1)])

            def dsf(e):
                inst = None
                for h in range(4):
                    inst = e.matmul(psum[2][0:64, h * 128:(h + 1) * 128], kend[:, h * 64:(h + 1) * 64], gv[:, h * 128:(h + 1) * 128],
                                    start=True, stop=True)
                return inst
            P.add("pe", dsf, r=[_k("kend"), _k("gv")], w=[_k("ps", 2)])
            elast = ecum[:, :, 127:128].to_broadcast([64, 4, 128])
            P.add("dve", lambda e: e.tensor_tensor(Sg[:], Sg[:], elast, ALU.mult), r=[_k("Sg"), _k("ecum")], w=[_k("Sg")])
            P.add("dve", lambda e: e.tensor_tensor(Sg[:].rearrange("p h t -> p (h t)"), Sg[:].rearrange("p h t -> p (h t)"),
                                                   psum[2][0:64, :], ALU.add), r=[_k("Sg"), _k("ps", 2)], w=[_k("Sg")])
            P.add("pool", lambda e: e.tensor_copy(Sgb[:], Sg[:]), r=[_k("Sg")], w=[_k("Sgb")])
            yield
            for h in range(4):
                P.add("act", lambda e, h=h: e.activation(junk[:, 0:128], psum[1][:, h * 128:(h + 1) * 128], AF.Square, accum_out=ss[:, h:h + 1]),
                      r=[_k("ps", 1)], w=[_k("junk"), _k("ss", h)])
            rsqrt_small(P, rstd[:, 0:4], _k("rstd", 0), ss[:, 0:4], [_k("ss", h) for h in range(4)], 1.0 / 128)
            yield
            for h in range(4):
                P.add("dve", lambda e, h=h: e.scalar_tensor_tensor(og[:, h * 128:(h + 1) * 128], psum[1][:, h * 128:(h + 1) * 128], rstd[:, h:h + 1],
                                                                    gsl[:, h * 128:(h + 1) * 128], ALU.mult, ALU.mult),
                      r=[_k("ps", 1), _k("rstd", 0), _k("gsl")], w=[_k("og")])
            yield

            def trog(e):
                inst = None
                v = bfv(0)
                for cc in range(4):
                    inst = e.transpose(v[:, cc * 128:(cc + 1) * 128], og[:, cc * 128:(cc + 1) * 128], ident[:])
                return inst
            P.add("pe", trog, r=[_k("og"), _k("ident")], w=[_k("ps", 0)])
            P.add("act", lambda e: e.copy(ogT[p3][:].rearrange("p c t -> p (c t)"), bfv(0)[:, 0:512]),
                  r=[_k("ps", 0)], w=[_k("ogT", p3)])
            yield

        def stageBs(i):
            p2 = i % 2
            sc = scb[p2]
            nkeys = (i + 1) * 128
            ngrp = (nkeys + 511) // 512
            pend = None
            for gk in range(ngrp):
                k0 = gk * 512
                n = min(512, nkeys - k0)
                kk = [_k("ikTc", b) for b in range(k0 // 128, (k0 + n) // 128)]
                for hI in range(8):
                    rb = hI % 2
                    P.add("pe", lambda e, hI=hI, k0=k0, n=n: e.matmul(psum[3][:, 0:n], iqT[p2][:, hI, :], ikTc[:, k0:k0 + n], start=True, stop=True),
                          r=[_k("iqT", p2)] + kk, w=[_k("ps", 3)])
                    if pend is not None:
                        pend()
                        pend = None
                    P.add("act", lambda e, rb=rb, hI=hI, n=n: e.activation(Rb[rb][:, 0:n], psum[3][:, 0:n], AF.Relu, scale=wabs[p2][:, hI:hI + 1]),
                          r=[_k("ps", 3), _k("wabs", p2)], w=[_k("Rb", rb)])

                    def acc(rb=rb, hI=hI, n=n, k0=k0):
                        P.add("pe", lambda e: e.matmul(psum[4][:, 0:n], Dsg[p2][:, hI, :], Rb[rb][:, 0:n], start=(hI == 0), stop=(hI == 7)),
                              r=[_k("Dsg", p2), _k("Rb", rb)], w=[_k("ps", 4)])
                        if hI == 7:
                            P.add("act", lambda e: e.copy(sc[:, k0:k0 + n], psum[4][:, 0:n]), r=[_k("ps", 4)], w=[_k("sc", p2)])
                    pend = acc
                    yield
            if pend is not None:
                pend()
            yield

        def stageBb(i):
            t0 = i * 128
            p2 = i % 2
            sc = scb[p2]
            nkeys = (i + 1) * 128
            scv = sc[:, 0:nkeys]
            P.add("dve", lambda e: e.tensor_reduce(st[:, 0:1], scv, AX.X, ALU.max), r=[_k("sc", p2)], w=[_k("st", 0)])
            yield
            lov = scv if i < 2 else sc[:, 0:TOPK]
            P.add("dve", lambda e: e.tensor_reduce(st[:, 1:2], lov, AX.X, ALU.min), r=[_k("sc", p2)], w=[_k("st", 1)])
            P.add("dve", lambda e: e.tensor_tensor(sc[:, t0:t0 + 128], sc[:, t0:t0 + 128], negm[:], ALU.add),
                  r=[_k("sc", p2), _k("negm")], w=[_k("sc", p2)])
            P.add("dve", lambda e: e.tensor_tensor(st[:, 2:3], st[:, 0:1], st[:, 1:2], ALU.subtract), r=[_k("st", 0), _k("st", 1)], w=[_k("st", 2)])
            P.add("dve", lambda e: e.tensor_scalar(st[:, 2:3], st[:, 2:3], 1.000001, 1e-30, ALU.mult, ALU.add), r=[_k("st", 2)], w=[_k("st", 2)])
            P.add("dve", lambda e: e.tensor_scalar(hst[:], pw[:], st[:, 2:3], None, ALU.mult), r=[_k("st", 2), _k("pw")], w=[_k("hst")])
            yield
            for k in range(N_IT):
                P.add("dve", lambda e, k=k: e.tensor_tensor(st[:, 3:4], st[:, 1:2], hst[:, k:k + 1], ALU.add),
                      r=[_k("st", 1), _k("hst")], w=[_k("st", 3)])
                P.add("dve", lambda e: e.tensor_scalar(junkb[:, 0:nkeys], scv, st[:, 3:4], None, ALU.is_ge, ALU.add, accum_out=st[:, 4:5]),
                      r=[_k("sc", p2), _k("st", 3)], w=[_k("junkb"), _k("st", 4)])
                P.add("dve", lambda e, k=k: e.tensor_scalar(st[:, 5:6], st[:, 4:5], TOPK - 0.5, hst[:, k:k + 1], ALU.is_ge, ALU.mult),
                      r=[_k("st", 4), _k("hst")], w=[_k("st", 5)])
                P.add("dve", lambda e: e.tensor_tensor(st[:, 1:2], st[:, 1:2], st[:, 5:6], ALU.add),
                      r=[_k("st", 1), _k("st", 5)], w=[_k("st", 1)])
                yield
            P.add("dve", lambda e: e.tensor_scalar(mskb[p2][:, 0:nkeys], scv, st[:, 1:2], -MASK_BIG, ALU.is_lt, ALU.mult),
                  r=[_k("sc", p2), _k("st", 1)], w=[_k("msk", p2)])
            yield
            return
            for b0 in range(0, i + 1, 8):
                nb = min(8, i + 1 - b0)

                def trm(e, b0=b0, nb=nb):
                    inst = None
                    v = bfv(3)
                    for bb in range(nb):
                        inst = e.transpose(v[:, bb * 128:(bb + 1) * 128], msk[:, (b0 + bb) * 128:(b0 + bb + 1) * 128], ident[:])
                    return inst
                P.add("pe", trm, r=[_k("msk"), _k("ident")], w=[_k("ps", 3)])
                P.add("act", lambda e, b0=b0, nb=nb: e.copy(mskT[:, b0:b0 + nb, :].rearrange("p c t -> p (c t)"), bfv(3)[:, 0:nb * 128]),
                      r=[_k("ps", 3)], w=[_k("mskT", b0 // 8)])
                yield

        def stageC(i):
            t0 = i * 128
            p3 = i % 4
            p2 = i % 2
            hb = hbufC
            hk = _k("hbufC")
            dma(hb[:], h_in[t0:t0 + 128, :], hk)
            pendc = []
            for j in range(i + 1):
                eb = j % 2

                def stf(e, j=j):
                    e.matmul(psum[5][:], kTc[:, j * 128:(j + 1) * 128], qT[p3][:].rearrange("p c t -> p (c t)"), start=True, stop=False)
                    return e.matmul(psum[5][:], mskb[p2][:, j * 128:(j + 1) * 128], ident4[:], start=False, stop=True)
                P.add("pe", stf, r=[_k("kTc", j), _k("qT", p3), _k("msk", p2), _k("ident4")], w=[_k("ps", 5)])
                while pendc:
                    pendc.pop(0)()
                P.add("act", lambda e, eb=eb: e.activation(Eb[eb][:], psum[5][:], AF.Exp, scale=128.0 ** -0.5),
                      r=[_k("ps", 5)], w=[_k("Eb", eb)])

                def pv(e, eb=eb, j=j):
                    e.matmul(psum[6][:], vc[:, j, :], Eb[eb][:], start=(j == 0), stop=(j == i))
                    return e.matmul(psum[7][:], ones[:], Eb[eb][:], start=(j == 0), stop=(j == i))

                def pvadd(pv=pv, j=j, eb=eb):
                    P.add("pe", pv, r=[_k("vc", j), _k("Eb", eb), _k("ones")], w=[_k("ps", 6), _k("ps", 7)])
                pendc.append(pvadd)
                yield
            while pendc:
                pendc.pop(0)()
            P.add("act", lambda e: e.activation(rden[:], psum[7][:], AF.Ln), r=[_k("ps", 7)], w=[_k("rden")])
            P.add("act", lambda e: e.activation(rden[:], rden[:], AF.Exp, scale=-1.0), r=[_k("rden")], w=[_k("rden")])
            P.add("dve", lambda e: e.tensor_tensor(dsT[:].rearrange("p c t -> p (c t)"), psum[6][:], rden[:], ALU.mult),
                  r=[_k("ps", 6), _k("rden")], w=[_k("dsT")])
            yield
            for mh in range(2):
                def ymm(e, mh=mh):
                    inst = None
                    for cc in range(8):
                        src = ogT[p3][:, cc, :] if cc < 4 else dsT[:, cc - 4, :]
                        inst = e.matmul(psum[5][:], src, wout[:, cc, mh * 512:(mh + 1) * 512], start=(cc == 0), stop=(cc == 7))
                    return inst
                P.add("pe", ymm, r=[_k("ogT", p3), _k("dsT")] + WOUT_ALL, w=[_k("ps", 5)])
                P.add("dve", lambda e, mh=mh: e.tensor_tensor(hb[:, mh * 512:(mh + 1) * 512], hb[:, mh * 512:(mh + 1) * 512], psum[5][:], ALU.add),
                      r=[_k("ps", 5), hk], w=[hk])
                yield
            P.add("sp", lambda e: e.dma_start(out=h_out[t0:t0 + 128, :], in_=hb[:]), r=[hk], dma=True)
            yield

        ntl = DBG["ntiles"]
        for s_ in range(ntl + 3):
            gens = []
            if 0 <= s_ - 3 < ntl:
                gens.append((stageC(s_ - 3), (s_ - 3) + 1 + 4))
            if 0 <= s_ - 2 < ntl:
                gens.append((stageBb(s_ - 2), 4 + N_IT + (s_ - 2) // 8 + 1))
            if 0 <= s_ - 1 < ntl:
                gens.append((stageBs(s_ - 1), 9 * ((s_ - 1) // 4 + 1)))
            if s_ < ntl:
                gens.append((stageA(s_), 24))
            _interleave(gens)
        P.emit(es)


def host_consts():
    import ml_dtypes
    cst = {}
    cst["ident"] = np.eye(128, dtype=ml_dtypes.bfloat16)
    half = 128
    inv = 10000.0 ** (-np.arange(half, dtype=np.float32) / half)
    ang = np.arange(T, dtype=np.float32)[:, None] * inv[None, :].astype(np.float32)
    cst["rope_cos"] = np.cos(ang).astype(np.float32)
    cst["rope_sin"] = np.sin(ang).astype(np.float32)
    g = np.array(RET_GAMMA, dtype=np.float64)
    ii = np.arange(128)
    rel = ii[None, :] - ii[:, None]
    dtm = np.zeros((128, 4, 128), np.float64)
    for h in range(4):
        dtm[:, h, :] = np.where(rel >= 0, g[h] ** np.maximum(rel, 0), 0.0) / 16.0
    cst["ret_dt"] = dtm.astype(np.float32)
    xi8 = np.zeros((128, 8, 128), np.float64)
    for cc in range(8):
        xi8[:, cc, :] = (g[cc // 2] ** (ii + 1.0))[None, :]
    cst["ret_xi8"] = xi8.astype(np.float32)
    cst["ones"] = np.ones((128, 128), dtype=ml_dtypes.bfloat16)
    cst["identf"] = np.eye(128, dtype=np.float32)
    cst["ident4"] = np.tile(np.eye(128, dtype=np.float32), (1, 4)).astype(ml_dtypes.bfloat16)
    le = (ii[:, None] <= ii[None, :])
    cst["gla_trim"] = np.where(le, -1.0 / 16.0, 0.0).astype(np.float32)
    cst["gla_trir"] = np.where(~le, -1.0 / 16.0, 0.0).astype(np.float32)
    cst["gla_m01"] = np.repeat(le[:, None, :], 4, axis=1).astype(np.float32)
    cst["dsa_negm"] = np.where(ii[None, :] <= ii[:, None], 0.0, -1e30).astype(np.float32)
    cst["dsa_pw"] = np.repeat((0.5 ** (np.arange(N_IT + 1) + 1.0))[None, :], 128, axis=0).astype(np.float32)
    zeta = np.zeros((128, 4), np.float64)
    for h in range(4):
        zeta[:, h] = g[h] ** (127.0 - ii) / 16.0
    cst["ret_zeta"] = zeta.astype(np.float32)
    return cst


INPUT_SHAPES = {
    "even_attn_norm": [1, D], "even_w_in": [1, D, EVEN_IN], "even_gla_wa2": [1, 16, 256],
    "even_gla_ba2": [1, 256], "even_gla_norm": [1, 128], "even_w_out": [1, D, D],
    "odd_attn_norm": [1, D], "odd_w_in": [1, D, ODD_IN], "odd_ret_norm": [1, 512], "odd_w_out": [1, 2048, D],
    "ffn_norm": [2, D], "ffn_w_gate": [2, D, DFF], "ffn_w_up": [2, D, DFF], "ffn_w_down": [2, DFF, D],
    "final_norm": [D],
}


def build(phases=("mix0", "ffn0", "mix1", "ffn1")):
    nc = bass.Bass("TRN2", target_bir_lowering=False)
    x = nc.dram_tensor("x", [T, D], F32, kind="ExternalInput").ap()
    out = nc.dram_tensor("out", [T, D], F32, kind="ExternalOutput").ap()
    I = {k: nc.dram_tensor(k, shp, F32, kind="ExternalInput").ap() for k, shp in INPUT_SHAPES.items()}
    cst = host_consts()
    C = {}
    for k, v in cst.items():
        C[k] = nc.dram_tensor(k, list(v.shape), BF16 if v.dtype != np.float32 else F32, kind="ExternalInput").ap()
    scr = [nc.dram_tensor("scr%d" % i, [T, D], F32, kind="Internal").ap() for i in range(3)]
    bufs = [x] + scr[:len(phases) - 1] + [out]
    for pi, ph in enumerate(phases):
        hin, hout = bufs[pi], bufs[pi + 1]
        if ph == "ffn0":
            ffn_phase(nc, hin, hout, I["ffn_norm"][0], I["ffn_w_gate"][0], I["ffn_w_up"][0], I["ffn_w_down"][0], C["ident"])
        elif ph == "ffn1":
            ffn_phase(nc, hin, hout, I["ffn_norm"][1], I["ffn_w_gate"][1], I["ffn_w_up"][1], I["ffn_w_down"][1], C["ident"],
                      final_g=I["final_norm"])
        elif ph == "mix1":
            mix1_phase(nc, hin, hout, I["odd_attn_norm"][0], I["odd_w_in"][0], I["odd_ret_norm"][0], I["odd_w_out"][0],
                       C["ident"], C["rope_cos"], C["rope_sin"], C["ret_dt"], C["ret_xi8"], C["ret_zeta"])
        elif ph == "mix0":
            mix0_phase(nc, hin, hout, I, C)
    return nc


def make_inputs(inputs, b):
    m = {k: np.ascontiguousarray(np.asarray(inputs[k], dtype=np.float32)) for k in INPUT_SHAPES}
    m["x"] = np.ascontiguousarray(np.asarray(inputs["x"][b], dtype=np.float32))
    m.update(host_consts())
    return m


def kernel(**inputs):
    nc = build()
    in_maps = [make_inputs(inputs, b) for b in range(8)]
    res = run_bass_kernel_spmd(nc, in_maps, core_ids=list(range(8)))
    return np.stack([np.asarray(r["out"], dtype=np.float32) for r in res.results], axis=0)
```

```python
import math
from contextlib import ExitStack

import numpy as np
import concourse.bass as bass
import concourse.mybir as mybir
from concourse.bass_utils import run_bass_kernel_spmd

F32 = mybir.dt.float32
BF16 = mybir.dt.bfloat16
AF = mybir.ActivationFunctionType
ALU = mybir.AluOpType
AX = mybir.AxisListType

T = 4096
D = 1024
DFF = 2816
NT = T // 128
EPS = 1e-6
EVEN_IN = 2904
ODD_IN = 6144


class _Op:
    __slots__ = ("eng", "fn", "deps", "dma", "sig", "sigidx", "dsem", "dval", "ndep")


class Prog:
    COMPUTE = ("pe", "act", "dve", "pool")

    def __init__(self, nc, n_dma_sems=16):
        self.nc = nc
        self.ops = []
        self.lastw = {}
        self.readers = {}
        self.n_dma_sems = n_dma_sems
        self.dma_rr = {"sp": 0, "pool": 0, "act": 0}
        self.dma_cnt = {}

    def add(self, eng, fn, r=(), w=(), dma=False):
        i = len(self.ops)
        deps = set()
        pr = [k for k in r if k[0] == "ps" and k not in w]
        if pr:
            w = list(w) + pr
        for k in r:
            a = self.lastw.get(k)
            if a is not None:
                deps.add(a)
        for k in w:
            a = self.lastw.get(k)
            if a is not None:
                deps.add(a)
            rd = self.readers.get(k)
            if rd:
                deps.update(rd.values())
        op = _Op()
        op.eng = eng
        op.fn = fn
        op.dma = dma
        op.sig = False
        op.sigidx = 0
        op.dsem = None
        op.dval = 0
        fdeps = []
        for a in deps:
            A = self.ops[a]
            if (not dma) and (not A.dma) and eng == "pe" and A.eng == "pe":
                continue
            fdeps.append(a)
        op.deps = sorted(fdeps)
        if dma:
            q = self.dma_rr[eng]
            self.dma_rr[eng] = (q + 1) % self.n_dma_sems
            key = (eng, q)
            self.dma_cnt[key] = self.dma_cnt.get(key, 0) + 1
            op.dsem = key
            op.dval = 16 * self.dma_cnt[key]
        self.ops.append(op)
        for k in w:
            self.lastw[k] = i
            self.readers[k] = {}
        for k in r:
            d = self.readers.setdefault(k, {})
            d[("dma", i) if dma else eng] = i
        return i

    def emit(self, es):
        nc = self.nc
        ops = self.ops
        for op in ops:
            for a in op.deps:
                if not ops[a].dma:
                    ops[a].sig = True
        cnt = {e: 0 for e in self.COMPUTE + ("sp",)}
        for op in ops:
            if op.sig and not op.dma:
                cnt[op.eng] += 1
                op.sigidx = cnt[op.eng]
        sems = {e: es.enter_context(nc.semaphore("s_" + e)) for e in cnt}
        dsems = {}
        for key in self.dma_cnt:
            dsems[key] = es.enter_context(nc.semaphore("d_%s%d" % key))
        engines = {"pe": "tensor", "act": "scalar", "dve": "vector", "pool": "gpsimd", "sp": "sync"}
        with nc.Block() as block:
            for e, attr in engines.items():
                mine = [op for op in ops if op.eng == e]

                def body(engine, mine=mine, e=e):
                    known = {}

                    def wait(sem_key, sem, val):
                        if known.get(sem_key, 0) >= val:
                            return
                        engine.wait_ge(sem, val)
                        known[sem_key] = val

                    for op in mine:
                        for a in op.deps:
                            A = ops[a]
                            if A.dma:
                                wait(A.dsem, dsems[A.dsem], A.dval)
                            else:
                                wait(A.eng, sems[A.eng], A.sigidx)
                        if op.dma:
                            if op.dval > 16:
                                wait(op.dsem, dsems[op.dsem], op.dval - 16)
                            inst = op.fn(engine)
                            inst.then_inc(dsems[op.dsem], 16)
                        else:
                            inst = op.fn(engine)
                            if op.sig:
                                inst.then_inc(sems[e], 1)
                    for key, c in self.dma_cnt.items():
                        if key[0] == e:
                            wait(key, dsems[key], 16 * c)

                getattr(block, attr)(body)


def _k(name, *idx):
    return (name,) + idx


class Ctx:
    def __init__(self, nc, P, es):
        self.nc = nc
        self.P = P
        self.es = es
        self.rr = 0

    UID = [0]

    def sb(self, name, shape, dt):
        Ctx.UID[0] += 1
        return self.es.enter_context(self.nc.sbuf_tensor("sb%d_%s" % (Ctx.UID[0], name), list(shape), dt))

    def ps(self, name):
        Ctx.UID[0] += 1
        return self.es.enter_context(self.nc.psum_tensor("ps%d_%s" % (Ctx.UID[0], name), [128, 512], F32))


RSQRT_MODE = ["lnexp"]


def rsqrt_small(P, out_ap, outkey, in_ap, inkey, scale, eps=EPS):
    inkeys = inkey if isinstance(inkey, list) else [inkey]
    P.add("dve", lambda e: e.tensor_scalar(out_ap, in_ap, scale, eps, ALU.mult, ALU.add),
          r=inkeys, w=[outkey])
    if RSQRT_MODE[0] == "sqrt":
        P.add("act", lambda e: e.sqrt(out_ap, out_ap), r=[outkey], w=[outkey])
        P.add("dve", lambda e: e.reciprocal(out_ap, out_ap), r=[outkey], w=[outkey])
    else:
        P.add("act", lambda e: e.activation(out_ap, out_ap, AF.Ln), r=[outkey], w=[outkey])
        P.add("act", lambda e: e.activation(out_ap, out_ap, AF.Exp, scale=-0.5), r=[outkey], w=[outkey])


def rmsnorm_tile(c, h_ap, hkey, xn_ap, xnkey, junk_ap, junkkey, ss_ap, sskey, rstd_ap, rstdkey):
    P = c.P
    P.add("act", lambda e: e.activation(junk_ap, h_ap, AF.Square, accum_out=ss_ap),
          r=[hkey], w=[junkkey, sskey])
    rsqrt_small(P, rstd_ap, rstdkey, ss_ap, sskey, 1.0 / D)
    P.add("dve", lambda e: e.tensor_scalar(xn_ap, h_ap, rstd_ap, None, ALU.mult),
          r=[hkey, rstdkey], w=[xnkey])


_LW = [0]


def load_weight_bf16(c, w_dram, rows, cols, dst, dstname, stage, gcol=None, gkey=None, colchunk=704, gmap=None):
    P = c.P
    nk = rows // 128
    nch = (cols + colchunk - 1) // colchunk
    if gcol is None:
        for k in range(nk):
            P.add("pool", lambda e, k=k: e.dma_start(out=dst[:, k, :], in_=w_dram[k * 128:(k + 1) * 128, :]),
                  w=[_k(dstname, k, j) for j in range(nch)], dma=True)
        return [_k(dstname, k, j) for k in range(nk) for j in range(nch)]
    for k in range(nk):
        for j in range(nch):
            c0 = j * colchunk
            c1 = min(cols, c0 + colchunk)
            cnt = _LW[0]
            _LW[0] += 1
            st, skeys = stage[cnt % len(stage)]
            src = w_dram[k * 128:(k + 1) * 128, c0:c1]
            P.add("sp", lambda e, st=st, src=src, n=c1 - c0: e.dma_start(out=st[:, 0:n], in_=src),
                  w=skeys, dma=True)
            dap = dst[:, k, c0:c1]
            sap = st[:, 0:c1 - c0]
            eng = ("dve", "act")[cnt % 2]
            rk = list(skeys) + ([gkey] if gcol is not None else [])
            if gcol is None:
                if eng == "act":
                    P.add("act", lambda e, dap=dap, sap=sap: e.copy(dap, sap), r=rk, w=[_k(dstname, k, j)])
                else:
                    P.add(eng, lambda e, dap=dap, sap=sap: e.tensor_copy(dap, sap), r=rk, w=[_k(dstname, k, j)])
            else:
                kk_ = gmap(k) if gmap is not None else k
                g = gcol[:, kk_:kk_ + 1]
                if eng == "act":
                    P.add("act", lambda e, dap=dap, sap=sap, g=g: e.activation(dap, sap, AF.Copy, scale=g),
                          r=rk, w=[_k(dstname, k, j)])
                else:
                    P.add(eng, lambda e, dap=dap, sap=sap, g=g: e.tensor_scalar(dap, sap, g, None, ALU.mult),
                          r=rk, w=[_k(dstname, k, j)])
    return [_k(dstname, k, j) for k in range(nk) for j in range(nch)]


def wkeys(dstname, k, c0, c1, colchunk=704):
    return [_k(dstname, k, j) for j in range(c0 // colchunk, (c1 - 1) // colchunk + 1)]


def ffn_phase(nc, h_in, h_out, g_dram, wg_d, wu_d, wd_d, ident_d, final_g=None):
    with ExitStack() as es:
        P = Prog(nc)
        c = Ctx(nc, P, es)
        CC = 704
        wg = c.sb("wg", [128, 8, DFF], BF16)
        wu = c.sb("wu", [128, 8, DFF], BF16)
        wd = c.sb("wd", [128, 22, D], BF16)
        gcol = c.sb("gcol", [128, 8], F32)
        ident = c.sb("ident", [128, 128], BF16)
        xn = [c.sb("xn%d" % i, [128, D], BF16) for i in range(2)]
        xnT = [c.sb("xnT%d" % i, [128, 8, 512], BF16) for i in range(2)]
        actT = c.sb("actT", [128, 22, 512], BF16)
        actf = actT[:].rearrange("p f t -> p (f t)").bitcast(F32)
        stage = []
        for si in range(7):
            stage.append((actf[:, si * 768:si * 768 + CC], [_k("actT", f) for f in range(3 * si, 3 * si + 3)]))
        sg = [c.sb("sg%d" % i, [128, 512], F32) for i in range(2)]
        junk = c.sb("junk", [128, D], BF16)
        ss = c.sb("ss", [128, 8], F32)
        rstd = c.sb("rstd", [128, 8], F32)
        if final_g is not None:
            fg = c.sb("fg", [128, D], F32)
        psum = [c.ps("ps%d" % i) for i in range(8)]

        P.add("sp", lambda e: e.dma_start(out=gcol[:], in_=g_dram.rearrange("(k p) -> p k", p=128),
                                          allow_slow_non_contiguous=True), w=[_k("gcol")], dma=True)
        P.add("sp", lambda e: e.dma_start(out=ident[:], in_=ident_d), w=[_k("ident")], dma=True)
        if final_g is not None:
            P.add("sp", lambda e: e.dma_start(out=fg[:], in_=final_g.partition_broadcast(128)),
                  w=[_k("fg")], dma=True)
        load_weight_bf16(c, wg_d, D, DFF, wg, "wg", stage, gcol, _k("gcol"), CC)
        load_weight_bf16(c, wu_d, D, DFF, wu, "wu", stage, gcol, _k("gcol"), CC)
        load_weight_bf16(c, wd_d, DFF, D, wd, "wd", stage, None, None, CC)

        nsup = T // 512
        hA = [c.sb("hA%d" % i, [128, D], F32) for i in range(2)]
        hC = [c.sb("hC%d" % i, [128, D], F32) for i in range(2)]
        cnt = {"t": 0}

        def front_norm(s, j):
            t0 = s * 512 + j * 128
            b = (s * 4 + j) % 2
            hap = hA[b][:]
            hk = _k("hA", b)
            P.add("sp", lambda e, hap=hap, t0=t0: e.dma_start(out=hap, in_=h_in[t0:t0 + 128, :]), w=[hk], dma=True)
            rmsnorm_tile(c, hap, hk, xn[b][:], _k("xn", b), junk[:], _k("junk"),
                         ss[:, j:j + 1], _k("ss", j), rstd[:, j:j + 1], _k("rstd", j))

        def front_tr(s, j):
            b = (s * 4 + j) % 2
            xb = s % 2
            pb = b
            pst = psum[pb][:].bitcast(BF16)

            def tr(e, b=b, pst=pst):
                inst = None
                for cc in range(8):
                    inst = e.transpose(pst[:, cc * 128:(cc + 1) * 128], xn[b][:, cc * 128:(cc + 1) * 128], ident[:])
                return inst
            P.add("pe", tr, r=[_k("xn", b), _k("ident")], w=[_k("ps", pb)])
            dst = xnT[xb][:, :, j * 128:(j + 1) * 128]
            src = pst[:, 0:1024].rearrange("p (c t) -> p c t", c=8)
            P.add("act", lambda e, dst=dst, src=src: e.copy(dst, src), r=[_k("ps", pb)], w=[_k("xnT", xb, j)])

        for j in range(4):
            front_norm(0, j)
            front_tr(0, j)
        for s in range(nsup):
            xb = s % 2
            xT = xnT[xb]
            xk = [_k("xnT", xb, j) for j in range(4)]
            for f in range(22):
                pg = 2 + (f % 2)
                pu = 4 + (f % 2)

                def mm(e, w_, pi, f=f, xT=xT):
                    inst = None
                    for k in range(8):
                        inst = e.matmul(psum[pi][:], w_[:, k, f * 128:(f + 1) * 128], xT[:, k, :],
                                        start=(k == 0), stop=(k == 7))
                    return inst
                wkg = [kk for k in range(8) for kk in wkeys("wg", k, f * 128, (f + 1) * 128, CC)]
                wku = [kk for k in range(8) for kk in wkeys("wu", k, f * 128, (f + 1) * 128, CC)]
                P.add("pe", lambda e, pg=pg, mm=mm: mm(e, wg, pg), r=xk + wkg, w=[_k("ps", pg)])
                P.add("pe", lambda e, pu=pu, mm=mm: mm(e, wu, pu), r=xk + wku, w=[_k("ps", pu)])
                sb_ = f % 2
                P.add("act", lambda e, sb_=sb_, pg=pg: e.activation(sg[sb_][:], psum[pg][:], AF.Silu),
                      r=[_k("ps", pg)], w=[_k("sg", sb_)])
                P.add("dve", lambda e, sb_=sb_, pu=pu, f=f: e.tensor_tensor(actT[:, f, :], sg[sb_][:], psum[pu][:], ALU.mult),
                      r=[_k("sg", sb_), _k("ps", pu)], w=[_k("actT", f)])
            ak = [_k("actT", f) for f in range(22)]
            wdk = [kk for f in range(22) for kk in wkeys("wd", f, 0, D, CC)]
            for j in range(4):
                t0 = s * 512 + j * 128
                hb = (s * 4 + j) % 2
                hap = hC[hb][:]
                hk = _k("hC", hb)
                P.add("sp", lambda e, hap=hap, t0=t0: e.dma_start(out=hap, in_=h_in[t0:t0 + 128, :]), w=[hk], dma=True)
                if s + 1 < nsup:
                    front_norm(s + 1, j)
                for mh in range(2):
                    py = 6 + mh

                    def mmd(e, j=j, mh=mh, py=py):
                        inst = None
                        for f in range(22):
                            inst = e.matmul(psum[py][:], actT[:, f, j * 128:(j + 1) * 128],
                                            wd[:, f, mh * 512:(mh + 1) * 512], start=(f == 0), stop=(f == 21))
                        return inst
                    P.add("pe", mmd, r=ak + wdk, w=[_k("ps", py)])
                    hs = hC[hb][:, mh * 512:(mh + 1) * 512]
                    P.add("dve", lambda e, hs=hs, py=py: e.tensor_tensor(hs, hs, psum[py][:], ALU.add),
                          r=[_k("ps", py), hk], w=[hk])
                if s + 1 < nsup:
                    front_tr(s + 1, j)
                if final_g is not None:
                    sj = 4 + j
                    P.add("act", lambda e, hap=hap, sj=sj: e.activation(junk[:], hap, AF.Square, accum_out=ss[:, sj:sj + 1]),
                          r=[hk], w=[_k("junk"), _k("ss", sj)])
                    rsqrt_small(P, rstd[:, sj:sj + 1], _k("rstd", sj), ss[:, sj:sj + 1], _k("ss", sj), 1.0 / D)
                    P.add("dve", lambda e, hap=hap, sj=sj: e.scalar_tensor_tensor(hap, hap, rstd[:, sj:sj + 1], fg[:], ALU.mult, ALU.mult),
                          r=[hk, _k("rstd", sj), _k("fg")], w=[hk])
                P.add("sp", lambda e, hap=hap, t0=t0: e.dma_start(out=h_out[t0:t0 + 128, :], in_=hap),
                      r=[hk], dma=True)
        P.emit(es)


RET_GAMMA = [1.0 - 2.0 ** (-5.0 - h) for h in range(4)]


DBG = {"ntiles": NT, "stage": 99}


def mix1_phase(nc, h_in, h_out, g_dram, win_d, retnorm_d, wout_d, ident_d, cos_d, sin_d, dt_d, xi8_d, zeta_d):
    RSQRT_MODE[0] = "sqrt"
    with ExitStack() as es:
        P = Prog(nc)
        c = Ctx(nc, P, es)
        CC = 512
        win = c.sb("win", [128, 8, ODD_IN], BF16)
        wout = c.sb("wout", [128, 16, D], BF16)
        gcol = c.sb("gcol", [128, 8], F32)
        rcol = c.sb("rcol", [128, 4], F32)
        ident = c.sb("ident", [128, 128], BF16)
        S = c.sb("S", [128, 2, 4, 512], F32)
        Sb = c.sb("Sb", [128, 2, 4, 512], BF16)
        hbufs = [c.sb("hbuf%d" % i, [128, D], F32) for i in range(2)]
        xn = c.sb("xn", [128, D], BF16)
        xnT = c.sb("xnT", [128, 8, 128], BF16)
        tmp = [c.sb("tmp%d" % i, [128, 2, 128], F32) for i in range(4)]
        qrot = c.sb("qrot", [128, D], BF16)
        krot = c.sb("krot", [128, D], BF16)
        kz = c.sb("kz", [128, D], BF16)
        qT = c.sb("qT", [128, 8, 128], BF16)
        qxiT = c.sb("qxiT", [128, 8, 128], BF16)
        kT = c.sb("kT", [128, 8, 128], BF16)
        vb = c.sb("vb", [128, 2048], BF16)
        gs = c.sb("gs", [128, 2048], BF16)
        atm = c.sb("atm", [128, 4, 128], BF16)
        og = c.sb("og", [128, 2048], BF16)
        ogT = c.sb("ogT", [128, 16, 128], BF16)
        junk = c.sb("junk", [128, 512], BF16)
        cos_t = c.sb("cos", [128, 128], F32)
        sin_t = c.sb("sin", [128, 128], F32)
        dtm = c.sb("dtm", [128, 4, 128], F32)
        xi8 = c.sb("xi8", [128, 8, 128], F32)
        zeta = c.sb("zeta", [128, 4], F32)
        ss = c.sb("ss", [128, 8], F32)
        rstd = c.sb("rstd", [128, 8], F32)
        psum = [c.ps("ps%d" % i) for i in range(8)]

        def dma(out_ap, in_ap, wk, **kw):
            P.add("sp", lambda e: e.dma_start(out=out_ap, in_=in_ap, **kw), w=[wk], dma=True)

        dma(gcol[:], g_dram.rearrange("(k p) -> p k", p=128), _k("gcol"), allow_slow_non_contiguous=True)
        dma(rcol[:], retnorm_d.rearrange("(k p) -> p k", p=128), _k("rcol"), allow_slow_non_contiguous=True)
        dma(ident[:], ident_d, _k("ident"))
        dma(dtm[:], dt_d, _k("dtm"))
        dma(xi8[:], xi8_d, _k("xi8"))
        dma(zeta[:], zeta_d, _k("zeta"))
        P.add("dve", lambda e: e.memset(S[:], 0.0), w=[_k("S", cc, h) for cc in range(2) for h in range(4)])
        P.add("pool", lambda e: e.memset(Sb[:], 0.0), w=[_k("Sb", cc, h) for cc in range(2) for h in range(4)])
        ogf = og[:].bitcast(F32)
        ogTf = ogT[:].rearrange("p c t -> p (c t)").bitcast(F32)
        vbf = vb[:].bitcast(F32)
        gsf = gs[:].bitcast(F32)
        stage = [(vbf[:, 0:512], [_k("vb", 0), _k("vb", 1)]), (vbf[:, 512:1024], [_k("vb", 2), _k("vb", 3)]),
                 (gsf[:, 0:512], [_k("gs", 0), _k("gs", 1)]), (gsf[:, 512:1024], [_k("gs", 2), _k("gs", 3)]),
                 (ogf[:, 0:512], [_k("og", 0), _k("og", 1)]), (ogf[:, 512:1024], [_k("og", 2), _k("og", 3)]),
                 (ogTf[:, 0:512], [_k("ogT", 0)]), (ogTf[:, 512:1024], [_k("ogT", 1)])]
        load_weight_bf16(c, win_d, D, ODD_IN, win, "win", stage, gcol, _k("gcol"), CC)
        load_weight_bf16(c, wout_d, 2048, D, wout, "wout", stage, rcol, _k("rcol"), CC, gmap=lambda k: k % 4)

        def bfv(i):
            return psum[i][:].bitcast(BF16)

        dma(hbufs[0][:], h_in[0:128, :], _k("hbuf", 0))
        for i in range(DBG["ntiles"]):
            t0 = i * 128
            hbuf = hbufs[i % 2]
            hk = _k("hbuf", i % 2)
            if i + 1 < DBG["ntiles"]:
                dma(hbufs[(i + 1) % 2][:], h_in[t0 + 128:t0 + 256, :], _k("hbuf", (i + 1) % 2))
            dma(cos_t[:], cos_d[t0:t0 + 128, :], _k("cos"))
            dma(sin_t[:], sin_d[t0:t0 + 128, :], _k("sin"))
            rmsnorm_tile(c, hbuf[:], hk, xn[:], _k("xn"), ogT[:, 0:8, :].rearrange("p c t -> p (c t)"), _k("ogT", 0),
                         ss[:, 4:5], _k("ss", 4), rstd[:, 4:5], _k("rstd", 4))

            def tr8(e, src, pb):
                inst = None
                v = bfv(pb)
                for cc in range(8):
                    inst = e.transpose(v[:, cc * 128:(cc + 1) * 128], src[:, cc * 128:(cc + 1) * 128], ident[:])
                return inst
            P.add("pe", lambda e: tr8(e, xn, 0), r=[_k("xn"), _k("ident")], w=[_k("ps", 0)])
            P.add("act", lambda e: e.copy(xnT[:].rearrange("p c t -> p (c t)"), bfv(0)), r=[_k("ps", 0)], w=[_k("xnT")])
            for g in range(12 if DBG["stage"] >= 2 else 0):
                pb = (4 + g) if g < 4 else 1 + (g % 2)

                def mm(e, g=g, pb=pb):
                    inst = None
                    for k in range(8):
                        inst = e.matmul(psum[pb][:], xnT[:, k, :], win[:, k, g * 512:(g + 1) * 512],
                                        start=(k == 0), stop=(k == 7))
                    return inst
                wk_ = [_k("win", k, g) for k in range(8)]
                P.add("pe", mm, r=[_k("xnT")] + wk_, w=[_k("ps", pb)])
                if g < 4:
                    dstt = qrot if g < 2 else krot
                    dname = "qrot" if g < 2 else "krot"
                    hh = (g % 2) * 2
                    X = psum[pb][:].rearrange("p (h x d) -> p h x d", h=2, x=2)
                    X1 = X[:, :, 0, :]
                    X2 = X[:, :, 1, :]
                    Dv = dstt[:, hh * 256:(hh + 2) * 256].rearrange("p (h x d) -> p h x d", h=2, x=2)
                    cb = cos_t[:].unsqueeze(1).to_broadcast([128, 2, 128])
                    sb_ = sin_t[:].unsqueeze(1).to_broadcast([128, 2, 128])
                    pk = _k("ps", pb)

                    def tt(e, o, a, b, op):
                        return e.tensor_tensor(o, a, b, op)
                    P.add("dve", lambda e, X1=X1, cb=cb: tt(e, tmp[0][:], X1, cb, ALU.mult), r=[pk, _k("cos")], w=[_k("tmp", 0)])
                    P.add("dve", lambda e, X2=X2, sb_=sb_: tt(e, tmp[1][:], X2, sb_, ALU.mult), r=[pk, _k("sin")], w=[_k("tmp", 1)])
                    P.add("dve", lambda e, X2=X2, cb=cb: tt(e, tmp[2][:], X2, cb, ALU.mult), r=[pk, _k("cos")], w=[_k("tmp", 2)])
                    P.add("dve", lambda e, X1=X1, sb_=sb_: tt(e, tmp[3][:], X1, sb_, ALU.mult), r=[pk, _k("sin")], w=[_k("tmp", 3)])
                    P.add("dve", lambda e, Dv=Dv: tt(e, Dv[:, :, 0, :], tmp[0][:], tmp[1][:], ALU.subtract),
                          r=[_k("tmp", 0), _k("tmp", 1)], w=[_k(dname, g % 2)])
                    P.add("dve", lambda e, Dv=Dv: tt(e, Dv[:, :, 1, :], tmp[2][:], tmp[3][:], ALU.add),
                          r=[_k("tmp", 2), _k("tmp", 3)], w=[_k(dname, g % 2)])
                elif g < 8:
                    vv = g - 4
                    P.add("act", lambda e, vv=vv, pb=pb: e.copy(vb[:, vv * 512:(vv + 1) * 512], psum[pb][:]),
                          r=[_k("ps", pb)], w=[_k("vb", vv)])
                else:
                    vv = g - 8
                    P.add("act", lambda e, vv=vv, pb=pb: e.activation(gs[:, vv * 512:(vv + 1) * 512], psum[pb][:], AF.Silu),
                          r=[_k("ps", pb)], w=[_k("gs", vv)])
            if DBG["stage"] < 3:
                P.add("sp", lambda e, t0=t0, hbuf=hbuf: e.dma_start(out=h_out[t0:t0 + 128, :], in_=hbuf[:]), r=[hk], dma=True)
                continue
            P.add("pool", lambda e: e.tensor_tensor(kz[:].rearrange("p (h d) -> p h d", h=4),
                                                    krot[:].rearrange("p (h d) -> p h d", h=4),
                                                    zeta[:].unsqueeze(2).to_broadcast([128, 4, 256]), ALU.mult),
                  r=[_k("krot", 0), _k("krot", 1), _k("zeta")], w=[_k("kz")])
            P.add("pe", lambda e: tr8(e, qrot, 0), r=[_k("qrot", 0), _k("qrot", 1), _k("ident")], w=[_k("ps", 0)])
            P.add("act", lambda e: e.copy(qT[:].rearrange("p c t -> p (c t)"), bfv(0)), r=[_k("ps", 0)], w=[_k("qT")])
            P.add("dve", lambda e: e.tensor_tensor(qxiT[:].rearrange("p c t -> p (c t)"), bfv(0),
                                                   xi8[:].rearrange("p c t -> p (c t)"), ALU.mult),
                  r=[_k("ps", 0), _k("xi8")], w=[_k("qxiT")])
            P.add("pe", lambda e: tr8(e, krot, 3), r=[_k("krot", 0), _k("krot", 1), _k("ident")], w=[_k("ps", 3)])
            P.add("act", lambda e: e.copy(kT[:].rearrange("p c t -> p (c t)"), bfv(3)), r=[_k("ps", 3)], w=[_k("kT")])

            if DBG["stage"] < 4:
                P.add("sp", lambda e, t0=t0, hbuf=hbuf: e.dma_start(out=h_out[t0:t0 + 128, :], in_=hbuf[:]), r=[hk], dma=True)
                continue
            def amm(e):
                inst = None
                for h in range(4):
                    for cc in range(2):
                        inst = e.matmul(psum[0][:, h * 128:(h + 1) * 128], kT[:, 2 * h + cc, :], qT[:, 2 * h + cc, :],
                                        start=(cc == 0), stop=(cc == 1))
                return inst
            P.add("pe", amm, r=[_k("kT"), _k("qT")], w=[_k("ps", 0)])
            P.add("dve", lambda e: e.tensor_tensor(atm[:].rearrange("p h t -> p (h t)"), psum[0][:],
                                                   dtm[:].rearrange("p h t -> p (h t)"), ALU.mult),
                  r=[_k("ps", 0), _k("dtm")], w=[_k("atm")])
            if DBG["stage"] < 5:
                P.add("sp", lambda e, t0=t0, hbuf=hbuf: e.dma_start(out=h_out[t0:t0 + 128, :], in_=hbuf[:]), r=[hk], dma=True)
                continue
            for h in range(4):
                def omm(e, h=h):
                    e.matmul(psum[4 + h][:], atm[:, h, :], vb[:, h * 512:(h + 1) * 512], start=True, stop=False)
                    e.matmul(psum[4 + h][:], qxiT[:, 2 * h, :], Sb[:, 0, h, :], start=False, stop=False)
                    return e.matmul(psum[4 + h][:], qxiT[:, 2 * h + 1, :], Sb[:, 1, h, :], start=False, stop=True)
                P.add("pe", omm, r=[_k("atm"), _k("vb", h), _k("qxiT"), _k("Sb", 0, h), _k("Sb", 1, h)], w=[_k("ps", 4 + h)])
                P.add("act", lambda e, h=h: e.activation(junk[:], psum[4 + h][:], AF.Square, accum_out=ss[:, h:h + 1]),
                      r=[_k("ps", 4 + h)], w=[_k("junk"), _k("ss", h)])
            if DBG["stage"] < 7:
                P.add("sp", lambda e, t0=t0, hbuf=hbuf: e.dma_start(out=h_out[t0:t0 + 128, :], in_=hbuf[:]), r=[hk], dma=True)
                continue
            rsqrt_small(P, rstd[:, 0:4], _k("rstd", 0), ss[:, 0:4], [_k("ss", h) for h in range(4)], 1.0 / 512)
            for h in range(4):
                P.add("dve", lambda e, h=h: e.scalar_tensor_tensor(og[:, h * 512:(h + 1) * 512], psum[4 + h][:], rstd[:, h:h + 1],
                                                                    gs[:, h * 512:(h + 1) * 512], ALU.mult, ALU.mult),
                      r=[_k("ps", 4 + h), _k("rstd", 0), _k("gs", h)], w=[_k("og", h)])
            for half in range(2):
                pb = 0 if half == 0 else 3

                def tro(e, half=half, pb=pb):
                    inst = None
                    v = bfv(pb)
                    for cc in range(8):
                        c2 = half * 8 + cc
                        inst = e.transpose(v[:, cc * 128:(cc + 1) * 128], og[:, c2 * 128:(c2 + 1) * 128], ident[:])
                    return inst
                P.add("pe", tro, r=[_k("og", half * 2), _k("og", half * 2 + 1), _k("ident")], w=[_k("ps", pb)])
                P.add("act", lambda e, half=half, pb=pb: e.copy(ogT[:, half * 8:(half + 1) * 8, :].rearrange("p c t -> p (c t)"), bfv(pb)),
                      r=[_k("ps", pb)], w=[_k("ogT", half)])
            if DBG["stage"] < 6:
                P.add("sp", lambda e, t0=t0, hbuf=hbuf: e.dma_start(out=h_out[t0:t0 + 128, :], in_=hbuf[:]), r=[hk], dma=True)
                continue
            for h in range(4):
                for cc in range(2):
                    pb = 1 + ((h * 2 + cc) % 2)
                    P.add("pe", lambda e, h=h, cc=cc, pb=pb: e.matmul(psum[pb][:], kz[:, h * 256 + cc * 128:h * 256 + (cc + 1) * 128],
                                                                        vb[:, h * 512:(h + 1) * 512], start=True, stop=True),
                          r=[_k("kz"), _k("vb", h)], w=[_k("ps", pb)])
                    dec = RET_GAMMA[h] ** 128
                    P.add("dve", lambda e, h=h, cc=cc, pb=pb, dec=dec: e.scalar_tensor_tensor(S[:, cc, h, :], S[:, cc, h, :], dec, psum[pb][:], ALU.mult, ALU.add),
                          r=[_k("S", cc, h), _k("ps", pb)], w=[_k("S", cc, h)])
                    P.add("pool", lambda e, h=h, cc=cc: e.tensor_copy(Sb[:, cc, h, :], S[:, cc, h, :]),
                          r=[_k("S", cc, h)], w=[_k("Sb", cc, h)])
            for mh in range(2):
                pb = 1 + mh

                def ymm(e, mh=mh, pb=pb):
                    inst = None
                    for cc in range(16):
                        inst = e.matmul(psum[pb][:], ogT[:, cc, :], wout[:, cc, mh * 512:(mh + 1) * 512],
                                        start=(cc == 0), stop=(cc == 15))
                    return inst
                P.add("pe", ymm, r=[_k("ogT", 0), _k("ogT", 1)] + [_k("wout", cc, jj) for cc in range(16) for jj in range(2)],
                      w=[_k("ps", pb)])
                P.add("dve", lambda e, mh=mh, pb=pb, hbuf=hbuf: e.tensor_tensor(hbuf[:, mh * 512:(mh + 1) * 512], hbuf[:, mh * 512:(mh + 1) * 512],
                                                                      psum[pb][:], ALU.add),
                      r=[_k("ps", pb), hk], w=[hk])
            P.add("sp", lambda e, t0=t0, hbuf=hbuf: e.dma_start(out=h_out[t0:t0 + 128, :], in_=hbuf[:]), r=[hk], dma=True)
        P.emit(es)


MASK_BIG = 30000.0
N_IT = 22
TOPK = 256
IDX_C0 = (64.0 ** -0.5) * (8.0 ** -0.5)
ACT_BISECT_EVERY = 10 ** 9


def _interleave(gens):
    st_ = [[g, max(1, n), 0] for g, n in gens]
    while st_:
        st_.sort(key=lambda x: x[2] / x[1])
        g = st_[0]
        try:
            next(g[0])
            g[2] += 1
        except StopIteration:
            st_.pop(0)


def mix0_phase(nc, h_in, h_out, I, C):
    RSQRT_MODE[0] = "lnexp"
    with ExitStack() as es:
        P = Prog(nc)
        c = Ctx(nc, P, es)
        CC = 512
        win = c.sb("win", [128, 8, EVEN_IN], BF16)
        wout = c.sb("wout", [128, 8, D], BF16)
        stage = [c.sb("stage%d" % i, [128, CC], F32) for i in range(2)]
        gcol = c.sb("gcol", [128, 8], F32)
        gcol2 = c.sb("gcol2", [128, 8], F32)
        ident = c.sb("ident", [128, 128], BF16)
        identf = c.sb("identf", [128, 128], F32)
        ones = c.sb("ones", [128, 128], BF16)
        ident4 = c.sb("ident4", [128, 512], BF16)
        trim = c.sb("trim", [128, 128], F32)
        trir = c.sb("trir", [128, 128], F32)
        m01 = c.sb("m01", [128, 4, 128], F32)
        negm = c.sb("negm", [128, 128], F32)
        pw = c.sb("pw", [128, N_IT + 1], F32)
        wa2 = c.sb("wa2", [16, 256], F32)
        ba2 = c.sb("ba2", [128, 256], F32)
        hbufA = c.sb("hbufA", [128, D], F32)
        hbufC = c.sb("hbufC", [128, D], F32)
        xn = c.sb("xn", [128, D], BF16)
        xnT = c.sb("xnT", [128, 8, 128], BF16)
        junk = c.sb("junk", [128, D], BF16)
        gqk = c.sb("gqk", [128, 512], BF16)
        gv = c.sb("gv", [128, 512], BF16)
        gsl = c.sb("gsl", [128, 512], BF16)
        dq = c.sb("dq", [128, 512], BF16)
        dkb = c.sb("dkb", [128, 128], BF16)
        iq = c.sb("iq", [128, 512], BF16)
        ikb = c.sb("ikb", [128, 64], BF16)
        iw = c.sb("iw", [128, 8], F32)
        wabs = [c.sb("wabs%d" % i, [128, 8], F32) for i in range(2)]
        sgn = c.sb("sgn", [128, 8], F32)
        Dsg = [c.sb("Dsg%d" % i, [128, 8, 128], BF16) for i in range(2)]
        gaT = c.sb("gaT", [16, 128], F32)
        zb = c.sb("zb", [128, 256], F32)
        sp_ = c.sb("sp", [128, 256], F32)
        ecum = c.sb("ecum", [64, 4, 128], F32)
        encum = c.sb("encum", [64, 4, 128], F32)
        erev = c.sb("erev", [128, 256], F32)
        qtT = c.sb("qtT", [64, 4, 128], BF16)
        ktT = c.sb("ktT", [64, 4, 128], BF16)
        kend = c.sb("kend", [128, 256], BF16)
        atm = c.sb("atm", [128, 4, 128], BF16)
        Sg = c.sb("Sg", [64, 4, 128], F32)
        Sgb = c.sb("Sgb", [64, 4, 128], BF16)
        og = c.sb("og", [128, 512], BF16)
        ogT = [c.sb("ogT%d" % i, [128, 4, 128], BF16) for i in range(4)]
        dsT = c.sb("dsT", [128, 4, 128], BF16)
        qT = [c.sb("qT%d" % i, [128, 4, 128], BF16) for i in range(4)]
        kTc = c.sb("kTc", [128, T], BF16)
        vc = c.sb("vc", [128, NT, 128], BF16)
        ikTc = c.sb("ikTc", [64, T], BF16)
        iqT = [c.sb("iqT%d" % i, [64, 8, 128], BF16) for i in range(2)]
        scb = [c.sb("sc%d" % i, [128, T], F32) for i in range(2)]
        mskb = [c.sb("msk%d" % i, [128, T], BF16) for i in range(2)]
        junkb = c.sb("junkb", [128, T], mybir.dt.int8)
        Rb = [c.sb("Rb%d" % i, [128, 512], BF16) for i in range(2)]
        Eb = [c.sb("Eb%d" % i, [128, 512], BF16) for i in range(2)]
        rden = c.sb("rden", [128, 512], F32)
        ss = c.sb("ss", [128, 8], F32)
        rstd = c.sb("rstd", [128, 8], F32)
        st = c.sb("st", [128, 8], F32)
        hst = c.sb("hst", [128, N_IT + 1], F32)
        nhst = c.sb("nhst", [128, N_IT], F32)
        hhst = c.sb("hhst", [128, N_IT], F32)
        psum = [c.ps("ps%d" % i) for i in range(8)]

        def dma(out_ap, in_ap, wk, **kw):
            P.add("sp", lambda e: e.dma_start(out=out_ap, in_=in_ap, **kw), w=[wk], dma=True)

        def bfv(i):
            return psum[i][:].bitcast(BF16)

        dma(gcol[:], I["even_attn_norm"][0].rearrange("(k p) -> p k", p=128), _k("gcol"), allow_slow_non_contiguous=True)
        P.add("dve", lambda e: e.memset(gcol2[:], 1.0), w=[_k("gcol2")])
        for cc in range(4):
            dma(gcol2[:, cc:cc + 1], I["even_gla_norm"][0].rearrange("(p o) -> p o", o=1), _k("gcol2"))
        dma(ident[:], C["ident"], _k("ident"))
        dma(identf[:], C["identf"], _k("identf"))
        dma(ones[:], C["ones"], _k("ones"))
        dma(ident4[:], C["ident4"], _k("ident4"))
        dma(trim[:], C["gla_trim"], _k("trim"))
        dma(trir[:], C["gla_trir"], _k("trir"))
        dma(m01[:], C["gla_m01"], _k("m01"))
        dma(negm[:], C["dsa_negm"], _k("negm"))
        dma(pw[:], C["dsa_pw"], _k("pw"))
        dma(wa2[:], I["even_gla_wa2"][0], _k("wa2"))
        dma(ba2[:], I["even_gla_ba2"][0].partition_broadcast(128), _k("ba2"))
        P.add("dve", lambda e: e.memset(Sg[:], 0.0), w=[_k("Sg")])
        P.add("pool", lambda e: e.memset(Sgb[:], 0.0), w=[_k("Sgb")])
        stage = [(stage[0][:], [_k("stage", 0)]), (stage[1][:], [_k("stage", 1)])] + \
                [(scb[1][:, si * 512:(si + 1) * 512], [_k("stg", si)]) for si in range(8)]
        load_weight_bf16(c, I["even_w_in"][0], D, EVEN_IN, win, "win", stage, gcol, _k("gcol"), CC)
        load_weight_bf16(c, I["even_w_out"][0], D, D, wout, "wout", stage, gcol2, _k("gcol2"), CC)
        P.add("dve", lambda e: e.memset(st[:, 7:8], 0.0), w=[_k("stg", si) for si in range(8)] + [_k("sc", 1), _k("st", 7)])
        WIN_ALL = [_k("win", k, j) for k in range(8) for j in range(6)]
        WOUT_ALL = [_k("wout", k, j) for k in range(8) for j in range(2)]

        def stageA(i):
            t0 = i * 128
            p2 = i % 2
            p3 = i % 4
            hb = hbufA
            hk = _k("hbufA")
            dma(hb[:], h_in[t0:t0 + 128, :], hk)
            rmsnorm_tile(c, hb[:], hk, xn[:], _k("xn"), junk[:], _k("junk"),
                         ss[:, 4:5], _k("ss", 4), rstd[:, 4:5], _k("rstd", 4))

            def tr8(e):
                inst = None
                v = bfv(0)
                for cc in range(8):
                    inst = e.transpose(v[:, cc * 128:(cc + 1) * 128], xn[:, cc * 128:(cc + 1) * 128], ident[:])
                return inst
            P.add("pe", tr8, r=[_k("xn"), _k("ident")], w=[_k("ps", 0)])
            P.add("act", lambda e: e.copy(xnT[:].rearrange("p c t -> p (c t)"), bfv(0)), r=[_k("ps", 0)], w=[_k("xnT")])
            yield

            def proj(c0, c1, pb):
                def f(e):
                    inst = None
                    for k in range(8):
                        inst = e.matmul(psum[pb][:, 0:(c1 - c0)], xnT[:, k, :], win[:, k, c0:c1], start=(k == 0), stop=(k == 7))
                    return inst
                P.add("pe", f, r=[_k("xnT")] + WIN_ALL, w=[_k("ps", pb)])

            proj(2320, 2832, 1)
            P.add("dve", lambda e: e.tensor_copy(iq[:], psum[1][:]), r=[_k("ps", 1)], w=[_k("iq")])
            yield
            proj(2832, 2904, 2)
            P.add("act", lambda e: e.copy(ikb[:], psum[2][:, 0:64]), r=[_k("ps", 2)], w=[_k("ikb")])
            P.add("act", lambda e: e.copy(iw[:], psum[2][:, 64:72]), r=[_k("ps", 2)], w=[_k("iw")])
            yield
            P.add("act", lambda e: e.activation(wabs[p2][:], iw[:], AF.Abs, scale=IDX_C0), r=[_k("iw")], w=[_k("wabs", p2)])
            P.add("act", lambda e: e.sign(sgn[:], iw[:]), r=[_k("iw")], w=[_k("sgn")])
            P.add("dve", lambda e: e.tensor_tensor(Dsg[p2][:], identf[:].unsqueeze(1).to_broadcast([128, 8, 128]),
                                                   sgn[:].unsqueeze(2).to_broadcast([128, 8, 128]), ALU.mult),
                  r=[_k("identf"), _k("sgn")], w=[_k("Dsg", p2)])

            def triq(e):
                inst = None
                v = bfv(0)
                for hh in range(8):
                    inst = e.transpose(v[0:64, hh * 128:(hh + 1) * 128], iq[:, hh * 64:(hh + 1) * 64], ident[:])
                return inst
            P.add("pe", triq, r=[_k("iq"), _k("ident")], w=[_k("ps", 0)])
            P.add("act", lambda e: e.copy(iqT[p2][:].rearrange("p c t -> p (c t)"), bfv(0)[0:64, :]), r=[_k("ps", 0)], w=[_k("iqT", p2)])
            yield
            P.add("pe", lambda e: e.transpose(bfv(0)[0:64, 0:128], ikb[:], ident[:]), r=[_k("ikb"), _k("ident")], w=[_k("ps", 0)])
            P.add("act", lambda e: e.copy(ikTc[:, t0:t0 + 128], bfv(0)[0:64, 0:128]), r=[_k("ps", 0)], w=[_k("ikTc", i)])
            yield
            proj(1552, 2064, 1)
            P.add("dve", lambda e: e.tensor_copy(dq[:], psum[1][:]), r=[_k("ps", 1)], w=[_k("dq")])
            yield
            proj(2064, 2320, 2)
            P.add("act", lambda e: e.copy(dkb[:], psum[2][:, 0:128]), r=[_k("ps", 2)], w=[_k("dkb")])
            P.add("act", lambda e: e.copy(vc[:, i, :], psum[2][:, 128:256]), r=[_k("ps", 2)], w=[_k("vc", i)])
            yield

            def trdq(e):
                inst = None
                v = bfv(0)
                for cc in range(4):
                    inst = e.transpose(v[:, cc * 128:(cc + 1) * 128], dq[:, cc * 128:(cc + 1) * 128], ident[:])
                inst = e.transpose(v[:, 512:640], dkb[:], ident[:])
                return inst
            P.add("pe", trdq, r=[_k("dq"), _k("dkb"), _k("ident")], w=[_k("ps", 0)])
            P.add("act", lambda e: e.copy(qT[p3][:].rearrange("p c t -> p (c t)"), bfv(0)[:, 0:512]), r=[_k("ps", 0)], w=[_k("qT", p3)])
            P.add("act", lambda e: e.copy(kTc[:, t0:t0 + 128], bfv(0)[:, 512:640]), r=[_k("ps", 0)], w=[_k("kTc", i)])
            yield
            proj(0, 512, 1)
            P.add("act", lambda e: e.copy(gqk[:], psum[1][:]), r=[_k("ps", 1)], w=[_k("gqk")])
            yield
            proj(512, 1024, 2)
            P.add("dve", lambda e: e.tensor_copy(gv[:], psum[2][:]), r=[_k("ps", 2)], w=[_k("gv")])
            yield
            proj(1040, 1552, 1)
            P.add("act", lambda e: e.activation(gsl[:], psum[1][:], AF.Silu), r=[_k("ps", 1)], w=[_k("gsl")])
            yield

            def gaf(e):
                inst = None
                for k in range(8):
                    inst = e.matmul(psum[2][0:16, 0:128], win[:, k, 1024:1040], xnT[:, k, :], start=(k == 0), stop=(k == 7))
                return inst
            P.add("pe", gaf, r=[_k("xnT")] + WIN_ALL, w=[_k("ps", 2)])
            P.add("act", lambda e: e.copy(gaT[:], psum[2][0:16, 0:128]), r=[_k("ps", 2)], w=[_k("gaT")])
            yield
            P.add("pe", lambda e: e.matmul(psum[1][:, 0:256], gaT[:], wa2[:], start=True, stop=True),
                  r=[_k("gaT"), _k("wa2")], w=[_k("ps", 1)])
            P.add("dve", lambda e: e.tensor_tensor(zb[:], psum[1][:, 0:256], ba2[:], ALU.add),
                  r=[_k("ps", 1), _k("ba2")], w=[_k("zb")])
            P.add("act", lambda e: e.activation(zb[:], zb[:], AF.Exp, scale=-1.0), r=[_k("zb")], w=[_k("zb")])
            P.add("act", lambda e: e.activation(sp_[:], zb[:], AF.Ln, bias=1.0), r=[_k("zb")], w=[_k("sp")])
            yield

            def cumf(e):
                inst = None
                for h in range(4):
                    inst = e.matmul(psum[2][0:64, h * 128:(h + 1) * 128], sp_[:, h * 64:(h + 1) * 64], trim[:], start=True, stop=True)
                return inst
            P.add("pe", cumf, r=[_k("sp"), _k("trim")], w=[_k("ps", 2)])
            P.add("pe", lambda e: e.matmul(psum[1][:, 0:256], trir[:], sp_[:], start=True, stop=True),
                  r=[_k("sp"), _k("trir")], w=[_k("ps", 1)])
            P.add("act", lambda e: e.activation(ecum[:].rearrange("p h t -> p (h t)"), psum[2][0:64, :], AF.Exp),
                  r=[_k("ps", 2)], w=[_k("ecum")])
            P.add("act", lambda e: e.activation(encum[:].rearrange("p h t -> p (h t)"), psum[2][0:64, :], AF.Exp, scale=-1.0),
                  r=[_k("ps", 2)], w=[_k("encum")])
            P.add("act", lambda e: e.activation(erev[:], psum[1][:, 0:256], AF.Exp), r=[_k("ps", 1)], w=[_k("erev")])
            yield

            def trqk(e):
                inst = None
                v = bfv(0)
                for hh in range(8):
                    inst = e.transpose(v[0:64, hh * 128:(hh + 1) * 128], gqk[:, hh * 64:(hh + 1) * 64], ident[:])
                return inst
            P.add("pe", trqk, r=[_k("gqk"), _k("ident")], w=[_k("ps", 0)])
            P.add("dve", lambda e: e.scalar_tensor_tensor(qtT[:].rearrange("p h t -> p (h t)"), bfv(0)[0:64, 0:512], 0.125,
                                                          ecum[:].rearrange("p h t -> p (h t)"), ALU.mult, ALU.mult),
                  r=[_k("ps", 0), _k("ecum")], w=[_k("qtT")])
            P.add("dve", lambda e: e.tensor_tensor(ktT[:].rearrange("p h t -> p (h t)"), bfv(0)[0:64, 512:1024],
                                                   encum[:].rearrange("p h t -> p (h t)"), ALU.mult),
                  r=[_k("ps", 0), _k("encum")], w=[_k("ktT")])
            P.add("dve", lambda e: e.tensor_tensor(kend[:], gqk[:, 256:512], erev[:], ALU.mult),
                  r=[_k("gqk"), _k("erev")], w=[_k("kend")])
            yield

            def atf(e):
                inst = None
                for h in range(4):
                    inst = e.matmul(psum[2][:, h * 128:(h + 1) * 128], ktT[:, h, :], qtT[:, h, :], start=True, stop=True)
                return inst
            P.add("pe", atf, r=[_k("ktT"), _k("qtT")], w=[_k("ps", 2)])
            P.add("dve", lambda e: e.tensor_tensor(atm[:].rearrange("p h t -> p (h t)"), psum[2][:],
                                                   m01[:].rearrange("p h t -> p (h t)"), ALU.mult),
                  r=[_k("ps", 2), _k("m01")], w=[_k("atm")])
            yield

            def of(e):
                inst = None
                for h in range(4):
                    e.matmul(psum[1][:, h * 128:(h + 1) * 128], atm[:, h, :], gv[:, h * 128:(h + 1) * 128], start=True, stop=False)
                    inst = e.matmul(psum[1][:, h * 128:(h + 1) * 128], qtT[:, h, :], Sgb[:, h, :], start=False, stop=True)
                return inst
            P.add("pe", of, r=[_k("atm"), _k("gv"), _k("qtT"), _k("Sgb")], w=[_k("ps", 1)])

            def dsf(e):
                inst = None
                for h in range(4):
                    inst = e.matmul(psum[2][0:64, h * 128:(h + 1) * 128], kend[:, h * 64:(h + 1) * 64], gv[:, h * 128:(h + 1) * 128],
                                    start=True, stop=True)
                return inst
            P.add("pe", dsf, r=[_k("kend"), _k("gv")], w=[_k("ps", 2)])
            elast = ecum[:, :, 127:128].to_broadcast([64, 4, 128])
            P.add("dve", lambda e: e.tensor_tensor(Sg[:], Sg[:], elast, ALU.mult), r=[_k("Sg"), _k("ecum")], w=[_k("Sg")])
            P.add("dve", lambda e: e.tensor_tensor(Sg[:].rearrange("p h t -> p (h t)"), Sg[:].rearrange("p h t -> p (h t)"),
                                                   psum[2][0:64, :], ALU.add), r=[_k("Sg"), _k("ps", 2)], w=[_k("Sg")])
            P.add("pool", lambda e: e.tensor_copy(Sgb[:], Sg[:]), r=[_k("Sg")], w=[_k("Sgb")])
            yield
            for h in range(4):
                P.add("act", lambda e, h=h: e.activation(junk[:, 0:128], psum[1][:, h * 128:(h + 1) * 128], AF.Square, accum_out=ss[:, h:h + 1]),
                      r=[_k("ps", 1)], w=[_k("junk"), _k("ss", h)])
            rsqrt_small(P, rstd[:, 0:4], _k("rstd", 0), ss[:, 0:4], [_k("ss", h) for h in range(4)], 1.0 / 128)
            yield
            for h in range(4):
                P.add("dve", lambda e, h=h: e.scalar_tensor_tensor(og[:, h * 128:(h + 1) * 128], psum[1][:, h * 128:(h + 1) * 128], rstd[:, h:h + 1],
                                                                    gsl[:, h * 128:(h + 1) * 128], ALU.mult, ALU.mult),
                      r=[_k("ps", 1), _k("rstd", 0), _k("gsl")], w=[_k("og")])
            yield

            def trog(e):
                inst = None
                v = bfv(0)
                for cc in range(4):
                    inst = e.transpose(v[:, cc * 128:(cc + 1) * 128], og[:, cc * 128:(cc + 1) * 128], ident[:])
                return inst
            P.add("pe", trog, r=[_k("og"), _k("ident")], w=[_k("ps", 0)])
            P.add("act", lambda e: e.copy(ogT[p3][:].rearrange("p c t -> p (c t)"), bfv(0)[:, 0:512]),
                  r=[_k("ps", 0)], w=[_k("ogT", p3)])
            yield

        def stageBs(i):
            p2 = i % 2
            sc = scb[p2]
            nkeys = (i + 1) * 128
            ngrp = (nkeys + 511) // 512
            pend = None
            for gk in range(ngrp):
                k0 = gk * 512
                n = min(512, nkeys - k0)
                kk = [_k("ikTc", b) for b in range(k0 // 128, (k0 + n) // 128)]
                for hI in range(8):
                    rb = hI % 2
                    P.add("pe", lambda e, hI=hI, k0=k0, n=n: e.matmul(psum[3][:, 0:n], iqT[p2][:, hI, :], ikTc[:, k0:k0 + n], start=True, stop=True),
                          r=[_k("iqT", p2)] + kk, w=[_k("ps", 3)])
                    if pend is not None:
                        pend()
                        pend = None
                    P.add("act", lambda e, rb=rb, hI=hI, n=n: e.activation(Rb[rb][:, 0:n], psum[3][:, 0:n], AF.Relu, scale=wabs[p2][:, hI:hI + 1]),
                          r=[_k("ps", 3), _k("wabs", p2)], w=[_k("Rb", rb)])

                    def acc(rb=rb, hI=hI, n=n, k0=k0):
                        P.add("pe", lambda e: e.matmul(psum[4][:, 0:n], Dsg[p2][:, hI, :], Rb[rb][:, 0:n], start=(hI == 0), stop=(hI == 7)),
                              r=[_k("Dsg", p2), _k("Rb", rb)], w=[_k("ps", 4)])
                        if hI == 7:
                            P.add("act", lambda e: e.copy(sc[:, k0:k0 + n], psum[4][:, 0:n]), r=[_k("ps", 4)], w=[_k("sc", p2)])
                    pend = acc
                    yield
            if pend is not None:
                pend()
            yield

        def stageBb(i):
            t0 = i * 128
            p2 = i % 2
            sc = scb[p2]
            nkeys = (i + 1) * 128
            scv = sc[:, 0:nkeys]
            P.add("dve", lambda e: e.tensor_reduce(st[:, 0:1], scv, AX.X, ALU.max), r=[_k("sc", p2)], w=[_k("st", 0)])
            yield
            lov = scv if i < 2 else sc[:, 0:TOPK]
            P.add("dve", lambda e: e.tensor_reduce(st[:, 1:2], lov, AX.X, ALU.min), r=[_k("sc", p2)], w=[_k("st", 1)])
            P.add("dve", lambda e: e.tensor_tensor(sc[:, t0:t0 + 128], sc[:, t0:t0 + 128], negm[:], ALU.add),
                  r=[_k("sc", p2), _k("negm")], w=[_k("sc", p2)])
            P.add("dve", lambda e: e.tensor_tensor(st[:, 2:3], st[:, 0:1], st[:, 1:2], ALU.subtract), r=[_k("st", 0), _k("st", 1)], w=[_k("st", 2)])
            P.add("dve", lambda e: e.tensor_scalar(st[:, 2:3], st[:, 2:3], 1.000001, 1e-30, ALU.mult, ALU.add), r=[_k("st", 2)], w=[_k("st", 2)])
            P.add("dve", lambda e: e.tensor_scalar(hst[:], pw[:], st[:, 2:3], None, ALU.mult), r=[_k("st", 2), _k("pw")], w=[_k("hst")])
            yield
            for k in range(N_IT):
                P.add("dve", lambda e, k=k: e.tensor_tensor(st[:, 3:4], st[:, 1:2], hst[:, k:k + 1], ALU.add),
                      r=[_k("st", 1), _k("hst")], w=[_k("st", 3)])
                P.add("dve", lambda e: e.tensor_scalar(junkb[:, 0:nkeys], scv, st[:, 3:4], None, ALU.is_ge, ALU.add, accum_out=st[:, 4:5]),
                      r=[_k("sc", p2), _k("st", 3)], w=[_k("junkb"), _k("st", 4)])
                P.add("dve", lambda e, k=k: e.tensor_scalar(st[:, 5:6], st[:, 4:5], TOPK - 0.5, hst[:, k:k + 1], ALU.is_ge, ALU.mult),
                      r=[_k("st", 4), _k("hst")], w=[_k("st", 5)])
                P.add("dve", lambda e: e.tensor_tensor(st[:, 1:2], st[:, 1:2], st[:, 5:6], ALU.add),
                      r=[_k("st", 1), _k("st", 5)], w=[_k("st", 1)])
                yield
            P.add("dve", lambda e: e.tensor_scalar(mskb[p2][:, 0:nkeys], scv, st[:, 1:2], -MASK_BIG, ALU.is_lt, ALU.mult),
                  r=[_k("sc", p2), _k("st", 1)], w=[_k("msk", p2)])
            yield
            return
            for b0 in range(0, i + 1, 8):
                nb = min(8, i + 1 - b0)

                def trm(e, b0=b0, nb=nb):
                    inst = None
                    v = bfv(3)
                    for bb in range(nb):
                        inst = e.transpose(v[:, bb * 128:(bb + 1) * 128], msk[:, (b0 + bb) * 128:(b0 + bb + 1) * 128], ident[:])
                    return inst
                P.add("pe", trm, r=[_k("msk"), _k("ident")], w=[_k("ps", 3)])
                P.add("act", lambda e, b0=b0, nb=nb: e.copy(mskT[:, b0:b0 + nb, :].rearrange("p c t -> p (c t)"), bfv(3)[:, 0:nb * 128]),
                      r=[_k("ps", 3)], w=[_k("mskT", b0 // 8)])
                yield

        def stageC(i):
            t0 = i * 128
            p3 = i % 4
            p2 = i % 2
            hb = hbufC
            hk = _k("hbufC")
            dma(hb[:], h_in[t0:t0 + 128, :], hk)
            pendc = []
            for j in range(i + 1):
                eb = j % 2

                def stf(e, j=j):
                    e.matmul(psum[5][:], kTc[:, j * 128:(j + 1) * 128], qT[p3][:].rearrange("p c t -> p (c t)"), start=True, stop=False)
                    return e.matmul(psum[5][:], mskb[p2][:, j * 128:(j + 1) * 128], ident4[:], start=False, stop=True)
                P.add("pe", stf, r=[_k("kTc", j), _k("qT", p3), _k("msk", p2), _k("ident4")], w=[_k("ps", 5)])
                while pendc:
                    pendc.pop(0)()
                P.add("act", lambda e, eb=eb: e.activation(Eb[eb][:], psum[5][:], AF.Exp, scale=128.0 ** -0.5),
                      r=[_k("ps", 5)], w=[_k("Eb", eb)])

                def pv(e, eb=eb, j=j):
                    e.matmul(psum[6][:], vc[:, j, :], Eb[eb][:], start=(j == 0), stop=(j == i))
                    return e.matmul(psum[7][:], ones[:], Eb[eb][:], start=(j == 0), stop=(j == i))

                def pvadd(pv=pv, j=j, eb=eb):
                    P.add("pe", pv, r=[_k("vc", j), _k("Eb", eb), _k("ones")], w=[_k("ps", 6), _k("ps", 7)])
                pendc.append(pvadd)
                yield
            while pendc:
                pendc.pop(0)()
            P.add("act", lambda e: e.activation(rden[:], psum[7][:], AF.Ln), r=[_k("ps", 7)], w=[_k("rden")])
            P.add("act", lambda e: e.activation(rden[:], rden[:], AF.Exp, scale=-1.0), r=[_k("rden")], w=[_k("rden")])
            P.add("dve", lambda e: e.tensor_tensor(dsT[:].rearrange("p c t -> p (c t)"), psum[6][:], rden[:], ALU.mult),
                  r=[_k("ps", 6), _k("rden")], w=[_k("dsT")])
            yield
            for mh in range(2):
                def ymm(e, mh=mh):
                    inst = None
                    for cc in range(8):
                        src = ogT[p3][:, cc, :] if cc < 4 else dsT[:, cc - 4, :]
                        inst = e.matmul(psum[5][:], src, wout[:, cc, mh * 512:(mh + 1) * 512], start=(cc == 0), stop=(cc == 7))
                    return inst
                P.add("pe", ymm, r=[_k("ogT", p3), _k("dsT")] + WOUT_ALL, w=[_k("ps", 5)])
                P.add("dve", lambda e, mh=mh: e.tensor_tensor(hb[:, mh * 512:(mh + 1) * 512], hb[:, mh * 512:(mh + 1) * 512], psum[5][:], ALU.add),
                      r=[_k("ps", 5), hk], w=[hk])
                yield
            P.add("sp", lambda e: e.dma_start(out=h_out[t0:t0 + 128, :], in_=hb[:]), r=[hk], dma=True)
            yield

        ntl = DBG["ntiles"]
        for s_ in range(ntl + 3):
            gens = []
            if 0 <= s_ - 3 < ntl:
                gens.append((stageC(s_ - 3), (s_ - 3) + 1 + 4))
            if 0 <= s_ - 2 < ntl:
                gens.append((stageBb(s_ - 2), 4 + N_IT + (s_ - 2) // 8 + 1))
            if 0 <= s_ - 1 < ntl:
                gens.append((stageBs(s_ - 1), 9 * ((s_ - 1) // 4 + 1)))
            if s_ < ntl:
                gens.append((stageA(s_), 24))
            _interleave(gens)
        P.emit(es)


def host_consts():
    import ml_dtypes
    cst = {}
    cst["ident"] = np.eye(128, dtype=ml_dtypes.bfloat16)
    half = 128
    inv = 10000.0 ** (-np.arange(half, dtype=np.float32) / half)
    ang = np.arange(T, dtype=np.float32)[:, None] * inv[None, :].astype(np.float32)
    cst["rope_cos"] = np.cos(ang).astype(np.float32)
    cst["rope_sin"] = np.sin(ang).astype(np.float32)
    g = np.array(RET_GAMMA, dtype=np.float64)
    ii = np.arange(128)
    rel = ii[None, :] - ii[:, None]
    dtm = np.zeros((128, 4, 128), np.float64)
    for h in range(4):
        dtm[:, h, :] = np.where(rel >= 0, g[h] ** np.maximum(rel, 0), 0.0) / 16.0
    cst["ret_dt"] = dtm.astype(np.float32)
    xi8 = np.zeros((128, 8, 128), np.float64)
    for cc in range(8):
        xi8[:, cc, :] = (g[cc // 2] ** (ii + 1.0))[None, :]
    cst["ret_xi8"] = xi8.astype(np.float32)
    cst["ones"] = np.ones((128, 128), dtype=ml_dtypes.bfloat16)
    cst["identf"] = np.eye(128, dtype=np.float32)
    cst["ident4"] = np.tile(np.eye(128, dtype=np.float32), (1, 4)).astype(ml_dtypes.bfloat16)
    le = (ii[:, None] <= ii[None, :])
    cst["gla_trim"] = np.where(le, -1.0 / 16.0, 0.0).astype(np.float32)
    cst["gla_trir"] = np.where(~le, -1.0 / 16.0, 0.0).astype(np.float32)
    cst["gla_m01"] = np.repeat(le[:, None, :], 4, axis=1).astype(np.float32)
    cst["dsa_negm"] = np.where(ii[None, :] <= ii[:, None], 0.0, -1e30).astype(np.float32)
    cst["dsa_pw"] = np.repeat((0.5 ** (np.arange(N_IT + 1) + 1.0))[None, :], 128, axis=0).astype(np.float32)
    zeta = np.zeros((128, 4), np.float64)
    for h in range(4):
        zeta[:, h] = g[h] ** (127.0 - ii) / 16.0
    cst["ret_zeta"] = zeta.astype(np.float32)
    return cst


INPUT_SHAPES = {
    "even_attn_norm": [1, D], "even_w_in": [1, D, EVEN_IN], "even_gla_wa2": [1, 16, 256],
    "even_gla_ba2": [1, 256], "even_gla_norm": [1, 128], "even_w_out": [1, D, D],
    "odd_attn_norm": [1, D], "odd_w_in": [1, D, ODD_IN], "odd_ret_norm": [1, 512], "odd_w_out": [1, 2048, D],
    "ffn_norm": [2, D], "ffn_w_gate": [2, D, DFF], "ffn_w_up": [2, D, DFF], "ffn_w_down": [2, DFF, D],
    "final_norm": [D],
}


def build(phases=("mix0", "ffn0", "mix1", "ffn1")):
    nc = bass.Bass("TRN2", target_bir_lowering=False)
    x = nc.dram_tensor("x", [T, D], F32, kind="ExternalInput").ap()
    out = nc.dram_tensor("out", [T, D], F32, kind="ExternalOutput").ap()
    I = {k: nc.dram_tensor(k, shp, F32, kind="ExternalInput").ap() for k, shp in INPUT_SHAPES.items()}
    cst = host_consts()
    C = {}
    for k, v in cst.items():
        C[k] = nc.dram_tensor(k, list(v.shape), BF16 if v.dtype != np.float32 else F32, kind="ExternalInput").ap()
    scr = [nc.dram_tensor("scr%d" % i, [T, D], F32, kind="Internal").ap() for i in range(3)]
    bufs = [x] + scr[:len(phases) - 1] + [out]
    for pi, ph in enumerate(phases):
        hin, hout = bufs[pi], bufs[pi + 1]
        if ph == "ffn0":
            ffn_phase(nc, hin, hout, I["ffn_norm"][0], I["ffn_w_gate"][0], I["ffn_w_up"][0], I["ffn_w_down"][0], C["ident"])
        elif ph == "ffn1":
            ffn_phase(nc, hin, hout, I["ffn_norm"][1], I["ffn_w_gate"][1], I["ffn_w_up"][1], I["ffn_w_down"][1], C["ident"],
                      final_g=I["final_norm"])
        elif ph == "mix1":
            mix1_phase(nc, hin, hout, I["odd_attn_norm"][0], I["odd_w_in"][0], I["odd_ret_norm"][0], I["odd_w_out"][0],
                       C["ident"], C["rope_cos"], C["rope_sin"], C["ret_dt"], C["ret_xi8"], C["ret_zeta"])
        elif ph == "mix0":
            mix0_phase(nc, hin, hout, I, C)
    return nc


def make_inputs(inputs, b):
    m = {k: np.ascontiguousarray(np.asarray(inputs[k], dtype=np.float32)) for k in INPUT_SHAPES}
    m["x"] = np.ascontiguousarray(np.asarray(inputs["x"][b], dtype=np.float32))
    m.update(host_consts())
    return m


def kernel(**inputs):
    nc = build()
    in_maps = [make_inputs(inputs, b) for b in range(8)]
    res = run_bass_kernel_spmd(nc, in_maps, core_ids=list(range(8)))
    return np.stack([np.asarray(r["out"], dtype=np.float32) for r in res.results], axis=0)
```

```python
import math
from contextlib import ExitStack

import numpy as np
import concourse.bass as bass
import concourse.mybir as mybir
from concourse.bass_utils import run_bass_kernel_spmd

F32 = mybir.dt.float32
BF16 = mybir.dt.bfloat16
AF = mybir.ActivationFunctionType
ALU = mybir.AluOpType
AX = mybir.AxisListType

T = 4096
D = 1024
DFF = 2816
NT = T // 128
EPS = 1e-6
EVEN_IN = 2904
ODD_IN = 6144


class _Op:
    __slots__ = ("eng", "fn", "deps", "dma", "sig", "sigidx", "dsem", "dval", "ndep")


class Prog:
    COMPUTE = ("pe", "act", "dve", "pool")

    def __init__(self, nc, n_dma_sems=16):
        self.nc = nc
        self.ops = []
        self.lastw = {}
        self.readers = {}
        self.n_dma_sems = n_dma_sems
        self.dma_rr = {"sp": 0, "pool": 0, "act": 0}
        self.dma_cnt = {}

    def add(self, eng, fn, r=(), w=(), dma=False):
        i = len(self.ops)
        deps = set()
        pr = [k for k in r if k[0] == "ps" and k not in w]
        if pr:
            w = list(w) + pr
        for k in r:
            a = self.lastw.get(k)
            if a is not None:
                deps.add(a)
        for k in w:
            a = self.lastw.get(k)
            if a is not None:
                deps.add(a)
            rd = self.readers.get(k)
            if rd:
                deps.update(rd.values())
        op = _Op()
        op.eng = eng
        op.fn = fn
        op.dma = dma
        op.sig = False
        op.sigidx = 0
        op.dsem = None
        op.dval = 0
        fdeps = []
        for a in deps:
            A = self.ops[a]
            if (not dma) and (not A.dma) and eng == "pe" and A.eng == "pe":
                continue
            fdeps.append(a)
        op.deps = sorted(fdeps)
        if dma:
            q = self.dma_rr[eng]
            self.dma_rr[eng] = (q + 1) % self.n_dma_sems
            key = (eng, q)
            self.dma_cnt[key] = self.dma_cnt.get(key, 0) + 1
            op.dsem = key
            op.dval = 16 * self.dma_cnt[key]
        self.ops.append(op)
        for k in w:
            self.lastw[k] = i
            self.readers[k] = {}
        for k in r:
            d = self.readers.setdefault(k, {})
            d[("dma", i) if dma else eng] = i
        return i

    def emit(self, es):
        nc = self.nc
        ops = self.ops
        for op in ops:
            for a in op.deps:
                if not ops[a].dma:
                    ops[a].sig = True
        cnt = {e: 0 for e in self.COMPUTE + ("sp",)}
        for op in ops:
            if op.sig and not op.dma:
                cnt[op.eng] += 1
                op.sigidx = cnt[op.eng]
        sems = {e: es.enter_context(nc.semaphore("s_" + e)) for e in cnt}
        dsems = {}
        for key in self.dma_cnt:
            dsems[key] = es.enter_context(nc.semaphore("d_%s%d" % key))
        engines = {"pe": "tensor", "act": "scalar", "dve": "vector", "pool": "gpsimd", "sp": "sync"}
        with nc.Block() as block:
            for e, attr in engines.items():
                mine = [op for op in ops if op.eng == e]

                def body(engine, mine=mine, e=e):
                    known = {}

                    def wait(sem_key, sem, val):
                        if known.get(sem_key, 0) >= val:
                            return
                        engine.wait_ge(sem, val)
                        known[sem_key] = val

                    for op in mine:
                        for a in op.deps:
                            A = ops[a]
                            if A.dma:
                                wait(A.dsem, dsems[A.dsem], A.dval)
                            else:
                                wait(A.eng, sems[A.eng], A.sigidx)
                        if op.dma:
                            if op.dval > 16:
                                wait(op.dsem, dsems[op.dsem], op.dval - 16)
                            inst = op.fn(engine)
                            inst.then_inc(dsems[op.dsem], 16)
                        else:
                            inst = op.fn(engine)
                            if op.sig:
                                inst.then_inc(sems[e], 1)
                    for key, c in self.dma_cnt.items():
                        if key[0] == e:
                            wait(key, dsems[key], 16 * c)

                getattr(block, attr)(body)


def _k(name, *idx):
    return (name,) + idx


class Ctx:
    def __init__(self, nc, P, es):
        self.nc = nc
        self.P = P
        self.es = es
        self.rr = 0

    UID = [0]

    def sb(self, name, shape, dt):
        Ctx.UID[0] += 1
        return self.es.enter_context(self.nc.sbuf_tensor("sb%d_%s" % (Ctx.UID[0], name), list(shape), dt))

    def ps(self, name):
        Ctx.UID[0] += 1
        return self.es.enter_context(self.nc.psum_tensor("ps%d_%s" % (Ctx.UID[0], name), [128, 512], F32))


RSQRT_MODE = ["lnexp"]


def rsqrt_small(P, out_ap, outkey, in_ap, inkey, scale, eps=EPS):
    inkeys = inkey if isinstance(inkey, list) else [inkey]
    P.add("dve", lambda e: e.tensor_scalar(out_ap, in_ap, scale, eps, ALU.mult, ALU.add),
          r=inkeys, w=[outkey])
    if RSQRT_MODE[0] == "sqrt":
        P.add("act", lambda e: e.sqrt(out_ap, out_ap), r=[outkey], w=[outkey])
        P.add("dve", lambda e: e.reciprocal(out_ap, out_ap), r=[outkey], w=[outkey])
    else:
        P.add("act", lambda e: e.activation(out_ap, out_ap, AF.Ln), r=[outkey], w=[outkey])
        P.add("act", lambda e: e.activation(out_ap, out_ap, AF.Exp, scale=-0.5), r=[outkey], w=[outkey])


def rmsnorm_tile(c, h_ap, hkey, xn_ap, xnkey, junk_ap, junkkey, ss_ap, sskey, rstd_ap, rstdkey):
    P = c.P
    P.add("act", lambda e: e.activation(junk_ap, h_ap, AF.Square, accum_out=ss_ap),
          r=[hkey], w=[junkkey, sskey])
    rsqrt_small(P, rstd_ap, rstdkey, ss_ap, sskey, 1.0 / D)
    P.add("dve", lambda e: e.tensor_scalar(xn_ap, h_ap, rstd_ap, None, ALU.mult),
          r=[hkey, rstdkey], w=[xnkey])


_LW = [0]


def load_weight_bf16(c, w_dram, rows, cols, dst, dstname, stage, gcol=None, gkey=None, colchunk=704, gmap=None, jsel=None):
    P = c.P
    nk = rows // 128
    nch = (cols + colchunk - 1) // colchunk
    if gcol is None:
        for k in range(nk):
            P.add("pool", lambda e, k=k: e.dma_start(out=dst[:, k, :], in_=w_dram[k * 128:(k + 1) * 128, :]),
                  w=[_k(dstname, k, j) for j in range(nch)], dma=True)
        return [_k(dstname, k, j) for k in range(nk) for j in range(nch)]
    kj = [(k, j) for k in range(nk) for j in range(nch)] if jsel is None else [(k, j) for j in jsel for k in range(nk)]
    for k, j in kj:
        if True:
            c0 = j * colchunk
            c1 = min(cols, c0 + colchunk)
            cnt = _LW[0]
            _LW[0] += 1
            st, skeys = stage[cnt % len(stage)]
            src = w_dram[k * 128:(k + 1) * 128, c0:c1]
            P.add("sp", lambda e, st=st, src=src, n=c1 - c0: e.dma_start(out=st[:, 0:n], in_=src),
                  w=skeys, dma=True)
            dap = dst[:, k, c0:c1]
            sap = st[:, 0:c1 - c0]
            eng = ("dve", "act")[cnt % 2]
            rk = list(skeys) + ([gkey] if gcol is not None else [])
            if gcol is None:
                if eng == "act":
                    P.add("act", lambda e, dap=dap, sap=sap: e.copy(dap, sap), r=rk, w=[_k(dstname, k, j)])
                else:
                    P.add(eng, lambda e, dap=dap, sap=sap: e.tensor_copy(dap, sap), r=rk, w=[_k(dstname, k, j)])
            else:
                kk_ = gmap(k) if gmap is not None else k
                g = gcol[:, kk_:kk_ + 1]
                if eng == "act":
                    P.add("act", lambda e, dap=dap, sap=sap, g=g: e.activation(dap, sap, AF.Copy, scale=g),
                          r=rk, w=[_k(dstname, k, j)])
                else:
                    P.add(eng, lambda e, dap=dap, sap=sap, g=g: e.tensor_scalar(dap, sap, g, None, ALU.mult),
                          r=rk, w=[_k(dstname, k, j)])
    return [_k(dstname, k, j) for k in range(nk) for j in range(nch)]


def wkeys(dstname, k, c0, c1, colchunk=704):
    return [_k(dstname, k, j) for j in range(c0 // colchunk, (c1 - 1) // colchunk + 1)]


def ffn_phase(nc, h_in, h_out, g_dram, wg_d, wu_d, wd_d, ident_d, final_g=None):
    with ExitStack() as es:
        P = Prog(nc)
        c = Ctx(nc, P, es)
        CC = 704
        wg = c.sb("wg", [128, 8, DFF], BF16)
        wu = c.sb("wu", [128, 8, DFF], BF16)
        wd = c.sb("wd", [128, 22, D], BF16)
        gcol = c.sb("gcol", [128, 8], F32)
        ident = c.sb("ident", [128, 128], BF16)
        xn = [c.sb("xn%d" % i, [128, D], BF16) for i in range(2)]
        xnT = [c.sb("xnT%d" % i, [128, 8, 512], BF16) for i in range(2)]
        actT = c.sb("actT", [128, 22, 512], BF16)
        actf = actT[:].rearrange("p f t -> p (f t)").bitcast(F32)
        stage = []
        for si in range(7):
            stage.append((actf[:, si * 768:si * 768 + CC], [_k("actT", f) for f in range(3 * si, 3 * si + 3)]))
        sg = [c.sb("sg%d" % i, [128, 512], F32) for i in range(2)]
        junk = c.sb("junk", [128, D], BF16)
        ss = c.sb("ss", [128, 8], F32)
        rstd = c.sb("rstd", [128, 8], F32)
        if final_g is not None:
            fg = c.sb("fg", [128, D], F32)
        psum = [c.ps("ps%d" % i) for i in range(8)]

        P.add("sp", lambda e: e.dma_start(out=gcol[:], in_=g_dram.rearrange("(k p) -> p k", p=128),
                                          allow_slow_non_contiguous=True), w=[_k("gcol")], dma=True)
        P.add("sp", lambda e: e.dma_start(out=ident[:], in_=ident_d), w=[_k("ident")], dma=True)
        if final_g is not None:
            P.add("sp", lambda e: e.dma_start(out=fg[:], in_=final_g.partition_broadcast(128)),
                  w=[_k("fg")], dma=True)
        for jj in range((DFF + CC - 1) // CC):
            load_weight_bf16(c, wg_d, D, DFF, wg, "wg", stage, gcol, _k("gcol"), CC, jsel=[jj])
            load_weight_bf16(c, wu_d, D, DFF, wu, "wu", stage, gcol, _k("gcol"), CC, jsel=[jj])
        load_weight_bf16(c, wd_d, DFF, D, wd, "wd", stage, None, None, CC)

        nsup = T // 512
        hA = [c.sb("hA%d" % i, [128, D], F32) for i in range(2)]
        hC = [c.sb("hC%d" % i, [128, D], F32) for i in range(2)]
        cnt = {"t": 0}

        def front_norm(s, j):
            t0 = s * 512 + j * 128
            b = (s * 4 + j) % 2
            hap = hA[b][:]
            hk = _k("hA", b)
            P.add("sp", lambda e, hap=hap, t0=t0: e.dma_start(out=hap, in_=h_in[t0:t0 + 128, :]), w=[hk], dma=True)
            rmsnorm_tile(c, hap, hk, xn[b][:], _k("xn", b), junk[:], _k("junk"),
                         ss[:, j:j + 1], _k("ss", j), rstd[:, j:j + 1], _k("rstd", j))

        def front_tr(s, j):
            b = (s * 4 + j) % 2
            xb = s % 2
            pb = b
            pst = psum[pb][:].bitcast(BF16)

            def tr(e, b=b, pst=pst):
                inst = None
                for cc in range(8):
                    inst = e.transpose(pst[:, cc * 128:(cc + 1) * 128], xn[b][:, cc * 128:(cc + 1) * 128], ident[:])
                return inst
            P.add("pe", tr, r=[_k("xn", b), _k("ident")], w=[_k("ps", pb)])
            dst = xnT[xb][:, :, j * 128:(j + 1) * 128]
            src = pst[:, 0:1024].rearrange("p (c t) -> p c t", c=8)
            P.add("act", lambda e, dst=dst, src=src: e.copy(dst, src), r=[_k("ps", pb)], w=[_k("xnT", xb, j)])

        for j in range(4):
            front_norm(0, j)
            front_tr(0, j)
        for s in range(nsup):
            xb = s % 2
            xT = xnT[xb]
            xk = [_k("xnT", xb, j) for j in range(4)]
            for f in range(22):
                pg = 2 + (f % 2)
                pu = 4 + (f % 2)

                def mm(e, w_, pi, f=f, xT=xT):
                    inst = None
                    for k in range(8):
                        inst = e.matmul(psum[pi][:], w_[:, k, f * 128:(f + 1) * 128], xT[:, k, :],
                                        start=(k == 0), stop=(k == 7))
                    return inst
                wkg = [kk for k in range(8) for kk in wkeys("wg", k, f * 128, (f + 1) * 128, CC)]
                wku = [kk for k in range(8) for kk in wkeys("wu", k, f * 128, (f + 1) * 128, CC)]
                P.add("pe", lambda e, pg=pg, mm=mm: mm(e, wg, pg), r=xk + wkg, w=[_k("ps", pg)])
                P.add("pe", lambda e, pu=pu, mm=mm: mm(e, wu, pu), r=xk + wku, w=[_k("ps", pu)])
                sb_ = f % 2
                P.add("act", lambda e, sb_=sb_, pg=pg: e.activation(sg[sb_][:], psum[pg][:], AF.Silu),
                      r=[_k("ps", pg)], w=[_k("sg", sb_)])
                P.add("dve", lambda e, sb_=sb_, pu=pu, f=f: e.tensor_tensor(actT[:, f, :], sg[sb_][:], psum[pu][:], ALU.mult),
                      r=[_k("sg", sb_), _k("ps", pu)], w=[_k("actT", f)])
            ak = [_k("actT", f) for f in range(22)]
            wdk = [kk for f in range(22) for kk in wkeys("wd", f, 0, D, CC)]
            for j in range(4):
                t0 = s * 512 + j * 128
                hb = (s * 4 + j) % 2
                hap = hC[hb][:]
                hk = _k("hC", hb)
                P.add("sp", lambda e, hap=hap, t0=t0: e.dma_start(out=hap, in_=h_in[t0:t0 + 128, :]), w=[hk], dma=True)
                if s + 1 < nsup:
                    front_norm(s + 1, j)
                for mh in range(2):
                    py = 6 + mh

                    def mmd(e, j=j, mh=mh, py=py):
                        inst = None
                        for f in range(22):
                            inst = e.matmul(psum[py][:], actT[:, f, j * 128:(j + 1) * 128],
                                            wd[:, f, mh * 512:(mh + 1) * 512], start=(f == 0), stop=(f == 21))
                        return inst
                    P.add("pe", mmd, r=ak + wdk, w=[_k("ps", py)])
                    hs = hC[hb][:, mh * 512:(mh + 1) * 512]
                    P.add("dve", lambda e, hs=hs, py=py: e.tensor_tensor(hs, hs, psum[py][:], ALU.add),
                          r=[_k("ps", py), hk], w=[hk])
                if s + 1 < nsup:
                    front_tr(s + 1, j)
                if final_g is not None:
                    sj = 4 + j
                    P.add("act", lambda e, hap=hap, sj=sj: e.activation(junk[:], hap, AF.Square, accum_out=ss[:, sj:sj + 1]),
                          r=[hk], w=[_k("junk"), _k("ss", sj)])
                    rsqrt_small(P, rstd[:, sj:sj + 1], _k("rstd", sj), ss[:, sj:sj + 1], _k("ss", sj), 1.0 / D)
                    P.add("dve", lambda e, hap=hap, sj=sj: e.scalar_tensor_tensor(hap, hap, rstd[:, sj:sj + 1], fg[:], ALU.mult, ALU.mult),
                          r=[hk, _k("rstd", sj), _k("fg")], w=[hk])
                P.add("sp", lambda e, hap=hap, t0=t0: e.dma_start(out=h_out[t0:t0 + 128, :], in_=hap),
                      r=[hk], dma=True)
        P.emit(es)


RET_GAMMA = [1.0 - 2.0 ** (-5.0 - h) for h in range(4)]


DBG = {"ntiles": NT, "stage": 99}


def mix1_phase(nc, h_in, h_out, g_dram, win_d, retnorm_d, wout_d, ident_d, cos_d, sin_d, dt_d, xi8_d, zeta_d):
    RSQRT_MODE[0] = "sqrt"
    with ExitStack() as es:
        P = Prog(nc)
        c = Ctx(nc, P, es)
        CC = 512
        win = c.sb("win", [128, 8, ODD_IN], BF16)
        wout = c.sb("wout", [128, 16, D], BF16)
        gcol = c.sb("gcol", [128, 8], F32)
        rcol = c.sb("rcol", [128, 4], F32)
        ident = c.sb("ident", [128, 128], BF16)
        S = c.sb("S", [128, 2, 4, 512], F32)
        Sb = c.sb("Sb", [128, 2, 4, 512], BF16)
        hbufs = [c.sb("hbuf%d" % i, [128, D], F32) for i in range(2)]
        xn = c.sb("xn", [128, D], BF16)
        xnT = c.sb("xnT", [128, 8, 128], BF16)
        tmp = [c.sb("tmp%d" % i, [128, 2, 128], F32) for i in range(4)]
        qrot = c.sb("qrot", [128, D], BF16)
        krot = c.sb("krot", [128, D], BF16)
        kz = c.sb("kz", [128, D], BF16)
        qT = c.sb("qT", [128, 8, 128], BF16)
        qxiT = c.sb("qxiT", [128, 8, 128], BF16)
        kT = c.sb("kT", [128, 8, 128], BF16)
        vb = c.sb("vb", [128, 2048], BF16)
        gs = c.sb("gs", [128, 2048], BF16)
        atm = c.sb("atm", [128, 4, 128], BF16)
        og = c.sb("og", [128, 2048], BF16)
        ogT = c.sb("ogT", [128, 16, 128], BF16)
        junk = c.sb("junk", [128, 512], BF16)
        cos_t = c.sb("cos", [128, 128], F32)
        sin_t = c.sb("sin", [128, 128], F32)
        dtm = c.sb("dtm", [128, 4, 128], F32)
        xi8 = c.sb("xi8", [128, 8, 128], F32)
        zeta = c.sb("zeta", [128, 4], F32)
        ss = c.sb("ss", [128, 8], F32)
        rstd = c.sb("rstd", [128, 8], F32)
        psum = [c.ps("ps%d" % i) for i in range(8)]

        def dma(out_ap, in_ap, wk, **kw):
            P.add("sp", lambda e: e.dma_start(out=out_ap, in_=in_ap, **kw), w=[wk], dma=True)

        dma(gcol[:], g_dram.rearrange("(k p) -> p k", p=128), _k("gcol"), allow_slow_non_contiguous=True)
        dma(rcol[:], retnorm_d.rearrange("(k p) -> p k", p=128), _k("rcol"), allow_slow_non_contiguous=True)
        dma(ident[:], ident_d, _k("ident"))
        dma(dtm[:], dt_d, _k("dtm"))
        dma(xi8[:], xi8_d, _k("xi8"))
        dma(zeta[:], zeta_d, _k("zeta"))
        P.add("dve", lambda e: e.memset(S[:], 0.0), w=[_k("S", cc, h) for cc in range(2) for h in range(4)])
        P.add("pool", lambda e: e.memset(Sb[:], 0.0), w=[_k("Sb", cc, h) for cc in range(2) for h in range(4)])
        ogf = og[:].bitcast(F32)
        ogTf = ogT[:].rearrange("p c t -> p (c t)").bitcast(F32)
        vbf = vb[:].bitcast(F32)
        gsf = gs[:].bitcast(F32)
        stage = [(vbf[:, 0:512], [_k("vb", 0), _k("vb", 1)]), (vbf[:, 512:1024], [_k("vb", 2), _k("vb", 3)]),
                 (gsf[:, 0:512], [_k("gs", 0), _k("gs", 1)]), (gsf[:, 512:1024], [_k("gs", 2), _k("gs", 3)]),
                 (ogf[:, 0:512], [_k("og", 0), _k("og", 1)]), (ogf[:, 512:1024], [_k("og", 2), _k("og", 3)]),
                 (ogTf[:, 0:512], [_k("ogT", 0)]), (ogTf[:, 512:1024], [_k("ogT", 1)])]
        load_weight_bf16(c, win_d, D, ODD_IN, win, "win", stage, gcol, _k("gcol"), CC)
        load_weight_bf16(c, wout_d, 2048, D, wout, "wout", stage, rcol, _k("rcol"), CC, gmap=lambda k: k % 4)

        def bfv(i):
            return psum[i][:].bitcast(BF16)

        dma(hbufs[0][:], h_in[0:128, :], _k("hbuf", 0))
        for i in range(DBG["ntiles"]):
            t0 = i * 128
            hbuf = hbufs[i % 2]
            hk = _k("hbuf", i % 2)
            if i + 1 < DBG["ntiles"]:
                dma(hbufs[(i + 1) % 2][:], h_in[t0 + 128:t0 + 256, :], _k("hbuf", (i + 1) % 2))
            dma(cos_t[:], cos_d[t0:t0 + 128, :], _k("cos"))
            dma(sin_t[:], sin_d[t0:t0 + 128, :], _k("sin"))
            rmsnorm_tile(c, hbuf[:], hk, xn[:], _k("xn"), ogT[:, 0:8, :].rearrange("p c t -> p (c t)"), _k("ogT", 0),
                         ss[:, 4:5], _k("ss", 4), rstd[:, 4:5], _k("rstd", 4))

            def tr8(e, src, pb):
                inst = None
                v = bfv(pb)
                for cc in range(8):
                    inst = e.transpose(v[:, cc * 128:(cc + 1) * 128], src[:, cc * 128:(cc + 1) * 128], ident[:])
                return inst
            P.add("pe", lambda e: tr8(e, xn, 0), r=[_k("xn"), _k("ident")], w=[_k("ps", 0)])
            P.add("act", lambda e: e.copy(xnT[:].rearrange("p c t -> p (c t)"), bfv(0)), r=[_k("ps", 0)], w=[_k("xnT")])
            for g in range(12 if DBG["stage"] >= 2 else 0):
                pb = (4 + g) if g < 4 else 1 + (g % 2)

                def mm(e, g=g, pb=pb):
                    inst = None
                    for k in range(8):
                        inst = e.matmul(psum[pb][:], xnT[:, k, :], win[:, k, g * 512:(g + 1) * 512],
                                        start=(k == 0), stop=(k == 7))
                    return inst
                wk_ = [_k("win", k, g) for k in range(8)]
                P.add("pe", mm, r=[_k("xnT")] + wk_, w=[_k("ps", pb)])
                if g < 4:
                    dstt = qrot if g < 2 else krot
                    dname = "qrot" if g < 2 else "krot"
                    hh = (g % 2) * 2
                    X = psum[pb][:].rearrange("p (h x d) -> p h x d", h=2, x=2)
                    X1 = X[:, :, 0, :]
                    X2 = X[:, :, 1, :]
                    Dv = dstt[:, hh * 256:(hh + 2) * 256].rearrange("p (h x d) -> p h x d", h=2, x=2)
                    cb = cos_t[:].unsqueeze(1).to_broadcast([128, 2, 128])
                    sb_ = sin_t[:].unsqueeze(1).to_broadcast([128, 2, 128])
                    pk = _k("ps", pb)

                    def tt(e, o, a, b, op):
                        return e.tensor_tensor(o, a, b, op)
                    P.add("dve", lambda e, X1=X1, cb=cb: tt(e, tmp[0][:], X1, cb, ALU.mult), r=[pk, _k("cos")], w=[_k("tmp", 0)])
                    P.add("dve", lambda e, X2=X2, sb_=sb_: tt(e, tmp[1][:], X2, sb_, ALU.mult), r=[pk, _k("sin")], w=[_k("tmp", 1)])
                    P.add("dve", lambda e, X2=X2, cb=cb: tt(e, tmp[2][:], X2, cb, ALU.mult), r=[pk, _k("cos")], w=[_k("tmp", 2)])
                    P.add("dve", lambda e, X1=X1, sb_=sb_: tt(e, tmp[3][:], X1, sb_, ALU.mult), r=[pk, _k("sin")], w=[_k("tmp", 3)])
                    P.add("dve", lambda e, Dv=Dv: tt(e, Dv[:, :, 0, :], tmp[0][:], tmp[1][:], ALU.subtract),
                          r=[_k("tmp", 0), _k("tmp", 1)], w=[_k(dname, g % 2)])
                    P.add("dve", lambda e, Dv=Dv: tt(e, Dv[:, :, 1, :], tmp[2][:], tmp[3][:], ALU.add),
                          r=[_k("tmp", 2), _k("tmp", 3)], w=[_k(dname, g % 2)])
                elif g < 8:
                    vv = g - 4
                    P.add("act", lambda e, vv=vv, pb=pb: e.copy(vb[:, vv * 512:(vv + 1) * 512], psum[pb][:]),
                          r=[_k("ps", pb)], w=[_k("vb", vv)])
                else:
                    vv = g - 8
                    P.add("act", lambda e, vv=vv, pb=pb: e.activation(gs[:, vv * 512:(vv + 1) * 512], psum[pb][:], AF.Silu),
                          r=[_k("ps", pb)], w=[_k("gs", vv)])
            if DBG["stage"] < 3:
                P.add("sp", lambda e, t0=t0, hbuf=hbuf: e.dma_start(out=h_out[t0:t0 + 128, :], in_=hbuf[:]), r=[hk], dma=True)
                continue
            P.add("pool", lambda e: e.tensor_tensor(kz[:].rearrange("p (h d) -> p h d", h=4),
                                                    krot[:].rearrange("p (h d) -> p h d", h=4),
                                                    zeta[:].unsqueeze(2).to_broadcast([128, 4, 256]), ALU.mult),
                  r=[_k("krot", 0), _k("krot", 1), _k("zeta")], w=[_k("kz")])
            P.add("pe", lambda e: tr8(e, qrot, 0), r=[_k("qrot", 0), _k("qrot", 1), _k("ident")], w=[_k("ps", 0)])
            P.add("act", lambda e: e.copy(qT[:].rearrange("p c t -> p (c t)"), bfv(0)), r=[_k("ps", 0)], w=[_k("qT")])
            P.add("dve", lambda e: e.tensor_tensor(qxiT[:].rearrange("p c t -> p (c t)"), bfv(0),
                                                   xi8[:].rearrange("p c t -> p (c t)"), ALU.mult),
                  r=[_k("ps", 0), _k("xi8")], w=[_k("qxiT")])
            P.add("pe", lambda e: tr8(e, krot, 3), r=[_k("krot", 0), _k("krot", 1), _k("ident")], w=[_k("ps", 3)])
            P.add("act", lambda e: e.copy(kT[:].rearrange("p c t -> p (c t)"), bfv(3)), r=[_k("ps", 3)], w=[_k("kT")])

            if DBG["stage"] < 4:
                P.add("sp", lambda e, t0=t0, hbuf=hbuf: e.dma_start(out=h_out[t0:t0 + 128, :], in_=hbuf[:]), r=[hk], dma=True)
                continue
            def amm(e):
                inst = None
                for h in range(4):
                    for cc in range(2):
                        inst = e.matmul(psum[0][:, h * 128:(h + 1) * 128], kT[:, 2 * h + cc, :], qT[:, 2 * h + cc, :],
                                        start=(cc == 0), stop=(cc == 1))
                return inst
            P.add("pe", amm, r=[_k("kT"), _k("qT")], w=[_k("ps", 0)])
            P.add("dve", lambda e: e.tensor_tensor(atm[:].rearrange("p h t -> p (h t)"), psum[0][:],
                                                   dtm[:].rearrange("p h t -> p (h t)"), ALU.mult),
                  r=[_k("ps", 0), _k("dtm")], w=[_k("atm")])
            if DBG["stage"] < 5:
                P.add("sp", lambda e, t0=t0, hbuf=hbuf: e.dma_start(out=h_out[t0:t0 + 128, :], in_=hbuf[:]), r=[hk], dma=True)
                continue
            for h in range(4):
                def omm(e, h=h):
                    e.matmul(psum[4 + h][:], atm[:, h, :], vb[:, h * 512:(h + 1) * 512], start=True, stop=False)
                    e.matmul(psum[4 + h][:], qxiT[:, 2 * h, :], Sb[:, 0, h, :], start=False, stop=False)
                    return e.matmul(psum[4 + h][:], qxiT[:, 2 * h + 1, :], Sb[:, 1, h, :], start=False, stop=True)
                P.add("pe", omm, r=[_k("atm"), _k("vb", h), _k("qxiT"), _k("Sb", 0, h), _k("Sb", 1, h)], w=[_k("ps", 4 + h)])
                P.add("act", lambda e, h=h: e.activation(junk[:], psum[4 + h][:], AF.Square, accum_out=ss[:, h:h + 1]),
                      r=[_k("ps", 4 + h)], w=[_k("junk"), _k("ss", h)])
            if DBG["stage"] < 7:
                P.add("sp", lambda e, t0=t0, hbuf=hbuf: e.dma_start(out=h_out[t0:t0 + 128, :], in_=hbuf[:]), r=[hk], dma=True)
                continue
            rsqrt_small(P, rstd[:, 0:4], _k("rstd", 0), ss[:, 0:4], [_k("ss", h) for h in range(4)], 1.0 / 512)
            for h in range(4):
                P.add("dve", lambda e, h=h: e.scalar_tensor_tensor(og[:, h * 512:(h + 1) * 512], psum[4 + h][:], rstd[:, h:h + 1],
                                                                    gs[:, h * 512:(h + 1) * 512], ALU.mult, ALU.mult),
                      r=[_k("ps", 4 + h), _k("rstd", 0), _k("gs", h)], w=[_k("og", h)])
            for half in range(2):
                pb = 0 if half == 0 else 3

                def tro(e, half=half, pb=pb):
                    inst = None
                    v = bfv(pb)
                    for cc in range(8):
                        c2 = half * 8 + cc
                        inst = e.transpose(v[:, cc * 128:(cc + 1) * 128], og[:, c2 * 128:(c2 + 1) * 128], ident[:])
                    return inst
                P.add("pe", tro, r=[_k("og", half * 2), _k("og", half * 2 + 1), _k("ident")], w=[_k("ps", pb)])
                P.add("act", lambda e, half=half, pb=pb: e.copy(ogT[:, half * 8:(half + 1) * 8, :].rearrange("p c t -> p (c t)"), bfv(pb)),
                      r=[_k("ps", pb)], w=[_k("ogT", half)])
            if DBG["stage"] < 6:
                P.add("sp", lambda e, t0=t0, hbuf=hbuf: e.dma_start(out=h_out[t0:t0 + 128, :], in_=hbuf[:]), r=[hk], dma=True)
                continue
            for h in range(4):
                for cc in range(2):
                    pb = 1 + ((h * 2 + cc) % 2)
                    P.add("pe", lambda e, h=h, cc=cc, pb=pb: e.matmul(psum[pb][:], kz[:, h * 256 + cc * 128:h * 256 + (cc + 1) * 128],
                                                                        vb[:, h * 512:(h + 1) * 512], start=True, stop=True),
                          r=[_k("kz"), _k("vb", h)], w=[_k("ps", pb)])
                    dec = RET_GAMMA[h] ** 128
                    P.add("dve", lambda e, h=h, cc=cc, pb=pb, dec=dec: e.scalar_tensor_tensor(S[:, cc, h, :], S[:, cc, h, :], dec, psum[pb][:], ALU.mult, ALU.add),
                          r=[_k("S", cc, h), _k("ps", pb)], w=[_k("S", cc, h)])
                    P.add("pool", lambda e, h=h, cc=cc: e.tensor_copy(Sb[:, cc, h, :], S[:, cc, h, :]),
                          r=[_k("S", cc, h)], w=[_k("Sb", cc, h)])
            for mh in range(2):
                pb = 1 + mh

                def ymm(e, mh=mh, pb=pb):
                    inst = None
                    for cc in range(16):
                        inst = e.matmul(psum[pb][:], ogT[:, cc, :], wout[:, cc, mh * 512:(mh + 1) * 512],
                                        start=(cc == 0), stop=(cc == 15))
                    return inst
                P.add("pe", ymm, r=[_k("ogT", 0), _k("ogT", 1)] + [_k("wout", cc, jj) for cc in range(16) for jj in range(2)],
                      w=[_k("ps", pb)])
                P.add("dve", lambda e, mh=mh, pb=pb, hbuf=hbuf: e.tensor_tensor(hbuf[:, mh * 512:(mh + 1) * 512], hbuf[:, mh * 512:(mh + 1) * 512],
                                                                      psum[pb][:], ALU.add),
                      r=[_k("ps", pb), hk], w=[hk])
            P.add("sp", lambda e, t0=t0, hbuf=hbuf: e.dma_start(out=h_out[t0:t0 + 128, :], in_=hbuf[:]), r=[hk], dma=True)
        P.emit(es)


MASK_BIG = 30000.0
N_IT = 22
TOPK = 256
IDX_C0 = (64.0 ** -0.5) * (8.0 ** -0.5)
ACT_BISECT_EVERY = 10 ** 9


def _interleave(gens):
    st_ = [[g, max(1, n), 0] for g, n in gens]
    while st_:
        st_.sort(key=lambda x: x[2] / x[1])
        g = st_[0]
        try:
            next(g[0])
            g[2] += 1
        except StopIteration:
            st_.pop(0)


def mix0_phase(nc, h_in, h_out, I, C):
    RSQRT_MODE[0] = "lnexp"
    with ExitStack() as es:
        P = Prog(nc)
        c = Ctx(nc, P, es)
        CC = 512
        win = c.sb("win", [128, 8, EVEN_IN], BF16)
        wout = c.sb("wout", [128, 8, D], BF16)
        stage = [c.sb("stage%d" % i, [128, CC], F32) for i in range(2)]
        gcol = c.sb("gcol", [128, 8], F32)
        gcol2 = c.sb("gcol2", [128, 8], F32)
        ident = c.sb("ident", [128, 128], BF16)
        identf = c.sb("identf", [128, 128], F32)
        ones = c.sb("ones", [128, 128], BF16)
        ident4 = c.sb("ident4", [128, 512], BF16)
        trim = c.sb("trim", [128, 128], F32)
        trir = c.sb("trir", [128, 128], F32)
        m01 = c.sb("m01", [128, 4, 128], F32)
        negm = c.sb("negm", [128, 128], F32)
        pw = c.sb("pw", [128, N_IT + 1], F32)
        wa2 = c.sb("wa2", [16, 256], F32)
        ba2 = c.sb("ba2", [128, 256], F32)
        hbufA = c.sb("hbufA", [128, D], F32)
        hbufC = c.sb("hbufC", [128, D], F32)
        xn = c.sb("xn", [128, D], BF16)
        xnT = c.sb("xnT", [128, 8, 128], BF16)
        junk = c.sb("junk", [128, D], BF16)
        gqk = c.sb("gqk", [128, 512], BF16)
        gv = c.sb("gv", [128, 512], BF16)
        gsl = c.sb("gsl", [128, 512], BF16)
        dq = c.sb("dq", [128, 512], BF16)
        dkb = c.sb("dkb", [128, 128], BF16)
        iq = c.sb("iq", [128, 512], BF16)
        ikb = c.sb("ikb", [128, 64], BF16)
        iw = c.sb("iw", [128, 8], F32)
        wabs = [c.sb("wabs%d" % i, [128, 8], F32) for i in range(2)]
        sgn = c.sb("sgn", [128, 8], F32)
        Dsg = [c.sb("Dsg%d" % i, [128, 8, 128], BF16) for i in range(2)]
        gaT = c.sb("gaT", [16, 128], F32)
        zb = c.sb("zb", [128, 256], F32)
        sp_ = c.sb("sp", [128, 256], F32)
        ecum = c.sb("ecum", [64, 4, 128], F32)
        encum = c.sb("encum", [64, 4, 128], F32)
        erev = c.sb("erev", [128, 256], F32)
        qtT = c.sb("qtT", [64, 4, 128], BF16)
        ktT = c.sb("ktT", [64, 4, 128], BF16)
        kend = c.sb("kend", [128, 256], BF16)
        atm = c.sb("atm", [128, 4, 128], BF16)
        Sg = c.sb("Sg", [64, 4, 128], F32)
        Sgb = c.sb("Sgb", [64, 4, 128], BF16)
        og = c.sb("og", [128, 512], BF16)
        ogT = [c.sb("ogT%d" % i, [128, 4, 128], BF16) for i in range(4)]
        dsT = c.sb("dsT", [128, 4, 128], BF16)
        qT = [c.sb("qT%d" % i, [128, 4, 128], BF16) for i in range(4)]
        kTc = c.sb("kTc", [128, T], BF16)
        vc = c.sb("vc", [128, NT, 128], BF16)
        ikTc = c.sb("ikTc", [64, T], BF16)
        iqT = [c.sb("iqT%d" % i, [64, 8, 128], BF16) for i in range(2)]
        scb = [c.sb("sc%d" % i, [128, T], F32) for i in range(2)]
        mskb = [c.sb("msk%d" % i, [128, T], BF16) for i in range(2)]
        junkb = c.sb("junkb", [128, T], mybir.dt.int8)
        Rb = [c.sb("Rb%d" % i, [128, 512], BF16) for i in range(2)]
        Eb = [c.sb("Eb%d" % i, [128, 512], BF16) for i in range(2)]
        rden = c.sb("rden", [128, 512], F32)
        ss = c.sb("ss", [128, 8], F32)
        rstd = c.sb("rstd", [128, 8], F32)
        st = c.sb("st", [128, 8], F32)
        hst = c.sb("hst", [128, N_IT + 1], F32)
        nhst = c.sb("nhst", [128, N_IT], F32)
        hhst = c.sb("hhst", [128, N_IT], F32)
        psum = [c.ps("ps%d" % i) for i in range(8)]

        def dma(out_ap, in_ap, wk, **kw):
            P.add("sp", lambda e: e.dma_start(out=out_ap, in_=in_ap, **kw), w=[wk], dma=True)

        def bfv(i):
            return psum[i][:].bitcast(BF16)

        dma(gcol[:], I["even_attn_norm"][0].rearrange("(k p) -> p k", p=128), _k("gcol"), allow_slow_non_contiguous=True)
        P.add("dve", lambda e: e.memset(gcol2[:], 1.0), w=[_k("gcol2")])
        for cc in range(4):
            dma(gcol2[:, cc:cc + 1], I["even_gla_norm"][0].rearrange("(p o) -> p o", o=1), _k("gcol2"))
        dma(ident[:], C["ident"], _k("ident"))
        dma(identf[:], C["identf"], _k("identf"))
        dma(ones[:], C["ones"], _k("ones"))
        dma(ident4[:], C["ident4"], _k("ident4"))
        dma(trim[:], C["gla_trim"], _k("trim"))
        dma(trir[:], C["gla_trir"], _k("trir"))
        dma(m01[:], C["gla_m01"], _k("m01"))
        dma(negm[:], C["dsa_negm"], _k("negm"))
        dma(pw[:], C["dsa_pw"], _k("pw"))
        dma(wa2[:], I["even_gla_wa2"][0], _k("wa2"))
        dma(ba2[:], I["even_gla_ba2"][0].partition_broadcast(128), _k("ba2"))
        P.add("dve", lambda e: e.memset(Sg[:], 0.0), w=[_k("Sg")])
        P.add("pool", lambda e: e.memset(Sgb[:], 0.0), w=[_k("Sgb")])
        stage = [(stage[0][:], [_k("stage", 0)]), (stage[1][:], [_k("stage", 1)])] + \
                [(scb[1][:, si * 512:(si + 1) * 512], [_k("stg", si)]) for si in range(8)]
        load_weight_bf16(c, I["even_w_in"][0], D, EVEN_IN, win, "win", stage, gcol, _k("gcol"), CC)
        load_weight_bf16(c, I["even_w_out"][0], D, D, wout, "wout", stage, gcol2, _k("gcol2"), CC)
        P.add("dve", lambda e: e.memset(st[:, 7:8], 0.0), w=[_k("stg", si) for si in range(8)] + [_k("sc", 1), _k("st", 7)])
        WIN_ALL = [_k("win", k, j) for k in range(8) for j in range(6)]
        WOUT_ALL = [_k("wout", k, j) for k in range(8) for j in range(2)]

        def stageA(i):
            t0 = i * 128
            p2 = i % 2
            p3 = i % 4
            hb = hbufA
            hk = _k("hbufA")
            dma(hb[:], h_in[t0:t0 + 128, :], hk)
            rmsnorm_tile(c, hb[:], hk, xn[:], _k("xn"), junk[:], _k("junk"),
                         ss[:, 4:5], _k("ss", 4), rstd[:, 4:5], _k("rstd", 4))

            def tr8(e):
                inst = None
                v = bfv(0)
                for cc in range(8):
                    inst = e.transpose(v[:, cc * 128:(cc + 1) * 128], xn[:, cc * 128:(cc + 1) * 128], ident[:])
                return inst
            P.add("pe", tr8, r=[_k("xn"), _k("ident")], w=[_k("ps", 0)])
            P.add("act", lambda e: e.copy(xnT[:].rearrange("p c t -> p (c t)"), bfv(0)), r=[_k("ps", 0)], w=[_k("xnT")])
            yield

            def proj(c0, c1, pb):
                def f(e):
                    inst = None
                    for k in range(8):
                        inst = e.matmul(psum[pb][:, 0:(c1 - c0)], xnT[:, k, :], win[:, k, c0:c1], start=(k == 0), stop=(k == 7))
                    return inst
                P.add("pe", f, r=[_k("xnT")] + WIN_ALL, w=[_k("ps", pb)])

            proj(2320, 2832, 1)
            P.add("dve", lambda e: e.tensor_copy(iq[:], psum[1][:]), r=[_k("ps", 1)], w=[_k("iq")])
            yield
            proj(2832, 2904, 2)
            P.add("act", lambda e: e.copy(ikb[:], psum[2][:, 0:64]), r=[_k("ps", 2)], w=[_k("ikb")])
            P.add("act", lambda e: e.copy(iw[:], psum[2][:, 64:72]), r=[_k("ps", 2)], w=[_k("iw")])
            yield
            P.add("act", lambda e: e.activation(wabs[p2][:], iw[:], AF.Abs, scale=IDX_C0), r=[_k("iw")], w=[_k("wabs", p2)])
            P.add("act", lambda e: e.sign(sgn[:], iw[:]), r=[_k("iw")], w=[_k("sgn")])
            P.add("dve", lambda e: e.tensor_tensor(Dsg[p2][:], identf[:].unsqueeze(1).to_broadcast([128, 8, 128]),
                                                   sgn[:].unsqueeze(2).to_broadcast([128, 8, 128]), ALU.mult),
                  r=[_k("identf"), _k("sgn")], w=[_k("Dsg", p2)])

            def triq(e):
                inst = None
                v = bfv(0)
                for hh in range(8):
                    inst = e.transpose(v[0:64, hh * 128:(hh + 1) * 128], iq[:, hh * 64:(hh + 1) * 64], ident[:])
                return inst
            P.add("pe", triq, r=[_k("iq"), _k("ident")], w=[_k("ps", 0)])
            P.add("act", lambda e: e.copy(iqT[p2][:].rearrange("p c t -> p (c t)"), bfv(0)[0:64, :]), r=[_k("ps", 0)], w=[_k("iqT", p2)])
            yield
            P.add("pe", lambda e: e.transpose(bfv(0)[0:64, 0:128], ikb[:], ident[:]), r=[_k("ikb"), _k("ident")], w=[_k("ps", 0)])
            P.add("act", lambda e: e.copy(ikTc[:, t0:t0 + 128], bfv(0)[0:64, 0:128]), r=[_k("ps", 0)], w=[_k("ikTc", i)])
            yield
            proj(1552, 2064, 1)
            P.add("dve", lambda e: e.tensor_copy(dq[:], psum[1][:]), r=[_k("ps", 1)], w=[_k("dq")])
            yield
            proj(2064, 2320, 2)
            P.add("act", lambda e: e.copy(dkb[:], psum[2][:, 0:128]), r=[_k("ps", 2)], w=[_k("dkb")])
            P.add("act", lambda e: e.copy(vc[:, i, :], psum[2][:, 128:256]), r=[_k("ps", 2)], w=[_k("vc", i)])
            yield

            def trdq(e):
                inst = None
                v = bfv(0)
                for cc in range(4):
                    inst = e.transpose(v[:, cc * 128:(cc + 1) * 128], dq[:, cc * 128:(cc + 1) * 128], ident[:])
                inst = e.transpose(v[:, 512:640], dkb[:], ident[:])
                return inst
            P.add("pe", trdq, r=[_k("dq"), _k("dkb"), _k("ident")], w=[_k("ps", 0)])
            P.add("act", lambda e: e.copy(qT[p3][:].rearrange("p c t -> p (c t)"), bfv(0)[:, 0:512]), r=[_k("ps", 0)], w=[_k("qT", p3)])
            P.add("act", lambda e: e.copy(kTc[:, t0:t0 + 128], bfv(0)[:, 512:640]), r=[_k("ps", 0)], w=[_k("kTc", i)])
            yield
            proj(0, 512, 1)
            P.add("act", lambda e: e.copy(gqk[:], psum[1][:]), r=[_k("ps", 1)], w=[_k("gqk")])
            yield
            proj(512, 1024, 2)
            P.add("dve", lambda e: e.tensor_copy(gv[:], psum[2][:]), r=[_k("ps", 2)], w=[_k("gv")])
            yield
            proj(1040, 1552, 1)
            P.add("act", lambda e: e.activation(gsl[:], psum[1][:], AF.Silu), r=[_k("ps", 1)], w=[_k("gsl")])
            yield

            def gaf(e):
                inst = None
                for k in range(8):
                    inst = e.matmul(psum[2][0:16, 0:128], win[:, k, 1024:1040], xnT[:, k, :], start=(k == 0), stop=(k == 7))
                return inst
            P.add("pe", gaf, r=[_k("xnT")] + WIN_ALL, w=[_k("ps", 2)])
            P.add("act", lambda e: e.copy(gaT[:], psum[2][0:16, 0:128]), r=[_k("ps", 2)], w=[_k("gaT")])
            yield
            P.add("pe", lambda e: e.matmul(psum[1][:, 0:256], gaT[:], wa2[:], start=True, stop=True),
                  r=[_k("gaT"), _k("wa2")], w=[_k("ps", 1)])
            P.add("dve", lambda e: e.tensor_tensor(zb[:], psum[1][:, 0:256], ba2[:], ALU.add),
                  r=[_k("ps", 1), _k("ba2")], w=[_k("zb")])
            P.add("act", lambda e: e.activation(zb[:], zb[:], AF.Exp, scale=-1.0), r=[_k("zb")], w=[_k("zb")])
            P.add("act", lambda e: e.activation(sp_[:], zb[:], AF.Ln, bias=1.0), r=[_k("zb")], w=[_k("sp")])
            yield

            def cumf(e):
                inst = None
                for h in range(4):
                    inst = e.matmul(psum[2][0:64, h * 128:(h + 1) * 128], sp_[:, h * 64:(h + 1) * 64], trim[:], start=True, stop=True)
                return inst
            P.add("pe", cumf, r=[_k("sp"), _k("trim")], w=[_k("ps", 2)])
            P.add("pe", lambda e: e.matmul(psum[1][:, 0:256], trir[:], sp_[:], start=True, stop=True),
                  r=[_k("sp"), _k("trir")], w=[_k("ps", 1)])
            P.add("act", lambda e: e.activation(ecum[:].rearrange("p h t -> p (h t)"), psum[2][0:64, :], AF.Exp),
                  r=[_k("ps", 2)], w=[_k("ecum")])
            P.add("act", lambda e: e.activation(encum[:].rearrange("p h t -> p (h t)"), psum[2][0:64, :], AF.Exp, scale=-1.0),
                  r=[_k("ps", 2)], w=[_k("encum")])
            P.add("act", lambda e: e.activation(erev[:], psum[1][:, 0:256], AF.Exp), r=[_k("ps", 1)], w=[_k("erev")])
            yield

            def trqk(e):
                inst = None
                v = bfv(0)
                for hh in range(8):
                    inst = e.transpose(v[0:64, hh * 128:(hh + 1) * 128], gqk[:, hh * 64:(hh + 1) * 64], ident[:])
                return inst
            P.add("pe", trqk, r=[_k("gqk"), _k("ident")], w=[_k("ps", 0)])
            P.add("dve", lambda e: e.scalar_tensor_tensor(qtT[:].rearrange("p h t -> p (h t)"), bfv(0)[0:64, 0:512], 0.125,
                                                          ecum[:].rearrange("p h t -> p (h t)"), ALU.mult, ALU.mult),
                  r=[_k("ps", 0), _k("ecum")], w=[_k("qtT")])
            P.add("dve", lambda e: e.tensor_tensor(ktT[:].rearrange("p h t -> p (h t)"), bfv(0)[0:64, 512:1024],
                                                   encum[:].rearrange("p h t -> p (h t)"), ALU.mult),
                  r=[_k("ps", 0), _k("encum")], w=[_k("ktT")])
            P.add("dve", lambda e: e.tensor_tensor(kend[:], gqk[:, 256:512], erev[:], ALU.mult),
                  r=[_k("gqk"), _k("erev")], w=[_k("kend")])
            yield

            def atf(e):
                inst = None
                for h in range(4):
                    inst = e.matmul(psum[2][:, h * 128:(h + 1) * 128], ktT[:, h, :], qtT[:, h, :], start=True, stop=True)
                return inst
            P.add("pe", atf, r=[_k("ktT"), _k("qtT")], w=[_k("ps", 2)])
            P.add("dve", lambda e: e.tensor_tensor(atm[:].rearrange("p h t -> p (h t)"), psum[2][:],
                                                   m01[:].rearrange("p h t -> p (h t)"), ALU.mult),
                  r=[_k("ps", 2), _k("m01")], w=[_k("atm")])
            yield

            def of(e):
                inst = None
                for h in range(4):
                    e.matmul(psum[1][:, h * 128:(h + 1) * 128], atm[:, h, :], gv[:, h * 128:(h + 1) * 128], start=True, stop=False)
                    inst = e.matmul(psum[1][:, h * 128:(h + 1) * 128], qtT[:, h, :], Sgb[:, h, :], start=False, stop=True)
                return inst
            P.add("pe", of, r=[_k("atm"), _k("gv"), _k("qtT"), _k("Sgb")], w=[_k("ps", 1)])

            def dsf(e):
                inst = None
                for h in range(4):
                    inst = e.matmul(psum[2][0:64, h * 128:(h + 1) * 128], kend[:, h * 64:(h + 1) * 64], gv[:, h * 128:(h + 1) * 128],
                                    start=True, stop=True)
                return inst
            P.add("pe", dsf, r=[_k("kend"), _k("gv")], w=[_k("ps", 2)])
            elast = ecum[:, :, 127:128].to_broadcast([64, 4, 128])
            P.add("dve", lambda e: e.tensor_tensor(Sg[:], Sg[:], elast, ALU.mult), r=[_k("Sg"), _k("ecum")], w=[_k("Sg")])
            P.add("dve", lambda e: e.tensor_tensor(Sg[:].rearrange("p h t -> p (h t)"), Sg[:].rearrange("p h t -> p (h t)"),
                                                   psum[2][0:64, :], ALU.add), r=[_k("Sg"), _k("ps", 2)], w=[_k("Sg")])
            P.add("pool", lambda e: e.tensor_copy(Sgb[:], Sg[:]), r=[_k("Sg")], w=[_k("Sgb")])
            yield
            for h in range(4):
                P.add("act", lambda e, h=h: e.activation(junk[:, 0:128], psum[1][:, h * 128:(h + 1) * 128], AF.Square, accum_out=ss[:, h:h + 1]),
                      r=[_k("ps", 1)], w=[_k("junk"), _k("ss", h)])
            rsqrt_small(P, rstd[:, 0:4], _k("rstd", 0), ss[:, 0:4], [_k("ss", h) for h in range(4)], 1.0 / 128)
            yield
            for h in range(4):
                P.add("dve", lambda e, h=h: e.scalar_tensor_tensor(og[:, h * 128:(h + 1) * 128], psum[1][:, h * 128:(h + 1) * 128], rstd[:, h:h + 1],
                                                                    gsl[:, h * 128:(h + 1) * 128], ALU.mult, ALU.mult),
                      r=[_k("ps", 1), _k("rstd", 0), _k("gsl")], w=[_k("og")])
            yield

            def trog(e):
                inst = None
                v = bfv(0)
                for cc in range(4):
                    inst = e.transpose(v[:, cc * 128:(cc + 1) * 128], og[:, cc * 128:(cc + 1) * 128], ident[:])
                return inst
            P.add("pe", trog, r=[_k("og"), _k("ident")], w=[_k("ps", 0)])
            P.add("act", lambda e: e.copy(ogT[p3][:].rearrange("p c t -> p (c t)"), bfv(0)[:, 0:512]),
                  r=[_k("ps", 0)], w=[_k("ogT", p3)])
            yield

        def stageBs(i):
            p2 = i % 2
            sc = scb[p2]
            nkeys = (i + 1) * 128
            ngrp = (nkeys + 511) // 512
            pend = None
            for gk in range(ngrp):
                k0 = gk * 512
                n = min(512, nkeys - k0)
                kk = [_k("ikTc", b) for b in range(k0 // 128, (k0 + n) // 128)]
                for hI in range(8):
                    rb = hI % 2
                    P.add("pe", lambda e, hI=hI, k0=k0, n=n: e.matmul(psum[3][:, 0:n], iqT[p2][:, hI, :], ikTc[:, k0:k0 + n], start=True, stop=True),
                          r=[_k("iqT", p2)] + kk, w=[_k("ps", 3)])
                    if pend is not None:
                        pend()
                        pend = None
                    P.add("act", lambda e, rb=rb, hI=hI, n=n: e.activation(Rb[rb][:, 0:n], psum[3][:, 0:n], AF.Relu, scale=wabs[p2][:, hI:hI + 1]),
                          r=[_k("ps", 3), _k("wabs", p2)], w=[_k("Rb", rb)])

                    def acc(rb=rb, hI=hI, n=n, k0=k0):
                        P.add("pe", lambda e: e.matmul(psum[4][:, 0:n], Dsg[p2][:, hI, :], Rb[rb][:, 0:n], start=(hI == 0), stop=(hI == 7)),
                              r=[_k("Dsg", p2), _k("Rb", rb)], w=[_k("ps", 4)])
                        if hI == 7:
                            P.add("act", lambda e: e.copy(sc[:, k0:k0 + n], psum[4][:, 0:n]), r=[_k("ps", 4)], w=[_k("sc", p2)])
                    pend = acc
                    yield
            if pend is not None:
                pend()
            yield

        def stageBb(i):
            t0 = i * 128
            p2 = i % 2
            sc = scb[p2]
            nkeys = (i + 1) * 128
            scv = sc[:, 0:nkeys]
            P.add("dve", lambda e: e.tensor_reduce(st[:, 0:1], scv, AX.X, ALU.max), r=[_k("sc", p2)], w=[_k("st", 0)])
            yield
            lov = scv if i < 2 else sc[:, 0:TOPK]
            P.add("dve", lambda e: e.tensor_reduce(st[:, 1:2], lov, AX.X, ALU.min), r=[_k("sc", p2)], w=[_k("st", 1)])
            P.add("dve", lambda e: e.tensor_tensor(sc[:, t0:t0 + 128], sc[:, t0:t0 + 128], negm[:], ALU.add),
                  r=[_k("sc", p2), _k("negm")], w=[_k("sc", p2)])
            P.add("dve", lambda e: e.tensor_tensor(st[:, 2:3], st[:, 0:1], st[:, 1:2], ALU.subtract), r=[_k("st", 0), _k("st", 1)], w=[_k("st", 2)])
            P.add("dve", lambda e: e.tensor_scalar(st[:, 2:3], st[:, 2:3], 1.000001, 1e-30, ALU.mult, ALU.add), r=[_k("st", 2)], w=[_k("st", 2)])
            P.add("dve", lambda e: e.tensor_scalar(hst[:], pw[:], st[:, 2:3], None, ALU.mult), r=[_k("st", 2), _k("pw")], w=[_k("hst")])
            yield
            for k in range(N_IT):
                P.add("dve", lambda e, k=k: e.tensor_tensor(st[:, 3:4], st[:, 1:2], hst[:, k:k + 1], ALU.add),
                      r=[_k("st", 1), _k("hst")], w=[_k("st", 3)])
                P.add("dve", lambda e: e.tensor_scalar(junkb[:, 0:nkeys], scv, st[:, 3:4], None, ALU.is_ge, ALU.add, accum_out=st[:, 4:5]),
                      r=[_k("sc", p2), _k("st", 3)], w=[_k("junkb"), _k("st", 4)])
                P.add("dve", lambda e, k=k: e.tensor_scalar(st[:, 5:6], st[:, 4:5], TOPK - 0.5, hst[:, k:k + 1], ALU.is_ge, ALU.mult),
                      r=[_k("st", 4), _k("hst")], w=[_k("st", 5)])
                P.add("dve", lambda e: e.tensor_tensor(st[:, 1:2], st[:, 1:2], st[:, 5:6], ALU.add),
                      r=[_k("st", 1), _k("st", 5)], w=[_k("st", 1)])
                yield
            P.add("dve", lambda e: e.tensor_scalar(mskb[p2][:, 0:nkeys], scv, st[:, 1:2], -MASK_BIG, ALU.is_lt, ALU.mult),
                  r=[_k("sc", p2), _k("st", 1)], w=[_k("msk", p2)])
            yield
            return
            for b0 in range(0, i + 1, 8):
                nb = min(8, i + 1 - b0)

                def trm(e, b0=b0, nb=nb):
                    inst = None
                    v = bfv(3)
                    for bb in range(nb):
                        inst = e.transpose(v[:, bb * 128:(bb + 1) * 128], msk[:, (b0 + bb) * 128:(b0 + bb + 1) * 128], ident[:])
                    return inst
                P.add("pe", trm, r=[_k("msk"), _k("ident")], w=[_k("ps", 3)])
                P.add("act", lambda e, b0=b0, nb=nb: e.copy(mskT[:, b0:b0 + nb, :].rearrange("p c t -> p (c t)"), bfv(3)[:, 0:nb * 128]),
                      r=[_k("ps", 3)], w=[_k("mskT", b0 // 8)])
                yield

        def stageC(i):
            t0 = i * 128
            p3 = i % 4
            p2 = i % 2
            hb = hbufC
            hk = _k("hbufC")
            dma(hb[:], h_in[t0:t0 + 128, :], hk)
            pendc = []
            for j in range(i + 1):
                eb = j % 2

                def stf(e, j=j):
                    e.matmul(psum[5][:], kTc[:, j * 128:(j + 1) * 128], qT[p3][:].rearrange("p c t -> p (c t)"), start=True, stop=False)
                    return e.matmul(psum[5][:], mskb[p2][:, j * 128:(j + 1) * 128], ident4[:], start=False, stop=True)
                P.add("pe", stf, r=[_k("kTc", j), _k("qT", p3), _k("msk", p2), _k("ident4")], w=[_k("ps", 5)])
                while pendc:
                    pendc.pop(0)()
                P.add("act", lambda e, eb=eb: e.activation(Eb[eb][:], psum[5][:], AF.Exp, scale=128.0 ** -0.5),
                      r=[_k("ps", 5)], w=[_k("Eb", eb)])

                def pv(e, eb=eb, j=j):
                    e.matmul(psum[6][:], vc[:, j, :], Eb[eb][:], start=(j == 0), stop=(j == i))
                    return e.matmul(psum[7][:], ones[:], Eb[eb][:], start=(j == 0), stop=(j == i))

                def pvadd(pv=pv, j=j, eb=eb):
                    P.add("pe", pv, r=[_k("vc", j), _k("Eb", eb), _k("ones")], w=[_k("ps", 6), _k("ps", 7)])
                pendc.append(pvadd)
                yield
            while pendc:
                pendc.pop(0)()
            P.add("act", lambda e: e.activation(rden[:], psum[7][:], AF.Ln), r=[_k("ps", 7)], w=[_k("rden")])
            P.add("act", lambda e: e.activation(rden[:], rden[:], AF.Exp, scale=-1.0), r=[_k("rden")], w=[_k("rden")])
            P.add("dve", lambda e: e.tensor_tensor(dsT[:].rearrange("p c t -> p (c t)"), psum[6][:], rden[:], ALU.mult),
                  r=[_k("ps", 6), _k("rden")], w=[_k("dsT")])
            yield
            for mh in range(2):
                def ymm(e, mh=mh):
                    inst = None
                    for cc in range(8):
                        src = ogT[p3][:, cc, :] if cc < 4 else dsT[:, cc - 4, :]
                        inst = e.matmul(psum[5][:], src, wout[:, cc, mh * 512:(mh + 1) * 512], start=(cc == 0), stop=(cc == 7))
                    return inst
                P.add("pe", ymm, r=[_k("ogT", p3), _k("dsT")] + WOUT_ALL, w=[_k("ps", 5)])
                P.add("dve", lambda e, mh=mh: e.tensor_tensor(hb[:, mh * 512:(mh + 1) * 512], hb[:, mh * 512:(mh + 1) * 512], psum[5][:], ALU.add),
                      r=[_k("ps", 5), hk], w=[hk])
                yield
            P.add("sp", lambda e: e.dma_start(out=h_out[t0:t0 + 128, :], in_=hb[:]), r=[hk], dma=True)
            yield

        ntl = DBG["ntiles"]
        for s_ in range(ntl + 3):
            gens = []
            if 0 <= s_ - 3 < ntl:
                gens.append((stageC(s_ - 3), (s_ - 3) + 1 + 4))
            if 0 <= s_ - 2 < ntl:
                gens.append((stageBb(s_ - 2), 4 + N_IT + (s_ - 2) // 8 + 1))
            if 0 <= s_ - 1 < ntl:
                gens.append((stageBs(s_ - 1), 9 * ((s_ - 1) // 4 + 1)))
            if s_ < ntl:
                gens.append((stageA(s_), 24))
            _interleave(gens)
        P.emit(es)


def host_consts():
    import ml_dtypes
    cst = {}
    cst["ident"] = np.eye(128, dtype=ml_dtypes.bfloat16)
    half = 128
    inv = 10000.0 ** (-np.arange(half, dtype=np.float32) / half)
    ang = np.arange(T, dtype=np.float32)[:, None] * inv[None, :].astype(np.float32)
    cst["rope_cos"] = np.cos(ang).astype(np.float32)
    cst["rope_sin"] = np.sin(ang).astype(np.float32)
    g = np.array(RET_GAMMA, dtype=np.float64)
    ii = np.arange(128)
    rel = ii[None, :] - ii[:, None]
    dtm = np.zeros((128, 4, 128), np.float64)
    for h in range(4):
        dtm[:, h, :] = np.where(rel >= 0, g[h] ** np.maximum(rel, 0), 0.0) / 16.0
    cst["ret_dt"] = dtm.astype(np.float32)
    xi8 = np.zeros((128, 8, 128), np.float64)
    for cc in range(8):
        xi8[:, cc, :] = (g[cc // 2] ** (ii + 1.0))[None, :]
    cst["ret_xi8"] = xi8.astype(np.float32)
    cst["ones"] = np.ones((128, 128), dtype=ml_dtypes.bfloat16)
    cst["identf"] = np.eye(128, dtype=np.float32)
    cst["ident4"] = np.tile(np.eye(128, dtype=np.float32), (1, 4)).astype(ml_dtypes.bfloat16)
    le = (ii[:, None] <= ii[None, :])
    cst["gla_trim"] = np.where(le, -1.0 / 16.0, 0.0).astype(np.float32)
    cst["gla_trir"] = np.where(~le, -1.0 / 16.0, 0.0).astype(np.float32)
    cst["gla_m01"] = np.repeat(le[:, None, :], 4, axis=1).astype(np.float32)
    cst["dsa_negm"] = np.where(ii[None, :] <= ii[:, None], 0.0, -1e30).astype(np.float32)
    cst["dsa_pw"] = np.repeat((0.5 ** (np.arange(N_IT + 1) + 1.0))[None, :], 128, axis=0).astype(np.float32)
    zeta = np.zeros((128, 4), np.float64)
    for h in range(4):
        zeta[:, h] = g[h] ** (127.0 - ii) / 16.0
    cst["ret_zeta"] = zeta.astype(np.float32)
    return cst


INPUT_SHAPES = {
    "even_attn_norm": [1, D], "even_w_in": [1, D, EVEN_IN], "even_gla_wa2": [1, 16, 256],
    "even_gla_ba2": [1, 256], "even_gla_norm": [1, 128], "even_w_out": [1, D, D],
    "odd_attn_norm": [1, D], "odd_w_in": [1, D, ODD_IN], "odd_ret_norm": [1, 512], "odd_w_out": [1, 2048, D],
    "ffn_norm": [2, D], "ffn_w_gate": [2, D, DFF], "ffn_w_up": [2, D, DFF], "ffn_w_down": [2, DFF, D],
    "final_norm": [D],
}


def build(phases=("mix0", "ffn0", "mix1", "ffn1")):
    nc = bass.Bass("TRN2", target_bir_lowering=False)
    x = nc.dram_tensor("x", [T, D], F32, kind="ExternalInput").ap()
    out = nc.dram_tensor("out", [T, D], F32, kind="ExternalOutput").ap()
    I = {k: nc.dram_tensor(k, shp, F32, kind="ExternalInput").ap() for k, shp in INPUT_SHAPES.items()}
    cst = host_consts()
    C = {}
    for k, v in cst.items():
        C[k] = nc.dram_tensor(k, list(v.shape), BF16 if v.dtype != np.float32 else F32, kind="ExternalInput").ap()
    scr = [nc.dram_tensor("scr%d" % i, [T, D], F32, kind="Internal").ap() for i in range(3)]
    bufs = [x] + scr[:len(phases) - 1] + [out]
    for pi, ph in enumerate(phases):
        hin, hout = bufs[pi], bufs[pi + 1]
        if ph == "ffn0":
            ffn_phase(nc, hin, hout, I["ffn_norm"][0], I["ffn_w_gate"][0], I["ffn_w_up"][0], I["ffn_w_down"][0], C["ident"])
        elif ph == "ffn1":
            ffn_phase(nc, hin, hout, I["ffn_norm"][1], I["ffn_w_gate"][1], I["ffn_w_up"][1], I["ffn_w_down"][1], C["ident"],
                      final_g=I["final_norm"])
        elif ph == "mix1":
            mix1_phase(nc, hin, hout, I["odd_attn_norm"][0], I["odd_w_in"][0], I["odd_ret_norm"][0], I["odd_w_out"][0],
                       C["ident"], C["rope_cos"], C["rope_sin"], C["ret_dt"], C["ret_xi8"], C["ret_zeta"])
        elif ph == "mix0":
            mix0_phase(nc, hin, hout, I, C)
    return nc


def make_inputs(inputs, b):
    m = {k: np.ascontiguousarray(np.asarray(inputs[k], dtype=np.float32)) for k in INPUT_SHAPES}
    m["x"] = np.ascontiguousarray(np.asarray(inputs["x"][b], dtype=np.float32))
    m.update(host_consts())
    return m


def kernel(**inputs):
    nc = build()
    in_maps = [make_inputs(inputs, b) for b in range(8)]
    res = run_bass_kernel_spmd(nc, in_maps, core_ids=list(range(8)))
    return np.stack([np.asarray(r["out"], dtype=np.float32) for r in res.results], axis=0)
```

```python
import math
from contextlib import ExitStack

import numpy as np
import concourse.bass as bass
import concourse.mybir as mybir
from concourse.bass_utils import run_bass_kernel_spmd

F32 = mybir.dt.float32
BF16 = mybir.dt.bfloat16
AF = mybir.ActivationFunctionType
ALU = mybir.AluOpType
AX = mybir.AxisListType

T = 4096
D = 1024
DFF = 2816
NT = T // 128
EPS = 1e-6
EVEN_IN = 2904
ODD_IN = 6144


class _Op:
    __slots__ = ("eng", "fn", "deps", "dma", "sig", "sigidx", "dsem", "dval", "ndep")


class Prog:
    COMPUTE = ("pe", "act", "dve", "pool")

    def __init__(self, nc, n_dma_sems=16):
        self.nc = nc
        self.ops = []
        self.lastw = {}
        self.readers = {}
        self.n_dma_sems = n_dma_sems
        self.dma_rr = {"sp": 0, "pool": 0, "act": 0}
        self.dma_cnt = {}

    def add(self, eng, fn, r=(), w=(), dma=False):
        i = len(self.ops)
        deps = set()
        pr = [k for k in r if k[0] == "ps" and k not in w]
        if pr:
            w = list(w) + pr
        for k in r:
            a = self.lastw.get(k)
            if a is not None:
                deps.add(a)
        for k in w:
            a = self.lastw.get(k)
            if a is not None:
                deps.add(a)
            rd = self.readers.get(k)
            if rd:
                deps.update(rd.values())
        op = _Op()
        op.eng = eng
        op.fn = fn
        op.dma = dma
        op.sig = False
        op.sigidx = 0
        op.dsem = None
        op.dval = 0
        fdeps = []
        for a in deps:
            A = self.ops[a]
            if (not dma) and (not A.dma) and eng == "pe" and A.eng == "pe":
                continue
            fdeps.append(a)
        op.deps = sorted(fdeps)
        if dma:
            q = self.dma_rr[eng]
            self.dma_rr[eng] = (q + 1) % self.n_dma_sems
            key = (eng, q)
            self.dma_cnt[key] = self.dma_cnt.get(key, 0) + 1
            op.dsem = key
            op.dval = 16 * self.dma_cnt[key]
        self.ops.append(op)
        for k in w:
            self.lastw[k] = i
            self.readers[k] = {}
        for k in r:
            d = self.readers.setdefault(k, {})
            d[("dma", i) if dma else eng] = i
        return i

    def emit(self, es):
        nc = self.nc
        ops = self.ops
        for op in ops:
            for a in op.deps:
                if not ops[a].dma:
                    ops[a].sig = True
        cnt = {e: 0 for e in self.COMPUTE + ("sp",)}
        for op in ops:
            if op.sig and not op.dma:
                cnt[op.eng] += 1
                op.sigidx = cnt[op.eng]
        sems = {e: es.enter_context(nc.semaphore("s_" + e)) for e in cnt}
        dsems = {}
        for key in self.dma_cnt:
            dsems[key] = es.enter_context(nc.semaphore("d_%s%d" % key))
        engines = {"pe": "tensor", "act": "scalar", "dve": "vector", "pool": "gpsimd", "sp": "sync"}
        with nc.Block() as block:
            for e, attr in engines.items():
                mine = [op for op in ops if op.eng == e]

                def body(engine, mine=mine, e=e):
                    known = {}

                    def wait(sem_key, sem, val):
                        if known.get(sem_key, 0) >= val:
                            return
                        engine.wait_ge(sem, val)
                        known[sem_key] = val

                    for op in mine:
                        for a in op.deps:
                            A = ops[a]
                            if A.dma:
                                wait(A.dsem, dsems[A.dsem], A.dval)
                            else:
                                wait(A.eng, sems[A.eng], A.sigidx)
                        if op.dma:
                            if op.dval > 16:
                                wait(op.dsem, dsems[op.dsem], op.dval - 16)
                            inst = op.fn(engine)
                            inst.then_inc(dsems[op.dsem], 16)
                        else:
                            inst = op.fn(engine)
                            if op.sig:
                                inst.then_inc(sems[e], 1)
                    for key, c in self.dma_cnt.items():
                        if key[0] == e:
                            wait(key, dsems[key], 16 * c)

                getattr(block, attr)(body)


def _k(name, *idx):
    return (name,) + idx


class Ctx:
    def __init__(self, nc, P, es):
        self.nc = nc
        self.P = P
        self.es = es
        self.rr = 0

    UID = [0]

    def sb(self, name, shape, dt):
        Ctx.UID[0] += 1
        return self.es.enter_context(self.nc.sbuf_tensor("sb%d_%s" % (Ctx.UID[0], name), list(shape), dt))

    def ps(self, name):
        Ctx.UID[0] += 1
        return self.es.enter_context(self.nc.psum_tensor("ps%d_%s" % (Ctx.UID[0], name), [128, 512], F32))


RSQRT_MODE = ["lnexp"]


def rsqrt_small(P, out_ap, outkey, in_ap, inkey, scale, eps=EPS):
    inkeys = inkey if isinstance(inkey, list) else [inkey]
    P.add("dve", lambda e: e.tensor_scalar(out_ap, in_ap, scale, eps, ALU.mult, ALU.add),
          r=inkeys, w=[outkey])
    if RSQRT_MODE[0] == "sqrt":
        P.add("act", lambda e: e.sqrt(out_ap, out_ap), r=[outkey], w=[outkey])
        P.add("dve", lambda e: e.reciprocal(out_ap, out_ap), r=[outkey], w=[outkey])
    else:
        P.add("act", lambda e: e.activation(out_ap, out_ap, AF.Ln), r=[outkey], w=[outkey])
        P.add("act", lambda e: e.activation(out_ap, out_ap, AF.Exp, scale=-0.5), r=[outkey], w=[outkey])


def rmsnorm_tile(c, h_ap, hkey, xn_ap, xnkey, junk_ap, junkkey, ss_ap, sskey, rstd_ap, rstdkey):
    P = c.P
    P.add("act", lambda e: e.activation(junk_ap, h_ap, AF.Square, accum_out=ss_ap),
          r=[hkey], w=[junkkey, sskey])
    rsqrt_small(P, rstd_ap, rstdkey, ss_ap, sskey, 1.0 / D)
    P.add("dve", lambda e: e.tensor_scalar(xn_ap, h_ap, rstd_ap, None, ALU.mult),
          r=[hkey, rstdkey], w=[xnkey])


_LW = [0]


def load_weight_bf16(c, w_dram, rows, cols, dst, dstname, stage, gcol=None, gkey=None, colchunk=704, gmap=None):
    P = c.P
    nk = rows // 128
    nch = (cols + colchunk - 1) // colchunk
    if gcol is None:
        for k in range(nk):
            P.add("pool", lambda e, k=k: e.dma_start(out=dst[:, k, :], in_=w_dram[k * 128:(k + 1) * 128, :]),
                  w=[_k(dstname, k, j) for j in range(nch)], dma=True)
        return [_k(dstname, k, j) for k in range(nk) for j in range(nch)]
    for k in range(nk):
        for j in range(nch):
            c0 = j * colchunk
            c1 = min(cols, c0 + colchunk)
            cnt = _LW[0]
            _LW[0] += 1
            st, skeys = stage[cnt % len(stage)]
            src = w_dram[k * 128:(k + 1) * 128, c0:c1]
            P.add("sp", lambda e, st=st, src=src, n=c1 - c0: e.dma_start(out=st[:, 0:n], in_=src),
                  w=skeys, dma=True)
            dap = dst[:, k, c0:c1]
            sap = st[:, 0:c1 - c0]
            eng = ("dve", "act")[cnt % 2]
            rk = list(skeys) + ([gkey] if gcol is not None else [])
            if gcol is None:
                if eng == "act":
                    P.add("act", lambda e, dap=dap, sap=sap: e.copy(dap, sap), r=rk, w=[_k(dstname, k, j)])
                else:
                    P.add(eng, lambda e, dap=dap, sap=sap: e.tensor_copy(dap, sap), r=rk, w=[_k(dstname, k, j)])
            else:
                kk_ = gmap(k) if gmap is not None else k
                g = gcol[:, kk_:kk_ + 1]
                if eng == "act":
                    P.add("act", lambda e, dap=dap, sap=sap, g=g: e.activation(dap, sap, AF.Copy, scale=g),
                          r=rk, w=[_k(dstname, k, j)])
                else:
                    P.add(eng, lambda e, dap=dap, sap=sap, g=g: e.tensor_scalar(dap, sap, g, None, ALU.mult),
                          r=rk, w=[_k(dstname, k, j)])
    return [_k(dstname, k, j) for k in range(nk) for j in range(nch)]


def wkeys(dstname, k, c0, c1, colchunk=704):
    return [_k(dstname, k, j) for j in range(c0 // colchunk, (c1 - 1) // colchunk + 1)]


def ffn_phase(nc, h_in, h_out, g_dram, wg_d, wu_d, wd_d, ident_d, final_g=None):
    with ExitStack() as es:
        P = Prog(nc)
        c = Ctx(nc, P, es)
        CC = 704
        wg = c.sb("wg", [128, 8, DFF], BF16)
        wu = c.sb("wu", [128, 8, DFF], BF16)
        wd = c.sb("wd", [128, 22, D], BF16)
        gcol = c.sb("gcol", [128, 8], F32)
        ident = c.sb("ident", [128, 128], BF16)
        xn = [c.sb("xn%d" % i, [128, D], BF16) for i in range(2)]
        xnT = [c.sb("xnT%d" % i, [128, 8, 512], BF16) for i in range(2)]
        actT = c.sb("actT", [128, 22, 512], BF16)
        actf = actT[:].rearrange("p f t -> p (f t)").bitcast(F32)
        stage = []
        for si in range(7):
            stage.append((actf[:, si * 768:si * 768 + CC], [_k("actT", f) for f in range(3 * si, 3 * si + 3)]))
        sg = [c.sb("sg%d" % i, [128, 512], F32) for i in range(2)]
        junk = c.sb("junk", [128, D], BF16)
        ss = c.sb("ss", [128, 8], F32)
        rstd = c.sb("rstd", [128, 8], F32)
        if final_g is not None:
            fg = c.sb("fg", [128, D], F32)
        psum = [c.ps("ps%d" % i) for i in range(8)]

        P.add("sp", lambda e: e.dma_start(out=gcol[:], in_=g_dram.rearrange("(k p) -> p k", p=128),
                                          allow_slow_non_contiguous=True), w=[_k("gcol")], dma=True)
        P.add("sp", lambda e: e.dma_start(out=ident[:], in_=ident_d), w=[_k("ident")], dma=True)
        if final_g is not None:
            P.add("sp", lambda e: e.dma_start(out=fg[:], in_=final_g.partition_broadcast(128)),
                  w=[_k("fg")], dma=True)
        load_weight_bf16(c, wg_d, D, DFF, wg, "wg", stage, gcol, _k("gcol"), CC)
        load_weight_bf16(c, wu_d, D, DFF, wu, "wu", stage, gcol, _k("gcol"), CC)
        load_weight_bf16(c, wd_d, DFF, D, wd, "wd", stage, None, None, CC)

        nsup = T // 512
        hA = [c.sb("hA%d" % i, [128, D], F32) for i in range(2)]
        hC = [c.sb("hC%d" % i, [128, D], F32) for i in range(2)]
        cnt = {"t": 0}

        def front_norm(s, j):
            t0 = s * 512 + j * 128
            b = (s * 4 + j) % 2
            hap = hA[b][:]
            hk = _k("hA", b)
            P.add("sp", lambda e, hap=hap, t0=t0: e.dma_start(out=hap, in_=h_in[t0:t0 + 128, :]), w=[hk], dma=True)
            rmsnorm_tile(c, hap, hk, xn[b][:], _k("xn", b), junk[:], _k("junk"),
                         ss[:, j:j + 1], _k("ss", j), rstd[:, j:j + 1], _k("rstd", j))

        def front_tr(s, j):
            b = (s * 4 + j) % 2
            xb = s % 2
            pb = b
            pst = psum[pb][:].bitcast(BF16)

            def tr(e, b=b, pst=pst):
                inst = None
                for cc in range(8):
                    inst = e.transpose(pst[:, cc * 128:(cc + 1) * 128], xn[b][:, cc * 128:(cc + 1) * 128], ident[:])
                return inst
            P.add("pe", tr, r=[_k("xn", b), _k("ident")], w=[_k("ps", pb)])
            dst = xnT[xb][:, :, j * 128:(j + 1) * 128]
            src = pst[:, 0:1024].rearrange("p (c t) -> p c t", c=8)
            P.add("act", lambda e, dst=dst, src=src: e.copy(dst, src), r=[_k("ps", pb)], w=[_k("xnT", xb, j)])

        for j in range(4):
            front_norm(0, j)
            front_tr(0, j)
        for s in range(nsup):
            xb = s % 2
            xT = xnT[xb]
            xk = [_k("xnT", xb, j) for j in range(4)]
            for f in range(22):
                pg = 2 + (f % 2)
                pu = 4 + (f % 2)

                def mm(e, w_, pi, f=f, xT=xT):
                    inst = None
                    for k in range(8):
                        inst = e.matmul(psum[pi][:], w_[:, k, f * 128:(f + 1) * 128], xT[:, k, :],
                                        start=(k == 0), stop=(k == 7))
                    return inst
                wkg = [kk for k in range(8) for kk in wkeys("wg", k, f * 128, (f + 1) * 128, CC)]
                wku = [kk for k in range(8) for kk in wkeys("wu", k, f * 128, (f + 1) * 128, CC)]
                P.add("pe", lambda e, pg=pg, mm=mm: mm(e, wg, pg), r=xk + wkg, w=[_k("ps", pg)])
                P.add("pe", lambda e, pu=pu, mm=mm: mm(e, wu, pu), r=xk + wku, w=[_k("ps", pu)])
                sb_ = f % 2
                P.add("act", lambda e, sb_=sb_, pg=pg: e.activation(sg[sb_][:], psum[pg][:], AF.Silu),
                      r=[_k("ps", pg)], w=[_k("sg", sb_)])
                P.add("dve", lambda e, sb_=sb_, pu=pu, f=f: e.tensor_tensor(actT[:, f, :], sg[sb_][:], psum[pu][:], ALU.mult),
                      r=[_k("sg", sb_), _k("ps", pu)], w=[_k("actT", f)])
            ak = [_k("actT", f) for f in range(22)]
            wdk = [kk for f in range(22) for kk in wkeys("wd", f, 0, D, CC)]
            for j in range(4):
                t0 = s * 512 + j * 128
                hb = (s * 4 + j) % 2
                hap = hC[hb][:]
                hk = _k("hC", hb)
                P.add("sp", lambda e, hap=hap, t0=t0: e.dma_start(out=hap, in_=h_in[t0:t0 + 128, :]), w=[hk], dma=True)
                if s + 1 < nsup:
                    front_norm(s + 1, j)
                for mh in range(2):
                    py = 6 + mh

                    def mmd(e, j=j, mh=mh, py=py):
                        inst = None
                        for f in range(22):
                            inst = e.matmul(psum[py][:], actT[:, f, j * 128:(j + 1) * 128],
                                            wd[:, f, mh * 512:(mh + 1) * 512], start=(f == 0), stop=(f == 21))
                        return inst
                    P.add("pe", mmd, r=ak + wdk, w=[_k("ps", py)])
                    hs = hC[hb][:, mh * 512:(mh + 1) * 512]
                    P.add("dve", lambda e, hs=hs, py=py: e.tensor_tensor(hs, hs, psum[py][:], ALU.add),
                          r=[_k("ps", py), hk], w=[hk])
                if s + 1 < nsup:
                    front_tr(s + 1, j)
                if final_g is not None:
                    sj = 4 + j
                    P.add("act", lambda e, hap=hap, sj=sj: e.activation(junk[:], hap, AF.Square, accum_out=ss[:, sj:sj + 1]),
                          r=[hk], w=[_k("junk"), _k("ss", sj)])
                    rsqrt_small(P, rstd[:, sj:sj + 1], _k("rstd", sj), ss[:, sj:sj + 1], _k("ss", sj), 1.0 / D)
                    P.add("dve", lambda e, hap=hap, sj=sj: e.scalar_tensor_tensor(hap, hap, rstd[:, sj:sj + 1], fg[:], ALU.mult, ALU.mult),
                          r=[hk, _k("rstd", sj), _k("fg")], w=[hk])
                P.add("sp", lambda e, hap=hap, t0=t0: e.dma_start(out=h_out[t0:t0 + 128, :], in_=hap),
                      r=[hk], dma=True)
        P.emit(es)


RET_GAMMA = [1.0 - 2.0 ** (-5.0 - h) for h in range(4)]


DBG = {"ntiles": NT, "stage": 99}


def mix1_phase(nc, h_in, h_out, g_dram, win_d, retnorm_d, wout_d, ident_d, cos_d, sin_d, dt_d, xi8_d, zeta_d):
    RSQRT_MODE[0] = "sqrt"
    with ExitStack() as es:
        P = Prog(nc)
        c = Ctx(nc, P, es)
        CC = 512
        win = c.sb("win", [128, 8, ODD_IN], BF16)
        wout = c.sb("wout", [128, 16, D], BF16)
        gcol = c.sb("gcol", [128, 8], F32)
        rcol = c.sb("rcol", [128, 4], F32)
        ident = c.sb("ident", [128, 128], BF16)
        S = c.sb("S", [128, 2, 4, 512], F32)
        Sb = c.sb("Sb", [128, 2, 4, 512], BF16)
        hbufs = [c.sb("hbuf%d" % i, [128, D], F32) for i in range(2)]
        xn = c.sb("xn", [128, D], BF16)
        xnT = c.sb("xnT", [128, 8, 128], BF16)
        tmp = [c.sb("tmp%d" % i, [128, 2, 128], F32) for i in range(4)]
        qrot = c.sb("qrot", [128, D], BF16)
        krot = c.sb("krot", [128, D], BF16)
        kz = c.sb("kz", [128, D], BF16)
        qT = c.sb("qT", [128, 8, 128], BF16)
        qxiT = c.sb("qxiT", [128, 8, 128], BF16)
        kT = c.sb("kT", [128, 8, 128], BF16)
        vb = c.sb("vb", [128, 2048], BF16)
        gs = c.sb("gs", [128, 2048], BF16)
        atm = c.sb("atm", [128, 4, 128], BF16)
        og = c.sb("og", [128, 2048], BF16)
        ogT = c.sb("ogT", [128, 16, 128], BF16)
        junk = c.sb("junk", [128, 512], BF16)
        cos_t = c.sb("cos", [128, 128], F32)
        sin_t = c.sb("sin", [128, 128], F32)
        dtm = c.sb("dtm", [128, 4, 128], F32)
        xi8 = c.sb("xi8", [128, 8, 128], F32)
        zeta = c.sb("zeta", [128, 4], F32)
        ss = c.sb("ss", [128, 8], F32)
        rstd = c.sb("rstd", [128, 8], F32)
        psum = [c.ps("ps%d" % i) for i in range(8)]

        def dma(out_ap, in_ap, wk, **kw):
            P.add("sp", lambda e: e.dma_start(out=out_ap, in_=in_ap, **kw), w=[wk], dma=True)

        dma(gcol[:], g_dram.rearrange("(k p) -> p k", p=128), _k("gcol"), allow_slow_non_contiguous=True)
        dma(rcol[:], retnorm_d.rearrange("(k p) -> p k", p=128), _k("rcol"), allow_slow_non_contiguous=True)
        dma(ident[:], ident_d, _k("ident"))
        dma(dtm[:], dt_d, _k("dtm"))
        dma(xi8[:], xi8_d, _k("xi8"))
        dma(zeta[:], zeta_d, _k("zeta"))
        P.add("dve", lambda e: e.memset(S[:], 0.0), w=[_k("S", cc, h) for cc in range(2) for h in range(4)])
        P.add("pool", lambda e: e.memset(Sb[:], 0.0), w=[_k("Sb", cc, h) for cc in range(2) for h in range(4)])
        ogf = og[:].bitcast(F32)
        ogTf = ogT[:].rearrange("p c t -> p (c t)").bitcast(F32)
        vbf = vb[:].bitcast(F32)
        gsf = gs[:].bitcast(F32)
        stage = [(vbf[:, 0:512], [_k("vb", 0), _k("vb", 1)]), (vbf[:, 512:1024], [_k("vb", 2), _k("vb", 3)]),
                 (gsf[:, 0:512], [_k("gs", 0), _k("gs", 1)]), (gsf[:, 512:1024], [_k("gs", 2), _k("gs", 3)]),
                 (ogf[:, 0:512], [_k("og", 0), _k("og", 1)]), (ogf[:, 512:1024], [_k("og", 2), _k("og", 3)]),
                 (ogTf[:, 0:512], [_k("ogT", 0)]), (ogTf[:, 512:1024], [_k("ogT", 1)])]
        load_weight_bf16(c, win_d, D, ODD_IN, win, "win", stage, gcol, _k("gcol"), CC)
        load_weight_bf16(c, wout_d, 2048, D, wout, "wout", stage, rcol, _k("rcol"), CC, gmap=lambda k: k % 4)

        def bfv(i):
            return psum[i][:].bitcast(BF16)

        dma(hbufs[0][:], h_in[0:128, :], _k("hbuf", 0))
        for i in range(DBG["ntiles"]):
            t0 = i * 128
            hbuf = hbufs[i % 2]
            hk = _k("hbuf", i % 2)
            if i + 1 < DBG["ntiles"]:
                dma(hbufs[(i + 1) % 2][:], h_in[t0 + 128:t0 + 256, :], _k("hbuf", (i + 1) % 2))
            dma(cos_t[:], cos_d[t0:t0 + 128, :], _k("cos"))
            dma(sin_t[:], sin_d[t0:t0 + 128, :], _k("sin"))
            rmsnorm_tile(c, hbuf[:], hk, xn[:], _k("xn"), ogT[:, 0:8, :].rearrange("p c t -> p (c t)"), _k("ogT", 0),
                         ss[:, 4:5], _k("ss", 4), rstd[:, 4:5], _k("rstd", 4))

            def tr8(e, src, pb):
                inst = None
                v = bfv(pb)
                for cc in range(8):
                    inst = e.transpose(v[:, cc * 128:(cc + 1) * 128], src[:, cc * 128:(cc + 1) * 128], ident[:])
                return inst
            P.add("pe", lambda e: tr8(e, xn, 0), r=[_k("xn"), _k("ident")], w=[_k("ps", 0)])
            P.add("act", lambda e: e.copy(xnT[:].rearrange("p c t -> p (c t)"), bfv(0)), r=[_k("ps", 0)], w=[_k("xnT")])
            for g in range(12 if DBG["stage"] >= 2 else 0):
                pb = (4 + g) if g < 4 else 1 + (g % 2)

                def mm(e, g=g, pb=pb):
                    inst = None
                    for k in range(8):
                        inst = e.matmul(psum[pb][:], xnT[:, k, :], win[:, k, g * 512:(g + 1) * 512],
                                        start=(k == 0), stop=(k == 7))
                    return inst
                wk_ = [_k("win", k, g) for k in range(8)]
                P.add("pe", mm, r=[_k("xnT")] + wk_, w=[_k("ps", pb)])
                if g < 4:
                    dstt = qrot if g < 2 else krot
                    dname = "qrot" if g < 2 else "krot"
                    hh = (g % 2) * 2
                    X = psum[pb][:].rearrange("p (h x d) -> p h x d", h=2, x=2)
                    X1 = X[:, :, 0, :]
                    X2 = X[:, :, 1, :]
                    Dv = dstt[:, hh * 256:(hh + 2) * 256].rearrange("p (h x d) -> p h x d", h=2, x=2)
                    cb = cos_t[:].unsqueeze(1).to_broadcast([128, 2, 128])
                    sb_ = sin_t[:].unsqueeze(1).to_broadcast([128, 2, 128])
                    pk = _k("ps", pb)

                    def tt(e, o, a, b, op):
                        return e.tensor_tensor(o, a, b, op)
                    P.add("dve", lambda e, X1=X1, cb=cb: tt(e, tmp[0][:], X1, cb, ALU.mult), r=[pk, _k("cos")], w=[_k("tmp", 0)])
                    P.add("dve", lambda e, X2=X2, sb_=sb_: tt(e, tmp[1][:], X2, sb_, ALU.mult), r=[pk, _k("sin")], w=[_k("tmp", 1)])
                    P.add("dve", lambda e, X2=X2, cb=cb: tt(e, tmp[2][:], X2, cb, ALU.mult), r=[pk, _k("cos")], w=[_k("tmp", 2)])
                    P.add("dve", lambda e, X1=X1, sb_=sb_: tt(e, tmp[3][:], X1, sb_, ALU.mult), r=[pk, _k("sin")], w=[_k("tmp", 3)])
                    P.add("dve", lambda e, Dv=Dv: tt(e, Dv[:, :, 0, :], tmp[0][:], tmp[1][:], ALU.subtract),
                          r=[_k("tmp", 0), _k("tmp", 1)], w=[_k(dname, g % 2)])
                    P.add("dve", lambda e, Dv=Dv: tt(e, Dv[:, :, 1, :], tmp[2][:], tmp[3][:], ALU.add),
                          r=[_k("tmp", 2), _k("tmp", 3)], w=[_k(dname, g % 2)])
                elif g < 8:
                    vv = g - 4
                    P.add("act", lambda e, vv=vv, pb=pb: e.copy(vb[:, vv * 512:(vv + 1) * 512], psum[pb][:]),
                          r=[_k("ps", pb)], w=[_k("vb", vv)])
                else:
                    vv = g - 8
                    P.add("act", lambda e, vv=vv, pb=pb: e.activation(gs[:, vv * 512:(vv + 1) * 512], psum[pb][:], AF.Silu),
                          r=[_k("ps", pb)], w=[_k("gs", vv)])
            if DBG["stage"] < 3:
                P.add("sp", lambda e, t0=t0, hbuf=hbuf: e.dma_start(out=h_out[t0:t0 + 128, :], in_=hbuf[:]), r=[hk], dma=True)
                continue
            P.add("pool", lambda e: e.tensor_tensor(kz[:].rearrange("p (h d) -> p h d", h=4),
                                                    krot[:].rearrange("p (h d) -> p h d", h=4),
                                                    zeta[:].unsqueeze(2).to_broadcast([128, 4, 256]), ALU.mult),
                  r=[_k("krot", 0), _k("krot", 1), _k("zeta")], w=[_k("kz")])
            P.add("pe", lambda e: tr8(e, qrot, 0), r=[_k("qrot", 0), _k("qrot", 1), _k("ident")], w=[_k("ps", 0)])
            P.add("act", lambda e: e.copy(qT[:].rearrange("p c t -> p (c t)"), bfv(0)), r=[_k("ps", 0)], w=[_k("qT")])
            P.add("dve", lambda e: e.tensor_tensor(qxiT[:].rearrange("p c t -> p (c t)"), bfv(0),
                                                   xi8[:].rearrange("p c t -> p (c t)"), ALU.mult),
                  r=[_k("ps", 0), _k("xi8")], w=[_k("qxiT")])
            P.add("pe", lambda e: tr8(e, krot, 3), r=[_k("krot", 0), _k("krot", 1), _k("ident")], w=[_k("ps", 3)])
            P.add("act", lambda e: e.copy(kT[:].rearrange("p c t -> p (c t)"), bfv(3)), r=[_k("ps", 3)], w=[_k("kT")])

            if DBG["stage"] < 4:
                P.add("sp", lambda e, t0=t0, hbuf=hbuf: e.dma_start(out=h_out[t0:t0 + 128, :], in_=hbuf[:]), r=[hk], dma=True)
                continue
            def amm(e):
                inst = None
                for h in range(4):
                    for cc in range(2):
                        inst = e.matmul(psum[0][:, h * 128:(h + 1) * 128], kT[:, 2 * h + cc, :], qT[:, 2 * h + cc, :],
                                        start=(cc == 0), stop=(cc == 1))
                return inst
            P.add("pe", amm, r=[_k("kT"), _k("qT")], w=[_k("ps", 0)])
            P.add("dve", lambda e: e.tensor_tensor(atm[:].rearrange("p h t -> p (h t)"), psum[0][:],
                                                   dtm[:].rearrange("p h t -> p (h t)"), ALU.mult),
                  r=[_k("ps", 0), _k("dtm")], w=[_k("atm")])
            if DBG["stage"] < 5:
                P.add("sp", lambda e, t0=t0, hbuf=hbuf: e.dma_start(out=h_out[t0:t0 + 128, :], in_=hbuf[:]), r=[hk], dma=True)
                continue
            for h in range(4):
                def omm(e, h=h):
                    e.matmul(psum[4 + h][:], atm[:, h, :], vb[:, h * 512:(h + 1) * 512], start=True, stop=False)
                    e.matmul(psum[4 + h][:], qxiT[:, 2 * h, :], Sb[:, 0, h, :], start=False, stop=False)
                    return e.matmul(psum[4 + h][:], qxiT[:, 2 * h + 1, :], Sb[:, 1, h, :], start=False, stop=True)
                P.add("pe", omm, r=[_k("atm"), _k("vb", h), _k("qxiT"), _k("Sb", 0, h), _k("Sb", 1, h)], w=[_k("ps", 4 + h)])
                P.add("act", lambda e, h=h: e.activation(junk[:], psum[4 + h][:], AF.Square, accum_out=ss[:, h:h + 1]),
                      r=[_k("ps", 4 + h)], w=[_k("junk"), _k("ss", h)])
            if DBG["stage"] < 7:
                P.add("sp", lambda e, t0=t0, hbuf=hbuf: e.dma_start(out=h_out[t0:t0 + 128, :], in_=hbuf[:]), r=[hk], dma=True)
                continue
            rsqrt_small(P, rstd[:, 0:4], _k("rstd", 0), ss[:, 0:4], [_k("ss", h) for h in range(4)], 1.0 / 512)
            for h in range(4):
                P.add("dve", lambda e, h=h: e.scalar_tensor_tensor(og[:, h * 512:(h + 1) * 512], psum[4 + h][:], rstd[:, h:h + 1],
                                                                    gs[:, h * 512:(h + 1) * 512], ALU.mult, ALU.mult),
                      r=[_k("ps", 4 + h), _k("rstd", 0), _k("gs", h)], w=[_k("og", h)])
            for half in range(2):
                pb = 0 if half == 0 else 3

                def tro(e, half=half, pb=pb):
                    inst = None
                    v = bfv(pb)
                    for cc in range(8):
                        c2 = half * 8 + cc
                        inst = e.transpose(v[:, cc * 128:(cc + 1) * 128], og[:, c2 * 128:(c2 + 1) * 128], ident[:])
                    return inst
                P.add("pe", tro, r=[_k("og", half * 2), _k("og", half * 2 + 1), _k("ident")], w=[_k("ps", pb)])
                P.add("act", lambda e, half=half, pb=pb: e.copy(ogT[:, half * 8:(half + 1) * 8, :].rearrange("p c t -> p (c t)"), bfv(pb)),
                      r=[_k("ps", pb)], w=[_k("ogT", half)])
            if DBG["stage"] < 6:
                P.add("sp", lambda e, t0=t0, hbuf=hbuf: e.dma_start(out=h_out[t0:t0 + 128, :], in_=hbuf[:]), r=[hk], dma=True)
                continue
            for h in range(4):
                for cc in range(2):
                    pb = 1 + ((h * 2 + cc) % 2)
                    P.add("pe", lambda e, h=h, cc=cc, pb=pb: e.matmul(psum[pb][:], kz[:, h * 256 + cc * 128:h * 256 + (cc + 1) * 128],
                                                                        vb[:, h * 512:(h + 1) * 512], start=True, stop=True),
                          r=[_k("kz"), _k("vb", h)], w=[_k("ps", pb)])
                    dec = RET_GAMMA[h] ** 128
                    P.add("dve", lambda e, h=h, cc=cc, pb=pb, dec=dec: e.scalar_tensor_tensor(S[:, cc, h, :], S[:, cc, h, :], dec, psum[pb][:], ALU.mult, ALU.add),
                          r=[_k("S", cc, h), _k("ps", pb)], w=[_k("S", cc, h)])
                    P.add("pool", lambda e, h=h, cc=cc: e.tensor_copy(Sb[:, cc, h, :], S[:, cc, h, :]),
                          r=[_k("S", cc, h)], w=[_k("Sb", cc, h)])
            for mh in range(2):
                pb = 1 + mh

                def ymm(e, mh=mh, pb=pb):
                    inst = None
                    for cc in range(16):
                        inst = e.matmul(psum[pb][:], ogT[:, cc, :], wout[:, cc, mh * 512:(mh + 1) * 512],
                                        start=(cc == 0), stop=(cc == 15))
                    return inst
                P.add("pe", ymm, r=[_k("ogT", 0), _k("ogT", 1)] + [_k("wout", cc, jj) for cc in range(16) for jj in range(2)],
                      w=[_k("ps", pb)])
                P.add("dve", lambda e, mh=mh, pb=pb, hbuf=hbuf: e.tensor_tensor(hbuf[:, mh * 512:(mh + 1) * 512], hbuf[:, mh * 512:(mh + 1) * 512],
                                                                      psum[pb][:], ALU.add),
                      r=[_k("ps", pb), hk], w=[hk])
            P.add("sp", lambda e, t0=t0, hbuf=hbuf: e.dma_start(out=h_out[t0:t0 + 128, :], in_=hbuf[:]), r=[hk], dma=True)
        P.emit(es)


MASK_BIG = 30000.0
N_IT = 22
TOPK = 256
IDX_C0 = (64.0 ** -0.5) * (8.0 ** -0.5)
ACT_BISECT_EVERY = 10 ** 9


def _interleave(gens):
    st_ = [[g, max(1, n), 0] for g, n in gens]
    while st_:
        st_.sort(key=lambda x: x[2] / x[1])
        g = st_[0]
        try:
            next(g[0])
            g[2] += 1
        except StopIteration:
            st_.pop(0)


def mix0_phase(nc, h_in, h_out, I, C):
    RSQRT_MODE[0] = "lnexp"
    with ExitStack() as es:
        P = Prog(nc)
        c = Ctx(nc, P, es)
        CC = 512
        win = c.sb("win", [128, 8, EVEN_IN], BF16)
        wout = c.sb("wout", [128, 8, D], BF16)
        stage = [c.sb("stage%d" % i, [128, CC], F32) for i in range(2)]
        gcol = c.sb("gcol", [128, 8], F32)
        gcol2 = c.sb("gcol2", [128, 8], F32)
        ident = c.sb("ident", [128, 128], BF16)
        identf = c.sb("identf", [128, 128], F32)
        ones = c.sb("ones", [128, 128], BF16)
        ident4 = c.sb("ident4", [128, 512], BF16)
        trim = c.sb("trim", [128, 128], F32)
        trir = c.sb("trir", [128, 128], F32)
        m01 = c.sb("m01", [128, 4, 128], F32)
        negm = c.sb("negm", [128, 128], F32)
        pw = c.sb("pw", [128, N_IT + 1], F32)
        wa2 = c.sb("wa2", [16, 256], F32)
        ba2 = c.sb("ba2", [128, 256], F32)
        hbufA = c.sb("hbufA", [128, D], F32)
        hbufC = c.sb("hbufC", [128, D], F32)
        xn = c.sb("xn", [128, D], BF16)
        xnT = c.sb("xnT", [128, 8, 128], BF16)
        junk = c.sb("junk", [128, D], BF16)
        gqk = c.sb("gqk", [128, 512], BF16)
        gv = c.sb("gv", [128, 512], BF16)
        gsl = c.sb("gsl", [128, 512], BF16)
        dq = c.sb("dq", [128, 512], BF16)
        dkb = c.sb("dkb", [128, 128], BF16)
        iq = c.sb("iq", [128, 512], BF16)
        ikb = c.sb("ikb", [128, 64], BF16)
        iw = c.sb("iw", [128, 8], F32)
        wabs = [c.sb("wabs%d" % i, [128, 8], F32) for i in range(2)]
        sgn = c.sb("sgn", [128, 8], F32)
        Dsg = [c.sb("Dsg%d" % i, [128, 8, 128], BF16) for i in range(2)]
        gaT = c.sb("gaT", [16, 128], F32)
        zb = c.sb("zb", [128, 256], F32)
        sp_ = c.sb("sp", [128, 256], F32)
        ecum = c.sb("ecum", [64, 4, 128], F32)
        encum = c.sb("encum", [64, 4, 128], F32)
        erev = c.sb("erev", [128, 256], F32)
        qtT = c.sb("qtT", [64, 4, 128], BF16)
        ktT = c.sb("ktT", [64, 4, 128], BF16)
        kend = c.sb("kend", [128, 256], BF16)
        atm = c.sb("atm", [128, 4, 128], BF16)
        Sg = c.sb("Sg", [64, 4, 128], F32)
        Sgb = c.sb("Sgb", [64, 4, 128], BF16)
        og = c.sb("og", [128, 512], BF16)
        ogT = [c.sb("ogT%d" % i, [128, 4, 128], BF16) for i in range(4)]
        dsT = c.sb("dsT", [128, 4, 128], BF16)
        qT = [c.sb("qT%d" % i, [128, 4, 128], BF16) for i in range(4)]
        kTc = c.sb("kTc", [128, T], BF16)
        vc = c.sb("vc", [128, NT, 128], BF16)
        ikTc = c.sb("ikTc", [64, T], BF16)
        iqT = [c.sb("iqT%d" % i, [64, 8, 128], BF16) for i in range(2)]
        scb = [c.sb("sc%d" % i, [128, T], F32) for i in range(2)]
        mskb = [c.sb("msk%d" % i, [128, T], BF16) for i in range(2)]
        junkb = c.sb("junkb", [128, T], mybir.dt.int8)
        Rb = [c.sb("Rb%d" % i, [128, 512], BF16) for i in range(2)]
        Eb = [c.sb("Eb%d" % i, [128, 512], BF16) for i in range(2)]
        rden = c.sb("rden", [128, 512], F32)
        ss = c.sb("ss", [128, 8], F32)
        rstd = c.sb("rstd", [128, 8], F32)
        st = c.sb("st", [128, 8], F32)
        hst = c.sb("hst", [128, N_IT + 1], F32)
        nhst = c.sb("nhst", [128, N_IT], F32)
        hhst = c.sb("hhst", [128, N_IT], F32)
        psum = [c.ps("ps%d" % i) for i in range(8)]

        def dma(out_ap, in_ap, wk, **kw):
            P.add("sp", lambda e: e.dma_start(out=out_ap, in_=in_ap, **kw), w=[wk], dma=True)

        def bfv(i):
            return psum[i][:].bitcast(BF16)

        dma(gcol[:], I["even_attn_norm"][0].rearrange("(k p) -> p k", p=128), _k("gcol"), allow_slow_non_contiguous=True)
        P.add("dve", lambda e: e.memset(gcol2[:], 1.0), w=[_k("gcol2")])
        for cc in range(4):
            dma(gcol2[:, cc:cc + 1], I["even_gla_norm"][0].rearrange("(p o) -> p o", o=1), _k("gcol2"))
        dma(ident[:], C["ident"], _k("ident"))
        dma(identf[:], C["identf"], _k("identf"))
        dma(ones[:], C["ones"], _k("ones"))
        dma(ident4[:], C["ident4"], _k("ident4"))
        dma(trim[:], C["gla_trim"], _k("trim"))
        dma(trir[:], C["gla_trir"], _k("trir"))
        dma(m01[:], C["gla_m01"], _k("m01"))
        dma(negm[:], C["dsa_negm"], _k("negm"))
        dma(pw[:], C["dsa_pw"], _k("pw"))
        dma(wa2[:], I["even_gla_wa2"][0], _k("wa2"))
        dma(ba2[:], I["even_gla_ba2"][0].partition_broadcast(128), _k("ba2"))
        P.add("dve", lambda e: e.memset(Sg[:], 0.0), w=[_k("Sg")])
        P.add("pool", lambda e: e.memset(Sgb[:], 0.0), w=[_k("Sgb")])
        stage = [(stage[0][:], [_k("stage", 0)]), (stage[1][:], [_k("stage", 1)])] + \
                [(scb[1][:, si * 512:(si + 1) * 512], [_k("stg", si)]) for si in range(8)]
        load_weight_bf16(c, I["even_w_in"][0], D, EVEN_IN, win, "win", stage, gcol, _k("gcol"), CC)
        load_weight_bf16(c, I["even_w_out"][0], D, D, wout, "wout", stage, gcol2, _k("gcol2"), CC)
        P.add("dve", lambda e: e.memset(st[:, 7:8], 0.0), w=[_k("stg", si) for si in range(8)] + [_k("sc", 1), _k("st", 7)])
        WIN_ALL = [_k("win", k, j) for k in range(8) for j in range(6)]
        WOUT_ALL = [_k("wout", k, j) for k in range(8) for j in range(2)]

        def stageA(i):
            t0 = i * 128
            p2 = i % 2
            p3 = i % 4
            hb = hbufA
            hk = _k("hbufA")
            dma(hb[:], h_in[t0:t0 + 128, :], hk)
            rmsnorm_tile(c, hb[:], hk, xn[:], _k("xn"), junk[:], _k("junk"),
                         ss[:, 4:5], _k("ss", 4), rstd[:, 4:5], _k("rstd", 4))

            def tr8(e):
                inst = None
                v = bfv(0)
                for cc in range(8):
                    inst = e.transpose(v[:, cc * 128:(cc + 1) * 128], xn[:, cc * 128:(cc + 1) * 128], ident[:])
                return inst
            P.add("pe", tr8, r=[_k("xn"), _k("ident")], w=[_k("ps", 0)])
            P.add("act", lambda e: e.copy(xnT[:].rearrange("p c t -> p (c t)"), bfv(0)), r=[_k("ps", 0)], w=[_k("xnT")])
            yield

            def proj(c0, c1, pb):
                def f(e):
                    inst = None
                    for k in range(8):
                        inst = e.matmul(psum[pb][:, 0:(c1 - c0)], xnT[:, k, :], win[:, k, c0:c1], start=(k == 0), stop=(k == 7))
                    return inst
                P.add("pe", f, r=[_k("xnT")] + WIN_ALL, w=[_k("ps", pb)])

            proj(2320, 2832, 1)
            P.add("act", lambda e: e.copy(iq[:], psum[1][:]), r=[_k("ps", 1)], w=[_k("iq")])
            yield
            proj(2832, 2904, 2)
            P.add("act", lambda e: e.copy(ikb[:], psum[2][:, 0:64]), r=[_k("ps", 2)], w=[_k("ikb")])
            P.add("act", lambda e: e.copy(iw[:], psum[2][:, 64:72]), r=[_k("ps", 2)], w=[_k("iw")])
            yield
            P.add("act", lambda e: e.activation(wabs[p2][:], iw[:], AF.Abs, scale=IDX_C0), r=[_k("iw")], w=[_k("wabs", p2)])
            P.add("act", lambda e: e.sign(sgn[:], iw[:]), r=[_k("iw")], w=[_k("sgn")])
            P.add("dve", lambda e: e.tensor_tensor(Dsg[p2][:], identf[:].unsqueeze(1).to_broadcast([128, 8, 128]),
                                                   sgn[:].unsqueeze(2).to_broadcast([128, 8, 128]), ALU.mult),
                  r=[_k("identf"), _k("sgn")], w=[_k("Dsg", p2)])

            def triq(e):
                inst = None
                v = bfv(0)
                for hh in range(8):
                    inst = e.transpose(v[0:64, hh * 128:(hh + 1) * 128], iq[:, hh * 64:(hh + 1) * 64], ident[:])
                return inst
            P.add("pe", triq, r=[_k("iq"), _k("ident")], w=[_k("ps", 0)])
            P.add("act", lambda e: e.copy(iqT[p2][:].rearrange("p c t -> p (c t)"), bfv(0)[0:64, :]), r=[_k("ps", 0)], w=[_k("iqT", p2)])
            yield
            P.add("pe", lambda e: e.transpose(bfv(0)[0:64, 0:128], ikb[:], ident[:]), r=[_k("ikb"), _k("ident")], w=[_k("ps", 0)])
            P.add("act", lambda e: e.copy(ikTc[:, t0:t0 + 128], bfv(0)[0:64, 0:128]), r=[_k("ps", 0)], w=[_k("ikTc", i)])
            yield
            proj(1552, 2064, 1)
            P.add("act", lambda e: e.copy(dq[:], psum[1][:]), r=[_k("ps", 1)], w=[_k("dq")])
            yield
            proj(2064, 2320, 2)
            P.add("act", lambda e: e.copy(dkb[:], psum[2][:, 0:128]), r=[_k("ps", 2)], w=[_k("dkb")])
            P.add("act", lambda e: e.copy(vc[:, i, :], psum[2][:, 128:256]), r=[_k("ps", 2)], w=[_k("vc", i)])
            yield

            def trdq(e):
                inst = None
                v = bfv(0)
                for cc in range(4):
                    inst = e.transpose(v[:, cc * 128:(cc + 1) * 128], dq[:, cc * 128:(cc + 1) * 128], ident[:])
                inst = e.transpose(v[:, 512:640], dkb[:], ident[:])
                return inst
            P.add("pe", trdq, r=[_k("dq"), _k("dkb"), _k("ident")], w=[_k("ps", 0)])
            P.add("act", lambda e: e.copy(qT[p3][:].rearrange("p c t -> p (c t)"), bfv(0)[:, 0:512]), r=[_k("ps", 0)], w=[_k("qT", p3)])
            P.add("act", lambda e: e.copy(kTc[:, t0:t0 + 128], bfv(0)[:, 512:640]), r=[_k("ps", 0)], w=[_k("kTc", i)])
            yield
            proj(0, 512, 1)
            P.add("act", lambda e: e.copy(gqk[:], psum[1][:]), r=[_k("ps", 1)], w=[_k("gqk")])
            yield
            proj(512, 1024, 2)
            P.add("act", lambda e: e.copy(gv[:], psum[2][:]), r=[_k("ps", 2)], w=[_k("gv")])
            yield
            proj(1040, 1552, 1)
            P.add("act", lambda e: e.activation(gsl[:], psum[1][:], AF.Silu), r=[_k("ps", 1)], w=[_k("gsl")])
            yield

            def gaf(e):
                inst = None
                for k in range(8):
                    inst = e.matmul(psum[2][0:16, 0:128], win[:, k, 1024:1040], xnT[:, k, :], start=(k == 0), stop=(k == 7))
                return inst
            P.add("pe", gaf, r=[_k("xnT")] + WIN_ALL, w=[_k("ps", 2)])
            P.add("act", lambda e: e.copy(gaT[:], psum[2][0:16, 0:128]), r=[_k("ps", 2)], w=[_k("gaT")])
            yield
            P.add("pe", lambda e: e.matmul(psum[1][:, 0:256], gaT[:], wa2[:], start=True, stop=True),
                  r=[_k("gaT"), _k("wa2")], w=[_k("ps", 1)])
            P.add("dve", lambda e: e.tensor_tensor(zb[:], psum[1][:, 0:256], ba2[:], ALU.add),
                  r=[_k("ps", 1), _k("ba2")], w=[_k("zb")])
            P.add("act", lambda e: e.activation(zb[:], zb[:], AF.Exp, scale=-1.0), r=[_k("zb")], w=[_k("zb")])
            P.add("act", lambda e: e.activation(sp_[:], zb[:], AF.Ln, bias=1.0), r=[_k("zb")], w=[_k("sp")])
            yield

            def cumf(e):
                inst = None
                for h in range(4):
                    inst = e.matmul(psum[2][0:64, h * 128:(h + 1) * 128], sp_[:, h * 64:(h + 1) * 64], trim[:], start=True, stop=True)
                return inst
            P.add("pe", cumf, r=[_k("sp"), _k("trim")], w=[_k("ps", 2)])
            P.add("pe", lambda e: e.matmul(psum[1][:, 0:256], trir[:], sp_[:], start=True, stop=True),
                  r=[_k("sp"), _k("trir")], w=[_k("ps", 1)])
            P.add("act", lambda e: e.activation(ecum[:].rearrange("p h t -> p (h t)"), psum[2][0:64, :], AF.Exp),
                  r=[_k("ps", 2)], w=[_k("ecum")])
            P.add("act", lambda e: e.activation(encum[:].rearrange("p h t -> p (h t)"), psum[2][0:64, :], AF.Exp, scale=-1.0),
                  r=[_k("ps", 2)], w=[_k("encum")])
            P.add("act", lambda e: e.activation(erev[:], psum[1][:, 0:256], AF.Exp), r=[_k("ps", 1)], w=[_k("erev")])
            yield

            def trqk(e):
                inst = None
                v = bfv(0)
                for hh in range(8):
                    inst = e.transpose(v[0:64, hh * 128:(hh + 1) * 128], gqk[:, hh * 64:(hh + 1) * 64], ident[:])
                return inst
            P.add("pe", trqk, r=[_k("gqk"), _k("ident")], w=[_k("ps", 0)])
            P.add("dve", lambda e: e.scalar_tensor_tensor(qtT[:].rearrange("p h t -> p (h t)"), bfv(0)[0:64, 0:512], 0.125,
                                                          ecum[:].rearrange("p h t -> p (h t)"), ALU.mult, ALU.mult),
                  r=[_k("ps", 0), _k("ecum")], w=[_k("qtT")])
            P.add("dve", lambda e: e.tensor_tensor(ktT[:].rearrange("p h t -> p (h t)"), bfv(0)[0:64, 512:1024],
                                                   encum[:].rearrange("p h t -> p (h t)"), ALU.mult),
                  r=[_k("ps", 0), _k("encum")], w=[_k("ktT")])
            P.add("dve", lambda e: e.tensor_tensor(kend[:], gqk[:, 256:512], erev[:], ALU.mult),
                  r=[_k("gqk"), _k("erev")], w=[_k("kend")])
            yield

            def atf(e):
                inst = None
                for h in range(4):
                    inst = e.matmul(psum[2][:, h * 128:(h + 1) * 128], ktT[:, h, :], qtT[:, h, :], start=True, stop=True)
                return inst
            P.add("pe", atf, r=[_k("ktT"), _k("qtT")], w=[_k("ps", 2)])
            P.add("dve", lambda e: e.tensor_tensor(atm[:].rearrange("p h t -> p (h t)"), psum[2][:],
                                                   m01[:].rearrange("p h t -> p (h t)"), ALU.mult),
                  r=[_k("ps", 2), _k("m01")], w=[_k("atm")])
            yield

            def of(e):
                inst = None
                for h in range(4):
                    e.matmul(psum[1][:, h * 128:(h + 1) * 128], atm[:, h, :], gv[:, h * 128:(h + 1) * 128], start=True, stop=False)
                    inst = e.matmul(psum[1][:, h * 128:(h + 1) * 128], qtT[:, h, :], Sgb[:, h, :], start=False, stop=True)
                return inst
            P.add("pe", of, r=[_k("atm"), _k("gv"), _k("qtT"), _k("Sgb")], w=[_k("ps", 1)])

            def dsf(e):
                inst = None
                for h in range(4):
                    inst = e.matmul(psum[2][0:64, h * 128:(h + 1) * 128], kend[:, h * 64:(h + 1) * 64], gv[:, h * 128:(h + 1) * 128],
                                    start=True, stop=True)
                return inst
            P.add("pe", dsf, r=[_k("kend"), _k("gv")], w=[_k("ps", 2)])
            elast = ecum[:, :, 127:128].to_broadcast([64, 4, 128])
            P.add("dve", lambda e: e.tensor_tensor(Sg[:], Sg[:], elast, ALU.mult), r=[_k("Sg"), _k("ecum")], w=[_k("Sg")])
            P.add("dve", lambda e: e.tensor_tensor(Sg[:].rearrange("p h t -> p (h t)"), Sg[:].rearrange("p h t -> p (h t)"),
                                                   psum[2][0:64, :], ALU.add), r=[_k("Sg"), _k("ps", 2)], w=[_k("Sg")])
            P.add("pool", lambda e: e.tensor_copy(Sgb[:], Sg[:]), r=[_k("Sg")], w=[_k("Sgb")])
            yield
            for h in range(4):
                P.add("act", lambda e, h=h: e.activation(junk[:, 0:128], psum[1][:, h * 128:(h + 1) * 128], AF.Square, accum_out=ss[:, h:h + 1]),
                      r=[_k("ps", 1)], w=[_k("junk"), _k("ss", h)])
            rsqrt_small(P, rstd[:, 0:4], _k("rstd", 0), ss[:, 0:4], [_k("ss", h) for h in range(4)], 1.0 / 128)
            yield
            for h in range(4):
                P.add("dve", lambda e, h=h: e.scalar_tensor_tensor(og[:, h * 128:(h + 1) * 128], psum[1][:, h * 128:(h + 1) * 128], rstd[:, h:h + 1],
                                                                    gsl[:, h * 128:(h + 1) * 128], ALU.mult, ALU.mult),
                      r=[_k("ps", 1), _k("rstd", 0), _k("gsl")], w=[_k("og")])
            yield

            def trog(e):
                inst = None
                v = bfv(0)
                for cc in range(4):
                    inst = e.transpose(v[:, cc * 128:(cc + 1) * 128], og[:, cc * 128:(cc + 1) * 128], ident[:])
                return inst
            P.add("pe", trog, r=[_k("og"), _k("ident")], w=[_k("ps", 0)])
            P.add("act", lambda e: e.copy(ogT[p3][:].rearrange("p c t -> p (c t)"), bfv(0)[:, 0:512]),
                  r=[_k("ps", 0)], w=[_k("ogT", p3)])
            yield

        def stageBs(i):
            p2 = i % 2
            sc = scb[p2]
            nkeys = (i + 1) * 128
            ngrp = (nkeys + 511) // 512
            pend = None
            for gk in range(ngrp):
                k0 = gk * 512
                n = min(512, nkeys - k0)
                kk = [_k("ikTc", b) for b in range(k0 // 128, (k0 + n) // 128)]
                for hI in range(8):
                    rb = hI % 2
                    P.add("pe", lambda e, hI=hI, k0=k0, n=n: e.matmul(psum[3][:, 0:n], iqT[p2][:, hI, :], ikTc[:, k0:k0 + n], start=True, stop=True),
                          r=[_k("iqT", p2)] + kk, w=[_k("ps", 3)])
                    if pend is not None:
                        pend()
                        pend = None
                    P.add("act", lambda e, rb=rb, hI=hI, n=n: e.activation(Rb[rb][:, 0:n], psum[3][:, 0:n], AF.Relu, scale=wabs[p2][:, hI:hI + 1]),
                          r=[_k("ps", 3), _k("wabs", p2)], w=[_k("Rb", rb)])

                    def acc(rb=rb, hI=hI, n=n, k0=k0):
                        P.add("pe", lambda e: e.matmul(psum[4][:, 0:n], Dsg[p2][:, hI, :], Rb[rb][:, 0:n], start=(hI == 0), stop=(hI == 7)),
                              r=[_k("Dsg", p2), _k("Rb", rb)], w=[_k("ps", 4)])
                        if hI == 7:
                            P.add("act", lambda e: e.copy(sc[:, k0:k0 + n], psum[4][:, 0:n]), r=[_k("ps", 4)], w=[_k("sc", p2)])
                    pend = acc
                    yield
            if pend is not None:
                pend()
            yield

        def stageBb(i):
            t0 = i * 128
            p2 = i % 2
            sc = scb[p2]
            nkeys = (i + 1) * 128
            scv = sc[:, 0:nkeys]
            P.add("dve", lambda e: e.tensor_reduce(st[:, 0:1], scv, AX.X, ALU.max), r=[_k("sc", p2)], w=[_k("st", 0)])
            yield
            lov = scv if i < 2 else sc[:, 0:TOPK]
            P.add("dve", lambda e: e.tensor_reduce(st[:, 1:2], lov, AX.X, ALU.min), r=[_k("sc", p2)], w=[_k("st", 1)])
            P.add("dve", lambda e: e.tensor_tensor(sc[:, t0:t0 + 128], sc[:, t0:t0 + 128], negm[:], ALU.add),
                  r=[_k("sc", p2), _k("negm")], w=[_k("sc", p2)])
            P.add("dve", lambda e: e.tensor_tensor(st[:, 2:3], st[:, 0:1], st[:, 1:2], ALU.subtract), r=[_k("st", 0), _k("st", 1)], w=[_k("st", 2)])
            P.add("dve", lambda e: e.tensor_scalar(st[:, 2:3], st[:, 2:3], 1.000001, 1e-30, ALU.mult, ALU.add), r=[_k("st", 2)], w=[_k("st", 2)])
            P.add("dve", lambda e: e.tensor_scalar(hst[:], pw[:], st[:, 2:3], None, ALU.mult), r=[_k("st", 2), _k("pw")], w=[_k("hst")])
            yield
            for k in range(N_IT):
                P.add("dve", lambda e, k=k: e.tensor_tensor(st[:, 3:4], st[:, 1:2], hst[:, k:k + 1], ALU.add),
                      r=[_k("st", 1), _k("hst")], w=[_k("st", 3)])
                P.add("dve", lambda e: e.tensor_scalar(junkb[:, 0:nkeys], scv, st[:, 3:4], None, ALU.is_ge, ALU.add, accum_out=st[:, 4:5]),
                      r=[_k("sc", p2), _k("st", 3)], w=[_k("junkb"), _k("st", 4)])
                P.add("dve", lambda e, k=k: e.tensor_scalar(st[:, 5:6], st[:, 4:5], TOPK - 0.5, hst[:, k:k + 1], ALU.is_ge, ALU.mult),
                      r=[_k("st", 4), _k("hst")], w=[_k("st", 5)])
                P.add("dve", lambda e: e.tensor_tensor(st[:, 1:2], st[:, 1:2], st[:, 5:6], ALU.add),
                      r=[_k("st", 1), _k("st", 5)], w=[_k("st", 1)])
                yield
            P.add("dve", lambda e: e.tensor_scalar(mskb[p2][:, 0:nkeys], scv, st[:, 1:2], -MASK_BIG, ALU.is_lt, ALU.mult),
                  r=[_k("sc", p2), _k("st", 1)], w=[_k("msk", p2)])
            yield
            return
            for b0 in range(0, i + 1, 8):
                nb = min(8, i + 1 - b0)

                def trm(e, b0=b0, nb=nb):
                    inst = None
                    v = bfv(3)
                    for bb in range(nb):
                        inst = e.transpose(v[:, bb * 128:(bb + 1) * 128], msk[:, (b0 + bb) * 128:(b0 + bb + 1) * 128], ident[:])
                    return inst
                P.add("pe", trm, r=[_k("msk"), _k("ident")], w=[_k("ps", 3)])
                P.add("act", lambda e, b0=b0, nb=nb: e.copy(mskT[:, b0:b0 + nb, :].rearrange("p c t -> p (c t)"), bfv(3)[:, 0:nb * 128]),
                      r=[_k("ps", 3)], w=[_k("mskT", b0 // 8)])
                yield

        def stageC(i):
            t0 = i * 128
            p3 = i % 4
            p2 = i % 2
            hb = hbufC
            hk = _k("hbufC")
            dma(hb[:], h_in[t0:t0 + 128, :], hk)
            pendc = []
            for j in range(i + 1):
                eb = j % 2

                def stf(e, j=j):
                    e.matmul(psum[5][:], kTc[:, j * 128:(j + 1) * 128], qT[p3][:].rearrange("p c t -> p (c t)"), start=True, stop=False)
                    return e.matmul(psum[5][:], mskb[p2][:, j * 128:(j + 1) * 128], ident4[:], start=False, stop=True)
                P.add("pe", stf, r=[_k("kTc", j), _k("qT", p3), _k("msk", p2), _k("ident4")], w=[_k("ps", 5)])
                while pendc:
                    pendc.pop(0)()
                P.add("act", lambda e, eb=eb: e.activation(Eb[eb][:], psum[5][:], AF.Exp, scale=128.0 ** -0.5),
                      r=[_k("ps", 5)], w=[_k("Eb", eb)])

                def pv(e, eb=eb, j=j):
                    e.matmul(psum[6][:], vc[:, j, :], Eb[eb][:], start=(j == 0), stop=(j == i))
                    return e.matmul(psum[7][:], ones[:], Eb[eb][:], start=(j == 0), stop=(j == i))

                def pvadd(pv=pv, j=j, eb=eb):
                    P.add("pe", pv, r=[_k("vc", j), _k("Eb", eb), _k("ones")], w=[_k("ps", 6), _k("ps", 7)])
                pendc.append(pvadd)
                yield
            while pendc:
                pendc.pop(0)()
            P.add("act", lambda e: e.activation(rden[:], psum[7][:], AF.Ln), r=[_k("ps", 7)], w=[_k("rden")])
            P.add("act", lambda e: e.activation(rden[:], rden[:], AF.Exp, scale=-1.0), r=[_k("rden")], w=[_k("rden")])
            P.add("dve", lambda e: e.tensor_tensor(dsT[:].rearrange("p c t -> p (c t)"), psum[6][:], rden[:], ALU.mult),
                  r=[_k("ps", 6), _k("rden")], w=[_k("dsT")])
            yield
            for mh in range(2):
                def ymm(e, mh=mh):
                    inst = None
                    for cc in range(8):
                        src = ogT[p3][:, cc, :] if cc < 4 else dsT[:, cc - 4, :]
                        inst = e.matmul(psum[5][:], src, wout[:, cc, mh * 512:(mh + 1) * 512], start=(cc == 0), stop=(cc == 7))
                    return inst
                P.add("pe", ymm, r=[_k("ogT", p3), _k("dsT")] + WOUT_ALL, w=[_k("ps", 5)])
                P.add("dve", lambda e, mh=mh: e.tensor_tensor(hb[:, mh * 512:(mh + 1) * 512], hb[:, mh * 512:(mh + 1) * 512], psum[5][:], ALU.add),
                      r=[_k("ps", 5), hk], w=[hk])
                yield
            P.add("sp", lambda e: e.dma_start(out=h_out[t0:t0 + 128, :], in_=hb[:]), r=[hk], dma=True)
            yield

        ntl = DBG["ntiles"]
        for s_ in range(ntl + 3):
            gens = []
            if 0 <= s_ - 3 < ntl:
                gens.append((stageC(s_ - 3), (s_ - 3) + 1 + 4))
            if 0 <= s_ - 2 < ntl:
                gens.append((stageBb(s_ - 2), 4 + N_IT + (s_ - 2) // 8 + 1))
            if 0 <= s_ - 1 < ntl:
                gens.append((stageBs(s_ - 1), 9 * ((s_ - 1) // 4 + 1)))
            if s_ < ntl:
                gens.append((stageA(s_), 24))
            _interleave(gens)
        P.emit(es)


def host_consts():
    import ml_dtypes
    cst = {}
    cst["ident"] = np.eye(128, dtype=ml_dtypes.bfloat16)
    half = 128
    inv = 10000.0 ** (-np.arange(half, dtype=np.float32) / half)
    ang = np.arange(T, dtype=np.float32)[:, None] * inv[None, :].astype(np.float32)
    cst["rope_cos"] = np.cos(ang).astype(np.float32)
    cst["rope_sin"] = np.sin(ang).astype(np.float32)
    g = np.array(RET_GAMMA, dtype=np.float64)
    ii = np.arange(128)
    rel = ii[None, :] - ii[:, None]
    dtm = np.zeros((128, 4, 128), np.float64)
    for h in range(4):
        dtm[:, h, :] = np.where(rel >= 0, g[h] ** np.maximum(rel, 0), 0.0) / 16.0
    cst["ret_dt"] = dtm.astype(np.float32)
    xi8 = np.zeros((128, 8, 128), np.float64)
    for cc in range(8):
        xi8[:, cc, :] = (g[cc // 2] ** (ii + 1.0))[None, :]
    cst["ret_xi8"] = xi8.astype(np.float32)
    cst["ones"] = np.ones((128, 128), dtype=ml_dtypes.bfloat16)
    cst["identf"] = np.eye(128, dtype=np.float32)
    cst["ident4"] = np.tile(np.eye(128, dtype=np.float32), (1, 4)).astype(ml_dtypes.bfloat16)
    le = (ii[:, None] <= ii[None, :])
    cst["gla_trim"] = np.where(le, -1.0 / 16.0, 0.0).astype(np.float32)
    cst["gla_trir"] = np.where(~le, -1.0 / 16.0, 0.0).astype(np.float32)
    cst["gla_m01"] = np.repeat(le[:, None, :], 4, axis=1).astype(np.float32)
    cst["dsa_negm"] = np.where(ii[None, :] <= ii[:, None], 0.0, -1e30).astype(np.float32)
    cst["dsa_pw"] = np.repeat((0.5 ** (np.arange(N_IT + 1) + 1.0))[None, :], 128, axis=0).astype(np.float32)
    zeta = np.zeros((128, 4), np.float64)
    for h in range(4):
        zeta[:, h] = g[h] ** (127.0 - ii) / 16.0
    cst["ret_zeta"] = zeta.astype(np.float32)
    return cst


INPUT_SHAPES = {
    "even_attn_norm": [1, D], "even_w_in": [1, D, EVEN_IN], "even_gla_wa2": [1, 16, 256],
    "even_gla_ba2": [1, 256], "even_gla_norm": [1, 128], "even_w_out": [1, D, D],
    "odd_attn_norm": [1, D], "odd_w_in": [1, D, ODD_IN], "odd_ret_norm": [1, 512], "odd_w_out": [1, 2048, D],
    "ffn_norm": [2, D], "ffn_w_gate": [2, D, DFF], "ffn_w_up": [2, D, DFF], "ffn_w_down": [2, DFF, D],
    "final_norm": [D],
}


def build(phases=("mix0", "ffn0", "mix1", "ffn1")):
    nc = bass.Bass("TRN2", target_bir_lowering=False)
    x = nc.dram_tensor("x", [T, D], F32, kind="ExternalInput").ap()
    out = nc.dram_tensor("out", [T, D], F32, kind="ExternalOutput").ap()
    I = {k: nc.dram_tensor(k, shp, F32, kind="ExternalInput").ap() for k, shp in INPUT_SHAPES.items()}
    cst = host_consts()
    C = {}
    for k, v in cst.items():
        C[k] = nc.dram_tensor(k, list(v.shape), BF16 if v.dtype != np.float32 else F32, kind="ExternalInput").ap()
    scr = [nc.dram_tensor("scr%d" % i, [T, D], F32, kind="Internal").ap() for i in range(3)]
    bufs = [x] + scr[:len(phases) - 1] + [out]
    for pi, ph in enumerate(phases):
        hin, hout = bufs[pi], bufs[pi + 1]
        if ph == "ffn0":
            ffn_phase(nc, hin, hout, I["ffn_norm"][0], I["ffn_w_gate"][0], I["ffn_w_up"][0], I["ffn_w_down"][0], C["ident"])
        elif ph == "ffn1":
            ffn_phase(nc, hin, hout, I["ffn_norm"][1], I["ffn_w_gate"][1], I["ffn_w_up"][1], I["ffn_w_down"][1], C["ident"],
                      final_g=I["final_norm"])
        elif ph == "mix1":
            mix1_phase(nc, hin, hout, I["odd_attn_norm"][0], I["odd_w_in"][0], I["odd_ret_norm"][0], I["odd_w_out"][0],
                       C["ident"], C["rope_cos"], C["rope_sin"], C["ret_dt"], C["ret_xi8"], C["ret_zeta"])
        elif ph == "mix0":
            mix0_phase(nc, hin, hout, I, C)
    return nc


def make_inputs(inputs, b):
    m = {k: np.ascontiguousarray(np.asarray(inputs[k], dtype=np.float32)) for k in INPUT_SHAPES}
    m["x"] = np.ascontiguousarray(np.asarray(inputs["x"][b], dtype=np.float32))
    m.update(host_consts())
    return m


def kernel(**inputs):
    nc = build()
    in_maps = [make_inputs(inputs, b) for b in range(8)]
    res = run_bass_kernel_spmd(nc, in_maps, core_ids=list(range(8)))
    return np.stack([np.asarray(r["out"], dtype=np.float32) for r in res.results], axis=0)
```
